# Optimizing a Trainium2 kernel written in Bass

```python
import jax, jax.numpy as jnp
from jax import lax
import numpy as np

D_MODEL = 1024
BATCH = 32
SEQ = 2048
DEPTH = 2

N_A_LAYERS = DEPTH // 2
N_B_LAYERS = DEPTH - N_A_LAYERS
A_HEADS = 8
A_KEY_DIM = D_MODEL // A_HEADS
A_VAL_DIM = D_MODEL // A_HEADS
A_F_DIM = A_HEADS * A_KEY_DIM
A_I_DIM = A_HEADS * A_VAL_DIM
A_CHUNK = 32
B_HEADS = 8
B_HEAD_DIM = D_MODEL // B_HEADS
MOBA_BLOCK = 256
MOBA_TOPK = 3
Q_BLOCK = 128
ROPE_THETA = 10000.0
NORM_EPS = 1e-6

kernel_name = "hgrn2_moba_yoco_hybrid"

F32 = jnp.float32


def rms_norm(x, g):
    xf = x.astype(F32)
    y = xf * lax.rsqrt(jnp.mean(xf * xf, axis=-1, keepdims=True) + NORM_EPS)
    return (y * g.astype(F32)).astype(x.dtype)


def modulation(c, w, b):
    m = (jax.nn.silu(c) @ w + b)[:, None, :]
    shift, scale, gate = jnp.split(m, 3, axis=-1)
    return shift, scale, gate


def rope_tables(positions):
    inv = ROPE_THETA ** (-jnp.arange(0, B_HEAD_DIM, 2, dtype=F32) / B_HEAD_DIM)
    ang = positions.astype(F32)[..., None] * inv
    return jnp.cos(ang)[:, :, None, :], jnp.sin(ang)[:, :, None, :]


def apply_rope(x, cos, sin):
    xf = x.astype(F32)
    x1, x2 = jnp.split(xf, 2, axis=-1)
    return jnp.concatenate([x1 * cos - x2 * sin, x2 * cos + x1 * sin], axis=-1).astype(x.dtype)


def hgrn2_mixer(u, w_in, w_out, g_norm, lb):
    bsz, T, _ = u.shape
    n_chunks = T // A_CHUNK
    proj = u @ w_in
    q, f, i, g = jnp.split(proj, [A_F_DIM, 2 * A_F_DIM, 2 * A_F_DIM + A_I_DIM], axis=-1)
    q = jax.nn.silu(q.astype(F32))
    fg = lb + (1.0 - lb) * jax.nn.sigmoid(f.astype(F32))
    k = 1.0 - fg
    logf = jnp.log(fg)

    def heads(t, d):
        return t.reshape(bsz, n_chunks, A_CHUNK, A_HEADS, d).transpose(1, 0, 3, 2, 4)

    qc, kc = heads(q, A_KEY_DIM), heads(k, A_KEY_DIM)
    vc = heads(i.astype(F32), A_VAL_DIM)
    bc = jnp.cumsum(heads(logf, A_KEY_DIM), axis=3)
    causal = jnp.tril(jnp.ones((A_CHUNK, A_CHUNK), dtype=bool))

    def step(S, inp):
        q_, k_, v_, b_ = inp
        diff = b_[:, :, :, None, :] - b_[:, :, None, :, :]
        decay = jnp.exp(jnp.where(causal[:, :, None], diff, -jnp.inf))
        att = jnp.einsum('bhtd,bhtsd,bhsd->bhts', q_, decay, k_)
        o = jnp.einsum('bhts,bhsv->bhtv', att, v_) + jnp.einsum('bhtd,bhdv->bhtv', q_ * jnp.exp(b_), S)
        b_last = b_[:, :, -1, :]
        S = jnp.exp(b_last)[..., None] * S + jnp.einsum(
            'bhsd,bhsv->bhdv', k_ * jnp.exp(b_last[:, :, None, :] - b_), v_)
        return S, o

    S0 = jnp.zeros((bsz, A_HEADS, A_KEY_DIM, A_VAL_DIM), F32)
    _, o = lax.scan(step, S0, (qc, kc, vc, bc))
    o = o.transpose(1, 0, 3, 2, 4).reshape(bsz, T, A_HEADS, A_VAL_DIM)
    o = o * lax.rsqrt(jnp.mean(o * o, axis=-1, keepdims=True) + NORM_EPS) * g_norm.astype(F32)
    o = o * jax.nn.silu(g.astype(F32).reshape(bsz, T, A_HEADS, A_VAL_DIM))
    return o.reshape(bsz, T, A_I_DIM).astype(u.dtype) @ w_out


def shared_kv(h, g, w_kv, cos, sin):
    bsz, T, _ = h.shape
    k, v = jnp.split(rms_norm(h, g) @ w_kv, 2, axis=-1)
    k = apply_rope(k.reshape(bsz, T, B_HEADS, B_HEAD_DIM), cos, sin)
    v = v.reshape(bsz, T, B_HEADS, B_HEAD_DIM)
    n_blocks = -(-T // MOBA_BLOCK)
    pad = n_blocks * MOBA_BLOCK - T
    k = jnp.pad(k, ((0, 0), (0, pad), (0, 0), (0, 0)))
    v = jnp.pad(v, ((0, 0), (0, pad), (0, 0), (0, 0)))
    kb = k.reshape(bsz, n_blocks, MOBA_BLOCK, B_HEADS, B_HEAD_DIM).transpose(0, 3, 1, 2, 4).astype(F32)
    vb = v.reshape(bsz, n_blocks, MOBA_BLOCK, B_HEADS, B_HEAD_DIM).transpose(0, 3, 1, 2, 4).astype(F32)
    kmean = jnp.mean(kb, axis=3)
    return kb, vb, kmean


def moba_mixer(u, w_in, w_out, kb, vb, kmean, cos, sin):
    bsz, T, _ = u.shape
    n_blocks = kb.shape[2]
    n_qblocks = T // Q_BLOCK
    k_past = min(MOBA_TOPK, n_blocks)
    n_slots = k_past + 1
    scale = B_HEAD_DIM ** -0.5
    q, z = jnp.split(u @ w_in, 2, axis=-1)
    q = apply_rope(q.reshape(bsz, T, B_HEADS, B_HEAD_DIM), cos, sin)
    qblocks = q.reshape(bsz, n_qblocks, Q_BLOCK, B_HEADS, B_HEAD_DIM).transpose(1, 0, 3, 2, 4)
    offs = jnp.arange(MOBA_BLOCK)
    is_own = jnp.arange(n_slots) == k_past

    def attend(args):
        qi, qb = args
        qf = qb.astype(F32)
        qpos = qi * Q_BLOCK + jnp.arange(Q_BLOCK)
        j = (qi * Q_BLOCK) // MOBA_BLOCK
        gate = jnp.einsum('bhqd,bhnd->bhqn', qf, kmean)
        gate = jnp.where(jnp.arange(n_blocks) < j, gate, -jnp.inf)
        _, top = lax.top_k(gate, k_past)
        idx = jnp.concatenate([top, jnp.full(top.shape[:-1] + (1,), j, top.dtype)], axis=-1)
        logits_blk = jnp.einsum('bhqd,bhnkd->bhqnk', qf, kb) * scale
        logits = jnp.take_along_axis(logits_blk, idx[..., None], axis=3)
        kpos = idx[..., None] * MOBA_BLOCK + offs
        valid = (kpos <= qpos[:, None, None]) & (is_own[:, None] | (idx < j)[..., None])
        logits = jnp.where(valid, logits, -jnp.inf)
        p = jax.nn.softmax(logits.reshape(logits.shape[:3] + (n_slots * MOBA_BLOCK,)), axis=-1)
        p = p.reshape(logits.shape)
        p_blk = jnp.einsum('bhqsk,bhqsn->bhqnk', p, jax.nn.one_hot(idx, n_blocks, dtype=F32))
        return jnp.einsum('bhqnk,bhnkd->bhqd', p_blk, vb)

    o = lax.map(attend, (jnp.arange(n_qblocks), qblocks))
    o = o.transpose(1, 0, 3, 2, 4).reshape(bsz, T, B_HEADS * B_HEAD_DIM)
    o = o * jax.nn.silu(z.astype(F32))
    return o.astype(u.dtype) @ w_out


def setup_inputs(seed: int = 0) -> dict:
    key = jax.random.key(seed)
    ks = jax.random.split(key, 16)
    D = D_MODEL
    s = D ** -0.5
    nrm = lambda k, shape, sc: jax.random.normal(k, shape, F32) * sc
    x = nrm(ks[0], (BATCH, SEQ, D), 1.0)
    c = nrm(ks[1], (BATCH, D), 1.0)
    positions = (jax.random.randint(ks[2], (BATCH, 1), 0, 1024) + jnp.arange(SEQ)[None, :]).astype(jnp.int32)
    mod_w = nrm(ks[3], (DEPTH, D, 3 * D), 0.5 * s)
    mod_b = nrm(ks[4], (DEPTH, 3 * D), 0.02)
    pre_norm_g = 1.0 + nrm(ks[5], (DEPTH, D), 0.02)
    post_norm_g = 1.0 + nrm(ks[6], (DEPTH, D), 0.02)
    a_w_in = nrm(ks[7], (N_A_LAYERS, D, 2 * A_F_DIM + 2 * A_I_DIM), s)
    a_w_out = nrm(ks[8], (N_A_LAYERS, A_I_DIM, D), A_I_DIM ** -0.5)
    a_out_norm_g = 1.0 + nrm(ks[9], (N_A_LAYERS, A_VAL_DIM), 0.02)
    a_lb_logits = nrm(ks[10], (N_A_LAYERS + 1, A_F_DIM), 0.5)
    kv_norm_g = 1.0 + nrm(ks[11], (D,), 0.02)
    w_kv = nrm(ks[12], (D, 2 * B_HEADS * B_HEAD_DIM), s)
    b_w_in = nrm(ks[13], (N_B_LAYERS, D, 2 * B_HEADS * B_HEAD_DIM), s)
    b_w_out = nrm(ks[14], (N_B_LAYERS, B_HEADS * B_HEAD_DIM, D), (B_HEADS * B_HEAD_DIM) ** -0.5)
    return {"x": x, "c": c, "positions": positions, "mod_w": mod_w, "mod_b": mod_b,
            "pre_norm_g": pre_norm_g, "post_norm_g": post_norm_g, "a_w_in": a_w_in,
            "a_w_out": a_w_out, "a_out_norm_g": a_out_norm_g, "a_lb_logits": a_lb_logits,
            "kv_norm_g": kv_norm_g, "w_kv": w_kv, "b_w_in": b_w_in, "b_w_out": b_w_out}


def reference(x, c, positions, mod_w, mod_b, pre_norm_g, post_norm_g, a_w_in, a_w_out,
              a_out_norm_g, a_lb_logits, kv_norm_g, w_kv, b_w_in, b_w_out):
    cos, sin = rope_tables(positions)
    lbs = jnp.cumsum(jax.nn.softmax(a_lb_logits.astype(F32), axis=0), axis=0)
    h = x
    kb = vb = kmean = None
    for layer in range(DEPTH):
        shift, scale, gate = modulation(c, mod_w[layer], mod_b[layer])
        u = rms_norm(h, pre_norm_g[layer]) * (1.0 + scale) + shift
        if layer < N_A_LAYERS:
            y = hgrn2_mixer(u, a_w_in[layer], a_w_out[layer], a_out_norm_g[layer], lbs[layer])
        else:
            if layer == N_A_LAYERS:
                kb, vb, kmean = shared_kv(h, kv_norm_g, w_kv, cos, sin)
            li = layer - N_A_LAYERS
            y = moba_mixer(u, b_w_in[li], b_w_out[li], kb, vb, kmean, cos, sin)
        h = h + gate * rms_norm(y, post_norm_g[layer])
    return h
```

```python
import numpy as np
from contextlib import ExitStack
import concourse.bass as bass
import concourse.mybir as mybir
from concourse.bass_utils import run_bass_kernel_spmd

F32 = mybir.dt.float32
BF16 = mybir.dt.bfloat16
I32 = mybir.dt.int32
ALU = mybir.AluOpType
AF = mybir.ActivationFunctionType
AX = mybir.AxisListType

NB = 4
T = 2048
D = 1024
NT = 16
EPS = 1e-6
BIG = 30000.0
HW = ("pe", "act", "dve", "pool", "sp")
PSUM_KEYS = ("pA", "pB", "pO", "pS", "pT0", "pM")

C_ID, C_CM, C_PM, C_SW, C_ON, C_ES, C_NEG, C_INV, C_SGN, NCF = 0, 128, 256, 384, 512, 640, 1664, 1728, 1729, 1730
NCB = 1664


class Op:
    __slots__ = ("hw", "fn", "reads", "writes", "sem", "inc", "signal", "value", "waits", "idx", "is_dma")


class Prog:
    def __init__(self):
        self.ops = []

    def op(self, hw, fn, reads=(), writes=()):
        o = Op()
        o.hw, o.fn, o.reads, o.writes = hw, fn, tuple(reads), tuple(writes)
        o.sem, o.inc, o.signal, o.value, o.waits, o.is_dma = hw, 1, False, 0, [], False
        o.idx = len(self.ops)
        self.ops.append(o)
        return o

    def dma(self, hw, slot, fn, reads=(), writes=()):
        o = self.op(hw, fn, reads, writes)
        o.sem, o.inc, o.signal, o.is_dma = "dma:" + slot, 16, True, True
        return o

    def analyze(self):
        last_w, readers = {}, {}
        for o in self.ops:
            deps = {}
            for k in o.reads:
                p = last_w.get(k)
                if p is not None:
                    deps[p.idx] = (p, "raw")
                kn = k[0] if isinstance(k, tuple) else k
                if kn in PSUM_KEYS:
                    for r in readers.get(k, {}).values():
                        if r.hw != o.hw and r.idx not in deps:
                            deps[r.idx] = (r, "rar")
            for k in o.writes:
                p = last_w.get(k)
                if p is not None and p.idx not in deps:
                    deps[p.idx] = (p, "waw")
                for r in readers.get(k, {}).values():
                    if r.idx not in deps and r is not o:
                        deps[r.idx] = (r, "war")
            for p, kind in deps.values():
                if (not p.is_dma) and (not o.is_dma) and p.hw == o.hw:
                    if o.hw == "pe" or kind != "raw":
                        continue
                o.waits.append(p)
                p.signal = True
            for k in o.reads:
                readers.setdefault(k, {})[("d", o.idx) if o.is_dma else o.hw] = o
            for k in o.writes:
                last_w[k] = o
                readers[k] = {}
        cnt = {}
        for o in self.ops:
            if o.signal:
                cnt[o.sem] = cnt.get(o.sem, 0) + o.inc
                o.value = cnt[o.sem]
        return cnt

    def emit(self, block, sems):
        streams = {h: [] for h in HW}
        for o in self.ops:
            streams[o.hw].append(o)

        def make(hwname):
            def body(eng):
                known = {}
                for o in streams[hwname]:
                    need = {}
                    for p in o.waits:
                        if p.value > need.get(p.sem, 0):
                            need[p.sem] = p.value
                    for s, v in need.items():
                        if known.get(s, 0) < v:
                            eng.wait_ge(sems[s], v)
                            known[s] = v
                    ins = o.fn(eng)
                    if o.signal:
                        ins.then_inc(sems[o.sem], o.inc)
            return body

        block.tensor(make("pe"))
        block.scalar(make("act"))
        block.vector(make("dve"))
        block.gpsimd(make("pool"))
        block.sync(make("sp"))


def make_consts():
    cf = np.zeros((128, NCF), np.float32)
    p = np.arange(128)
    cf[:, C_ID:C_ID + 128] = np.eye(128, dtype=np.float32)
    cf[:, C_CM:C_CM + 128] = np.where(p[:, None] > p[None, :], -BIG, 0.0)
    cf[:, C_PM:C_PM + 128] = ((p[:, None] // 64 == p[None, :] // 64) & (p[:, None] <= p[None, :])).astype(np.float32)
    cf[:, C_SW:C_SW + 128] = (p[:, None] == (p[None, :] + 64) % 128).astype(np.float32)
    cf[:, C_ON:C_ON + 128] = 1.0
    for n in range(8):
        cf[n, C_ES + n * 128:C_ES + (n + 1) * 128] = 1.0
    neg = np.zeros((8, 8), np.float32)
    for i in range(8):
        j = (8 + i) // 2
        neg[i, j:] = -1e30
    cf[:, C_NEG:C_NEG + 64] = neg.reshape(1, 64)
    inv = np.float32(10000.0) ** (-np.arange(0, 128, 2, dtype=np.float32) / np.float32(128))
    cf[:, C_INV] = np.concatenate([inv, inv]).astype(np.float32)
    cf[:, C_SGN] = np.where(p < 64, -1.0, 1.0)
    return cf


def build(stop_after_l0=False, nseq=NB, limit=None):
    nc = bass.Bass("TRN2", target_bir_lowering=False)

    def din(name, shape, dt=F32):
        return nc.dram_tensor(name, list(shape), dt, kind="ExternalInput").ap()

    x = din("x", [NB, T, D])
    cin = din("c", [NB, D])
    pos = din("pos", [NB, T], I32)
    mod_w = din("mod_w", [2, D, 3 * D])
    mod_b = din("mod_b", [2, 3 * D])
    pre_g = din("pre_g", [2, D])
    post_g = din("post_g", [2, D])
    a_w_in = din("a_w_in", [D, 4 * D])
    a_w_out = din("a_w_out", [D, D])
    a_gn = din("a_gn", [128, 1])
    a_lb = din("a_lb", [2, D])
    kv_g = din("kv_g", [1, D])
    w_kv = din("w_kv", [D, 2 * D])
    b_w_in = din("b_w_in", [D, 2 * D])
    b_w_out = din("b_w_out", [D, D])
    cf = din("cf", [128, NCF])
    out = nc.dram_tensor("out", [NB, T, D], F32, kind="ExternalOutput").ap()
    scr = nc.dram_tensor("scr", [2, 3, NB, D], F32, kind="Internal").ap()

    P = Prog()
    es = ExitStack()
    with es:
        def sb(name, shape, dt):
            return es.enter_context(nc.sbuf_tensor(name, list(shape), dt))

        def ps(name, shape, dt):
            return es.enter_context(nc.psum_tensor(name, list(shape), dt))

        cb = sb("cb", [128, NCB], BF16)
        cfs = sb("cfs", [128, NCF - NCB], F32)
        ident = cb[:, C_ID:C_ID + 128]
        cmask = cb[:, C_CM:C_CM + 128]
        pmask = cb[:, C_PM:C_PM + 128]
        swp = cb[:, C_SW:C_SW + 128]
        onesb = cb[:, C_ON:C_ON + 128]
        negm = cfs[:, 0:64]
        invf = cfs[:, 64:65]
        sgn = cfs[:, 65:66]

        xnT = sb("xnT", [128, 8, T], BF16)
        ogT = sb("ogT", [128, 8, T], BF16)
        h1 = sb("h1", [128, NT * D], F32)
        wbf = [sb("wbf%d" % i, [128, 8, 512], BF16) for i in range(2)]
        hb = [sb("hb%d" % i, [128, T], BF16) for i in range(6)]
        cosT = sb("cosT", [128, T], BF16)
        sinT = sb("sinT", [128, T], BF16)
        xin = [sb("xin%d" % i, [128, D], F32) for i in range(2)]
        xs = [sb("xs0", [128, D], BF16)] * 2
        G_bc = sb("G_bc", [128, D], F32)
        PT = [sb("PT%d" % i, [128, 512], BF16) for i in range(3)]
        rsb = sb("rsb", [128, 512], F32)
        otb = sb("otb", [128, 512], F32)
        biasT = sb("biasT", [128, 1024], BF16)
        gbuf = sb("gbuf", [128, 64], F32)
        cmpb = sb("cmpb", [128, 8, 8, 8], F32)
        rankb = sb("rankb", [128, 64], F32)
        biasq = sb("biasq", [128, 64], BF16)
        small = sb("small", [128, 256], F32)
        smallb = sb("smallb", [128, 160], BF16)
        ss = small[:, 0:16]
        rstd = small[:, 16:32]
        tmpc = small[:, 32:48]
        aT = small[:, 48:56]
        shT = small[:, 56:64]
        gkT = small[:, 64:72]
        lbT = small[:, 72:88]
        oml = small[:, 88:96]
        gn = small[:, 96:97]
        bcol = small[:, 100:104]
        nbf = small[:, 104:105]
        ssy = small[:, 108:112]
        Bl = small[:, 112:144]
        Bl0 = small[:, 144:176]
        km = small[:, 176:184]
        cT = small[:, 192:224]
        shTb = smallb[:, 0:8]
        kmb = smallb[:, 8:16]
        birow = smallb[0:1, 16:144]
        scT = sb("scT", [128, 32], BF16)
        bmat = sb("bmat", [128, 128], BF16)

        def h1f(off, n):
            return h1[:, off:off + n]

        def h1b(off, n):
            return h1[:, off:off + n // 2].bitcast(BF16)

        o_ = 0
        T_q = h1f(o_, 512); o_ += 512
        T_s = h1f(o_, 512); o_ += 512
        T_d0 = h1f(o_, 512); o_ += 512
        T_d1 = h1f(o_, 512); o_ += 512
        T_B = h1f(o_, 512); o_ += 512
        attS = [h1b(o_, 512), h1b(o_ + 256, 512)]; o_ += 512
        osb = h1f(o_, 512); o_ += 512
        sqb = h1b(o_, 512); o_ += 256
        rst = h1f(o_, 512); o_ += 512
        tt_ = h1f(o_, 512); o_ += 512
        Ub = h1f(o_, 4096); o_ += 4096
        Sbf = h1b(o_, 4096); o_ += 2048
        arep = h1f(o_, 2048); RA = h1f(o_, 1024); RAi = h1[:, o_:o_ + 1024].bitcast(I32); o_ += 2048
        Shalf = h1f(o_, 2048); RB = h1f(o_, 1024); RBi = h1[:, o_:o_ + 1024].bitcast(I32); o_ += 2048
        qrawb = h1b(o_, 512); o_ += 256
        assert o_ <= NT * D
        msb = h1f(0, 3072)
        modbb = h1f(3072, 3072)
        pgb = h1f(6144, 2048)
        rows = h1f(8192, 3072)

        pA = [ps("pA%d" % i, [128, 512], F32) for i in range(2)]
        pB = [ps("pB%d" % i, [128, 512], F32) for i in range(2)]
        pO = ps("pO", [128, 512], F32)
        pS = ps("pS", [128, 512], F32)
        pT0 = ps("pT0", [128, 1024], BF16)
        pM = ps("pM", [128, 512], F32)

        H1A = "h1all"

        def WK(k, g0=0, g1=4):
            return [("wbf", k, g) for g in range(g0, g1)]

        def top(hw, fn, reads=(), writes=()):
            return P.op(hw, fn, tuple(reads) + (H1A,), writes)

        def tdma(hw, slot, fn, reads=(), writes=()):
            return P.dma(hw, slot, fn, tuple(reads) + (H1A,), writes)

        def barrier():
            for e in ("act", "dve", "pool"):
                P.op(e, (lambda eng, e=e: (eng.memset(small[:, 250:251], 0.0) if e != "act" else
                                            eng.activation(out=small[:, 251:252], in_=small[:, 252:253], func=AF.Copy))),
                     writes=[("bar", e)])
            P.op("pe", lambda eng: eng.matmul(pM[0:1, 0:1], lhsT=onesb[:, 0:1], rhs=onesb[:, 0:1], start=True, stop=True),
                 reads=["cb"], writes=["pM", ("bar", "pe")])
            for e in ("act", "dve", "pool", "pe", "sp"):
                if e == "pe":
                    P.op("pe", lambda eng: eng.matmul(pM[0:1, 0:1], lhsT=onesb[:, 0:1], rhs=onesb[:, 0:1], start=True, stop=True),
                         reads=["cb"] + [("bar", q) for q in ("act", "dve", "pool")], writes=["pM"])
                elif e == "sp":
                    P.op("sp", lambda eng: eng.nop(), reads=[("bar", q) for q in ("act", "dve", "pool", "pe")])
                else:
                    P.op(e, (lambda eng, e=e: (eng.memset(small[:, 253:254], 0.0) if e == "dve" else
                                                eng.memset(small[:, 254:255], 0.0) if e == "pool" else
                                                eng.activation(out=small[:, 255:256], in_=small[:, 252:253], func=AF.Copy))),
                         reads=[("bar", q) for q in ("act", "dve", "pool", "pe") if q != e])

        P.dma("pool", "cb", lambda e: e.dma_start(out=cb[:], in_=cf[:, 0:NCB]), writes=["cb"])
        P.dma("sp", "cfs", lambda e: e.dma_start(out=cfs[:], in_=cf[:, NCB:NCF]), writes=["cfs"])
        P.op("dve", lambda e: e.memset(small[:], 0.0), writes=["small"])
        P.op("dve", lambda e: e.memset(bmat[:], 0.0), writes=["birow"])
        P.op("dve", lambda e: e.memset(biasT[:], 0.0), writes=["biasT"])
        P.op("pool", lambda e: e.memset(hb[3][:], 0.0), writes=[("kt", 0), ("kt", 1)])
        P.op("pool", lambda e: e.memset(hb[5][:], 0.0), writes=[("kt", 0), ("kt", 1)])
        with nc.allow_non_contiguous_dma(reason="tiny one-time transposed loads"):
            pass
        for bb in range(NB):
            P.dma("sp", "s0", lambda e, bb=bb: e.dma_start(out=cT.rearrange("p (c b) -> p c b", b=NB)[:, :, bb], in_=cin[bb].rearrange("(c p) -> p c", p=128),
                                                         allow_slow_non_contiguous=True), reads=["small"], writes=[("cT", bb)])
        P.dma("sp", "s1", lambda e: e.dma_start(out=gkT, in_=kv_g[0].rearrange("(c p) -> p c", p=128), allow_slow_non_contiguous=True),
              reads=["small"], writes=["gkT"])
        for l in range(2):
            P.dma("sp", "s2", lambda e, l=l: e.dma_start(out=lbT[:, l * 8:(l + 1) * 8], in_=a_lb[l].rearrange("(c p) -> p c", p=128),
                                                       allow_slow_non_contiguous=True), reads=["small"], writes=[("lbT", l)])
        P.dma("sp", "s3", lambda e: e.dma_start(out=gn, in_=a_gn), reads=["small"], writes=["gn"])
        P.op("act", lambda e: e.activation(out=scT[:], in_=cT, func=AF.Silu), reads=[("cT", q) for q in range(NB)], writes=["scT"])
        P.op("dve", lambda e: e.tensor_tensor(out=oml, in0=lbT[:, 8:16], in1=lbT[:, 0:8], op=ALU.subtract), reads=[("lbT", 0), ("lbT", 1)], writes=["oml"])
        P.op("act", lambda e: e.activation(out=oml, in_=oml, func=AF.Sigmoid), reads=["oml"], writes=["oml"])
        wi = 0
        for l in range(2):
            tdma("sp", "s4", lambda e, l=l: e.dma_start(out=modbb[0:NB, :], in_=mod_b[l].partition_broadcast(NB)), writes=["modbb"])
            tdma("sp", "s5", lambda e, l=l: e.dma_start(out=pgb[0:NB, 0:1024], in_=pre_g[l].partition_broadcast(NB)), writes=["pgb0"])
            tdma("sp", "s6", lambda e, l=l: e.dma_start(out=pgb[0:NB, 1024:2048], in_=post_g[l].partition_broadcast(NB)), writes=["pgb1"])
            for cbk in range(6):
                k = wi % 2
                wi += 1
                P.dma("pool", "wbf%d" % k, lambda e, l=l, cbk=cbk, k=k: e.dma_start(
                    out=wbf[k][:], in_=mod_w[l][:, cbk * 512:(cbk + 1) * 512].rearrange("(c p) n -> p c n", p=128)),
                    writes=WK(k))
                for c in range(8):
                    P.op("pe", lambda e, c=c, k=k: e.matmul(pM[0:NB, :], lhsT=scT[:, c * NB:(c + 1) * NB], rhs=wbf[k][:, c, :],
                                                            start=(c == 0), stop=(c == 7)),
                         reads=["scT"] + WK(k), writes=["pM"])
                top("dve", lambda e, cbk=cbk: e.tensor_tensor(
                    out=msb[0:NB, cbk * 512:(cbk + 1) * 512], in0=pM[0:NB, :],
                    in1=modbb[0:NB, cbk * 512:(cbk + 1) * 512], op=ALU.add),
                    reads=["pM", "modbb"], writes=[("msb", cbk)])
            mk = [("msb", i) for i in range(6)]
            top("dve", lambda e: e.scalar_tensor_tensor(out=rows[0:NB, 0:1024], in0=msb[0:NB, 1024:2048],
                                                        scalar=1.0, in1=pgb[0:NB, 0:1024], op0=ALU.add, op1=ALU.mult),
                reads=mk + ["pgb0"], writes=["rows"])
            top("dve", lambda e: e.tensor_copy(out=rows[0:NB, 1024:2048], in_=msb[0:NB, 0:1024]), reads=mk, writes=["rows"])
            top("dve", lambda e: e.tensor_tensor(out=rows[0:NB, 2048:3072], in0=msb[0:NB, 2048:3072], in1=pgb[0:NB, 1024:2048], op=ALU.mult),
                reads=mk + ["pgb1"], writes=["rows"])
            for kind in range(3):
                tdma("sp", "s7", lambda e, l=l, kind=kind: e.dma_start(out=scr[l, kind], in_=rows[0:NB, kind * 1024:(kind + 1) * 1024]),
                     reads=["rows"], writes=[("scr", l, kind)])
        barrier()

        xin_i = [0]

        def layer_vectors(l, b):
            P.dma("sp", "v0", lambda e: e.dma_start(out=aT, in_=scr[l, 0, b].rearrange("(c p) -> p c", p=128), allow_slow_non_contiguous=True),
                  reads=[("scr", l, 0)], writes=["aT"])
            P.dma("sp", "v1", lambda e: e.dma_start(out=shT, in_=scr[l, 1, b].rearrange("(c p) -> p c", p=128), allow_slow_non_contiguous=True),
                  reads=[("scr", l, 1)], writes=["shT"])
            P.dma("sp", "v2", lambda e: e.dma_start(out=G_bc[:], in_=scr[l, 2, b].partition_broadcast(128)), reads=[("scr", l, 2)], writes=["G_bc"])
            P.op("dve", lambda e: e.tensor_copy(out=shTb, in_=shT), reads=["shT"], writes=["shTb"])

        def rstd_from(col_in, col_out, keyin, keyout):
            P.op("dve", lambda e: e.tensor_scalar(out=col_out, in0=col_in, scalar1=1.0 / D, scalar2=EPS, op0=ALU.mult, op1=ALU.add),
                 reads=[keyin], writes=[keyout])
            P.op("act", lambda e: e.activation(out=col_out, in_=col_out, func=AF.Sqrt), reads=[keyout], writes=[keyout])
            P.op("dve", lambda e: e.reciprocal(out=col_out, in_=col_out), reads=[keyout], writes=[keyout])

        def pre_phase(l, b):
            for tt in range(NT):
                k = tt % 2
                if l == 0:
                    xi = xin_i[0] % 2
                    xin_i[0] += 1
                    P.dma("sp", "xin%d" % xi, lambda e, tt=tt, xi=xi: e.dma_start(out=xin[xi][:], in_=x[b, tt * 128:(tt + 1) * 128, :]),
                          writes=[("xin", xi)])
                    src, skey = xin[xi][:], ("xin", xi)
                else:
                    src, skey = h1[:, tt * D:(tt + 1) * D], ("h1", tt)
                P.op("dve", lambda e, tt=tt: e.memset(ss[:, tt:tt + 1], 0.0), writes=[("ss", tt)])
                P.op("act", lambda e, src=src, tt=tt: e.activation(out=xs[0][:], in_=src, func=AF.Square, accum_out=ss[:, tt:tt + 1]),
                     reads=[skey, ("ss", tt)], writes=[("xs", 0), ("ss", tt)])
                rstd_from(ss[:, tt:tt + 1], rstd[:, tt:tt + 1], ("ss", tt), ("rstd", tt))
                P.op("dve", lambda e, src=src, tt=tt, k=k: e.tensor_scalar(out=xs[k][:], in0=src, scalar1=rstd[:, tt:tt + 1], scalar2=None, op0=ALU.mult),
                     reads=[skey, ("rstd", tt)], writes=[("xs", 0)])
                for c in range(8):
                    P.op("pe", lambda e, c=c, k=k: e.transpose(out=pT0[:, c * 128:(c + 1) * 128], in_=xs[k][:, c * 128:(c + 1) * 128], identity=ident),
                         reads=[("xs", 0), "cb"], writes=["pT0"])
                P.op("act", lambda e, tt=tt: e.activation(out=xnT[:, :, tt * 128:(tt + 1) * 128], in_=pT0[:].rearrange("p (c t) -> p c t", c=8), func=AF.Copy),
                     reads=["pT0"], writes=[("xnT", tt)])

        wslot = [0]

        def load_head_weights(pieces):
            k = wslot[0] % 2
            wslot[0] += 1
            for g, ap in enumerate(pieces):
                P.dma("pool", "wbf%d" % k, lambda e, g=g, ap=ap, k=k: e.dma_start(
                    out=wbf[k][:, :, g * 128:(g + 1) * 128], in_=ap.rearrange("(c p) n -> p c n", p=128)),
                    writes=[("wbf", k, g)])
            return k

        def bias_cols(k, groups):
            for j, g in enumerate(groups):
                for c in range(8):
                    P.op("pe", lambda e, j=j, g=g, c=c: e.matmul(pM[:, j:j + 1], lhsT=wbf[k][:, c, g * 128:(g + 1) * 128], rhs=shTb[:, c:c + 1],
                                                               start=(c == 0), stop=(c == 7)),
                         reads=[("wbf", k, g), "shTb"], writes=["pM"])
            P.op("dve", lambda e: e.tensor_copy(out=bcol[:, 0:len(groups)], in_=pM[:, 0:len(groups)]), reads=["pM"], writes=["bcol"])

        def scale_w(k, c0, c1, vec, vkey):
            P.op("pool", lambda e: e.tensor_tensor(out=wbf[k][:, :, c0:c1], in0=wbf[k][:, :, c0:c1],
                                                   in1=vec.unsqueeze(2).to_broadcast([128, 8, c1 - c0]), op=ALU.mult),
                 reads=WK(k, c0 // 128, c1 // 128) + [vkey], writes=WK(k, c0 // 128, c1 // 128))

        pa_i = [0]

        def proj_fm(k, g, tb):
            i = pa_i[0] % 2
            pa_i[0] += 1
            for c in range(8):
                P.op("pe", lambda e, c=c, i=i: e.matmul(pA[i][:], lhsT=wbf[k][:, c, g * 128:(g + 1) * 128], rhs=xnT[:, c, tb * 512:(tb + 1) * 512],
                                                      start=(c == 0), stop=(c == 7)),
                     reads=[("wbf", k, g)] + [("xnT", 4 * tb + q) for q in range(4)], writes=[("pA", i)])
            return i

        def proj_tm(k, g, dst, dkey, bias_row=False):
            for t4 in range(4):
                i = t4 % 2
                for j in range(4):
                    tt = t4 * 4 + j
                    for c in range(8):
                        P.op("pe", lambda e, c=c, tt=tt, j=j, i=i: e.matmul(pB[i][:, j * 128:(j + 1) * 128], lhsT=xnT[:, c, tt * 128:(tt + 1) * 128],
                                                                          rhs=wbf[k][:, c, g * 128:(g + 1) * 128], start=(c == 0),
                                                                          stop=(c == 7 and not bias_row)),
                             reads=[("wbf", k, g), ("xnT", tt)], writes=[("pB", i)])
                    if bias_row:
                        P.op("pe", lambda e, j=j, i=i: e.matmul(pB[i][:, j * 128:(j + 1) * 128], lhsT=onesb, rhs=bmat[:], start=False, stop=True),
                             reads=["cb", "birow"], writes=[("pB", i)])
                P.op("act", lambda e, t4=t4, i=i: e.activation(out=dst[:, t4 * 512:(t4 + 1) * 512], in_=pB[i][:], func=AF.Copy),
                     reads=[("pB", i)], writes=[(dkey, t4)])

        def post_phase(l, b, wout, last):
            for cbk in range(2):
                P.dma("pool", "wbf%d" % cbk, lambda e, cbk=cbk: e.dma_start(
                    out=wbf[cbk][:], in_=wout[:, cbk * 512:(cbk + 1) * 512].rearrange("(c p) n -> p c n", p=128)), writes=WK(cbk))
            wslot[0] = 0
            for tt in range(NT):
                for cbk in range(2):
                    for c in range(8):
                        P.op("pe", lambda e, c=c, cbk=cbk, tt=tt: e.matmul(pA[cbk][:], lhsT=ogT[:, c, tt * 128:(tt + 1) * 128], rhs=wbf[cbk][:, c, :],
                                                                         start=(c == 0), stop=(c == 7)),
                             reads=WK(cbk) + [("ogT", hh, tt // 4) for hh in range(8)], writes=[("pA", cbk)])
                    P.op("dve", lambda e, cbk=cbk: e.memset(ssy[:, cbk:cbk + 1], 0.0), writes=[("ssy", cbk)])
                    P.op("act", lambda e, cbk=cbk: e.activation(out=PT[cbk][:], in_=pA[cbk][:], func=AF.Square, accum_out=ssy[:, cbk:cbk + 1]),
                         reads=[("pA", cbk), ("ssy", cbk)], writes=[("PT", cbk), ("ssy", cbk)])
                P.op("dve", lambda e: e.tensor_tensor(out=ssy[:, 2:3], in0=ssy[:, 0:1], in1=ssy[:, 1:2], op=ALU.add),
                     reads=[("ssy", 0), ("ssy", 1)], writes=[("ssy", 2)])
                rstd_from(ssy[:, 2:3], ssy[:, 3:4], ("ssy", 2), ("ssy", 3))
                xi = xin_i[0] % 2
                xin_i[0] += 1
                if l == 0:
                    P.dma("sp", "xin%d" % xi, lambda e, tt=tt, xi=xi: e.dma_start(out=xin[xi][:], in_=x[b, tt * 128:(tt + 1) * 128, :]),
                          writes=[("xin", xi)])
                    dest, dkey, res, rkey = h1[:, tt * D:(tt + 1) * D], ("h1", tt), xin[xi][:], ("xin", xi)
                    extra_w = [H1A]
                else:
                    dest, dkey, res, rkey = xin[xi][:], ("xin", xi), h1[:, tt * D:(tt + 1) * D], ("h1", tt)
                    extra_w = [H1A]
                for cbk in range(2):
                    P.op("dve", lambda e, cbk=cbk, dest=dest: e.scalar_tensor_tensor(
                        out=dest[:, cbk * 512:(cbk + 1) * 512], in0=pA[cbk][:], scalar=ssy[:, 3:4], in1=G_bc[:, cbk * 512:(cbk + 1) * 512],
                        op0=ALU.mult, op1=ALU.mult), reads=[("pA", cbk), ("ssy", 3), "G_bc"], writes=[dkey] + (extra_w if l == 0 else []))
                P.op("dve", lambda e, dest=dest, res=res: e.tensor_tensor(out=dest, in0=dest, in1=res, op=ALU.add),
                     reads=[dkey, rkey], writes=[dkey] + extra_w)
                if last:
                    P.dma("sp", "xin%d" % xi, lambda e, tt=tt, dest=dest: e.dma_start(out=out[b, tt * 128:(tt + 1) * 128, :], in_=dest),
                          reads=[dkey], writes=[("out", b, tt)])

        def rope_tables(b):
            for hf in range(2):
                cs = slice(hf * 1024, (hf + 1) * 1024)
                tdma("sp", "ra", lambda e, cs=cs: e.dma_start(out=RAi, in_=pos[b, cs].partition_broadcast(128)), writes=["arep"])
                top("dve", lambda e: e.tensor_copy(out=RA, in_=RAi), reads=["arep"], writes=["arep"])
                top("dve", lambda e: e.tensor_scalar(out=RA, in0=RA, scalar1=invf, scalar2=None, op0=ALU.mult), reads=["arep", "cfs"], writes=["arep"])
                for which in range(2):
                    off = 0.0 if which == 0 else float(np.pi / 2)
                    top("dve", lambda e, off=off: e.tensor_scalar(out=RB, in0=RA, scalar1=off, scalar2=float(1.0 / (2 * np.pi)), op0=ALU.add, op1=ALU.mult),
                        reads=["arep"], writes=["Shalf"])
                    top("dve", lambda e: e.tensor_copy(out=RBi, in_=RB), reads=["Shalf"], writes=["Shalf"])
                    top("dve", lambda e: e.tensor_copy(out=RB, in_=RBi), reads=["Shalf"], writes=["Shalf"])
                    top("dve", lambda e: e.tensor_scalar(out=RB, in0=RB, scalar1=float(-2 * np.pi), scalar2=None, op0=ALU.mult), reads=["Shalf"], writes=["Shalf"])
                    top("dve", lambda e, off=off: e.scalar_tensor_tensor(out=RB, in0=RA, scalar=off, in1=RB, op0=ALU.add, op1=ALU.add),
                        reads=["arep", "Shalf"], writes=["Shalf"])
                    top("dve", lambda e: e.tensor_scalar(out=RB, in0=RB, scalar1=3.14159, scalar2=-3.14159, op0=ALU.min, op1=ALU.max),
                        reads=["Shalf"], writes=["Shalf"])
                    if which == 0:
                        top("act", lambda e: e.activation(out=RB, in_=RB, func=AF.Sin), reads=["Shalf"], writes=["Shalf"])
                        top("dve", lambda e, cs=cs: e.tensor_scalar(out=sinT[:, cs], in0=RB, scalar1=sgn, scalar2=None, op0=ALU.mult),
                            reads=["Shalf", "cfs"], writes=["sinT"])
                    else:
                        top("act", lambda e, cs=cs: e.activation(out=cosT[:, cs], in_=RB, func=AF.Sin), reads=["Shalf"], writes=["cosT"])

        def l0_head(h):
            qtT, ktT, sgT, kt, vv, ktB = hb[0], hb[1], hb[2], hb[3], hb[4], hb[5]
            k = load_head_weights([a_w_in[:, g * 1024 + h * 128:g * 1024 + (h + 1) * 128] for g in range(4)])
            bias_cols(k, [0, 1, 3])
            P.op("dve", lambda e: e.tensor_scalar(out=nbf, in0=bcol[:, 1:2], scalar1=-1.0, scalar2=None, op0=ALU.mult), reads=["bcol"], writes=["nbf"])
            for c in range(8):
                P.op("pe", lambda e, c=c: e.matmul(pM[0:1, 128:256], lhsT=shTb[:, c:c + 1], rhs=wbf[k][:, c, 256:384], start=(c == 0), stop=(c == 7)),
                     reads=[("wbf", k, 2), "shTb"], writes=["pM"])
            P.op("dve", lambda e: e.tensor_copy(out=bmat[0:1, :], in_=pM[0:1, 128:256]), reads=["pM"], writes=["birow"])
            scale_w(k, 0, 512, aT, "aT")
            for tb in range(4):
                bs = slice(tb * 512, (tb + 1) * 512)
                i = proj_fm(k, 0, tb)
                top("act", lambda e, i=i: e.activation(out=T_q, in_=pA[i][:], func=AF.Silu, bias=bcol[:, 0:1]), reads=[("pA", i), "bcol"], writes=["T_q"])
                i = proj_fm(k, 3, tb)
                P.op("act", lambda e, i=i, bs=bs: e.activation(out=sgT[:, bs], in_=pA[i][:], func=AF.Silu, bias=bcol[:, 2:3]),
                     reads=[("pA", i), "bcol"], writes=[("sgT", tb)])
                i = proj_fm(k, 1, tb)
                top("act", lambda e, i=i: e.activation(out=T_s, in_=pA[i][:], func=AF.Sigmoid, bias=nbf, scale=-1.0), reads=[("pA", i), "nbf"], writes=["T_s"])
                top("dve", lambda e: e.tensor_scalar(out=T_s, in0=T_s, scalar1=oml[:, h:h + 1], scalar2=None, op0=ALU.mult), reads=["T_s", "oml"], writes=["T_s"])
                top("dve", lambda e: e.tensor_scalar(out=T_d0, in0=T_s, scalar1=-1.0, scalar2=1.0, op0=ALU.mult, op1=ALU.add), reads=["T_s"], writes=["T_d0"])
                if tb == 0 and h == 0:
                    top("dve", lambda e: e.memset(T_d1, 0.0), writes=["T_d1"])
                d0v = T_d0.rearrange("p (n c) -> p n c", c=64)[:, :, 0:1]
                d1v = T_d1.rearrange("p (n c) -> p n c", c=64)[:, :, 0:1]
                top("dve", lambda e, d0v=d0v, d1v=d1v: e.tensor_copy(out=d1v, in_=d0v), reads=["T_d0", "T_d1"], writes=["T_d1"])
                top("dve", lambda e, d0v=d0v: e.memset(d0v, 0.0), reads=["T_d0", "T_d1"], writes=["T_d0"])
                top("dve", lambda e: e.tensor_tensor_scan(out=T_B, data0=T_d0, data1=T_d1, initial=0.0, op0=ALU.mult, op1=ALU.add),
                    reads=["T_d0", "T_d1"], writes=["T_B"])
                top("dve", lambda e, tb=tb: e.tensor_copy(out=Bl[:, tb * 8:(tb + 1) * 8].unsqueeze(2),
                                                          in_=T_B.rearrange("p (n c) -> p n c", c=64)[:, :, 63:64]), reads=["T_B"], writes=["Bl"])
                top("dve", lambda e, bs=bs: e.tensor_tensor(out=qtT[:, bs], in0=T_q, in1=T_B, op=ALU.mult), reads=["T_q", "T_B"], writes=[("qtT", tb)])
                top("dve", lambda e: e.reciprocal(out=T_d0, in_=T_B), reads=["T_B"], writes=["T_d0"])
                top("dve", lambda e, bs=bs: e.tensor_tensor(out=ktT[:, bs], in0=T_s, in1=T_d0, op=ALU.mult), reads=["T_s", "T_d0"], writes=[("ktT", tb)])
            if limit == "h0a":
                return
            proj_tm(k, 2, vv, "vv", bias_row=True)
            for t8 in range(2):
                for j in range(8):
                    tt = t8 * 8 + j
                    P.op("pe", lambda e, tt=tt, j=j: e.transpose(out=pT0[:, j * 128:(j + 1) * 128], in_=ktT[:, tt * 128:(tt + 1) * 128], identity=ident),
                         reads=[("ktT", tt // 4), "cb"], writes=["pT0"])
                P.op("act", lambda e, t8=t8: e.activation(out=kt[0:64, t8 * 1024:(t8 + 1) * 1024], in_=pT0[0:64, :], func=AF.Copy), reads=["pT0"], writes=[("kt", t8)])
                P.op("act", lambda e, t8=t8: e.activation(out=ktB[64:128, t8 * 1024:(t8 + 1) * 1024], in_=pT0[64:128, :], func=AF.Copy), reads=["pT0"], writes=[("kt", t8)])
            if limit == "h0b":
                return
            Uv = Ub.rearrange("p (v n) -> p n v", n=32)
            for ng in range(8):
                i = ng % 2
                for j in range(4):
                    n = ng * 4 + j
                    tt, half = n // 2, n % 2
                    ksrc = kt if half == 0 else ktB
                    P.op("pe", lambda e, j=j, tt=tt, ksrc=ksrc, i=i: e.matmul(pB[i][:, j * 128:(j + 1) * 128], lhsT=ksrc[:, tt * 128:(tt + 1) * 128],
                                                                            rhs=vv[:, tt * 128:(tt + 1) * 128], start=True, stop=True),
                         reads=[("kt", tt // 8), ("vv", tt // 4)], writes=[("pB", i)])
                top("dve", lambda e, ng=ng, i=i: e.tensor_tensor(out=Uv[:, ng * 4:(ng + 1) * 4, :], in0=pB[i][:].rearrange("p (n v) -> p n v", n=4),
                                                                 in1=Bl[:, ng * 4:(ng + 1) * 4].unsqueeze(2).to_broadcast([128, 4, 128]), op=ALU.mult),
                    reads=[("pB", i), "Bl"], writes=["Ub"])
            if limit == "h0c1":
                return
            top("dve", lambda e: e.tensor_copy(out=Bl0, in_=Bl), reads=["Bl"], writes=["Bl0"])
            top("dve", lambda e: e.memset(Bl0[:, 0:1], 0.0), reads=["Bl0"], writes=["Bl0"])
            top("dve", lambda e: e.tensor_copy(out=arep.rearrange("p (v n) -> p v n", n=32), in_=Bl0.unsqueeze(1).to_broadcast([128, 64, 32])),
                reads=["Bl0"], writes=["arep"])
            if limit == "h0c2":
                return
            for vh in range(2):
                top("dve", lambda e, vh=vh: e.tensor_tensor_scan(out=Shalf, data0=arep, data1=Ub[:, vh * 2048:(vh + 1) * 2048], initial=0.0,
                                                                 op0=ALU.mult, op1=ALU.add),
                    reads=["arep", "Ub"], writes=["Shalf"])
                if limit == "h0c3":
                    continue
                top("act", lambda e, vh=vh: e.activation(out=Sbf.rearrange("p (n v) -> p v n", n=32)[:, vh * 64:(vh + 1) * 64, :],
                                                         in_=Shalf.rearrange("p (v n) -> p v n", n=32), func=AF.Copy),
                    reads=["Shalf"], writes=["Sbf"])
            if limit in ("h0c", "h0c3"):
                return
            for pg in range(4):
                i = pg % 2
                for j in range(4):
                    pr = pg * 4 + j
                    P.op("pe", lambda e, j=j, pr=pr, i=i: e.matmul(pB[i][:, j * 128:(j + 1) * 128], lhsT=ktT[:, pr * 128:(pr + 1) * 128],
                                                                 rhs=qtT[:, pr * 128:(pr + 1) * 128], start=True, stop=True),
                         reads=[("ktT", pg), ("qtT", pg)], writes=[("pB", i)])
                top("dve", lambda e, i=i: e.tensor_tensor(out=attS[i].rearrange("p (n t) -> p n t", n=4), in0=pB[i][:].rearrange("p (n t) -> p n t", n=4),
                                                          in1=pmask.unsqueeze(1).to_broadcast([128, 4, 128]), op=ALU.mult),
                    reads=[("pB", i), "cb"], writes=[("attS", i)])
                for j in range(4):
                    pr = pg * 4 + j
                    P.op("pe", lambda e, j=j, pr=pr, i=i: e.matmul(pO[:, j * 128:(j + 1) * 128], lhsT=vv[:, pr * 128:(pr + 1) * 128],
                                                                 rhs=attS[i][:, j * 128:(j + 1) * 128], start=True, stop=False, skip_group_check=True),
                         reads=[("vv", pg), ("attS", i), H1A], writes=["pO"])
                    if pr > 0:
                        P.op("pe", lambda e, j=j, pr=pr: e.matmul(pO[:, j * 128:j * 128 + 64], lhsT=Sbf[:, (2 * pr - 1) * 128:(2 * pr) * 128],
                                                                rhs=qtT[:, pr * 128:pr * 128 + 64], start=False, stop=False, skip_group_check=True),
                             reads=["Sbf", ("qtT", pg), H1A], writes=["pO"])
                    P.op("pe", lambda e, j=j, pr=pr: e.matmul(pO[:, j * 128 + 64:(j + 1) * 128], lhsT=Sbf[:, (2 * pr) * 128:(2 * pr + 1) * 128],
                                                            rhs=qtT[:, pr * 128 + 64:(pr + 1) * 128], start=False, stop=True, skip_group_check=True),
                         reads=["Sbf", ("qtT", pg), H1A], writes=["pO"])
                top("act", lambda e: e.activation(out=osb, in_=pO[:], func=AF.Copy), reads=["pO"], writes=["osb"])
                top("act", lambda e: e.activation(out=sqb, in_=pO[:], func=AF.Square), reads=["pO"], writes=["sqb"])
                P.op("pe", lambda e: e.matmul(pS[:], lhsT=onesb, rhs=sqb, start=True, stop=True), reads=["sqb", "cb", H1A], writes=["pS"])
                top("dve", lambda e: e.tensor_scalar(out=rst, in0=pS[:], scalar1=1.0 / 128, scalar2=EPS, op0=ALU.mult, op1=ALU.add), reads=["pS"], writes=["rst"])
                top("act", lambda e: e.activation(out=rst, in_=rst, func=AF.Sqrt), reads=["rst"], writes=["rst"])
                top("dve", lambda e: e.reciprocal(out=rst, in_=rst), reads=["rst"], writes=["rst"])
                top("dve", lambda e: e.scalar_tensor_tensor(out=tt_, in0=osb, scalar=gn, in1=rst, op0=ALU.mult, op1=ALU.mult),
                    reads=["osb", "rst", "gn"], writes=["tt_"])
                top("dve", lambda e, pg=pg: e.tensor_tensor(out=ogT[:, h, pg * 512:(pg + 1) * 512], in0=tt_, in1=sgT[:, pg * 512:(pg + 1) * 512], op=ALU.mult),
                    reads=["tt_", ("sgT", pg)], writes=[("ogT", h, pg)])

        pt_i = [0]

        def l1_head(h):
            QT, KT, szT, V = hb[0], hb[1], hb[2], hb[4]
            k = load_head_weights([b_w_in[:, h * 128:(h + 1) * 128], b_w_in[:, 1024 + h * 128:1024 + (h + 1) * 128],
                                   w_kv[:, h * 128:(h + 1) * 128], w_kv[:, 1024 + h * 128:1024 + (h + 1) * 128]])
            bias_cols(k, [0, 1])
            scale_w(k, 0, 256, aT, "aT")
            scale_w(k, 256, 512, gkT, "gkT")
            if limit == "l1a0":
                return
            for tb in range(4):
                bs = slice(tb * 512, (tb + 1) * 512)
                for which in range(2):
                    if limit == "l1a1" and (tb, which) == (0, 1):
                        return
                    if limit == "l1a2" and (tb, which) == (1, 0):
                        return
                    dst = QT if which == 0 else KT
                    dkey = "QT" if which == 0 else "KT"
                    i = proj_fm(k, 0 if which == 0 else 2, tb)
                    j = (tb * 2 + which) % 2
                    if which == 0:
                        P.op("dve", lambda e, i=i: e.tensor_scalar(out=PT[0][:], in0=pA[i][:], scalar1=bcol[:, 0:1], scalar2=None, op0=ALU.add),
                            reads=[("pA", i), "bcol"], writes=[("PT", 0)])
                        P.op("dve", lambda e, i=i, bs=bs: e.scalar_tensor_tensor(out=rsb[:], in0=pA[i][:], scalar=bcol[:, 0:1], in1=cosT[:, bs], op0=ALU.add, op1=ALU.mult),
                            reads=[("pA", i), "bcol", "cosT"], writes=["rsb"])
                    else:
                        P.op("act", lambda e, i=i: e.activation(out=PT[0][:], in_=pA[i][:], func=AF.Copy), reads=[("pA", i)], writes=[("PT", 0)])
                        P.op("dve", lambda e, i=i, bs=bs: e.tensor_tensor(out=rsb[:], in0=pA[i][:], in1=cosT[:, bs], op=ALU.mult),
                            reads=[("pA", i), "cosT"], writes=["rsb"])
                    P.op("pe", lambda e, j=j: e.matmul(pB[j][:], lhsT=swp, rhs=PT[0][:], start=True, stop=True), reads=[("PT", 0), "cb"], writes=[("pB", j)])
                    P.op("dve", lambda e, j=j, bs=bs: e.tensor_tensor(out=otb[:], in0=pB[j][:], in1=sinT[:, bs], op=ALU.mult), reads=[("pB", j), "sinT"], writes=["otb"])
                    P.op("dve", lambda e, dst=dst, bs=bs: e.tensor_tensor(out=dst[:, bs], in0=rsb[:], in1=otb[:], op=ALU.add), reads=["rsb", "otb"], writes=[(dkey, tb)])
                i = proj_fm(k, 1, tb)
                P.op("act", lambda e, i=i, bs=bs: e.activation(out=szT[:, bs], in_=pA[i][:], func=AF.Silu, bias=bcol[:, 1:2]),
                     reads=[("pA", i), "bcol"], writes=[("szT", tb)])
            if limit == "l1a":
                return
            proj_tm(k, 3, V, "V")
            P.op("dve", lambda e: e.tensor_reduce(out=km, in_=KT[:].rearrange("p (n k) -> p n k", k=256), axis=AX.X, op=ALU.add),
                 reads=[("KT", q) for q in range(4)], writes=["km"])
            P.op("dve", lambda e: e.tensor_scalar(out=kmb, in0=km, scalar1=1.0 / 256, scalar2=None, op0=ALU.mult), reads=["km"], writes=["kmb"])
            for i8 in range(8):
                P.op("pe", lambda e, i8=i8: e.matmul(pM[:, i8 * 8:(i8 + 1) * 8], lhsT=QT[:, (8 + i8) * 128:(9 + i8) * 128], rhs=kmb, start=True, stop=True),
                     reads=[("QT", (8 + i8) // 4), "kmb"], writes=["pM"])
            P.op("dve", lambda e: e.tensor_tensor(out=gbuf[:], in0=pM[:, 0:64], in1=negm, op=ALU.add), reads=["pM", "cfs"], writes=["gbuf"])
            g3 = gbuf[:].rearrange("p (i n) -> p i n", n=8)
            P.op("dve", lambda e: e.tensor_tensor(out=cmpb[:], in0=g3.unsqueeze(2).to_broadcast([128, 8, 8, 8]),
                                                  in1=g3.unsqueeze(3).to_broadcast([128, 8, 8, 8]), op=ALU.is_gt), reads=["gbuf"], writes=["cmpb"])
            P.op("dve", lambda e: e.tensor_reduce(out=rankb[:], in_=cmpb[:].rearrange("p i n m -> p (i n) m"), axis=AX.X, op=ALU.add),
                 reads=["cmpb"], writes=["rankb"])
            bq_pad = cmpb[:].rearrange("p a b c -> p (a b c)").bitcast(BF16).rearrange("p (i c) -> p i c", c=128)
            P.op("dve", lambda e: e.memset(bq_pad, 0.0), reads=["rankb"], writes=["cmpb"])
            P.op("dve", lambda e: e.tensor_scalar(out=bq_pad[:, :, 0:8], in0=rankb[:].rearrange("p (i n) -> p i n", n=8), scalar1=2.5, scalar2=-BIG,
                                                  op0=ALU.is_ge, op1=ALU.mult), reads=["rankb", "cmpb"], writes=["cmpb"])
            for i8 in range(8):
                P.op("pe", lambda e, i8=i8: e.transpose(out=pT0[:, i8 * 128:(i8 + 1) * 128], in_=bq_pad[:, i8, :], identity=ident),
                     reads=["cmpb", "cb"], writes=["pT0"])
            P.op("act", lambda e: e.activation(out=biasT[:], in_=pT0[:], func=AF.Copy), reads=["pT0"], writes=["biasT"])
            if limit == "l1b":
                return
            sc = float(128 ** -0.5)
            for g in range(4):
                nkt = 4 * g + 4
                for ktile in range(nkt):
                    c0 = max(ktile - 4 * g, 0)
                    cl = slice(c0 * 128, 512)
                    j = pt_i[0] % 2
                    r = pt_i[0] % 3
                    pt_i[0] += 1
                    mm = [(cl, KT[:, ktile * 128:(ktile + 1) * 128], QT[:, g * 512 + c0 * 128:(g + 1) * 512], [("KT", ktile // 4), ("QT", g)])]
                    if g >= 2:
                        n = ktile // 2
                        lo = max(2 * n + 2 - 4 * g, c0)
                        if lo < 4:
                            mm.append((slice(lo * 128, 512), cb[:, C_ES + n * 128:C_ES + (n + 1) * 128],
                                       biasT[:, (g - 2) * 512 + lo * 128:(g - 2) * 512 + 512], ["biasT", "cb"]))
                    if ktile >= 4 * g:
                        mm.append((slice(c0 * 128, c0 * 128 + 128), ident, cmask, ["cb"]))
                    for mi, (csl, l_, r_, rk) in enumerate(mm):
                        P.op("pe", lambda e, csl=csl, l_=l_, r_=r_, mi=mi, j=j, last=(mi == len(mm) - 1): e.matmul(
                            pB[j][:, csl], lhsT=l_, rhs=r_, start=(mi == 0), stop=last, skip_group_check=True),
                            reads=rk, writes=[("pB", j)])
                    P.op("act", lambda e, j=j, r=r, cl=cl: e.activation(out=PT[r][:, cl], in_=pB[j][:, cl], func=AF.Exp, scale=sc),
                         reads=[("pB", j)], writes=[("PT", r)])
                    P.op("pe", lambda e, r=r, cl=cl, ktile=ktile, nkt=nkt: e.matmul(pO[:, cl], lhsT=V[:, ktile * 128:(ktile + 1) * 128], rhs=PT[r][:, cl],
                                                                                   start=(ktile == 0), stop=(ktile == nkt - 1), skip_group_check=True),
                         reads=[("V", ktile // 4), ("PT", r)], writes=["pO"])
                    P.op("pe", lambda e, r=r, cl=cl, ktile=ktile, nkt=nkt: e.matmul(pS[:, cl], lhsT=onesb, rhs=PT[r][:, cl],
                                                                                   start=(ktile == 0), stop=(ktile == nkt - 1), skip_group_check=True),
                         reads=["cb", ("PT", r)], writes=["pS"])
                P.op("dve", lambda e: e.reciprocal(out=rsb[:], in_=pS[:]), reads=["pS"], writes=["rsb"])
                P.op("dve", lambda e: e.tensor_tensor(out=otb[:], in0=pO[:], in1=rsb[:], op=ALU.mult), reads=["pO", "rsb"], writes=["otb"])
                P.op("dve", lambda e, g=g: e.tensor_tensor(out=ogT[:, h, g * 512:(g + 1) * 512], in0=otb[:], in1=szT[:, g * 512:(g + 1) * 512], op=ALU.mult),
                     reads=["otb", ("szT", g)], writes=[("ogT", h, g)])

        for b in range(nseq):
            if limit == "setup":
                break
            rope_tables(b)
            layer_vectors(0, b)
            if limit == "rope":
                break
            pre_phase(0, b)
            if limit == "pre":
                break
            for h in range(8):
                l0_head(h)
                if limit in ("head0", "h0a", "h0b", "h0c", "h0c1", "h0c2", "h0c3"):
                    break
            if limit in ("head0", "heads", "h0a", "h0b", "h0c", "h0c1", "h0c2", "h0c3"):
                break
            post_phase(0, b, a_w_out, last=stop_after_l0)
            if stop_after_l0:
                continue
            layer_vectors(1, b)
            pre_phase(1, b)
            if limit == "l1pre":
                break
            for h in range(8):
                l1_head(h)
                if limit in ("l1a", "l1b", "l1c", "l1a0", "l1a1", "l1a2"):
                    break
            if limit in ("l1a", "l1b", "l1c", "l1a0", "l1a1", "l1a2"):
                break
            post_phase(1, b, b_w_out, last=True)
        if limit is not None:
            P.dma("sp", "dbg", lambda e: e.dma_start(out=out[0, 0:128, :], in_=xin[0][:]), reads=[("xin", 0)], writes=[("out", 0, 0)])
            P.op("sp", lambda e: e.nop(), reads=[("out", 0, 0)])
            for e_ in ("act", "dve", "pool", "pe"):
                pass
        else:
            P.op("sp", lambda e: e.nop(), reads=[("out", b, tt) for b in range(nseq) for tt in range(NT)])

        cnt = P.analyze()
        names = set(cnt.keys()) | set(HW)
        sems = {s: es.enter_context(nc.semaphore(s.replace(":", "_"))) for s in sorted(names)}
        with nc.Block() as block:
            P.emit(block, sems)
    return nc, len(P.ops), cnt


_CACHE = {}


def kernel(x, c, positions, mod_w, mod_b, pre_norm_g, post_norm_g, a_w_in, a_w_out, a_out_norm_g,
           a_lb_logits, kv_norm_g, w_kv, b_w_in, b_w_out):
    if "nc" not in _CACHE:
        _CACHE["nc"] = build()[0]
    nc = _CACHE["nc"]
    f = lambda a: np.ascontiguousarray(np.asarray(a), dtype=np.float32)
    shared = {
        "mod_w": f(mod_w), "mod_b": f(mod_b), "pre_g": f(pre_norm_g), "post_g": f(post_norm_g),
        "a_w_in": f(a_w_in)[0], "a_w_out": f(a_w_out)[0], "a_gn": f(a_out_norm_g).reshape(128, 1),
        "a_lb": f(a_lb_logits), "kv_g": f(kv_norm_g).reshape(1, D), "w_kv": f(w_kv),
        "b_w_in": f(b_w_in)[0], "b_w_out": f(b_w_out)[0], "cf": make_consts(),
    }
    x = f(x)
    c = f(c)
    positions = np.ascontiguousarray(np.asarray(positions), dtype=np.int32)
    in_maps = []
    for i in range(8):
        m = dict(shared)
        m["x"] = x[i * NB:(i + 1) * NB]
        m["c"] = c[i * NB:(i + 1) * NB]
        m["pos"] = positions[i * NB:(i + 1) * NB]
        in_maps.append(m)
    res = run_bass_kernel_spmd(nc, in_maps, core_ids=list(range(8)))
    return np.concatenate([r["out"] for r in res.results], axis=0)
```

```python
import numpy as np
from contextlib import ExitStack
import concourse.bass as bass
import concourse.mybir as mybir
from concourse.bass_utils import run_bass_kernel_spmd

F32 = mybir.dt.float32
BF16 = mybir.dt.bfloat16
I32 = mybir.dt.int32
ALU = mybir.AluOpType
AF = mybir.ActivationFunctionType
AX = mybir.AxisListType

NB = 4
T = 2048
D = 1024
NT = 16
EPS = 1e-6
BIG = 30000.0
HW = ("pe", "act", "dve", "pool", "sp")
PSUM_KEYS = ("pA", "pB", "pO", "pS", "pT0", "pM")

C_ID, C_CM, C_PM, C_SW, C_ON, C_ES, C_NEG, C_INV, C_SGN, NCF = 0, 128, 256, 384, 512, 640, 1664, 1728, 1729, 1730
NCB = 1664


class Op:
    __slots__ = ("hw", "fn", "reads", "writes", "sem", "inc", "signal", "value", "waits", "idx", "is_dma")


class Prog:
    def __init__(self):
        self.ops = []

    def op(self, hw, fn, reads=(), writes=()):
        o = Op()
        o.hw, o.fn, o.reads, o.writes = hw, fn, tuple(reads), tuple(writes)
        o.sem, o.inc, o.signal, o.value, o.waits, o.is_dma = hw, 1, False, 0, [], False
        o.idx = len(self.ops)
        self.ops.append(o)
        return o

    def dma(self, hw, slot, fn, reads=(), writes=()):
        o = self.op(hw, fn, reads, writes)
        o.sem, o.inc, o.signal, o.is_dma = "dma:" + slot, 16, True, True
        return o

    def analyze(self):
        last_w, readers = {}, {}
        for o in self.ops:
            deps = {}
            for k in o.reads:
                p = last_w.get(k)
                if p is not None:
                    deps[p.idx] = (p, "raw")
                kn = k[0] if isinstance(k, tuple) else k
                if kn in PSUM_KEYS:
                    for r in readers.get(k, {}).values():
                        if r.hw != o.hw and r.idx not in deps:
                            deps[r.idx] = (r, "rar")
            for k in o.writes:
                p = last_w.get(k)
                if p is not None and p.idx not in deps:
                    deps[p.idx] = (p, "waw")
                for r in readers.get(k, {}).values():
                    if r.idx not in deps and r is not o:
                        deps[r.idx] = (r, "war")
            for p, kind in deps.values():
                if (not p.is_dma) and (not o.is_dma) and p.hw == o.hw:
                    if o.hw == "pe" or kind != "raw":
                        continue
                o.waits.append(p)
                p.signal = True
            for k in o.reads:
                readers.setdefault(k, {})[("d", o.idx) if o.is_dma else o.hw] = o
            for k in o.writes:
                last_w[k] = o
                readers[k] = {}
        cnt = {}
        for o in self.ops:
            if o.signal:
                cnt[o.sem] = cnt.get(o.sem, 0) + o.inc
                o.value = cnt[o.sem]
        return cnt

    def emit(self, block, sems):
        streams = {h: [] for h in HW}
        for o in self.ops:
            streams[o.hw].append(o)

        def make(hwname):
            def body(eng):
                known = {}
                for o in streams[hwname]:
                    need = {}
                    for p in o.waits:
                        if p.value > need.get(p.sem, 0):
                            need[p.sem] = p.value
                    for s, v in need.items():
                        if known.get(s, 0) < v:
                            eng.wait_ge(sems[s], v)
                            known[s] = v
                    ins = o.fn(eng)
                    if o.signal:
                        ins.then_inc(sems[o.sem], o.inc)
            return body

        block.tensor(make("pe"))
        block.scalar(make("act"))
        block.vector(make("dve"))
        block.gpsimd(make("pool"))
        block.sync(make("sp"))


def make_consts():
    cf = np.zeros((128, NCF), np.float32)
    p = np.arange(128)
    cf[:, C_ID:C_ID + 128] = np.eye(128, dtype=np.float32)
    cf[:, C_CM:C_CM + 128] = np.where(p[:, None] > p[None, :], -BIG, 0.0)
    cf[:, C_PM:C_PM + 128] = ((p[:, None] // 64 == p[None, :] // 64) & (p[:, None] <= p[None, :])).astype(np.float32)
    cf[:, C_SW:C_SW + 128] = (p[:, None] == (p[None, :] + 64) % 128).astype(np.float32)
    cf[:, C_ON:C_ON + 128] = 1.0
    for n in range(8):
        cf[n, C_ES + n * 128:C_ES + (n + 1) * 128] = 1.0
    neg = np.zeros((8, 8), np.float32)
    for i in range(8):
        j = (8 + i) // 2
        neg[i, j:] = -1e30
    cf[:, C_NEG:C_NEG + 64] = neg.reshape(1, 64)
    inv = np.float32(10000.0) ** (-np.arange(0, 128, 2, dtype=np.float32) / np.float32(128))
    cf[:, C_INV] = np.concatenate([inv, inv]).astype(np.float32)
    cf[:, C_SGN] = np.where(p < 64, -1.0, 1.0)
    return cf


def build(stop_after_l0=False, nseq=NB, limit=None):
    nc = bass.Bass("TRN2", target_bir_lowering=False)

    def din(name, shape, dt=F32):
        return nc.dram_tensor(name, list(shape), dt, kind="ExternalInput").ap()

    x = din("x", [NB, T, D])
    cin = din("c", [NB, D])
    pos = din("pos", [NB, T], I32)
    mod_w = din("mod_w", [2, D, 3 * D])
    mod_b = din("mod_b", [2, 3 * D])
    pre_g = din("pre_g", [2, D])
    post_g = din("post_g", [2, D])
    a_w_in = din("a_w_in", [D, 4 * D])
    a_w_out = din("a_w_out", [D, D])
    a_gn = din("a_gn", [128, 1])
    a_lb = din("a_lb", [2, D])
    kv_g = din("kv_g", [1, D])
    w_kv = din("w_kv", [D, 2 * D])
    b_w_in = din("b_w_in", [D, 2 * D])
    b_w_out = din("b_w_out", [D, D])
    cf = din("cf", [128, NCF])
    out = nc.dram_tensor("out", [NB, T, D], F32, kind="ExternalOutput").ap()
    scr = nc.dram_tensor("scr", [2, 3, NB, D], F32, kind="Internal").ap()

    P = Prog()
    es = ExitStack()
    with es:
        def sb(name, shape, dt):
            return es.enter_context(nc.sbuf_tensor(name, list(shape), dt))

        def ps(name, shape, dt):
            return es.enter_context(nc.psum_tensor(name, list(shape), dt))

        cb = sb("cb", [128, NCB], BF16)
        cfs = sb("cfs", [128, NCF - NCB], F32)
        ident = cb[:, C_ID:C_ID + 128]
        cmask = cb[:, C_CM:C_CM + 128]
        pmask = cb[:, C_PM:C_PM + 128]
        swp = cb[:, C_SW:C_SW + 128]
        onesb = cb[:, C_ON:C_ON + 128]
        negm = cfs[:, 0:64]
        invf = cfs[:, 64:65]
        sgn = cfs[:, 65:66]

        xnT = sb("xnT", [128, 8, T], BF16)
        ogT = sb("ogT", [128, 8, T], BF16)
        h1 = sb("h1", [128, NT * D], F32)
        wbf = [sb("wbf%d" % i, [128, 8, 512], BF16) for i in range(2)]
        hb = [sb("hb%d" % i, [128, T], BF16) for i in range(6)]
        cosT = sb("cosT", [128, T], BF16)
        sinT = sb("sinT", [128, T], BF16)
        xin = [sb("xin%d" % i, [128, D], F32) for i in range(2)]
        xs = [sb("xs0", [128, D], BF16)] * 2
        G_bc = sb("G_bc", [128, D], F32)
        PT = [sb("PT%d" % i, [128, 512], BF16) for i in range(3)]
        rsb = sb("rsb", [128, 512], F32)
        otb = sb("otb", [128, 512], F32)
        biasT = sb("biasT", [128, 1024], BF16)
        gbuf = sb("gbuf", [128, 64], F32)
        cmpb = sb("cmpb", [128, 8, 8, 8], F32)
        rankb = sb("rankb", [128, 64], F32)
        biasq = sb("biasq", [128, 64], BF16)
        small = sb("small", [128, 256], F32)
        smallb = sb("smallb", [128, 160], BF16)
        ss = small[:, 0:16]
        rstd = small[:, 16:32]
        tmpc = small[:, 32:48]
        aT = small[:, 48:56]
        shT = small[:, 56:64]
        gkT = small[:, 64:72]
        lbT = small[:, 72:88]
        oml = small[:, 88:96]
        noml = small[:, 184:192]
        gn = small[:, 96:97]
        bcol = small[:, 100:104]
        nbf = small[:, 104:105]
        ssy = small[:, 108:112]
        Bl = small[:, 112:144]
        Bl0 = small[:, 144:176]
        km = small[:, 176:184]
        cT = small[:, 192:224]
        shTb = smallb[:, 0:8]
        kmb = smallb[:, 8:16]
        birow = smallb[0:1, 16:144]
        scT = sb("scT", [128, 32], BF16)
        bmat = sb("bmat", [128, 128], BF16)

        def h1f(off, n):
            return h1[:, off:off + n]

        def h1b(off, n):
            return h1[:, off:off + n // 2].bitcast(BF16)

        o_ = 0
        T_q = h1f(o_, 512); o_ += 512
        T_s = h1f(o_, 512); o_ += 512
        T_d0 = h1f(o_, 512); o_ += 512
        T_d1 = h1f(o_, 512); o_ += 512
        T_B = h1f(o_, 512); o_ += 512
        attS = [h1b(o_, 512), h1b(o_ + 256, 512)]; o_ += 512
        osb = h1f(o_, 512); o_ += 512
        sqb = h1b(o_, 512); o_ += 256
        rst = h1f(o_, 512); o_ += 512
        tt_ = h1f(o_, 512); o_ += 512
        Ub = h1f(o_, 4096); o_ += 4096
        Sbf = h1b(o_, 4096); o_ += 2048
        arep = h1f(o_, 2048); RA = h1f(o_, 1024); RAi = h1[:, o_:o_ + 1024].bitcast(I32); o_ += 2048
        Shalf = h1f(o_, 2048); RB = h1f(o_, 1024); RBi = h1[:, o_:o_ + 1024].bitcast(I32); o_ += 2048
        T_q2 = h1f(o_, 512); o_ += 512
        T_B2 = h1f(o_, 512); o_ += 512
        assert o_ <= NT * D
        msb = h1f(0, 3072)
        modbb = h1f(3072, 3072)
        pgb = h1f(6144, 2048)
        rows = h1f(8192, 3072)

        pA = [ps("pA%d" % i, [128, 512], F32) for i in range(2)]
        pB = [ps("pB%d" % i, [128, 512], F32) for i in range(2)]
        pO = ps("pO", [128, 512], F32)
        pS = ps("pS", [128, 512], F32)
        pT0 = ps("pT0", [128, 1024], BF16)
        pM = ps("pM", [128, 512], F32)

        H1A = "h1all"

        def WK(k, g0=0, g1=4):
            return [("wbf", k, g) for g in range(g0, g1)]

        def top(hw, fn, reads=(), writes=()):
            return P.op(hw, fn, tuple(reads) + (H1A,), writes)

        def tdma(hw, slot, fn, reads=(), writes=()):
            return P.dma(hw, slot, fn, tuple(reads) + (H1A,), writes)

        def barrier():
            for e in ("act", "dve", "pool"):
                P.op(e, (lambda eng, e=e: (eng.memset(small[:, 250:251], 0.0) if e != "act" else
                                            eng.activation(out=small[:, 251:252], in_=small[:, 252:253], func=AF.Copy))),
                     writes=[("bar", e)])
            P.op("pe", lambda eng: eng.matmul(pM[0:1, 0:1], lhsT=onesb[:, 0:1], rhs=onesb[:, 0:1], start=True, stop=True),
                 reads=["cb"], writes=["pM", ("bar", "pe")])
            for e in ("act", "dve", "pool", "pe", "sp"):
                if e == "pe":
                    P.op("pe", lambda eng: eng.matmul(pM[0:1, 0:1], lhsT=onesb[:, 0:1], rhs=onesb[:, 0:1], start=True, stop=True),
                         reads=["cb"] + [("bar", q) for q in ("act", "dve", "pool")], writes=["pM"])
                elif e == "sp":
                    P.op("sp", lambda eng: eng.nop(), reads=[("bar", q) for q in ("act", "dve", "pool", "pe")])
                else:
                    P.op(e, (lambda eng, e=e: (eng.memset(small[:, 253:254], 0.0) if e == "dve" else
                                                eng.memset(small[:, 254:255], 0.0) if e == "pool" else
                                                eng.activation(out=small[:, 255:256], in_=small[:, 252:253], func=AF.Copy))),
                         reads=[("bar", q) for q in ("act", "dve", "pool", "pe") if q != e])

        P.dma("pool", "cb", lambda e: e.dma_start(out=cb[:], in_=cf[:, 0:NCB]), writes=["cb"])
        P.dma("sp", "cfs", lambda e: e.dma_start(out=cfs[:], in_=cf[:, NCB:NCF]), writes=["cfs"])
        P.op("dve", lambda e: e.memset(small[:], 0.0), writes=["small"])
        P.op("dve", lambda e: e.memset(bmat[:], 0.0), writes=["birow"])
        P.op("dve", lambda e: e.memset(biasT[:], 0.0), writes=["biasT"])
        P.op("pool", lambda e: e.memset(hb[3][:], 0.0), writes=[("kt", 0), ("kt", 1)])
        P.op("pool", lambda e: e.memset(hb[5][:], 0.0), writes=[("kt", 0), ("kt", 1)])
        with nc.allow_non_contiguous_dma(reason="tiny one-time transposed loads"):
            pass
        for bb in range(NB):
            P.dma("sp", "s0", lambda e, bb=bb: e.dma_start(out=cT.rearrange("p (c b) -> p c b", b=NB)[:, :, bb], in_=cin[bb].rearrange("(c p) -> p c", p=128),
                                                         allow_slow_non_contiguous=True), reads=["small"], writes=[("cT", bb)])
        P.dma("sp", "s1", lambda e: e.dma_start(out=gkT, in_=kv_g[0].rearrange("(c p) -> p c", p=128), allow_slow_non_contiguous=True),
              reads=["small"], writes=["gkT"])
        for l in range(2):
            P.dma("sp", "s2", lambda e, l=l: e.dma_start(out=lbT[:, l * 8:(l + 1) * 8], in_=a_lb[l].rearrange("(c p) -> p c", p=128),
                                                       allow_slow_non_contiguous=True), reads=["small"], writes=[("lbT", l)])
        P.dma("sp", "s3", lambda e: e.dma_start(out=gn, in_=a_gn), reads=["small"], writes=["gn"])
        P.op("act", lambda e: e.activation(out=scT[:], in_=cT, func=AF.Silu), reads=[("cT", q) for q in range(NB)], writes=["scT"])
        P.op("dve", lambda e: e.tensor_tensor(out=oml, in0=lbT[:, 8:16], in1=lbT[:, 0:8], op=ALU.subtract), reads=[("lbT", 0), ("lbT", 1)], writes=["oml"])
        P.op("act", lambda e: e.activation(out=oml, in_=oml, func=AF.Sigmoid), reads=["oml"], writes=["oml"])
        P.op("dve", lambda e: e.tensor_scalar(out=noml, in0=oml, scalar1=-1.0, scalar2=None, op0=ALU.mult), reads=["oml", "small"], writes=["noml"])
        wi = 0
        for l in range(2):
            tdma("sp", "s4", lambda e, l=l: e.dma_start(out=modbb[0:NB, :], in_=mod_b[l].partition_broadcast(NB)), writes=["modbb"])
            tdma("sp", "s5", lambda e, l=l: e.dma_start(out=pgb[0:NB, 0:1024], in_=pre_g[l].partition_broadcast(NB)), writes=["pgb0"])
            tdma("sp", "s6", lambda e, l=l: e.dma_start(out=pgb[0:NB, 1024:2048], in_=post_g[l].partition_broadcast(NB)), writes=["pgb1"])
            for cbk in range(6):
                k = wi % 2
                wi += 1
                P.dma("pool", "wbf%d" % k, lambda e, l=l, cbk=cbk, k=k: e.dma_start(
                    out=wbf[k][:], in_=mod_w[l][:, cbk * 512:(cbk + 1) * 512].rearrange("(c p) n -> p c n", p=128)),
                    writes=WK(k))
                for c in range(8):
                    P.op("pe", lambda e, c=c, k=k: e.matmul(pM[0:NB, :], lhsT=scT[:, c * NB:(c + 1) * NB], rhs=wbf[k][:, c, :],
                                                            start=(c == 0), stop=(c == 7)),
                         reads=["scT"] + WK(k), writes=["pM"])
                top("dve", lambda e, cbk=cbk: e.tensor_tensor(
                    out=msb[0:NB, cbk * 512:(cbk + 1) * 512], in0=pM[0:NB, :],
                    in1=modbb[0:NB, cbk * 512:(cbk + 1) * 512], op=ALU.add),
                    reads=["pM", "modbb"], writes=[("msb", cbk)])
            mk = [("msb", i) for i in range(6)]
            top("dve", lambda e: e.scalar_tensor_tensor(out=rows[0:NB, 0:1024], in0=msb[0:NB, 1024:2048],
                                                        scalar=1.0, in1=pgb[0:NB, 0:1024], op0=ALU.add, op1=ALU.mult),
                reads=mk + ["pgb0"], writes=["rows"])
            top("dve", lambda e: e.tensor_copy(out=rows[0:NB, 1024:2048], in_=msb[0:NB, 0:1024]), reads=mk, writes=["rows"])
            top("dve", lambda e: e.tensor_tensor(out=rows[0:NB, 2048:3072], in0=msb[0:NB, 2048:3072], in1=pgb[0:NB, 1024:2048], op=ALU.mult),
                reads=mk + ["pgb1"], writes=["rows"])
            for kind in range(3):
                tdma("sp", "s7", lambda e, l=l, kind=kind: e.dma_start(out=scr[l, kind], in_=rows[0:NB, kind * 1024:(kind + 1) * 1024]),
                     reads=["rows"], writes=[("scr", l, kind)])
        barrier()

        xin_i = [0]

        def layer_vectors(l, b):
            P.dma("sp", "v0", lambda e: e.dma_start(out=aT, in_=scr[l, 0, b].rearrange("(c p) -> p c", p=128), allow_slow_non_contiguous=True),
                  reads=[("scr", l, 0)], writes=["aT"])
            P.dma("sp", "v1", lambda e: e.dma_start(out=shT, in_=scr[l, 1, b].rearrange("(c p) -> p c", p=128), allow_slow_non_contiguous=True),
                  reads=[("scr", l, 1)], writes=["shT"])
            P.dma("sp", "v2", lambda e: e.dma_start(out=G_bc[:], in_=scr[l, 2, b].partition_broadcast(128)), reads=[("scr", l, 2)], writes=["G_bc"])
            P.op("dve", lambda e: e.tensor_copy(out=shTb, in_=shT), reads=["shT"], writes=["shTb"])

        def rstd_from(col_in, col_out, keyin, keyout):
            P.op("dve", lambda e: e.tensor_scalar(out=col_out, in0=col_in, scalar1=1.0 / D, scalar2=EPS, op0=ALU.mult, op1=ALU.add),
                 reads=[keyin], writes=[keyout])
            P.op("act", lambda e: e.activation(out=col_out, in_=col_out, func=AF.Sqrt), reads=[keyout], writes=[keyout])
            P.op("dve", lambda e: e.reciprocal(out=col_out, in_=col_out), reads=[keyout], writes=[keyout])

        def pre_phase(l, b):
            for tt in range(NT):
                k = tt % 2
                if l == 0:
                    xi = xin_i[0] % 2
                    xin_i[0] += 1
                    P.dma("sp", "xin%d" % xi, lambda e, tt=tt, xi=xi: e.dma_start(out=xin[xi][:], in_=x[b, tt * 128:(tt + 1) * 128, :]),
                          writes=[("xin", xi)])
                    src, skey = xin[xi][:], ("xin", xi)
                else:
                    src, skey = h1[:, tt * D:(tt + 1) * D], ("h1", tt)
                P.op("dve", lambda e, tt=tt: e.memset(ss[:, tt:tt + 1], 0.0), writes=[("ss", tt)])
                P.op("act", lambda e, src=src, tt=tt: e.activation(out=xs[0][:], in_=src, func=AF.Square, accum_out=ss[:, tt:tt + 1]),
                     reads=[skey, ("ss", tt)], writes=[("xs", 0), ("ss", tt)])
                rstd_from(ss[:, tt:tt + 1], rstd[:, tt:tt + 1], ("ss", tt), ("rstd", tt))
                P.op("dve", lambda e, src=src, tt=tt, k=k: e.tensor_scalar(out=xs[k][:], in0=src, scalar1=rstd[:, tt:tt + 1], scalar2=None, op0=ALU.mult),
                     reads=[skey, ("rstd", tt)], writes=[("xs", 0)])
                for c in range(8):
                    P.op("pe", lambda e, c=c, k=k: e.transpose(out=pT0[:, c * 128:(c + 1) * 128], in_=xs[k][:, c * 128:(c + 1) * 128], identity=ident),
                         reads=[("xs", 0), "cb"], writes=["pT0"])
                P.op("act", lambda e, tt=tt: e.activation(out=xnT[:, :, tt * 128:(tt + 1) * 128], in_=pT0[:].rearrange("p (c t) -> p c t", c=8), func=AF.Copy),
                     reads=["pT0"], writes=[("xnT", tt)])

        wslot = [0]

        def load_head_weights(pieces):
            k = wslot[0] % 2
            wslot[0] += 1
            for g, ap in enumerate(pieces):
                P.dma("pool", "wbf%d" % k, lambda e, g=g, ap=ap, k=k: e.dma_start(
                    out=wbf[k][:, :, g * 128:(g + 1) * 128], in_=ap.rearrange("(c p) n -> p c n", p=128)),
                    writes=[("wbf", k, g)])
            return k

        def bias_cols(k, groups):
            for j, g in enumerate(groups):
                for c in range(8):
                    P.op("pe", lambda e, j=j, g=g, c=c: e.matmul(pM[:, j:j + 1], lhsT=wbf[k][:, c, g * 128:(g + 1) * 128], rhs=shTb[:, c:c + 1],
                                                               start=(c == 0), stop=(c == 7)),
                         reads=[("wbf", k, g), "shTb"], writes=["pM"])
            P.op("dve", lambda e: e.tensor_copy(out=bcol[:, 0:len(groups)], in_=pM[:, 0:len(groups)]), reads=["pM"], writes=["bcol"])

        def scale_w(k, c0, c1, vec, vkey):
            P.op("pool", lambda e: e.tensor_tensor(out=wbf[k][:, :, c0:c1], in0=wbf[k][:, :, c0:c1],
                                                   in1=vec.unsqueeze(2).to_broadcast([128, 8, c1 - c0]), op=ALU.mult),
                 reads=WK(k, c0 // 128, c1 // 128) + [vkey], writes=WK(k, c0 // 128, c1 // 128))

        pa_i = [0]

        def proj_fm(k, g, tb):
            i = pa_i[0] % 2
            pa_i[0] += 1
            for c in range(8):
                P.op("pe", lambda e, c=c, i=i: e.matmul(pA[i][:], lhsT=wbf[k][:, c, g * 128:(g + 1) * 128], rhs=xnT[:, c, tb * 512:(tb + 1) * 512],
                                                      start=(c == 0), stop=(c == 7)),
                     reads=[("wbf", k, g)] + [("xnT", 4 * tb + q) for q in range(4)], writes=[("pA", i)])
            return i

        def proj_tm(k, g, dst, dkey, bias_row=False):
            for t4 in range(4):
                i = t4 % 2
                for j in range(4):
                    tt = t4 * 4 + j
                    for c in range(8):
                        P.op("pe", lambda e, c=c, tt=tt, j=j, i=i: e.matmul(pB[i][:, j * 128:(j + 1) * 128], lhsT=xnT[:, c, tt * 128:(tt + 1) * 128],
                                                                          rhs=wbf[k][:, c, g * 128:(g + 1) * 128], start=(c == 0),
                                                                          stop=(c == 7 and not bias_row)),
                             reads=[("wbf", k, g), ("xnT", tt)], writes=[("pB", i)])
                    if bias_row:
                        P.op("pe", lambda e, j=j, i=i: e.matmul(pB[i][:, j * 128:(j + 1) * 128], lhsT=onesb, rhs=bmat[:], start=False, stop=True),
                             reads=["cb", "birow"], writes=[("pB", i)])
                P.op("act", lambda e, t4=t4, i=i: e.activation(out=dst[:, t4 * 512:(t4 + 1) * 512], in_=pB[i][:], func=AF.Copy),
                     reads=[("pB", i)], writes=[(dkey, t4)])

        def post_phase(l, b, wout, last):
            for cbk in range(2):
                P.dma("pool", "wbf%d" % cbk, lambda e, cbk=cbk: e.dma_start(
                    out=wbf[cbk][:], in_=wout[:, cbk * 512:(cbk + 1) * 512].rearrange("(c p) n -> p c n", p=128)), writes=WK(cbk))
            wslot[0] = 0
            for tt in range(NT):
                for cbk in range(2):
                    for c in range(8):
                        P.op("pe", lambda e, c=c, cbk=cbk, tt=tt: e.matmul(pA[cbk][:], lhsT=ogT[:, c, tt * 128:(tt + 1) * 128], rhs=wbf[cbk][:, c, :],
                                                                         start=(c == 0), stop=(c == 7)),
                             reads=WK(cbk) + [("ogT", hh, tt // 4) for hh in range(8)], writes=[("pA", cbk)])
                    P.op("dve", lambda e, cbk=cbk: e.memset(ssy[:, cbk:cbk + 1], 0.0), writes=[("ssy", cbk)])
                    P.op("act", lambda e, cbk=cbk: e.activation(out=PT[cbk][:], in_=pA[cbk][:], func=AF.Square, accum_out=ssy[:, cbk:cbk + 1]),
                         reads=[("pA", cbk), ("ssy", cbk)], writes=[("PT", cbk), ("ssy", cbk)])
                P.op("dve", lambda e: e.tensor_tensor(out=ssy[:, 2:3], in0=ssy[:, 0:1], in1=ssy[:, 1:2], op=ALU.add),
                     reads=[("ssy", 0), ("ssy", 1)], writes=[("ssy", 2)])
                rstd_from(ssy[:, 2:3], ssy[:, 3:4], ("ssy", 2), ("ssy", 3))
                xi = xin_i[0] % 2
                xin_i[0] += 1
                if l == 0:
                    P.dma("sp", "xin%d" % xi, lambda e, tt=tt, xi=xi: e.dma_start(out=xin[xi][:], in_=x[b, tt * 128:(tt + 1) * 128, :]),
                          writes=[("xin", xi)])
                    dest, dkey, res, rkey = h1[:, tt * D:(tt + 1) * D], ("h1", tt), xin[xi][:], ("xin", xi)
                    extra_w = [H1A]
                else:
                    dest, dkey, res, rkey = xin[xi][:], ("xin", xi), h1[:, tt * D:(tt + 1) * D], ("h1", tt)
                    extra_w = [H1A]
                for cbk in range(2):
                    P.op("dve", lambda e, cbk=cbk, dest=dest: e.scalar_tensor_tensor(
                        out=dest[:, cbk * 512:(cbk + 1) * 512], in0=pA[cbk][:], scalar=ssy[:, 3:4], in1=G_bc[:, cbk * 512:(cbk + 1) * 512],
                        op0=ALU.mult, op1=ALU.mult), reads=[("pA", cbk), ("ssy", 3), "G_bc"], writes=[dkey] + (extra_w if l == 0 else []))
                P.op("dve", lambda e, dest=dest, res=res: e.tensor_tensor(out=dest, in0=dest, in1=res, op=ALU.add),
                     reads=[dkey, rkey], writes=[dkey] + extra_w)
                if last:
                    P.dma("sp", "xin%d" % xi, lambda e, tt=tt, dest=dest: e.dma_start(out=out[b, tt * 128:(tt + 1) * 128, :], in_=dest),
                          reads=[dkey], writes=[("out", b, tt)])

        def rope_tables(b):
            for hf in range(2):
                cs = slice(hf * 1024, (hf + 1) * 1024)
                tdma("sp", "ra", lambda e, cs=cs: e.dma_start(out=RAi, in_=pos[b, cs].partition_broadcast(128)), writes=["arep"])
                top("dve", lambda e: e.tensor_copy(out=RA, in_=RAi), reads=["arep"], writes=["arep"])
                top("dve", lambda e: e.tensor_scalar(out=RA, in0=RA, scalar1=invf, scalar2=None, op0=ALU.mult), reads=["arep", "cfs"], writes=["arep"])
                for which in range(2):
                    off = 0.0 if which == 0 else float(np.pi / 2)
                    top("dve", lambda e, off=off: e.tensor_scalar(out=RB, in0=RA, scalar1=off, scalar2=float(1.0 / (2 * np.pi)), op0=ALU.add, op1=ALU.mult),
                        reads=["arep"], writes=["Shalf"])
                    top("dve", lambda e: e.tensor_copy(out=RBi, in_=RB), reads=["Shalf"], writes=["Shalf"])
                    top("dve", lambda e: e.tensor_copy(out=RB, in_=RBi), reads=["Shalf"], writes=["Shalf"])
                    top("dve", lambda e: e.tensor_scalar(out=RB, in0=RB, scalar1=float(-2 * np.pi), scalar2=None, op0=ALU.mult), reads=["Shalf"], writes=["Shalf"])
                    top("dve", lambda e, off=off: e.scalar_tensor_tensor(out=RB, in0=RA, scalar=off, in1=RB, op0=ALU.add, op1=ALU.add),
                        reads=["arep", "Shalf"], writes=["Shalf"])
                    top("dve", lambda e: e.tensor_scalar(out=RB, in0=RB, scalar1=3.14159, scalar2=-3.14159, op0=ALU.min, op1=ALU.max),
                        reads=["Shalf"], writes=["Shalf"])
                    if which == 0:
                        top("act", lambda e: e.activation(out=RB, in_=RB, func=AF.Sin), reads=["Shalf"], writes=["Shalf"])
                        top("dve", lambda e, cs=cs: e.tensor_scalar(out=sinT[:, cs], in0=RB, scalar1=sgn, scalar2=None, op0=ALU.mult),
                            reads=["Shalf", "cfs"], writes=["sinT"])
                    else:
                        top("act", lambda e, cs=cs: e.activation(out=cosT[:, cs], in_=RB, func=AF.Sin), reads=["Shalf"], writes=["cosT"])

        def l0_head(h, k):
            qtT, ktT, sgT, kt, vv, ktB = hb[0], hb[1], hb[2], hb[3], hb[4], hb[5]
            bias_cols(k, [0, 1, 3])
            P.op("dve", lambda e: e.tensor_scalar(out=nbf, in0=bcol[:, 1:2], scalar1=-1.0, scalar2=None, op0=ALU.mult), reads=["bcol"], writes=["nbf"])
            for c in range(8):
                P.op("pe", lambda e, c=c: e.matmul(pM[0:1, 128:256], lhsT=shTb[:, c:c + 1], rhs=wbf[k][:, c, 256:384], start=(c == 0), stop=(c == 7)),
                     reads=[("wbf", k, 2), "shTb"], writes=["pM"])
            P.op("dve", lambda e: e.tensor_copy(out=bmat[0:1, :], in_=pM[0:1, 128:256]), reads=["pM"], writes=["birow"])
            scale_w(k, 0, 512, aT, "aT")
            knext = None
            if h < 7:
                knext = load_head_weights([a_w_in[:, g * 1024 + (h + 1) * 128:g * 1024 + (h + 2) * 128] for g in range(4)])
            for tb in range(4):
                bs = slice(tb * 512, (tb + 1) * 512)
                Tq, TB = (T_q, T_B) if tb % 2 == 0 else (T_q2, T_B2)
                kq, kB = ("T_q", "T_B") if tb % 2 == 0 else ("T_q2", "T_B2")
                i = proj_fm(k, 0, tb)
                top("act", lambda e, i=i, Tq=Tq: e.activation(out=Tq, in_=pA[i][:], func=AF.Silu, bias=bcol[:, 0:1]), reads=[("pA", i), "bcol"], writes=[kq])
                i = proj_fm(k, 3, tb)
                P.op("act", lambda e, i=i, bs=bs: e.activation(out=sgT[:, bs], in_=pA[i][:], func=AF.Silu, bias=bcol[:, 2:3]),
                     reads=[("pA", i), "bcol"], writes=[("sgT", tb)])
                i = proj_fm(k, 1, tb)
                top("act", lambda e, i=i: e.activation(out=T_s, in_=pA[i][:], func=AF.Sigmoid, bias=nbf, scale=-1.0), reads=[("pA", i), "nbf"], writes=["T_s"])
                top("dve", lambda e: e.tensor_scalar(out=T_d0, in0=T_s, scalar1=noml[:, h:h + 1], scalar2=1.0, op0=ALU.mult, op1=ALU.add),
                    reads=["T_s", "noml"], writes=["T_d0"])
                if tb == 0 and h == 0:
                    top("dve", lambda e: e.memset(T_d1, 0.0), writes=["T_d1"])
                d0v = T_d0.rearrange("p (n c) -> p n c", c=64)[:, :, 0:1]
                d1v = T_d1.rearrange("p (n c) -> p n c", c=64)[:, :, 0:1]
                top("dve", lambda e, d0v=d0v, d1v=d1v: e.tensor_copy(out=d1v, in_=d0v), reads=["T_d0", "T_d1"], writes=["T_d1"])
                top("dve", lambda e, d0v=d0v: e.memset(d0v, 0.0), reads=["T_d0", "T_d1"], writes=["T_d0"])
                top("dve", lambda e, TB=TB: e.tensor_tensor_scan(out=TB, data0=T_d0, data1=T_d1, initial=0.0, op0=ALU.mult, op1=ALU.add),
                    reads=["T_d0", "T_d1"], writes=[kB])
                top("dve", lambda e, tb=tb, TB=TB: e.tensor_copy(out=Bl[:, tb * 8:(tb + 1) * 8].unsqueeze(2),
                                                                 in_=TB.rearrange("p (n c) -> p n c", c=64)[:, :, 63:64]), reads=[kB], writes=["Bl"])
                top("pool", lambda e, bs=bs, Tq=Tq, TB=TB: e.tensor_tensor(out=qtT[:, bs], in0=Tq, in1=TB, op=ALU.mult), reads=[kq, kB], writes=[("qtT", tb)])
                top("dve", lambda e, TB=TB: e.reciprocal(out=T_d0, in_=TB), reads=[kB], writes=["T_d0"])
                top("dve", lambda e, bs=bs: e.scalar_tensor_tensor(out=ktT[:, bs], in0=T_s, scalar=oml[:, h:h + 1], in1=T_d0, op0=ALU.mult, op1=ALU.mult),
                    reads=["T_s", "T_d0", "oml"], writes=[("ktT", tb)])
            if limit == "h0a":
                return knext
            proj_tm(k, 2, vv, "vv", bias_row=True)
            for t8 in range(2):
                for j in range(8):
                    tt = t8 * 8 + j
                    P.op("pe", lambda e, tt=tt, j=j: e.transpose(out=pT0[:, j * 128:(j + 1) * 128], in_=ktT[:, tt * 128:(tt + 1) * 128], identity=ident),
                         reads=[("ktT", tt // 4), "cb"], writes=["pT0"])
                P.op("act", lambda e, t8=t8: e.activation(out=kt[0:64, t8 * 1024:(t8 + 1) * 1024], in_=pT0[0:64, :], func=AF.Copy), reads=["pT0"], writes=[("kt", t8)])
                P.op("act", lambda e, t8=t8: e.activation(out=ktB[64:128, t8 * 1024:(t8 + 1) * 1024], in_=pT0[64:128, :], func=AF.Copy), reads=["pT0"], writes=[("kt", t8)])
            if limit == "h0b":
                return knext
            Uv = Ub.rearrange("p (v n) -> p n v", n=32)
            for ng in range(8):
                i = ng % 2
                for j in range(4):
                    n = ng * 4 + j
                    tt, half = n // 2, n % 2
                    ksrc = kt if half == 0 else ktB
                    P.op("pe", lambda e, j=j, tt=tt, ksrc=ksrc, i=i: e.matmul(pB[i][:, j * 128:(j + 1) * 128], lhsT=ksrc[:, tt * 128:(tt + 1) * 128],
                                                                            rhs=vv[:, tt * 128:(tt + 1) * 128], start=True, stop=True),
                         reads=[("kt", tt // 8), ("vv", tt // 4)], writes=[("pB", i)])
                top("dve", lambda e, ng=ng, i=i: e.tensor_tensor(out=Uv[:, ng * 4:(ng + 1) * 4, :], in0=pB[i][:].rearrange("p (n v) -> p n v", n=4),
                                                                 in1=Bl[:, ng * 4:(ng + 1) * 4].unsqueeze(2).to_broadcast([128, 4, 128]), op=ALU.mult),
                    reads=[("pB", i), "Bl"], writes=["Ub"])
            if limit == "h0c1":
                return knext
            top("dve", lambda e: e.tensor_copy(out=Bl0, in_=Bl), reads=["Bl"], writes=["Bl0"])
            top("dve", lambda e: e.memset(Bl0[:, 0:1], 0.0), reads=["Bl0"], writes=["Bl0"])
            top("pool", lambda e: e.tensor_copy(out=arep.rearrange("p (v n) -> p v n", n=32), in_=Bl0.unsqueeze(1).to_broadcast([128, 64, 32])),
                reads=["Bl0"], writes=["arep"])
            if limit == "h0c2":
                return knext
            for vh in range(2):
                top("dve", lambda e, vh=vh: e.tensor_tensor_scan(out=Shalf, data0=arep, data1=Ub[:, vh * 2048:(vh + 1) * 2048], initial=0.0,
                                                                 op0=ALU.mult, op1=ALU.add),
                    reads=["arep", "Ub"], writes=["Shalf"])
                if limit == "h0c3":
                    continue
                top("act", lambda e, vh=vh: e.activation(out=Sbf.rearrange("p (n v) -> p v n", n=32)[:, vh * 64:(vh + 1) * 64, :],
                                                         in_=Shalf.rearrange("p (v n) -> p v n", n=32), func=AF.Copy),
                    reads=["Shalf"], writes=["Sbf"])
            if limit in ("h0c", "h0c3"):
                return knext
            for pg in range(4):
                i = pg % 2
                for j in range(4):
                    pr = pg * 4 + j
                    P.op("pe", lambda e, j=j, pr=pr, i=i: e.matmul(pB[i][:, j * 128:(j + 1) * 128], lhsT=ktT[:, pr * 128:(pr + 1) * 128],
                                                                 rhs=qtT[:, pr * 128:(pr + 1) * 128], start=True, stop=True),
                         reads=[("ktT", pg), ("qtT", pg)], writes=[("pB", i)])
                top("dve", lambda e, i=i: e.tensor_tensor(out=attS[i].rearrange("p (n t) -> p n t", n=4), in0=pB[i][:].rearrange("p (n t) -> p n t", n=4),
                                                          in1=pmask.unsqueeze(1).to_broadcast([128, 4, 128]), op=ALU.mult),
                    reads=[("pB", i), "cb"], writes=[("attS", i)])
                for j in range(4):
                    pr = pg * 4 + j
                    P.op("pe", lambda e, j=j, pr=pr, i=i: e.matmul(pO[:, j * 128:(j + 1) * 128], lhsT=vv[:, pr * 128:(pr + 1) * 128],
                                                                 rhs=attS[i][:, j * 128:(j + 1) * 128], start=True, stop=False, skip_group_check=True),
                         reads=[("vv", pg), ("attS", i), H1A], writes=["pO"])
                    if pr > 0:
                        P.op("pe", lambda e, j=j, pr=pr: e.matmul(pO[:, j * 128:j * 128 + 64], lhsT=Sbf[:, (2 * pr - 1) * 128:(2 * pr) * 128],
                                                                rhs=qtT[:, pr * 128:pr * 128 + 64], start=False, stop=False, skip_group_check=True),
                             reads=["Sbf", ("qtT", pg), H1A], writes=["pO"])
                    P.op("pe", lambda e, j=j, pr=pr: e.matmul(pO[:, j * 128 + 64:(j + 1) * 128], lhsT=Sbf[:, (2 * pr) * 128:(2 * pr + 1) * 128],
                                                            rhs=qtT[:, pr * 128 + 64:(pr + 1) * 128], start=False, stop=True, skip_group_check=True),
                         reads=["Sbf", ("qtT", pg), H1A], writes=["pO"])
                top("act", lambda e: e.activation(out=osb, in_=pO[:], func=AF.Copy), reads=["pO"], writes=["osb"])
                top("act", lambda e: e.activation(out=sqb, in_=pO[:], func=AF.Square), reads=["pO"], writes=["sqb"])
                P.op("pe", lambda e: e.matmul(pS[:], lhsT=onesb, rhs=sqb, start=True, stop=True), reads=["sqb", "cb", H1A], writes=["pS"])
                top("dve", lambda e: e.tensor_scalar(out=rst, in0=pS[:], scalar1=1.0 / 128, scalar2=EPS, op0=ALU.mult, op1=ALU.add), reads=["pS"], writes=["rst"])
                top("act", lambda e: e.activation(out=rst, in_=rst, func=AF.Sqrt), reads=["rst"], writes=["rst"])
                top("dve", lambda e: e.reciprocal(out=rst, in_=rst), reads=["rst"], writes=["rst"])
                top("dve", lambda e: e.scalar_tensor_tensor(out=tt_, in0=osb, scalar=gn, in1=rst, op0=ALU.mult, op1=ALU.mult),
                    reads=["osb", "rst", "gn"], writes=["tt_"])
                top("dve", lambda e, pg=pg: e.tensor_tensor(out=ogT[:, h, pg * 512:(pg + 1) * 512], in0=tt_, in1=sgT[:, pg * 512:(pg + 1) * 512], op=ALU.mult),
                    reads=["tt_", ("sgT", pg)], writes=[("ogT", h, pg)])
            return knext

        pt_i = [0]

        def l1_pieces(h):
            return [b_w_in[:, h * 128:(h + 1) * 128], b_w_in[:, 1024 + h * 128:1024 + (h + 1) * 128],
                    w_kv[:, h * 128:(h + 1) * 128], w_kv[:, 1024 + h * 128:1024 + (h + 1) * 128]]

        def l1_head(h, k):
            QT, KT, szT, V = hb[0], hb[1], hb[2], hb[4]
            bias_cols(k, [0, 1])
            scale_w(k, 0, 256, aT, "aT")
            scale_w(k, 256, 512, gkT, "gkT")
            knext = load_head_weights(l1_pieces(h + 1)) if h < 7 else None
            if limit == "l1a0":
                return knext
            for tb in range(4):
                bs = slice(tb * 512, (tb + 1) * 512)
                for which in range(2):
                    if limit == "l1a1" and (tb, which) == (0, 1):
                        return knext
                    if limit == "l1a2" and (tb, which) == (1, 0):
                        return knext
                    dst = QT if which == 0 else KT
                    dkey = "QT" if which == 0 else "KT"
                    i = proj_fm(k, 0 if which == 0 else 2, tb)
                    j = (tb * 2 + which) % 2
                    if which == 0:
                        P.op("dve", lambda e, i=i: e.tensor_scalar(out=PT[0][:], in0=pA[i][:], scalar1=bcol[:, 0:1], scalar2=None, op0=ALU.add),
                            reads=[("pA", i), "bcol"], writes=[("PT", 0)])
                        P.op("dve", lambda e, i=i, bs=bs: e.scalar_tensor_tensor(out=rsb[:], in0=pA[i][:], scalar=bcol[:, 0:1], in1=cosT[:, bs], op0=ALU.add, op1=ALU.mult),
                            reads=[("pA", i), "bcol", "cosT"], writes=["rsb"])
                    else:
                        P.op("act", lambda e, i=i: e.activation(out=PT[0][:], in_=pA[i][:], func=AF.Copy), reads=[("pA", i)], writes=[("PT", 0)])
                        P.op("dve", lambda e, i=i, bs=bs: e.tensor_tensor(out=rsb[:], in0=pA[i][:], in1=cosT[:, bs], op=ALU.mult),
                            reads=[("pA", i), "cosT"], writes=["rsb"])
                    P.op("pe", lambda e, j=j: e.matmul(pB[j][:], lhsT=swp, rhs=PT[0][:], start=True, stop=True), reads=[("PT", 0), "cb"], writes=[("pB", j)])
                    P.op("dve", lambda e, j=j, bs=bs: e.tensor_tensor(out=otb[:], in0=pB[j][:], in1=sinT[:, bs], op=ALU.mult), reads=[("pB", j), "sinT"], writes=["otb"])
                    P.op("dve", lambda e, dst=dst, bs=bs: e.tensor_tensor(out=dst[:, bs], in0=rsb[:], in1=otb[:], op=ALU.add), reads=["rsb", "otb"], writes=[(dkey, tb)])
                i = proj_fm(k, 1, tb)
                P.op("act", lambda e, i=i, bs=bs: e.activation(out=szT[:, bs], in_=pA[i][:], func=AF.Silu, bias=bcol[:, 1:2]),
                     reads=[("pA", i), "bcol"], writes=[("szT", tb)])
            if limit == "l1a":
                return knext
            proj_tm(k, 3, V, "V")
            P.op("dve", lambda e: e.tensor_reduce(out=km, in_=KT[:].rearrange("p (n k) -> p n k", k=256), axis=AX.X, op=ALU.add),
                 reads=[("KT", q) for q in range(4)], writes=["km"])
            P.op("dve", lambda e: e.tensor_scalar(out=kmb, in0=km, scalar1=1.0 / 256, scalar2=None, op0=ALU.mult), reads=["km"], writes=["kmb"])
            for i8 in range(8):
                P.op("pe", lambda e, i8=i8: e.matmul(pM[:, i8 * 8:(i8 + 1) * 8], lhsT=QT[:, (8 + i8) * 128:(9 + i8) * 128], rhs=kmb, start=True, stop=True),
                     reads=[("QT", (8 + i8) // 4), "kmb"], writes=["pM"])
            P.op("dve", lambda e: e.tensor_tensor(out=gbuf[:], in0=pM[:, 0:64], in1=negm, op=ALU.add), reads=["pM", "cfs"], writes=["gbuf"])
            g3 = gbuf[:].rearrange("p (i n) -> p i n", n=8)
            P.op("dve", lambda e: e.tensor_tensor(out=cmpb[:], in0=g3.unsqueeze(2).to_broadcast([128, 8, 8, 8]),
                                                  in1=g3.unsqueeze(3).to_broadcast([128, 8, 8, 8]), op=ALU.is_gt), reads=["gbuf"], writes=["cmpb"])
            P.op("dve", lambda e: e.tensor_reduce(out=rankb[:], in_=cmpb[:].rearrange("p i n m -> p (i n) m"), axis=AX.X, op=ALU.add),
                 reads=["cmpb"], writes=["rankb"])
            bq_pad = cmpb[:].rearrange("p a b c -> p (a b c)").bitcast(BF16).rearrange("p (i c) -> p i c", c=128)
            P.op("dve", lambda e: e.memset(bq_pad, 0.0), reads=["rankb"], writes=["cmpb"])
            P.op("dve", lambda e: e.tensor_scalar(out=bq_pad[:, :, 0:8], in0=rankb[:].rearrange("p (i n) -> p i n", n=8), scalar1=2.5, scalar2=-BIG,
                                                  op0=ALU.is_ge, op1=ALU.mult), reads=["rankb", "cmpb"], writes=["cmpb"])
            for i8 in range(8):
                P.op("pe", lambda e, i8=i8: e.transpose(out=pT0[:, i8 * 128:(i8 + 1) * 128], in_=bq_pad[:, i8, :], identity=ident),
                     reads=["cmpb", "cb"], writes=["pT0"])
            P.op("act", lambda e: e.activation(out=biasT[:], in_=pT0[:], func=AF.Copy), reads=["pT0"], writes=["biasT"])
            if limit == "l1b":
                return knext
            sc = float(128 ** -0.5)
            pT0f = pT0[:].bitcast(F32)
            for g in range(4):
                nkt = 4 * g + 4
                if g % 2 == 0:
                    aO, aS, kO, kS = pO[:], pS[:], "pO", "pS"
                else:
                    aO, aS, kO, kS = pT0f, pM[:], "pT0", "pM"
                for ktile in range(nkt):
                    c0 = max(ktile - 4 * g, 0)
                    cl = slice(c0 * 128, 512)
                    j = pt_i[0] % 2
                    r = pt_i[0] % 3
                    pt_i[0] += 1
                    mm = [(cl, KT[:, ktile * 128:(ktile + 1) * 128], QT[:, g * 512 + c0 * 128:(g + 1) * 512], [("KT", ktile // 4), ("QT", g)])]
                    if g >= 2:
                        n = ktile // 2
                        lo = max(2 * n + 2 - 4 * g, c0)
                        if lo < 4:
                            mm.append((slice(lo * 128, 512), cb[:, C_ES + n * 128:C_ES + (n + 1) * 128],
                                       biasT[:, (g - 2) * 512 + lo * 128:(g - 2) * 512 + 512], ["biasT", "cb"]))
                    if ktile >= 4 * g:
                        mm.append((slice(c0 * 128, c0 * 128 + 128), ident, cmask, ["cb"]))
                    for mi, (csl, l_, r_, rk) in enumerate(mm):
                        P.op("pe", lambda e, csl=csl, l_=l_, r_=r_, mi=mi, j=j, last=(mi == len(mm) - 1): e.matmul(
                            pB[j][:, csl], lhsT=l_, rhs=r_, start=(mi == 0), stop=last, skip_group_check=True),
                            reads=rk, writes=[("pB", j)])
                    P.op("act", lambda e, j=j, r=r, cl=cl: e.activation(out=PT[r][:, cl], in_=pB[j][:, cl], func=AF.Exp, scale=sc),
                         reads=[("pB", j)], writes=[("PT", r)])
                    P.op("pe", lambda e, r=r, cl=cl, ktile=ktile, nkt=nkt, aO=aO: e.matmul(aO[:, cl], lhsT=V[:, ktile * 128:(ktile + 1) * 128], rhs=PT[r][:, cl],
                                                                                          start=(ktile == 0), stop=(ktile == nkt - 1), skip_group_check=True),
                         reads=[("V", ktile // 4), ("PT", r)], writes=[kO])
                    P.op("pe", lambda e, r=r, cl=cl, ktile=ktile, nkt=nkt, aS=aS: e.matmul(aS[:, cl], lhsT=onesb, rhs=PT[r][:, cl],
                                                                                          start=(ktile == 0), stop=(ktile == nkt - 1), skip_group_check=True),
                         reads=["cb", ("PT", r)], writes=[kS])
                P.op("dve", lambda e, aS=aS: e.reciprocal(out=rsb[:], in_=aS), reads=[kS], writes=["rsb"])
                P.op("dve", lambda e, aO=aO: e.tensor_tensor(out=otb[:], in0=aO, in1=rsb[:], op=ALU.mult), reads=[kO, "rsb"], writes=["otb"])
                P.op("dve", lambda e, g=g: e.tensor_tensor(out=ogT[:, h, g * 512:(g + 1) * 512], in0=otb[:], in1=szT[:, g * 512:(g + 1) * 512], op=ALU.mult),
                     reads=["otb", ("szT", g)], writes=[("ogT", h, g)])
            return knext

        for b in range(nseq):
            if limit == "setup":
                break
            rope_tables(b)
            layer_vectors(0, b)
            if limit == "rope":
                break
            kw = load_head_weights([a_w_in[:, g * 1024:g * 1024 + 128] for g in range(4)])
            pre_phase(0, b)
            if limit == "pre":
                break
            for h in range(8):
                kw = l0_head(h, kw)
                if limit in ("head0", "h0a", "h0b", "h0c", "h0c1", "h0c2", "h0c3"):
                    break
            if limit in ("head0", "heads", "h0a", "h0b", "h0c", "h0c1", "h0c2", "h0c3"):
                break
            post_phase(0, b, a_w_out, last=stop_after_l0)
            if stop_after_l0:
                continue
            layer_vectors(1, b)
            kw = load_head_weights(l1_pieces(0))
            pre_phase(1, b)
            if limit == "l1pre":
                break
            for h in range(8):
                kw = l1_head(h, kw)
                if limit in ("l1a", "l1b", "l1c", "l1a0", "l1a1", "l1a2"):
                    break
            if limit in ("l1a", "l1b", "l1c", "l1a0", "l1a1", "l1a2"):
                break
            post_phase(1, b, b_w_out, last=True)
        if limit is not None:
            P.dma("sp", "dbg", lambda e: e.dma_start(out=out[0, 0:128, :], in_=xin[0][:]), reads=[("xin", 0)], writes=[("out", 0, 0)])
            P.op("sp", lambda e: e.nop(), reads=[("out", 0, 0)])
            for e_ in ("act", "dve", "pool", "pe"):
                pass
        else:
            P.op("sp", lambda e: e.nop(), reads=[("out", b, tt) for b in range(nseq) for tt in range(NT)])

        cnt = P.analyze()
        names = set(cnt.keys()) | set(HW)
        sems = {s: es.enter_context(nc.semaphore(s.replace(":", "_"))) for s in sorted(names)}
        with nc.Block() as block:
            P.emit(block, sems)
    return nc, len(P.ops), cnt


_CACHE = {}


def kernel(x, c, positions, mod_w, mod_b, pre_norm_g, post_norm_g, a_w_in, a_w_out, a_out_norm_g,
           a_lb_logits, kv_norm_g, w_kv, b_w_in, b_w_out):
    if "nc" not in _CACHE:
        _CACHE["nc"] = build()[0]
    nc = _CACHE["nc"]
    f = lambda a: np.ascontiguousarray(np.asarray(a), dtype=np.float32)
    shared = {
        "mod_w": f(mod_w), "mod_b": f(mod_b), "pre_g": f(pre_norm_g), "post_g": f(post_norm_g),
        "a_w_in": f(a_w_in)[0], "a_w_out": f(a_w_out)[0], "a_gn": f(a_out_norm_g).reshape(128, 1),
        "a_lb": f(a_lb_logits), "kv_g": f(kv_norm_g).reshape(1, D), "w_kv": f(w_kv),
        "b_w_in": f(b_w_in)[0], "b_w_out": f(b_w_out)[0], "cf": make_consts(),
    }
    x = f(x)
    c = f(c)
    positions = np.ascontiguousarray(np.asarray(positions), dtype=np.int32)
    in_maps = []
    for i in range(8):
        m = dict(shared)
        m["x"] = x[i * NB:(i + 1) * NB]
        m["c"] = c[i * NB:(i + 1) * NB]
        m["pos"] = positions[i * NB:(i + 1) * NB]
        in_maps.append(m)
    res = run_bass_kernel_spmd(nc, in_maps, core_ids=list(range(8)))
    return np.concatenate([r["out"] for r in res.results], axis=0)
```

```python
import numpy as np
from contextlib import ExitStack
import concourse.bass as bass
import concourse.mybir as mybir
from concourse.bass_utils import run_bass_kernel_spmd

F32 = mybir.dt.float32
BF16 = mybir.dt.bfloat16
I32 = mybir.dt.int32
ALU = mybir.AluOpType
AF = mybir.ActivationFunctionType
AX = mybir.AxisListType

NB = 4
T = 2048
D = 1024
NT = 16
EPS = 1e-6
BIG = 30000.0
HW = ("pe", "act", "dve", "pool", "sp")
PSUM_KEYS = ("pA", "pB", "pO", "pS", "pT0", "pM")

C_ID, C_CM, C_PM, C_SW, C_ON, C_ES, C_NEG, C_INV, C_SGN, NCF = 0, 128, 256, 384, 512, 640, 1664, 1728, 1729, 1730
NCB = 1664


class Op:
    __slots__ = ("hw", "fn", "reads", "writes", "sem", "inc", "signal", "value", "waits", "idx", "is_dma")


class Prog:
    def __init__(self):
        self.ops = []

    def op(self, hw, fn, reads=(), writes=()):
        o = Op()
        o.hw, o.fn, o.reads, o.writes = hw, fn, tuple(reads), tuple(writes)
        o.sem, o.inc, o.signal, o.value, o.waits, o.is_dma = hw, 1, False, 0, [], False
        o.idx = len(self.ops)
        self.ops.append(o)
        return o

    def dma(self, hw, slot, fn, reads=(), writes=()):
        o = self.op(hw, fn, reads, writes)
        o.sem, o.inc, o.signal, o.is_dma = "dma:" + slot, 16, True, True
        return o

    def analyze(self):
        last_w, readers = {}, {}
        for o in self.ops:
            deps = {}
            for k in o.reads:
                p = last_w.get(k)
                if p is not None:
                    deps[p.idx] = (p, "raw")
                kn = k[0] if isinstance(k, tuple) else k
                if kn in PSUM_KEYS:
                    for r in readers.get(k, {}).values():
                        if r.hw != o.hw and r.idx not in deps:
                            deps[r.idx] = (r, "rar")
            for k in o.writes:
                p = last_w.get(k)
                if p is not None and p.idx not in deps:
                    deps[p.idx] = (p, "waw")
                for r in readers.get(k, {}).values():
                    if r.idx not in deps and r is not o:
                        deps[r.idx] = (r, "war")
            for p, kind in deps.values():
                if (not p.is_dma) and (not o.is_dma) and p.hw == o.hw:
                    if o.hw == "pe" or kind != "raw":
                        continue
                o.waits.append(p)
                p.signal = True
            for k in o.reads:
                readers.setdefault(k, {})[("d", o.idx) if o.is_dma else o.hw] = o
            for k in o.writes:
                last_w[k] = o
                readers[k] = {}
        cnt = {}
        for o in self.ops:
            if o.signal:
                cnt[o.sem] = cnt.get(o.sem, 0) + o.inc
                o.value = cnt[o.sem]
        return cnt

    def emit(self, block, sems):
        streams = {h: [] for h in HW}
        for o in self.ops:
            streams[o.hw].append(o)

        def make(hwname):
            def body(eng):
                known = {}
                for o in streams[hwname]:
                    need = {}
                    for p in o.waits:
                        if p.value > need.get(p.sem, 0):
                            need[p.sem] = p.value
                    for s, v in need.items():
                        if known.get(s, 0) < v:
                            eng.wait_ge(sems[s], v)
                            known[s] = v
                    ins = o.fn(eng)
                    if o.signal:
                        ins.then_inc(sems[o.sem], o.inc)
            return body

        block.tensor(make("pe"))
        block.scalar(make("act"))
        block.vector(make("dve"))
        block.gpsimd(make("pool"))
        block.sync(make("sp"))


def make_consts():
    cf = np.zeros((128, NCF), np.float32)
    p = np.arange(128)
    cf[:, C_ID:C_ID + 128] = np.eye(128, dtype=np.float32)
    cf[:, C_CM:C_CM + 128] = np.where(p[:, None] > p[None, :], -BIG, 0.0)
    cf[:, C_PM:C_PM + 128] = ((p[:, None] // 64 == p[None, :] // 64) & (p[:, None] <= p[None, :])).astype(np.float32)
    cf[:, C_SW:C_SW + 128] = (p[:, None] == (p[None, :] + 64) % 128).astype(np.float32)
    cf[:, C_ON:C_ON + 128] = 1.0
    for n in range(8):
        cf[n, C_ES + n * 128:C_ES + (n + 1) * 128] = 1.0
    neg = np.zeros((8, 8), np.float32)
    for i in range(8):
        j = (8 + i) // 2
        neg[i, j:] = -1e30
    cf[:, C_NEG:C_NEG + 64] = neg.reshape(1, 64)
    inv = np.float32(10000.0) ** (-np.arange(0, 128, 2, dtype=np.float32) / np.float32(128))
    cf[:, C_INV] = np.concatenate([inv, inv]).astype(np.float32)
    cf[:, C_SGN] = np.where(p < 64, -1.0, 1.0)
    return cf


def build(stop_after_l0=False, nseq=NB, limit=None):
    nc = bass.Bass("TRN2", target_bir_lowering=False)

    def din(name, shape, dt=F32):
        return nc.dram_tensor(name, list(shape), dt, kind="ExternalInput").ap()

    x = din("x", [NB, T, D])
    cin = din("c", [NB, D])
    pos = din("pos", [NB, T], I32)
    mod_w = din("mod_w", [2, D, 3 * D])
    mod_b = din("mod_b", [2, 3 * D])
    pre_g = din("pre_g", [2, D])
    post_g = din("post_g", [2, D])
    a_w_in = din("a_w_in", [D, 4 * D])
    a_w_out = din("a_w_out", [D, D])
    a_gn = din("a_gn", [128, 1])
    a_lb = din("a_lb", [2, D])
    kv_g = din("kv_g", [1, D])
    w_kv = din("w_kv", [D, 2 * D])
    b_w_in = din("b_w_in", [D, 2 * D])
    b_w_out = din("b_w_out", [D, D])
    cf = din("cf", [128, NCF])
    out = nc.dram_tensor("out", [NB, T, D], F32, kind="ExternalOutput").ap()
    scr = nc.dram_tensor("scr", [2, 3, NB, D], F32, kind="Internal").ap()

    P = Prog()
    es = ExitStack()
    with es:
        def sb(name, shape, dt):
            return es.enter_context(nc.sbuf_tensor(name, list(shape), dt))

        def ps(name, shape, dt):
            return es.enter_context(nc.psum_tensor(name, list(shape), dt))

        cb = sb("cb", [128, NCB], BF16)
        cfs = sb("cfs", [128, NCF - NCB], F32)
        ident = cb[:, C_ID:C_ID + 128]
        cmask = cb[:, C_CM:C_CM + 128]
        pmask = cb[:, C_PM:C_PM + 128]
        swp = cb[:, C_SW:C_SW + 128]
        onesb = cb[:, C_ON:C_ON + 128]
        negm = cfs[:, 0:64]
        invf = cfs[:, 64:65]
        sgn = cfs[:, 65:66]

        xnT = sb("xnT", [128, 8, T], BF16)
        ogT = sb("ogT", [128, 8, T], BF16)
        h1 = sb("h1", [128, NT * D], F32)
        wbf = [sb("wbf%d" % i, [128, 8, 512], BF16) for i in range(2)]
        hb = [sb("hb%d" % i, [128, T], BF16) for i in range(6)]
        cosT = sb("cosT", [128, T], BF16)
        sinT = sb("sinT", [128, T], BF16)
        xin = [sb("xin%d" % i, [128, D], F32) for i in range(2)]
        xs = [sb("xs0", [128, D], BF16)] * 2
        G_bc = sb("G_bc", [128, D], F32)
        PT = [sb("PT%d" % i, [128, 512], BF16) for i in range(3)]
        rsb = sb("rsb", [128, 512], F32)
        otb = sb("otb", [128, 512], F32)
        biasT = sb("biasT", [128, 1024], BF16)
        gbuf = sb("gbuf", [128, 64], F32)
        cmpb = sb("cmpb", [128, 8, 8, 8], F32)
        rankb = sb("rankb", [128, 64], F32)
        biasq = sb("biasq", [128, 64], BF16)
        small = sb("small", [128, 256], F32)
        smallb = sb("smallb", [128, 160], BF16)
        ss = small[:, 0:16]
        rstd = small[:, 16:32]
        tmpc = small[:, 32:48]
        aT = small[:, 48:56]
        shT = small[:, 56:64]
        gkT = small[:, 64:72]
        lbT = small[:, 72:88]
        oml = small[:, 88:96]
        noml = small[:, 184:192]
        gn = small[:, 96:97]
        bcol = small[:, 100:104]
        nbf = small[:, 104:105]
        ssy = small[:, 108:112]
        Bl = small[:, 112:144]
        Bl0 = small[:, 144:176]
        km = small[:, 176:184]
        cT = small[:, 192:224]
        shTb = smallb[:, 0:8]
        kmb = smallb[:, 8:16]
        birow = smallb[0:1, 16:144]
        scT = sb("scT", [128, 32], BF16)
        bmat = sb("bmat", [128, 128], BF16)

        def h1f(off, n):
            return h1[:, off:off + n]

        def h1b(off, n):
            return h1[:, off:off + n // 2].bitcast(BF16)

        o_ = 0
        T_q = h1f(o_, 512); o_ += 512
        T_s = h1f(o_, 512); o_ += 512
        T_d0 = h1f(o_, 512); o_ += 512
        T_d1 = h1f(o_, 512); o_ += 512
        T_B = h1f(o_, 512); o_ += 512
        attS = [h1b(o_, 512), h1b(o_ + 256, 512)]; o_ += 512
        osb = h1f(o_, 512); o_ += 512
        sqb = h1b(o_, 512); o_ += 256
        rst = h1f(o_, 512); o_ += 512
        tt_ = h1f(o_, 512); o_ += 512
        Ub = h1f(o_, 4096); o_ += 4096
        Sbf = h1b(o_, 4096); o_ += 2048
        arep = h1f(o_, 1024); RA = h1f(o_, 1024); RAi = h1[:, o_:o_ + 1024].bitcast(I32); o_ += 1024
        Shalf = h1f(o_, 1024); RB = h1f(o_, 1024); RBi = h1[:, o_:o_ + 1024].bitcast(I32); o_ += 1024
        Shalf2 = h1f(o_, 1024); o_ += 1024
        T_s2 = h1f(o_, 512); o_ += 512
        T_q2 = h1f(o_, 512); o_ += 512
        T_B2 = h1f(o_, 512); o_ += 512
        assert o_ <= NT * D
        msb = h1f(0, 3072)
        modbb = h1f(3072, 3072)
        pgb = h1f(6144, 2048)
        rows = h1f(8192, 3072)

        pA = [ps("pA%d" % i, [128, 512], F32) for i in range(2)]
        pB = [ps("pB%d" % i, [128, 512], F32) for i in range(2)]
        pO = ps("pO", [128, 512], F32)
        pS = ps("pS", [128, 512], F32)
        pT0 = ps("pT0", [128, 1024], BF16)
        pM = ps("pM", [128, 512], F32)

        H1A = "h1all"

        def WK(k, g0=0, g1=4):
            return [("wbf", k, g) for g in range(g0, g1)]

        def top(hw, fn, reads=(), writes=()):
            return P.op(hw, fn, tuple(reads) + (H1A,), writes)

        def tdma(hw, slot, fn, reads=(), writes=()):
            return P.dma(hw, slot, fn, tuple(reads) + (H1A,), writes)

        def barrier():
            for e in ("act", "dve", "pool"):
                P.op(e, (lambda eng, e=e: (eng.memset(small[:, 250:251], 0.0) if e != "act" else
                                            eng.activation(out=small[:, 251:252], in_=small[:, 252:253], func=AF.Copy))),
                     writes=[("bar", e)])
            P.op("pe", lambda eng: eng.matmul(pM[0:1, 0:1], lhsT=onesb[:, 0:1], rhs=onesb[:, 0:1], start=True, stop=True),
                 reads=["cb"], writes=["pM", ("bar", "pe")])
            for e in ("act", "dve", "pool", "pe", "sp"):
                if e == "pe":
                    P.op("pe", lambda eng: eng.matmul(pM[0:1, 0:1], lhsT=onesb[:, 0:1], rhs=onesb[:, 0:1], start=True, stop=True),
                         reads=["cb"] + [("bar", q) for q in ("act", "dve", "pool")], writes=["pM"])
                elif e == "sp":
                    P.op("sp", lambda eng: eng.nop(), reads=[("bar", q) for q in ("act", "dve", "pool", "pe")])
                else:
                    P.op(e, (lambda eng, e=e: (eng.memset(small[:, 253:254], 0.0) if e == "dve" else
                                                eng.memset(small[:, 254:255], 0.0) if e == "pool" else
                                                eng.activation(out=small[:, 255:256], in_=small[:, 252:253], func=AF.Copy))),
                         reads=[("bar", q) for q in ("act", "dve", "pool", "pe") if q != e])

        P.dma("pool", "cb", lambda e: e.dma_start(out=cb[:], in_=cf[:, 0:NCB]), writes=["cb"])
        P.dma("sp", "cfs", lambda e: e.dma_start(out=cfs[:], in_=cf[:, NCB:NCF]), writes=["cfs"])
        P.op("dve", lambda e: e.memset(small[:], 0.0), writes=["small"])
        P.op("dve", lambda e: e.memset(bmat[:], 0.0), writes=["birow"])
        P.op("dve", lambda e: e.memset(biasT[:], 0.0), writes=["biasT"])
        P.op("pool", lambda e: e.memset(hb[3][:], 0.0), writes=[("kt", 0), ("kt", 1)])
        P.op("pool", lambda e: e.memset(hb[5][:], 0.0), writes=[("kt", 0), ("kt", 1)])
        with nc.allow_non_contiguous_dma(reason="tiny one-time transposed loads"):
            pass
        for bb in range(NB):
            P.dma("sp", "s0", lambda e, bb=bb: e.dma_start(out=cT.rearrange("p (c b) -> p c b", b=NB)[:, :, bb], in_=cin[bb].rearrange("(c p) -> p c", p=128),
                                                         allow_slow_non_contiguous=True), reads=["small"], writes=[("cT", bb)])
        P.dma("sp", "s1", lambda e: e.dma_start(out=gkT, in_=kv_g[0].rearrange("(c p) -> p c", p=128), allow_slow_non_contiguous=True),
              reads=["small"], writes=["gkT"])
        for l in range(2):
            P.dma("sp", "s2", lambda e, l=l: e.dma_start(out=lbT[:, l * 8:(l + 1) * 8], in_=a_lb[l].rearrange("(c p) -> p c", p=128),
                                                       allow_slow_non_contiguous=True), reads=["small"], writes=[("lbT", l)])
        P.dma("sp", "s3", lambda e: e.dma_start(out=gn, in_=a_gn), reads=["small"], writes=["gn"])
        P.op("act", lambda e: e.activation(out=scT[:], in_=cT, func=AF.Silu), reads=[("cT", q) for q in range(NB)], writes=["scT"])
        P.op("dve", lambda e: e.tensor_tensor(out=oml, in0=lbT[:, 8:16], in1=lbT[:, 0:8], op=ALU.subtract), reads=[("lbT", 0), ("lbT", 1)], writes=["oml"])
        P.op("act", lambda e: e.activation(out=oml, in_=oml, func=AF.Sigmoid), reads=["oml"], writes=["oml"])
        P.op("dve", lambda e: e.tensor_scalar(out=noml, in0=oml, scalar1=-1.0, scalar2=None, op0=ALU.mult), reads=["oml", "small"], writes=["noml"])
        wi = 0
        for l in range(2):
            tdma("sp", "s4", lambda e, l=l: e.dma_start(out=modbb[0:NB, :], in_=mod_b[l].partition_broadcast(NB)), writes=["modbb"])
            tdma("sp", "s5", lambda e, l=l: e.dma_start(out=pgb[0:NB, 0:1024], in_=pre_g[l].partition_broadcast(NB)), writes=["pgb0"])
            tdma("sp", "s6", lambda e, l=l: e.dma_start(out=pgb[0:NB, 1024:2048], in_=post_g[l].partition_broadcast(NB)), writes=["pgb1"])
            for cbk in range(6):
                k = wi % 2
                wi += 1
                P.dma("pool", "wbf%d" % k, lambda e, l=l, cbk=cbk, k=k: e.dma_start(
                    out=wbf[k][:], in_=mod_w[l][:, cbk * 512:(cbk + 1) * 512].rearrange("(c p) n -> p c n", p=128)),
                    writes=WK(k))
                for c in range(8):
                    P.op("pe", lambda e, c=c, k=k: e.matmul(pM[0:NB, :], lhsT=scT[:, c * NB:(c + 1) * NB], rhs=wbf[k][:, c, :],
                                                            start=(c == 0), stop=(c == 7)),
                         reads=["scT"] + WK(k), writes=["pM"])
                top("dve", lambda e, cbk=cbk: e.tensor_tensor(
                    out=msb[0:NB, cbk * 512:(cbk + 1) * 512], in0=pM[0:NB, :],
                    in1=modbb[0:NB, cbk * 512:(cbk + 1) * 512], op=ALU.add),
                    reads=["pM", "modbb"], writes=[("msb", cbk)])
            mk = [("msb", i) for i in range(6)]
            top("dve", lambda e: e.scalar_tensor_tensor(out=rows[0:NB, 0:1024], in0=msb[0:NB, 1024:2048],
                                                        scalar=1.0, in1=pgb[0:NB, 0:1024], op0=ALU.add, op1=ALU.mult),
                reads=mk + ["pgb0"], writes=["rows"])
            top("dve", lambda e: e.tensor_copy(out=rows[0:NB, 1024:2048], in_=msb[0:NB, 0:1024]), reads=mk, writes=["rows"])
            top("dve", lambda e: e.tensor_tensor(out=rows[0:NB, 2048:3072], in0=msb[0:NB, 2048:3072], in1=pgb[0:NB, 1024:2048], op=ALU.mult),
                reads=mk + ["pgb1"], writes=["rows"])
            for kind in range(3):
                tdma("sp", "s7", lambda e, l=l, kind=kind: e.dma_start(out=scr[l, kind], in_=rows[0:NB, kind * 1024:(kind + 1) * 1024]),
                     reads=["rows"], writes=[("scr", l, kind)])
        barrier()

        xin_i = [0]

        def layer_vectors(l, b):
            P.dma("sp", "v0", lambda e: e.dma_start(out=aT, in_=scr[l, 0, b].rearrange("(c p) -> p c", p=128), allow_slow_non_contiguous=True),
                  reads=[("scr", l, 0)], writes=["aT"])
            P.dma("sp", "v1", lambda e: e.dma_start(out=shT, in_=scr[l, 1, b].rearrange("(c p) -> p c", p=128), allow_slow_non_contiguous=True),
                  reads=[("scr", l, 1)], writes=["shT"])
            P.dma("sp", "v2", lambda e: e.dma_start(out=G_bc[:], in_=scr[l, 2, b].partition_broadcast(128)), reads=[("scr", l, 2)], writes=["G_bc"])
            P.op("dve", lambda e: e.tensor_copy(out=shTb, in_=shT), reads=["shT"], writes=["shTb"])

        def rstd_from(col_in, col_out, keyin, keyout):
            P.op("dve", lambda e: e.tensor_scalar(out=col_out, in0=col_in, scalar1=1.0 / D, scalar2=EPS, op0=ALU.mult, op1=ALU.add),
                 reads=[keyin], writes=[keyout])
            P.op("act", lambda e: e.activation(out=col_out, in_=col_out, func=AF.Sqrt), reads=[keyout], writes=[keyout])
            P.op("dve", lambda e: e.reciprocal(out=col_out, in_=col_out), reads=[keyout], writes=[keyout])

        def pre_phase(l, b):
            for tt in range(NT):
                k = tt % 2
                if l == 0:
                    xi = xin_i[0] % 2
                    xin_i[0] += 1
                    P.dma("sp", "xin%d" % xi, lambda e, tt=tt, xi=xi: e.dma_start(out=xin[xi][:], in_=x[b, tt * 128:(tt + 1) * 128, :]),
                          writes=[("xin", xi)])
                    src, skey = xin[xi][:], ("xin", xi)
                else:
                    src, skey = h1[:, tt * D:(tt + 1) * D], ("h1", tt)
                P.op("dve", lambda e, tt=tt: e.memset(ss[:, tt:tt + 1], 0.0), writes=[("ss", tt)])
                P.op("act", lambda e, src=src, tt=tt: e.activation(out=xs[0][:], in_=src, func=AF.Square, accum_out=ss[:, tt:tt + 1]),
                     reads=[skey, ("ss", tt)], writes=[("xs", 0), ("ss", tt)])
                rstd_from(ss[:, tt:tt + 1], rstd[:, tt:tt + 1], ("ss", tt), ("rstd", tt))
                P.op("dve", lambda e, src=src, tt=tt, k=k: e.tensor_scalar(out=xs[k][:], in0=src, scalar1=rstd[:, tt:tt + 1], scalar2=None, op0=ALU.mult),
                     reads=[skey, ("rstd", tt)], writes=[("xs", 0)])
                for c in range(8):
                    P.op("pe", lambda e, c=c, k=k: e.transpose(out=pT0[:, c * 128:(c + 1) * 128], in_=xs[k][:, c * 128:(c + 1) * 128], identity=ident),
                         reads=[("xs", 0), "cb"], writes=["pT0"])
                P.op("act", lambda e, tt=tt: e.activation(out=xnT[:, :, tt * 128:(tt + 1) * 128], in_=pT0[:].rearrange("p (c t) -> p c t", c=8), func=AF.Copy),
                     reads=["pT0"], writes=[("xnT", tt)])

        wslot = [0]

        def load_head_weights(pieces):
            k = wslot[0] % 2
            wslot[0] += 1
            for g, ap in enumerate(pieces):
                P.dma("pool", "wbf%d" % k, lambda e, g=g, ap=ap, k=k: e.dma_start(
                    out=wbf[k][:, :, g * 128:(g + 1) * 128], in_=ap.rearrange("(c p) n -> p c n", p=128)),
                    writes=[("wbf", k, g)])
            return k

        def bias_cols(k, groups):
            for j, g in enumerate(groups):
                for c in range(8):
                    P.op("pe", lambda e, j=j, g=g, c=c: e.matmul(pM[:, j:j + 1], lhsT=wbf[k][:, c, g * 128:(g + 1) * 128], rhs=shTb[:, c:c + 1],
                                                               start=(c == 0), stop=(c == 7)),
                         reads=[("wbf", k, g), "shTb"], writes=["pM"])
            P.op("dve", lambda e: e.tensor_copy(out=bcol[:, 0:len(groups)], in_=pM[:, 0:len(groups)]), reads=["pM"], writes=["bcol"])

        def scale_w(k, c0, c1, vec, vkey):
            P.op("pool", lambda e: e.tensor_tensor(out=wbf[k][:, :, c0:c1], in0=wbf[k][:, :, c0:c1],
                                                   in1=vec.unsqueeze(2).to_broadcast([128, 8, c1 - c0]), op=ALU.mult),
                 reads=WK(k, c0 // 128, c1 // 128) + [vkey], writes=WK(k, c0 // 128, c1 // 128))

        pa_i = [0]

        def proj_fm(k, g, tb):
            i = pa_i[0] % 2
            pa_i[0] += 1
            for c in range(8):
                P.op("pe", lambda e, c=c, i=i: e.matmul(pA[i][:], lhsT=wbf[k][:, c, g * 128:(g + 1) * 128], rhs=xnT[:, c, tb * 512:(tb + 1) * 512],
                                                      start=(c == 0), stop=(c == 7)),
                     reads=[("wbf", k, g)] + [("xnT", 4 * tb + q) for q in range(4)], writes=[("pA", i)])
            return i

        def proj_tm(k, g, dst, dkey, bias_row=False):
            for t4 in range(4):
                i = t4 % 2
                for j in range(4):
                    tt = t4 * 4 + j
                    for c in range(8):
                        P.op("pe", lambda e, c=c, tt=tt, j=j, i=i: e.matmul(pB[i][:, j * 128:(j + 1) * 128], lhsT=xnT[:, c, tt * 128:(tt + 1) * 128],
                                                                          rhs=wbf[k][:, c, g * 128:(g + 1) * 128], start=(c == 0),
                                                                          stop=(c == 7 and not bias_row)),
                             reads=[("wbf", k, g), ("xnT", tt)], writes=[("pB", i)])
                    if bias_row:
                        P.op("pe", lambda e, j=j, i=i: e.matmul(pB[i][:, j * 128:(j + 1) * 128], lhsT=onesb, rhs=bmat[:], start=False, stop=True),
                             reads=["cb", "birow"], writes=[("pB", i)])
                P.op("act", lambda e, t4=t4, i=i: e.activation(out=dst[:, t4 * 512:(t4 + 1) * 512], in_=pB[i][:], func=AF.Copy),
                     reads=[("pB", i)], writes=[(dkey, t4)])

        def post_phase(l, b, wout, last):
            for cbk in range(2):
                P.dma("pool", "wbf%d" % cbk, lambda e, cbk=cbk: e.dma_start(
                    out=wbf[cbk][:], in_=wout[:, cbk * 512:(cbk + 1) * 512].rearrange("(c p) n -> p c n", p=128)), writes=WK(cbk))
            wslot[0] = 0
            for tt in range(NT):
                pp, pk = (pA, "pA") if tt % 2 == 0 else (pB, "pB")
                for cbk in range(2):
                    for c in range(8):
                        P.op("pe", lambda e, c=c, cbk=cbk, tt=tt, pp=pp: e.matmul(pp[cbk][:], lhsT=ogT[:, c, tt * 128:(tt + 1) * 128], rhs=wbf[cbk][:, c, :],
                                                                         start=(c == 0), stop=(c == 7)),
                             reads=WK(cbk) + [("ogT", hh, tt // 4) for hh in range(8)], writes=[(pk, cbk)])
                    P.op("dve", lambda e, cbk=cbk: e.memset(ssy[:, cbk:cbk + 1], 0.0), writes=[("ssy", cbk)])
                    P.op("act", lambda e, cbk=cbk, pp=pp: e.activation(out=PT[cbk][:], in_=pp[cbk][:], func=AF.Square, accum_out=ssy[:, cbk:cbk + 1]),
                         reads=[(pk, cbk), ("ssy", cbk)], writes=[("PT", cbk), ("ssy", cbk)])
                P.op("dve", lambda e: e.tensor_tensor(out=ssy[:, 2:3], in0=ssy[:, 0:1], in1=ssy[:, 1:2], op=ALU.add),
                     reads=[("ssy", 0), ("ssy", 1)], writes=[("ssy", 2)])
                rstd_from(ssy[:, 2:3], ssy[:, 3:4], ("ssy", 2), ("ssy", 3))
                xi = xin_i[0] % 2
                xin_i[0] += 1
                if l == 0:
                    P.dma("sp", "xin%d" % xi, lambda e, tt=tt, xi=xi: e.dma_start(out=xin[xi][:], in_=x[b, tt * 128:(tt + 1) * 128, :]),
                          writes=[("xin", xi)])
                    dest, dkey, res, rkey = h1[:, tt * D:(tt + 1) * D], ("h1", tt), xin[xi][:], ("xin", xi)
                    extra_w = [H1A]
                else:
                    dest, dkey, res, rkey = xin[xi][:], ("xin", xi), h1[:, tt * D:(tt + 1) * D], ("h1", tt)
                    extra_w = [H1A]
                for cbk in range(2):
                    P.op("dve", lambda e, cbk=cbk, dest=dest, pp=pp: e.scalar_tensor_tensor(
                        out=dest[:, cbk * 512:(cbk + 1) * 512], in0=pp[cbk][:], scalar=ssy[:, 3:4], in1=G_bc[:, cbk * 512:(cbk + 1) * 512],
                        op0=ALU.mult, op1=ALU.mult), reads=[(pk, cbk), ("ssy", 3), "G_bc"], writes=[dkey] + (extra_w if l == 0 else []))
                P.op("dve", lambda e, dest=dest, res=res: e.tensor_tensor(out=dest, in0=dest, in1=res, op=ALU.add),
                     reads=[dkey, rkey], writes=[dkey] + extra_w)
                if last:
                    P.dma("sp", "xin%d" % xi, lambda e, tt=tt, dest=dest: e.dma_start(out=out[b, tt * 128:(tt + 1) * 128, :], in_=dest),
                          reads=[dkey], writes=[("out", b, tt)])

        def rope_tables(b):
            for hf in range(2):
                cs = slice(hf * 1024, (hf + 1) * 1024)
                tdma("sp", "ra", lambda e, cs=cs: e.dma_start(out=RAi, in_=pos[b, cs].partition_broadcast(128)), writes=["arep"])
                top("dve", lambda e: e.tensor_copy(out=RA, in_=RAi), reads=["arep"], writes=["arep"])
                top("dve", lambda e: e.tensor_scalar(out=RA, in0=RA, scalar1=invf, scalar2=None, op0=ALU.mult), reads=["arep", "cfs"], writes=["arep"])
                for which in range(2):
                    off = 0.0 if which == 0 else float(np.pi / 2)
                    top("dve", lambda e, off=off: e.tensor_scalar(out=RB, in0=RA, scalar1=off, scalar2=float(1.0 / (2 * np.pi)), op0=ALU.add, op1=ALU.mult),
                        reads=["arep"], writes=["Shalf"])
                    top("dve", lambda e: e.tensor_copy(out=RBi, in_=RB), reads=["Shalf"], writes=["Shalf"])
                    top("dve", lambda e: e.tensor_copy(out=RB, in_=RBi), reads=["Shalf"], writes=["Shalf"])
                    top("dve", lambda e: e.tensor_scalar(out=RB, in0=RB, scalar1=float(-2 * np.pi), scalar2=None, op0=ALU.mult), reads=["Shalf"], writes=["Shalf"])
                    top("dve", lambda e, off=off: e.scalar_tensor_tensor(out=RB, in0=RA, scalar=off, in1=RB, op0=ALU.add, op1=ALU.add),
                        reads=["arep", "Shalf"], writes=["Shalf"])
                    top("dve", lambda e: e.tensor_scalar(out=RB, in0=RB, scalar1=3.14159, scalar2=-3.14159, op0=ALU.min, op1=ALU.max),
                        reads=["Shalf"], writes=["Shalf"])
                    if which == 0:
                        top("act", lambda e: e.activation(out=RB, in_=RB, func=AF.Sin), reads=["Shalf"], writes=["Shalf"])
                        top("dve", lambda e, cs=cs: e.tensor_scalar(out=sinT[:, cs], in0=RB, scalar1=sgn, scalar2=None, op0=ALU.mult),
                            reads=["Shalf", "cfs"], writes=["sinT"])
                    else:
                        top("act", lambda e, cs=cs: e.activation(out=cosT[:, cs], in_=RB, func=AF.Sin), reads=["Shalf"], writes=["cosT"])

        def l0_head(h, k):
            qtT, ktT, sgT, kt, vv, ktB = hb[0], hb[1], hb[2], hb[3], hb[4], hb[5]
            bias_cols(k, [0, 1, 3])
            P.op("dve", lambda e: e.tensor_scalar(out=nbf, in0=bcol[:, 1:2], scalar1=-1.0, scalar2=None, op0=ALU.mult), reads=["bcol"], writes=["nbf"])
            for c in range(8):
                P.op("pe", lambda e, c=c: e.matmul(pM[0:1, 128:256], lhsT=shTb[:, c:c + 1], rhs=wbf[k][:, c, 256:384], start=(c == 0), stop=(c == 7)),
                     reads=[("wbf", k, 2), "shTb"], writes=["pM"])
            P.op("dve", lambda e: e.tensor_copy(out=bmat[0:1, :], in_=pM[0:1, 128:256]), reads=["pM"], writes=["birow"])
            scale_w(k, 0, 512, aT, "aT")
            knext = None
            if h < 7:
                knext = load_head_weights([a_w_in[:, g * 1024 + (h + 1) * 128:g * 1024 + (h + 2) * 128] for g in range(4)])
            for tb in range(4):
                bs = slice(tb * 512, (tb + 1) * 512)
                Tq, TB, Ts = (T_q, T_B, T_s) if tb % 2 == 0 else (T_q2, T_B2, T_s2)
                kq, kB, ks = ("T_q", "T_B", "T_s") if tb % 2 == 0 else ("T_q2", "T_B2", "T_s2")
                i = proj_fm(k, 0, tb)
                top("act", lambda e, i=i, Tq=Tq: e.activation(out=Tq, in_=pA[i][:], func=AF.Silu, bias=bcol[:, 0:1]), reads=[("pA", i), "bcol"], writes=[kq])
                i = proj_fm(k, 3, tb)
                P.op("act", lambda e, i=i, bs=bs: e.activation(out=sgT[:, bs], in_=pA[i][:], func=AF.Silu, bias=bcol[:, 2:3]),
                     reads=[("pA", i), "bcol"], writes=[("sgT", tb)])
                i = proj_fm(k, 1, tb)
                top("act", lambda e, i=i, Ts=Ts: e.activation(out=Ts, in_=pA[i][:], func=AF.Sigmoid, bias=nbf, scale=-1.0), reads=[("pA", i), "nbf"], writes=[ks])
                top("dve", lambda e, Ts=Ts: e.tensor_scalar(out=T_d0, in0=Ts, scalar1=noml[:, h:h + 1], scalar2=1.0, op0=ALU.mult, op1=ALU.add),
                    reads=[ks, "noml"], writes=["T_d0"])
                if tb == 0 and h == 0:
                    top("dve", lambda e: e.memset(T_d1, 0.0), writes=["T_d1"])
                d0v = T_d0.rearrange("p (n c) -> p n c", c=64)[:, :, 0:1]
                d1v = T_d1.rearrange("p (n c) -> p n c", c=64)[:, :, 0:1]
                top("dve", lambda e, d0v=d0v, d1v=d1v: e.tensor_copy(out=d1v, in_=d0v), reads=["T_d0", "T_d1"], writes=["T_d1"])
                top("dve", lambda e, d0v=d0v: e.memset(d0v, 0.0), reads=["T_d0", "T_d1"], writes=["T_d0"])
                top("dve", lambda e, TB=TB: e.tensor_tensor_scan(out=TB, data0=T_d0, data1=T_d1, initial=0.0, op0=ALU.mult, op1=ALU.add),
                    reads=["T_d0", "T_d1"], writes=[kB])
                top("dve", lambda e, tb=tb, TB=TB: e.tensor_copy(out=Bl[:, tb * 8:(tb + 1) * 8].unsqueeze(2),
                                                                 in_=TB.rearrange("p (n c) -> p n c", c=64)[:, :, 63:64]), reads=[kB], writes=["Bl"])
                top("pool", lambda e, bs=bs, Tq=Tq, TB=TB: e.tensor_tensor(out=qtT[:, bs], in0=Tq, in1=TB, op=ALU.mult), reads=[kq, kB], writes=[("qtT", tb)])
                top("dve", lambda e, TB=TB: e.reciprocal(out=T_d0, in_=TB), reads=[kB], writes=["T_d0"])
                top("dve", lambda e, bs=bs, Ts=Ts: e.scalar_tensor_tensor(out=ktT[:, bs], in0=Ts, scalar=oml[:, h:h + 1], in1=T_d0, op0=ALU.mult, op1=ALU.mult),
                    reads=[ks, "T_d0", "oml"], writes=[("ktT", tb)])
            if limit == "h0a":
                return knext
            proj_tm(k, 2, vv, "vv", bias_row=True)
            for t8 in range(2):
                for j in range(8):
                    tt = t8 * 8 + j
                    P.op("pe", lambda e, tt=tt, j=j: e.transpose(out=pT0[:, j * 128:(j + 1) * 128], in_=ktT[:, tt * 128:(tt + 1) * 128], identity=ident),
                         reads=[("ktT", tt // 4), "cb"], writes=["pT0"])
                P.op("act", lambda e, t8=t8: e.activation(out=kt[0:64, t8 * 1024:(t8 + 1) * 1024], in_=pT0[0:64, :], func=AF.Copy), reads=["pT0"], writes=[("kt", t8)])
                P.op("act", lambda e, t8=t8: e.activation(out=ktB[64:128, t8 * 1024:(t8 + 1) * 1024], in_=pT0[64:128, :], func=AF.Copy), reads=["pT0"], writes=[("kt", t8)])
            if limit == "h0b":
                return knext
            Uv = Ub.rearrange("p (v n) -> p n v", n=32)
            for ng in range(8):
                i = ng % 2
                for j in range(4):
                    n = ng * 4 + j
                    tt, half = n // 2, n % 2
                    ksrc = kt if half == 0 else ktB
                    P.op("pe", lambda e, j=j, tt=tt, ksrc=ksrc, i=i: e.matmul(pB[i][:, j * 128:(j + 1) * 128], lhsT=ksrc[:, tt * 128:(tt + 1) * 128],
                                                                            rhs=vv[:, tt * 128:(tt + 1) * 128], start=True, stop=True),
                         reads=[("kt", tt // 8), ("vv", tt // 4)], writes=[("pB", i)])
                top("dve", lambda e, ng=ng, i=i: e.tensor_tensor(out=Uv[:, ng * 4:(ng + 1) * 4, :], in0=pB[i][:].rearrange("p (n v) -> p n v", n=4),
                                                                 in1=Bl[:, ng * 4:(ng + 1) * 4].unsqueeze(2).to_broadcast([128, 4, 128]), op=ALU.mult),
                    reads=[("pB", i), "Bl"], writes=["Ub"])
            if limit == "h0c1":
                return knext
            top("dve", lambda e: e.tensor_copy(out=Bl0, in_=Bl), reads=["Bl"], writes=["Bl0"])
            top("dve", lambda e: e.memset(Bl0[:, 0:1], 0.0), reads=["Bl0"], writes=["Bl0"])
            top("pool", lambda e: e.tensor_copy(out=arep.rearrange("p (v n) -> p v n", n=32), in_=Bl0.unsqueeze(1).to_broadcast([128, 32, 32])),
                reads=["Bl0"], writes=["arep"])
            for vq in range(4):
                Sq, ksq = (Shalf, "Shalf") if vq % 2 == 0 else (Shalf2, "Shalf2")
                top("dve", lambda e, vq=vq, Sq=Sq: e.tensor_tensor_scan(out=Sq, data0=arep, data1=Ub[:, vq * 1024:(vq + 1) * 1024], initial=0.0,
                                                                        op0=ALU.mult, op1=ALU.add),
                    reads=["arep", "Ub"], writes=[ksq])
                dstv = Sbf.rearrange("p (n v) -> p v n", n=32)[:, vq * 32:(vq + 1) * 32, :]
                if vq % 2 == 0:
                    top("act", lambda e, dstv=dstv, Sq=Sq: e.activation(out=dstv, in_=Sq.rearrange("p (v n) -> p v n", n=32), func=AF.Copy),
                        reads=[ksq], writes=[("Sbf", vq)])
                else:
                    top("pool", lambda e, dstv=dstv, Sq=Sq: e.tensor_copy(out=dstv, in_=Sq.rearrange("p (v n) -> p v n", n=32)),
                        reads=[ksq], writes=[("Sbf", vq)])
            def att_stage(pg):
                i = pg % 2
                for j in range(4):
                    pr = pg * 4 + j
                    P.op("pe", lambda e, j=j, pr=pr, i=i: e.matmul(pB[i][:, j * 128:(j + 1) * 128], lhsT=ktT[:, pr * 128:(pr + 1) * 128],
                                                                 rhs=qtT[:, pr * 128:(pr + 1) * 128], start=True, stop=True),
                         reads=[("ktT", pg), ("qtT", pg)], writes=[("pB", i)])
                top("dve", lambda e, i=i: e.tensor_tensor(out=attS[i].rearrange("p (n t) -> p n t", n=4), in0=pB[i][:].rearrange("p (n t) -> p n t", n=4),
                                                          in1=pmask.unsqueeze(1).to_broadcast([128, 4, 128]), op=ALU.mult),
                    reads=[("pB", i), "cb"], writes=[("attS", i)])

            att_stage(0)
            for pg in range(4):
                i = pg % 2
                if pg < 3:
                    att_stage(pg + 1)
                for j in range(4):
                    pr = pg * 4 + j
                    P.op("pe", lambda e, j=j, pr=pr, i=i: e.matmul(pO[:, j * 128:(j + 1) * 128], lhsT=vv[:, pr * 128:(pr + 1) * 128],
                                                                 rhs=attS[i][:, j * 128:(j + 1) * 128], start=True, stop=False, skip_group_check=True),
                         reads=[("vv", pg), ("attS", i), H1A], writes=["pO"])
                    if pr > 0:
                        P.op("pe", lambda e, j=j, pr=pr: e.matmul(pO[:, j * 128:j * 128 + 64], lhsT=Sbf[:, (2 * pr - 1) * 128:(2 * pr) * 128],
                                                                rhs=qtT[:, pr * 128:pr * 128 + 64], start=False, stop=False, skip_group_check=True),
                             reads=[("Sbf", 0), ("Sbf", 1), ("Sbf", 2), ("Sbf", 3), ("qtT", pg), H1A], writes=["pO"])
                    P.op("pe", lambda e, j=j, pr=pr: e.matmul(pO[:, j * 128 + 64:(j + 1) * 128], lhsT=Sbf[:, (2 * pr) * 128:(2 * pr + 1) * 128],
                                                            rhs=qtT[:, pr * 128 + 64:(pr + 1) * 128], start=False, stop=True, skip_group_check=True),
                         reads=[("Sbf", 0), ("Sbf", 1), ("Sbf", 2), ("Sbf", 3), ("qtT", pg), H1A], writes=["pO"])
                top("act", lambda e: e.activation(out=osb, in_=pO[:], func=AF.Copy), reads=["pO"], writes=["osb"])
                top("act", lambda e: e.activation(out=sqb, in_=pO[:], func=AF.Square), reads=["pO"], writes=["sqb"])
                P.op("pe", lambda e: e.matmul(pS[:], lhsT=onesb, rhs=sqb, start=True, stop=True), reads=["sqb", "cb", H1A], writes=["pS"])
                top("dve", lambda e: e.tensor_scalar(out=rst, in0=pS[:], scalar1=1.0 / 128, scalar2=EPS, op0=ALU.mult, op1=ALU.add), reads=["pS"], writes=["rst"])
                top("act", lambda e: e.activation(out=rst, in_=rst, func=AF.Sqrt), reads=["rst"], writes=["rst"])
                top("dve", lambda e: e.reciprocal(out=rst, in_=rst), reads=["rst"], writes=["rst"])
                top("dve", lambda e: e.scalar_tensor_tensor(out=tt_, in0=osb, scalar=gn, in1=rst, op0=ALU.mult, op1=ALU.mult),
                    reads=["osb", "rst", "gn"], writes=["tt_"])
                top("dve", lambda e, pg=pg: e.tensor_tensor(out=ogT[:, h, pg * 512:(pg + 1) * 512], in0=tt_, in1=sgT[:, pg * 512:(pg + 1) * 512], op=ALU.mult),
                    reads=["tt_", ("sgT", pg)], writes=[("ogT", h, pg)])
            return knext

        pt_i = [0]

        def l1_pieces(h):
            return [b_w_in[:, h * 128:(h + 1) * 128], b_w_in[:, 1024 + h * 128:1024 + (h + 1) * 128],
                    w_kv[:, h * 128:(h + 1) * 128], w_kv[:, 1024 + h * 128:1024 + (h + 1) * 128]]

        def l1_head(h, k):
            QT, KT, szT, V = hb[0], hb[1], hb[2], hb[4]
            bias_cols(k, [0, 1])
            scale_w(k, 0, 256, aT, "aT")
            scale_w(k, 256, 512, gkT, "gkT")
            knext = load_head_weights(l1_pieces(h + 1)) if h < 7 else None
            if limit == "l1a0":
                return knext
            for tb in range(4):
                bs = slice(tb * 512, (tb + 1) * 512)
                for which in range(2):
                    if limit == "l1a1" and (tb, which) == (0, 1):
                        return knext
                    if limit == "l1a2" and (tb, which) == (1, 0):
                        return knext
                    dst = QT if which == 0 else KT
                    dkey = "QT" if which == 0 else "KT"
                    i = proj_fm(k, 0 if which == 0 else 2, tb)
                    j = (tb * 2 + which) % 2
                    if which == 0:
                        P.op("dve", lambda e, i=i: e.tensor_scalar(out=PT[0][:], in0=pA[i][:], scalar1=bcol[:, 0:1], scalar2=None, op0=ALU.add),
                            reads=[("pA", i), "bcol"], writes=[("PT", 0)])
                        P.op("dve", lambda e, i=i, bs=bs: e.scalar_tensor_tensor(out=rsb[:], in0=pA[i][:], scalar=bcol[:, 0:1], in1=cosT[:, bs], op0=ALU.add, op1=ALU.mult),
                            reads=[("pA", i), "bcol", "cosT"], writes=["rsb"])
                    else:
                        P.op("act", lambda e, i=i: e.activation(out=PT[0][:], in_=pA[i][:], func=AF.Copy), reads=[("pA", i)], writes=[("PT", 0)])
                        P.op("dve", lambda e, i=i, bs=bs: e.tensor_tensor(out=rsb[:], in0=pA[i][:], in1=cosT[:, bs], op=ALU.mult),
                            reads=[("pA", i), "cosT"], writes=["rsb"])
                    P.op("pe", lambda e, j=j: e.matmul(pB[j][:], lhsT=swp, rhs=PT[0][:], start=True, stop=True), reads=[("PT", 0), "cb"], writes=[("pB", j)])
                    P.op("dve", lambda e, j=j, bs=bs: e.tensor_tensor(out=otb[:], in0=pB[j][:], in1=sinT[:, bs], op=ALU.mult), reads=[("pB", j), "sinT"], writes=["otb"])
                    P.op("dve", lambda e, dst=dst, bs=bs: e.tensor_tensor(out=dst[:, bs], in0=rsb[:], in1=otb[:], op=ALU.add), reads=["rsb", "otb"], writes=[(dkey, tb)])
                i = proj_fm(k, 1, tb)
                P.op("act", lambda e, i=i, bs=bs: e.activation(out=szT[:, bs], in_=pA[i][:], func=AF.Silu, bias=bcol[:, 1:2]),
                     reads=[("pA", i), "bcol"], writes=[("szT", tb)])
            if limit == "l1a":
                return knext
            proj_tm(k, 3, V, "V")
            P.op("dve", lambda e: e.tensor_reduce(out=km, in_=KT[:].rearrange("p (n k) -> p n k", k=256), axis=AX.X, op=ALU.add),
                 reads=[("KT", q) for q in range(4)], writes=["km"])
            P.op("dve", lambda e: e.tensor_scalar(out=kmb, in0=km, scalar1=1.0 / 256, scalar2=None, op0=ALU.mult), reads=["km"], writes=["kmb"])
            for i8 in range(8):
                P.op("pe", lambda e, i8=i8: e.matmul(pM[:, i8 * 8:(i8 + 1) * 8], lhsT=QT[:, (8 + i8) * 128:(9 + i8) * 128], rhs=kmb, start=True, stop=True),
                     reads=[("QT", (8 + i8) // 4), "kmb"], writes=["pM"])
            P.op("dve", lambda e: e.tensor_tensor(out=gbuf[:], in0=pM[:, 0:64], in1=negm, op=ALU.add), reads=["pM", "cfs"], writes=["gbuf"])
            g3 = gbuf[:].rearrange("p (i n) -> p i n", n=8)
            P.op("dve", lambda e: e.tensor_tensor(out=cmpb[:], in0=g3.unsqueeze(2).to_broadcast([128, 8, 8, 8]),
                                                  in1=g3.unsqueeze(3).to_broadcast([128, 8, 8, 8]), op=ALU.is_gt), reads=["gbuf"], writes=["cmpb"])
            P.op("dve", lambda e: e.tensor_reduce(out=rankb[:], in_=cmpb[:].rearrange("p i n m -> p (i n) m"), axis=AX.X, op=ALU.add),
                 reads=["cmpb"], writes=["rankb"])
            bq_pad = cmpb[:].rearrange("p a b c -> p (a b c)").bitcast(BF16).rearrange("p (i c) -> p i c", c=128)
            P.op("dve", lambda e: e.memset(bq_pad, 0.0), reads=["rankb"], writes=["cmpb"])
            P.op("dve", lambda e: e.tensor_scalar(out=bq_pad[:, :, 0:8], in0=rankb[:].rearrange("p (i n) -> p i n", n=8), scalar1=2.5, scalar2=-BIG,
                                                  op0=ALU.is_ge, op1=ALU.mult), reads=["rankb", "cmpb"], writes=["cmpb"])
            for i8 in range(8):
                P.op("pe", lambda e, i8=i8: e.transpose(out=pT0[:, i8 * 128:(i8 + 1) * 128], in_=bq_pad[:, i8, :], identity=ident),
                     reads=["cmpb", "cb"], writes=["pT0"])
            P.op("act", lambda e: e.activation(out=biasT[:], in_=pT0[:], func=AF.Copy), reads=["pT0"], writes=["biasT"])
            if limit == "l1b":
                return knext
            sc = float(128 ** -0.5)
            pT0f = pT0[:].bitcast(F32)
            tiles = [(g, ktile) for g in range(4) for ktile in range(4 * g + 4)]

            def acc_of(g):
                if g % 2 == 0:
                    return pO[:], pS[:], "pO", "pS"
                return pT0f, pM[:], "pT0", "pM"

            def qk_stage(idx):
                g, ktile = tiles[idx]
                c0 = max(ktile - 4 * g, 0)
                cl = slice(c0 * 128, 512)
                j, r = idx % 2, idx % 3
                mm = [(cl, KT[:, ktile * 128:(ktile + 1) * 128], QT[:, g * 512 + c0 * 128:(g + 1) * 512], [("KT", ktile // 4), ("QT", g)])]
                if g >= 2:
                    n = ktile // 2
                    lo = max(2 * n + 2 - 4 * g, c0)
                    if lo < 4:
                        mm.append((slice(lo * 128, 512), cb[:, C_ES + n * 128:C_ES + (n + 1) * 128],
                                   biasT[:, (g - 2) * 512 + lo * 128:(g - 2) * 512 + 512], ["biasT", "cb"]))
                if ktile >= 4 * g:
                    mm.append((slice(c0 * 128, c0 * 128 + 128), ident, cmask, ["cb"]))
                for mi, (csl, l_, r_, rk) in enumerate(mm):
                    P.op("pe", lambda e, csl=csl, l_=l_, r_=r_, mi=mi, j=j, last=(mi == len(mm) - 1): e.matmul(
                        pB[j][:, csl], lhsT=l_, rhs=r_, start=(mi == 0), stop=last, skip_group_check=True),
                        reads=rk, writes=[("pB", j)])
                P.op("act", lambda e, j=j, r=r, cl=cl: e.activation(out=PT[r][:, cl], in_=pB[j][:, cl], func=AF.Exp, scale=sc),
                     reads=[("pB", j)], writes=[("PT", r)])

            def pv_stage(idx):
                g, ktile = tiles[idx]
                nkt = 4 * g + 4
                c0 = max(ktile - 4 * g, 0)
                cl = slice(c0 * 128, 512)
                r = idx % 3
                aO, aS, kO, kS = acc_of(g)
                P.op("pe", lambda e, r=r, cl=cl, ktile=ktile, nkt=nkt, aO=aO: e.matmul(aO[:, cl], lhsT=V[:, ktile * 128:(ktile + 1) * 128], rhs=PT[r][:, cl],
                                                                                      start=(ktile == 0), stop=(ktile == nkt - 1), skip_group_check=True),
                     reads=[("V", ktile // 4), ("PT", r)], writes=[kO])
                P.op("pe", lambda e, r=r, cl=cl, ktile=ktile, nkt=nkt, aS=aS: e.matmul(aS[:, cl], lhsT=onesb, rhs=PT[r][:, cl],
                                                                                      start=(ktile == 0), stop=(ktile == nkt - 1), skip_group_check=True),
                     reads=["cb", ("PT", r)], writes=[kS])
                if ktile == nkt - 1:
                    P.op("dve", lambda e, aS=aS: e.reciprocal(out=rsb[:], in_=aS), reads=[kS], writes=["rsb"])
                    P.op("dve", lambda e, aO=aO: e.tensor_tensor(out=otb[:], in0=aO, in1=rsb[:], op=ALU.mult), reads=[kO, "rsb"], writes=["otb"])
                    P.op("dve", lambda e, g=g: e.tensor_tensor(out=ogT[:, h, g * 512:(g + 1) * 512], in0=otb[:], in1=szT[:, g * 512:(g + 1) * 512], op=ALU.mult),
                         reads=["otb", ("szT", g)], writes=[("ogT", h, g)])

            qk_stage(0)
            for idx in range(len(tiles)):
                if idx + 1 < len(tiles):
                    qk_stage(idx + 1)
                pv_stage(idx)
            return knext

        for b in range(nseq):
            if limit == "setup":
                break
            rope_tables(b)
            layer_vectors(0, b)
            if limit == "rope":
                break
            kw = load_head_weights([a_w_in[:, g * 1024:g * 1024 + 128] for g in range(4)])
            pre_phase(0, b)
            if limit == "pre":
                break
            for h in range(8):
                kw = l0_head(h, kw)
                if limit in ("head0", "h0a", "h0b", "h0c", "h0c1", "h0c2", "h0c3"):
                    break
            if limit in ("head0", "heads", "h0a", "h0b", "h0c", "h0c1", "h0c2", "h0c3"):
                break
            post_phase(0, b, a_w_out, last=stop_after_l0)
            if stop_after_l0:
                continue
            layer_vectors(1, b)
            kw = load_head_weights(l1_pieces(0))
            pre_phase(1, b)
            if limit == "l1pre":
                break
            for h in range(8):
                kw = l1_head(h, kw)
                if limit in ("l1a", "l1b", "l1c", "l1a0", "l1a1", "l1a2"):
                    break
            if limit in ("l1a", "l1b", "l1c", "l1a0", "l1a1", "l1a2"):
                break
            post_phase(1, b, b_w_out, last=True)
        if limit is not None:
            P.dma("sp", "dbg", lambda e: e.dma_start(out=out[0, 0:128, :], in_=xin[0][:]), reads=[("xin", 0)], writes=[("out", 0, 0)])
            P.op("sp", lambda e: e.nop(), reads=[("out", 0, 0)])
            for e_ in ("act", "dve", "pool", "pe"):
                pass
        else:
            P.op("sp", lambda e: e.nop(), reads=[("out", b, tt) for b in range(nseq) for tt in range(NT)])

        cnt = P.analyze()
        names = set(cnt.keys()) | set(HW)
        sems = {s: es.enter_context(nc.semaphore(s.replace(":", "_"))) for s in sorted(names)}
        with nc.Block() as block:
            P.emit(block, sems)
    return nc, len(P.ops), cnt


_CACHE = {}


def kernel(x, c, positions, mod_w, mod_b, pre_norm_g, post_norm_g, a_w_in, a_w_out, a_out_norm_g,
           a_lb_logits, kv_norm_g, w_kv, b_w_in, b_w_out):
    if "nc" not in _CACHE:
        _CACHE["nc"] = build()[0]
    nc = _CACHE["nc"]
    f = lambda a: np.ascontiguousarray(np.asarray(a), dtype=np.float32)
    shared = {
        "mod_w": f(mod_w), "mod_b": f(mod_b), "pre_g": f(pre_norm_g), "post_g": f(post_norm_g),
        "a_w_in": f(a_w_in)[0], "a_w_out": f(a_w_out)[0], "a_gn": f(a_out_norm_g).reshape(128, 1),
        "a_lb": f(a_lb_logits), "kv_g": f(kv_norm_g).reshape(1, D), "w_kv": f(w_kv),
        "b_w_in": f(b_w_in)[0], "b_w_out": f(b_w_out)[0], "cf": make_consts(),
    }
    x = f(x)
    c = f(c)
    positions = np.ascontiguousarray(np.asarray(positions), dtype=np.int32)
    in_maps = []
    for i in range(8):
        m = dict(shared)
        m["x"] = x[i * NB:(i + 1) * NB]
        m["c"] = c[i * NB:(i + 1) * NB]
        m["pos"] = positions[i * NB:(i + 1) * NB]
        in_maps.append(m)
    res = run_bass_kernel_spmd(nc, in_maps, core_ids=list(range(8)))
    return np.concatenate([r["out"] for r in res.results], axis=0)
```

```python
import numpy as np
from contextlib import ExitStack
import concourse.bass as bass
import concourse.mybir as mybir
from concourse.bass_utils import run_bass_kernel_spmd

F32 = mybir.dt.float32
BF16 = mybir.dt.bfloat16
I32 = mybir.dt.int32
ALU = mybir.AluOpType
AF = mybir.ActivationFunctionType
AX = mybir.AxisListType

NB = 4
T = 2048
D = 1024
NT = 16
EPS = 1e-6
BIG = 30000.0
HW = ("pe", "act", "dve", "pool", "sp")
PSUM_KEYS = ("pA", "pB", "pO", "pS", "pT0", "pM")

C_ID, C_CM, C_PM, C_SW, C_ON, C_ES, C_NEG, C_INV, C_SGN, NCF = 0, 128, 256, 384, 512, 640, 1664, 1728, 1729, 1730
NCB = 1664


class Op:
    __slots__ = ("hw", "fn", "reads", "writes", "sem", "inc", "signal", "value", "waits", "idx", "is_dma")


class Prog:
    def __init__(self):
        self.ops = []

    def op(self, hw, fn, reads=(), writes=()):
        o = Op()
        o.hw, o.fn, o.reads, o.writes = hw, fn, tuple(reads), tuple(writes)
        o.sem, o.inc, o.signal, o.value, o.waits, o.is_dma = hw, 1, False, 0, [], False
        o.idx = len(self.ops)
        self.ops.append(o)
        return o

    def dma(self, hw, slot, fn, reads=(), writes=()):
        o = self.op(hw, fn, reads, writes)
        o.sem, o.inc, o.signal, o.is_dma = "dma:" + slot, 16, True, True
        return o

    def analyze(self):
        last_w, readers = {}, {}
        for o in self.ops:
            deps = {}
            for k in o.reads:
                p = last_w.get(k)
                if p is not None:
                    deps[p.idx] = (p, "raw")
                kn = k[0] if isinstance(k, tuple) else k
                if kn in PSUM_KEYS:
                    for r in readers.get(k, {}).values():
                        if r.hw != o.hw and r.idx not in deps:
                            deps[r.idx] = (r, "rar")
            for k in o.writes:
                p = last_w.get(k)
                if p is not None and p.idx not in deps:
                    deps[p.idx] = (p, "waw")
                for r in readers.get(k, {}).values():
                    if r.idx not in deps and r is not o:
                        deps[r.idx] = (r, "war")
            for p, kind in deps.values():
                if (not p.is_dma) and (not o.is_dma) and p.hw == o.hw:
                    if o.hw == "pe" or kind != "raw":
                        continue
                o.waits.append(p)
                p.signal = True
            for k in o.reads:
                readers.setdefault(k, {})[("d", o.idx) if o.is_dma else o.hw] = o
            for k in o.writes:
                last_w[k] = o
                readers[k] = {}
        cnt = {}
        for o in self.ops:
            if o.signal:
                cnt[o.sem] = cnt.get(o.sem, 0) + o.inc
                o.value = cnt[o.sem]
        return cnt

    def emit(self, block, sems):
        streams = {h: [] for h in HW}
        for o in self.ops:
            streams[o.hw].append(o)

        def make(hwname):
            def body(eng):
                known = {}
                for o in streams[hwname]:
                    need = {}
                    for p in o.waits:
                        if p.value > need.get(p.sem, 0):
                            need[p.sem] = p.value
                    for s, v in need.items():
                        if known.get(s, 0) < v:
                            eng.wait_ge(sems[s], v)
                            known[s] = v
                    ins = o.fn(eng)
                    if o.signal:
                        ins.then_inc(sems[o.sem], o.inc)
            return body

        block.tensor(make("pe"))
        block.scalar(make("act"))
        block.vector(make("dve"))
        block.gpsimd(make("pool"))
        block.sync(make("sp"))


def make_consts():
    cf = np.zeros((128, NCF), np.float32)
    p = np.arange(128)
    cf[:, C_ID:C_ID + 128] = np.eye(128, dtype=np.float32)
    cf[:, C_CM:C_CM + 128] = np.where(p[:, None] > p[None, :], -BIG, 0.0)
    cf[:, C_PM:C_PM + 128] = ((p[:, None] // 64 == p[None, :] // 64) & (p[:, None] <= p[None, :])).astype(np.float32)
    cf[:, C_SW:C_SW + 128] = (p[:, None] == (p[None, :] + 64) % 128).astype(np.float32)
    cf[:, C_ON:C_ON + 128] = 1.0
    for n in range(8):
        cf[n, C_ES + n * 128:C_ES + (n + 1) * 128] = 1.0
    neg = np.zeros((8, 8), np.float32)
    for i in range(8):
        j = (8 + i) // 2
        neg[i, j:] = -1e30
    cf[:, C_NEG:C_NEG + 64] = neg.reshape(1, 64)
    inv = np.float32(10000.0) ** (-np.arange(0, 128, 2, dtype=np.float32) / np.float32(128))
    cf[:, C_INV] = np.concatenate([inv, inv]).astype(np.float32)
    cf[:, C_SGN] = np.where(p < 64, -1.0, 1.0)
    return cf


def build(stop_after_l0=False, nseq=NB, limit=None):
    nc = bass.Bass("TRN2", target_bir_lowering=False)

    def din(name, shape, dt=F32):
        return nc.dram_tensor(name, list(shape), dt, kind="ExternalInput").ap()

    x = din("x", [NB, T, D])
    cin = din("c", [NB, D])
    pos = din("pos", [NB, T], I32)
    mod_w = din("mod_w", [2, D, 3 * D])
    mod_b = din("mod_b", [2, 3 * D])
    pre_g = din("pre_g", [2, D])
    post_g = din("post_g", [2, D])
    a_w_in = din("a_w_in", [D, 4 * D])
    a_w_out = din("a_w_out", [D, D])
    a_gn = din("a_gn", [128, 1])
    a_lb = din("a_lb", [2, D])
    kv_g = din("kv_g", [1, D])
    w_kv = din("w_kv", [D, 2 * D])
    b_w_in = din("b_w_in", [D, 2 * D])
    b_w_out = din("b_w_out", [D, D])
    cf = din("cf", [128, NCF])
    out = nc.dram_tensor("out", [NB, T, D], F32, kind="ExternalOutput").ap()
    scr = nc.dram_tensor("scr", [2, 3, NB, D], F32, kind="Internal").ap()

    P = Prog()
    es = ExitStack()
    with es:
        def sb(name, shape, dt):
            return es.enter_context(nc.sbuf_tensor(name, list(shape), dt))

        def ps(name, shape, dt):
            return es.enter_context(nc.psum_tensor(name, list(shape), dt))

        cb = sb("cb", [128, NCB], BF16)
        cfs = sb("cfs", [128, NCF - NCB], F32)
        ident = cb[:, C_ID:C_ID + 128]
        cmask = cb[:, C_CM:C_CM + 128]
        pmask = cb[:, C_PM:C_PM + 128]
        swp = cb[:, C_SW:C_SW + 128]
        onesb = cb[:, C_ON:C_ON + 128]
        negm = cfs[:, 0:64]
        invf = cfs[:, 64:65]
        sgn = cfs[:, 65:66]

        xnT = sb("xnT", [128, 8, T], BF16)
        ogT = sb("ogT", [128, 8, T], BF16)
        h1 = sb("h1", [128, NT * D], F32)
        wbf = [sb("wbf%d" % i, [128, 8, 512], BF16) for i in range(2)]
        hb = [sb("hb%d" % i, [128, T], BF16) for i in range(6)]
        cosT = sb("cosT", [128, T], BF16)
        sinT = sb("sinT", [128, T], BF16)
        xin = [sb("xin%d" % i, [128, D], F32) for i in range(2)]
        xs = [sb("xs0", [128, D], BF16)] * 2
        G_bc = sb("G_bc", [128, D], F32)
        PT = [sb("PT%d" % i, [128, 512], BF16) for i in range(3)]
        rsb = sb("rsb", [128, 512], F32)
        otb = sb("otb", [128, 512], F32)
        biasT = sb("biasT", [128, 1024], BF16)
        gbuf = sb("gbuf", [128, 64], F32)
        cmpb = sb("cmpb", [128, 8, 8, 8], F32)
        rankb = sb("rankb", [128, 64], F32)
        biasq = sb("biasq", [128, 64], BF16)
        small = sb("small", [128, 256], F32)
        smallb = sb("smallb", [128, 160], BF16)
        ss = small[:, 0:16]
        rstd = small[:, 16:32]
        tmpc = small[:, 32:48]
        aT = small[:, 48:56]
        shT = small[:, 56:64]
        gkT = small[:, 64:72]
        lbT = small[:, 72:88]
        oml = small[:, 88:96]
        noml = small[:, 184:192]
        gn = small[:, 96:97]
        bcol = small[:, 100:104]
        nbf = small[:, 104:105]
        ssy = small[:, 108:112]
        Bl = small[:, 112:144]
        Bl0 = small[:, 144:176]
        km = small[:, 176:184]
        cT = small[:, 192:224]
        shTb = smallb[:, 0:8]
        kmb = smallb[:, 8:16]
        birow = smallb[0:1, 16:144]
        scT = sb("scT", [128, 32], BF16)
        bmat = sb("bmat", [128, 128], BF16)

        def h1f(off, n):
            return h1[:, off:off + n]

        def h1b(off, n):
            return h1[:, off:off + n // 2].bitcast(BF16)

        o_ = 0
        T_q = h1f(o_, 512); o_ += 512
        T_s = h1f(o_, 512); o_ += 512
        T_d0 = h1f(o_, 512); o_ += 512
        T_d1 = h1f(o_, 512); o_ += 512
        T_B = h1f(o_, 512); o_ += 512
        attS = [h1b(o_, 512), h1b(o_ + 256, 512)]; o_ += 512
        osb = h1f(o_, 512); o_ += 512
        sqb = h1b(o_, 512); o_ += 256
        rst = h1f(o_, 512); o_ += 512
        tt_ = h1f(o_, 512); o_ += 512
        Ub = h1f(o_, 4096); o_ += 4096
        Sbf = h1b(o_, 4096); o_ += 2048
        arep = h1f(o_, 1024); RA = h1f(o_, 1024); RAi = h1[:, o_:o_ + 1024].bitcast(I32); o_ += 1024
        Shalf = h1f(o_, 1024); RB = h1f(o_, 1024); RBi = h1[:, o_:o_ + 1024].bitcast(I32); o_ += 1024
        Shalf2 = h1f(o_, 1024); o_ += 1024
        T_s2 = h1f(o_, 512); o_ += 512
        T_q2 = h1f(o_, 512); o_ += 512
        T_B2 = h1f(o_, 512); o_ += 512
        assert o_ <= NT * D
        msb = h1f(0, 3072)
        modbb = h1f(3072, 3072)
        pgb = h1f(6144, 2048)
        rows = h1f(8192, 3072)

        pA = [ps("pA%d" % i, [128, 512], F32) for i in range(2)]
        pB = [ps("pB%d" % i, [128, 512], F32) for i in range(2)]
        pO = ps("pO", [128, 512], F32)
        pS = ps("pS", [128, 512], F32)
        pT0 = ps("pT0", [128, 1024], BF16)
        pM = ps("pM", [128, 512], F32)

        H1A = "h1all"

        def WK(k, g0=0, g1=4):
            return [("wbf", k, g) for g in range(g0, g1)]

        def top(hw, fn, reads=(), writes=()):
            return P.op(hw, fn, tuple(reads) + (H1A,), writes)

        def tdma(hw, slot, fn, reads=(), writes=()):
            return P.dma(hw, slot, fn, tuple(reads) + (H1A,), writes)

        def barrier():
            for e in ("act", "dve", "pool"):
                P.op(e, (lambda eng, e=e: (eng.memset(small[:, 250:251], 0.0) if e != "act" else
                                            eng.activation(out=small[:, 251:252], in_=small[:, 252:253], func=AF.Copy))),
                     writes=[("bar", e)])
            P.op("pe", lambda eng: eng.matmul(pM[0:1, 0:1], lhsT=onesb[:, 0:1], rhs=onesb[:, 0:1], start=True, stop=True),
                 reads=["cb"], writes=["pM", ("bar", "pe")])
            for e in ("act", "dve", "pool", "pe", "sp"):
                if e == "pe":
                    P.op("pe", lambda eng: eng.matmul(pM[0:1, 0:1], lhsT=onesb[:, 0:1], rhs=onesb[:, 0:1], start=True, stop=True),
                         reads=["cb"] + [("bar", q) for q in ("act", "dve", "pool")], writes=["pM"])
                elif e == "sp":
                    P.op("sp", lambda eng: eng.nop(), reads=[("bar", q) for q in ("act", "dve", "pool", "pe")])
                else:
                    P.op(e, (lambda eng, e=e: (eng.memset(small[:, 253:254], 0.0) if e == "dve" else
                                                eng.memset(small[:, 254:255], 0.0) if e == "pool" else
                                                eng.activation(out=small[:, 255:256], in_=small[:, 252:253], func=AF.Copy))),
                         reads=[("bar", q) for q in ("act", "dve", "pool", "pe") if q != e])

        P.dma("pool", "cb", lambda e: e.dma_start(out=cb[:], in_=cf[:, 0:NCB]), writes=["cb"])
        P.dma("sp", "cfs", lambda e: e.dma_start(out=cfs[:], in_=cf[:, NCB:NCF]), writes=["cfs"])
        P.op("dve", lambda e: e.memset(small[:], 0.0), writes=["small"])
        P.op("dve", lambda e: e.memset(bmat[:], 0.0), writes=["birow"])
        P.op("dve", lambda e: e.memset(biasT[:], 0.0), writes=["biasT"])
        P.op("pool", lambda e: e.memset(hb[3][:], 0.0), writes=[("kt", 0), ("kt", 1)])
        P.op("pool", lambda e: e.memset(hb[5][:], 0.0), writes=[("kt", 0), ("kt", 1)])
        with nc.allow_non_contiguous_dma(reason="tiny one-time transposed loads"):
            pass
        for bb in range(NB):
            P.dma("sp", "s0", lambda e, bb=bb: e.dma_start(out=cT.rearrange("p (c b) -> p c b", b=NB)[:, :, bb], in_=cin[bb].rearrange("(c p) -> p c", p=128),
                                                         allow_slow_non_contiguous=True), reads=["small"], writes=[("cT", bb)])
        P.dma("sp", "s1", lambda e: e.dma_start(out=gkT, in_=kv_g[0].rearrange("(c p) -> p c", p=128), allow_slow_non_contiguous=True),
              reads=["small"], writes=["gkT"])
        for l in range(2):
            P.dma("sp", "s2", lambda e, l=l: e.dma_start(out=lbT[:, l * 8:(l + 1) * 8], in_=a_lb[l].rearrange("(c p) -> p c", p=128),
                                                       allow_slow_non_contiguous=True), reads=["small"], writes=[("lbT", l)])
        P.dma("sp", "s3", lambda e: e.dma_start(out=gn, in_=a_gn), reads=["small"], writes=["gn"])
        P.op("act", lambda e: e.activation(out=scT[:], in_=cT, func=AF.Silu), reads=[("cT", q) for q in range(NB)], writes=["scT"])
        P.op("dve", lambda e: e.tensor_tensor(out=oml, in0=lbT[:, 8:16], in1=lbT[:, 0:8], op=ALU.subtract), reads=[("lbT", 0), ("lbT", 1)], writes=["oml"])
        P.op("act", lambda e: e.activation(out=oml, in_=oml, func=AF.Sigmoid), reads=["oml"], writes=["oml"])
        P.op("dve", lambda e: e.tensor_scalar(out=noml, in0=oml, scalar1=-1.0, scalar2=None, op0=ALU.mult), reads=["oml", "small"], writes=["noml"])
        wi = 0
        for l in range(2):
            tdma("sp", "s4", lambda e, l=l: e.dma_start(out=modbb[0:NB, :], in_=mod_b[l].partition_broadcast(NB)), writes=["modbb"])
            tdma("sp", "s5", lambda e, l=l: e.dma_start(out=pgb[0:NB, 0:1024], in_=pre_g[l].partition_broadcast(NB)), writes=["pgb0"])
            tdma("sp", "s6", lambda e, l=l: e.dma_start(out=pgb[0:NB, 1024:2048], in_=post_g[l].partition_broadcast(NB)), writes=["pgb1"])
            for cbk in range(6):
                k = wi % 2
                wi += 1
                P.dma("pool", "wbf%d" % k, lambda e, l=l, cbk=cbk, k=k: e.dma_start(
                    out=wbf[k][:], in_=mod_w[l][:, cbk * 512:(cbk + 1) * 512].rearrange("(c p) n -> p c n", p=128)),
                    writes=WK(k))
                for c in range(8):
                    P.op("pe", lambda e, c=c, k=k: e.matmul(pM[0:NB, :], lhsT=scT[:, c * NB:(c + 1) * NB], rhs=wbf[k][:, c, :],
                                                            start=(c == 0), stop=(c == 7)),
                         reads=["scT"] + WK(k), writes=["pM"])
                top("dve", lambda e, cbk=cbk: e.tensor_tensor(
                    out=msb[0:NB, cbk * 512:(cbk + 1) * 512], in0=pM[0:NB, :],
                    in1=modbb[0:NB, cbk * 512:(cbk + 1) * 512], op=ALU.add),
                    reads=["pM", "modbb"], writes=[("msb", cbk)])
            mk = [("msb", i) for i in range(6)]
            top("dve", lambda e: e.scalar_tensor_tensor(out=rows[0:NB, 0:1024], in0=msb[0:NB, 1024:2048],
                                                        scalar=1.0, in1=pgb[0:NB, 0:1024], op0=ALU.add, op1=ALU.mult),
                reads=mk + ["pgb0"], writes=["rows"])
            top("dve", lambda e: e.tensor_copy(out=rows[0:NB, 1024:2048], in_=msb[0:NB, 0:1024]), reads=mk, writes=["rows"])
            top("dve", lambda e: e.tensor_tensor(out=rows[0:NB, 2048:3072], in0=msb[0:NB, 2048:3072], in1=pgb[0:NB, 1024:2048], op=ALU.mult),
                reads=mk + ["pgb1"], writes=["rows"])
            for kind in range(3):
                tdma("sp", "s7", lambda e, l=l, kind=kind: e.dma_start(out=scr[l, kind], in_=rows[0:NB, kind * 1024:(kind + 1) * 1024]),
                     reads=["rows"], writes=[("scr", l, kind)])
        barrier()

        xin_i = [0]

        def layer_vectors(l, b):
            P.dma("sp", "v0", lambda e: e.dma_start(out=aT, in_=scr[l, 0, b].rearrange("(c p) -> p c", p=128), allow_slow_non_contiguous=True),
                  reads=[("scr", l, 0)], writes=["aT"])
            P.dma("sp", "v1", lambda e: e.dma_start(out=shT, in_=scr[l, 1, b].rearrange("(c p) -> p c", p=128), allow_slow_non_contiguous=True),
                  reads=[("scr", l, 1)], writes=["shT"])
            P.dma("sp", "v2", lambda e: e.dma_start(out=G_bc[:], in_=scr[l, 2, b].partition_broadcast(128)), reads=[("scr", l, 2)], writes=["G_bc"])
            P.op("dve", lambda e: e.tensor_copy(out=shTb, in_=shT), reads=["shT"], writes=["shTb"])

        def rstd_from(col_in, col_out, keyin, keyout):
            P.op("dve", lambda e: e.tensor_scalar(out=col_out, in0=col_in, scalar1=1.0 / D, scalar2=EPS, op0=ALU.mult, op1=ALU.add),
                 reads=[keyin], writes=[keyout])
            P.op("act", lambda e: e.activation(out=col_out, in_=col_out, func=AF.Sqrt), reads=[keyout], writes=[keyout])
            P.op("dve", lambda e: e.reciprocal(out=col_out, in_=col_out), reads=[keyout], writes=[keyout])

        def pre_phase(l, b):
            for tt in range(NT):
                k = tt % 2
                if l == 0:
                    xi = xin_i[0] % 2
                    xin_i[0] += 1
                    P.dma("sp", "xin%d" % xi, lambda e, tt=tt, xi=xi: e.dma_start(out=xin[xi][:], in_=x[b, tt * 128:(tt + 1) * 128, :]),
                          writes=[("xin", xi)])
                    src, skey = xin[xi][:], ("xin", xi)
                else:
                    src, skey = h1[:, tt * D:(tt + 1) * D], ("h1", tt)
                P.op("dve", lambda e, tt=tt: e.memset(ss[:, tt:tt + 1], 0.0), writes=[("ss", tt)])
                P.op("act", lambda e, src=src, tt=tt: e.activation(out=xs[0][:], in_=src, func=AF.Square, accum_out=ss[:, tt:tt + 1]),
                     reads=[skey, ("ss", tt)], writes=[("xs", 0), ("ss", tt)])
                rstd_from(ss[:, tt:tt + 1], rstd[:, tt:tt + 1], ("ss", tt), ("rstd", tt))
                P.op("dve", lambda e, src=src, tt=tt, k=k: e.tensor_scalar(out=xs[k][:], in0=src, scalar1=rstd[:, tt:tt + 1], scalar2=None, op0=ALU.mult),
                     reads=[skey, ("rstd", tt)], writes=[("xs", 0)])
                for c in range(8):
                    P.op("pe", lambda e, c=c, k=k: e.transpose(out=pT0[:, c * 128:(c + 1) * 128], in_=xs[k][:, c * 128:(c + 1) * 128], identity=ident),
                         reads=[("xs", 0), "cb"], writes=["pT0"])
                P.op("act", lambda e, tt=tt: e.activation(out=xnT[:, :, tt * 128:(tt + 1) * 128], in_=pT0[:].rearrange("p (c t) -> p c t", c=8), func=AF.Copy),
                     reads=["pT0"], writes=[("xnT", tt)])

        wslot = [0]

        def load_head_weights(pieces):
            k = wslot[0] % 2
            wslot[0] += 1
            for g, ap in enumerate(pieces):
                P.dma("pool", "wbf%d" % k, lambda e, g=g, ap=ap, k=k: e.dma_start(
                    out=wbf[k][:, :, g * 128:(g + 1) * 128], in_=ap.rearrange("(c p) n -> p c n", p=128)),
                    writes=[("wbf", k, g)])
            return k

        def bias_cols(k, groups):
            for j, g in enumerate(groups):
                for c in range(8):
                    P.op("pe", lambda e, j=j, g=g, c=c: e.matmul(pM[:, j:j + 1], lhsT=wbf[k][:, c, g * 128:(g + 1) * 128], rhs=shTb[:, c:c + 1],
                                                               start=(c == 0), stop=(c == 7)),
                         reads=[("wbf", k, g), "shTb"], writes=["pM"])
            P.op("dve", lambda e: e.tensor_copy(out=bcol[:, 0:len(groups)], in_=pM[:, 0:len(groups)]), reads=["pM"], writes=["bcol"])

        def scale_w(k, c0, c1, vec, vkey):
            P.op("pool", lambda e: e.tensor_tensor(out=wbf[k][:, :, c0:c1], in0=wbf[k][:, :, c0:c1],
                                                   in1=vec.unsqueeze(2).to_broadcast([128, 8, c1 - c0]), op=ALU.mult),
                 reads=WK(k, c0 // 128, c1 // 128) + [vkey], writes=WK(k, c0 // 128, c1 // 128))

        pa_i = [0]

        def proj_fm(k, g, tb):
            i = pa_i[0] % 2
            pa_i[0] += 1
            for c in range(8):
                P.op("pe", lambda e, c=c, i=i: e.matmul(pA[i][:], lhsT=wbf[k][:, c, g * 128:(g + 1) * 128], rhs=xnT[:, c, tb * 512:(tb + 1) * 512],
                                                      start=(c == 0), stop=(c == 7)),
                     reads=[("wbf", k, g)] + [("xnT", 4 * tb + q) for q in range(4)], writes=[("pA", i)])
            return i

        def proj_tm(k, g, dst, dkey, bias_row=False):
            for t4 in range(4):
                i = t4 % 2
                for j in range(4):
                    tt = t4 * 4 + j
                    for c in range(8):
                        P.op("pe", lambda e, c=c, tt=tt, j=j, i=i: e.matmul(pB[i][:, j * 128:(j + 1) * 128], lhsT=xnT[:, c, tt * 128:(tt + 1) * 128],
                                                                          rhs=wbf[k][:, c, g * 128:(g + 1) * 128], start=(c == 0),
                                                                          stop=(c == 7 and not bias_row)),
                             reads=[("wbf", k, g), ("xnT", tt)], writes=[("pB", i)])
                    if bias_row:
                        P.op("pe", lambda e, j=j, i=i: e.matmul(pB[i][:, j * 128:(j + 1) * 128], lhsT=onesb, rhs=bmat[:], start=False, stop=True),
                             reads=["cb", "birow"], writes=[("pB", i)])
                P.op("act", lambda e, t4=t4, i=i: e.activation(out=dst[:, t4 * 512:(t4 + 1) * 512], in_=pB[i][:], func=AF.Copy),
                     reads=[("pB", i)], writes=[(dkey, t4)])

        def post_phase(l, b, wout, last):
            for cbk in range(2):
                P.dma("pool", "wbf%d" % cbk, lambda e, cbk=cbk: e.dma_start(
                    out=wbf[cbk][:], in_=wout[:, cbk * 512:(cbk + 1) * 512].rearrange("(c p) n -> p c n", p=128)), writes=WK(cbk))
            wslot[0] = 0
            for tt in range(NT):
                pp, pk = (pA, "pA") if tt % 2 == 0 else (pB, "pB")
                for cbk in range(2):
                    for c in range(8):
                        P.op("pe", lambda e, c=c, cbk=cbk, tt=tt, pp=pp: e.matmul(pp[cbk][:], lhsT=ogT[:, c, tt * 128:(tt + 1) * 128], rhs=wbf[cbk][:, c, :],
                                                                         start=(c == 0), stop=(c == 7)),
                             reads=WK(cbk) + [("ogT", hh, tt // 4) for hh in range(8)], writes=[(pk, cbk)])
                    P.op("dve", lambda e, cbk=cbk: e.memset(ssy[:, cbk:cbk + 1], 0.0), writes=[("ssy", cbk)])
                    P.op("act", lambda e, cbk=cbk, pp=pp: e.activation(out=PT[cbk][:], in_=pp[cbk][:], func=AF.Square, accum_out=ssy[:, cbk:cbk + 1]),
                         reads=[(pk, cbk), ("ssy", cbk)], writes=[("PT", cbk), ("ssy", cbk)])
                P.op("dve", lambda e: e.tensor_tensor(out=ssy[:, 2:3], in0=ssy[:, 0:1], in1=ssy[:, 1:2], op=ALU.add),
                     reads=[("ssy", 0), ("ssy", 1)], writes=[("ssy", 2)])
                rstd_from(ssy[:, 2:3], ssy[:, 3:4], ("ssy", 2), ("ssy", 3))
                xi = xin_i[0] % 2
                xin_i[0] += 1
                if l == 0:
                    P.dma("sp", "xin%d" % xi, lambda e, tt=tt, xi=xi: e.dma_start(out=xin[xi][:], in_=x[b, tt * 128:(tt + 1) * 128, :]),
                          writes=[("xin", xi)])
                    dest, dkey, res, rkey = h1[:, tt * D:(tt + 1) * D], ("h1", tt), xin[xi][:], ("xin", xi)
                    extra_w = [H1A]
                else:
                    dest, dkey, res, rkey = xin[xi][:], ("xin", xi), h1[:, tt * D:(tt + 1) * D], ("h1", tt)
                    extra_w = [H1A]
                for cbk in range(2):
                    P.op("dve", lambda e, cbk=cbk, dest=dest, pp=pp: e.scalar_tensor_tensor(
                        out=dest[:, cbk * 512:(cbk + 1) * 512], in0=pp[cbk][:], scalar=ssy[:, 3:4], in1=G_bc[:, cbk * 512:(cbk + 1) * 512],
                        op0=ALU.mult, op1=ALU.mult), reads=[(pk, cbk), ("ssy", 3), "G_bc"], writes=[dkey] + (extra_w if l == 0 else []))
                P.op("dve", lambda e, dest=dest, res=res: e.tensor_tensor(out=dest, in0=dest, in1=res, op=ALU.add),
                     reads=[dkey, rkey], writes=[dkey] + extra_w)
                if last:
                    P.dma("sp", "xin%d" % xi, lambda e, tt=tt, dest=dest: e.dma_start(out=out[b, tt * 128:(tt + 1) * 128, :], in_=dest),
                          reads=[dkey], writes=[("out", b, tt)])

        def rope_tables(b):
            for hf in range(2):
                cs = slice(hf * 1024, (hf + 1) * 1024)
                tdma("sp", "ra", lambda e, cs=cs: e.dma_start(out=RAi, in_=pos[b, cs].partition_broadcast(128)), writes=["arep"])
                top("dve", lambda e: e.tensor_copy(out=RA, in_=RAi), reads=["arep"], writes=["arep"])
                top("dve", lambda e: e.tensor_scalar(out=RA, in0=RA, scalar1=invf, scalar2=None, op0=ALU.mult), reads=["arep", "cfs"], writes=["arep"])
                for which in range(2):
                    off = 0.0 if which == 0 else float(np.pi / 2)
                    top("dve", lambda e, off=off: e.tensor_scalar(out=RB, in0=RA, scalar1=off, scalar2=float(1.0 / (2 * np.pi)), op0=ALU.add, op1=ALU.mult),
                        reads=["arep"], writes=["Shalf"])
                    top("dve", lambda e: e.tensor_copy(out=RBi, in_=RB), reads=["Shalf"], writes=["Shalf"])
                    top("dve", lambda e: e.tensor_copy(out=RB, in_=RBi), reads=["Shalf"], writes=["Shalf"])
                    top("dve", lambda e: e.tensor_scalar(out=RB, in0=RB, scalar1=float(-2 * np.pi), scalar2=None, op0=ALU.mult), reads=["Shalf"], writes=["Shalf"])
                    top("dve", lambda e, off=off: e.scalar_tensor_tensor(out=RB, in0=RA, scalar=off, in1=RB, op0=ALU.add, op1=ALU.add),
                        reads=["arep", "Shalf"], writes=["Shalf"])
                    top("dve", lambda e: e.tensor_scalar(out=RB, in0=RB, scalar1=3.14159, scalar2=-3.14159, op0=ALU.min, op1=ALU.max),
                        reads=["Shalf"], writes=["Shalf"])
                    if which == 0:
                        top("act", lambda e: e.activation(out=RB, in_=RB, func=AF.Sin), reads=["Shalf"], writes=["Shalf"])
                        top("dve", lambda e, cs=cs: e.tensor_scalar(out=sinT[:, cs], in0=RB, scalar1=sgn, scalar2=None, op0=ALU.mult),
                            reads=["Shalf", "cfs"], writes=["sinT"])
                    else:
                        top("act", lambda e, cs=cs: e.activation(out=cosT[:, cs], in_=RB, func=AF.Sin), reads=["Shalf"], writes=["cosT"])

        def l0_head(h, k):
            qtT, ktT, sgT, kt, vv, ktB = hb[0], hb[1], hb[2], hb[3], hb[4], hb[5]
            bias_cols(k, [0, 1, 3])
            P.op("dve", lambda e: e.tensor_scalar(out=nbf, in0=bcol[:, 1:2], scalar1=-1.0, scalar2=None, op0=ALU.mult), reads=["bcol"], writes=["nbf"])
            for c in range(8):
                P.op("pe", lambda e, c=c: e.matmul(pM[0:1, 128:256], lhsT=shTb[:, c:c + 1], rhs=wbf[k][:, c, 256:384], start=(c == 0), stop=(c == 7)),
                     reads=[("wbf", k, 2), "shTb"], writes=["pM"])
            P.op("dve", lambda e: e.tensor_copy(out=bmat[0:1, :], in_=pM[0:1, 128:256]), reads=["pM"], writes=["birow"])
            scale_w(k, 0, 512, aT, "aT")
            knext = None
            if h < 7:
                knext = load_head_weights([a_w_in[:, g * 1024 + (h + 1) * 128:g * 1024 + (h + 2) * 128] for g in range(4)])
            for tb in range(4):
                bs = slice(tb * 512, (tb + 1) * 512)
                Tq, TB, Ts = (T_q, T_B, T_s) if tb % 2 == 0 else (T_q2, T_B2, T_s2)
                kq, kB, ks = ("T_q", "T_B", "T_s") if tb % 2 == 0 else ("T_q2", "T_B2", "T_s2")
                i = proj_fm(k, 0, tb)
                top("act", lambda e, i=i, Tq=Tq: e.activation(out=Tq, in_=pA[i][:], func=AF.Silu, bias=bcol[:, 0:1]), reads=[("pA", i), "bcol"], writes=[kq])
                i = proj_fm(k, 3, tb)
                P.op("act", lambda e, i=i, bs=bs: e.activation(out=sgT[:, bs], in_=pA[i][:], func=AF.Silu, bias=bcol[:, 2:3]),
                     reads=[("pA", i), "bcol"], writes=[("sgT", tb)])
                i = proj_fm(k, 1, tb)
                top("act", lambda e, i=i, Ts=Ts: e.activation(out=Ts, in_=pA[i][:], func=AF.Sigmoid, bias=nbf, scale=-1.0), reads=[("pA", i), "nbf"], writes=[ks])
                top("dve", lambda e, Ts=Ts: e.tensor_scalar(out=T_d0, in0=Ts, scalar1=noml[:, h:h + 1], scalar2=1.0, op0=ALU.mult, op1=ALU.add),
                    reads=[ks, "noml"], writes=["T_d0"])
                if tb == 0 and h == 0:
                    top("dve", lambda e: e.memset(T_d1, 0.0), writes=["T_d1"])
                d0v = T_d0.rearrange("p (n c) -> p n c", c=64)[:, :, 0:1]
                d1v = T_d1.rearrange("p (n c) -> p n c", c=64)[:, :, 0:1]
                top("dve", lambda e, d0v=d0v, d1v=d1v: e.tensor_copy(out=d1v, in_=d0v), reads=["T_d0", "T_d1"], writes=["T_d1"])
                top("dve", lambda e, d0v=d0v: e.memset(d0v, 0.0), reads=["T_d0", "T_d1"], writes=["T_d0"])
                top("dve", lambda e, TB=TB: e.tensor_tensor_scan(out=TB, data0=T_d0, data1=T_d1, initial=0.0, op0=ALU.mult, op1=ALU.add),
                    reads=["T_d0", "T_d1"], writes=[kB])
                top("dve", lambda e, tb=tb, TB=TB: e.tensor_copy(out=Bl[:, tb * 8:(tb + 1) * 8].unsqueeze(2),
                                                                 in_=TB.rearrange("p (n c) -> p n c", c=64)[:, :, 63:64]), reads=[kB], writes=["Bl"])
                top("pool", lambda e, bs=bs, Tq=Tq, TB=TB: e.tensor_tensor(out=qtT[:, bs], in0=Tq, in1=TB, op=ALU.mult), reads=[kq, kB], writes=[("qtT", tb)])
                top("dve", lambda e, TB=TB: e.reciprocal(out=T_d0, in_=TB), reads=[kB], writes=["T_d0"])
                top("dve", lambda e, bs=bs, Ts=Ts: e.scalar_tensor_tensor(out=ktT[:, bs], in0=Ts, scalar=oml[:, h:h + 1], in1=T_d0, op0=ALU.mult, op1=ALU.mult),
                    reads=[ks, "T_d0", "oml"], writes=[("ktT", tb)])
            top("dve", lambda e: e.tensor_copy(out=Bl0, in_=Bl), reads=["Bl"], writes=["Bl0"])
            top("dve", lambda e: e.memset(Bl0[:, 0:1], 0.0), reads=["Bl0"], writes=["Bl0"])
            top("pool", lambda e: e.tensor_copy(out=arep.rearrange("p (v n) -> p v n", n=32), in_=Bl0.unsqueeze(1).to_broadcast([128, 32, 32])),
                reads=["Bl0"], writes=["arep"])
            if limit == "h0a":
                return knext
            proj_tm(k, 2, vv, "vv", bias_row=True)
            for t8 in range(2):
                for j in range(8):
                    tt = t8 * 8 + j
                    P.op("pe", lambda e, tt=tt, j=j: e.transpose(out=pT0[:, j * 128:(j + 1) * 128], in_=ktT[:, tt * 128:(tt + 1) * 128], identity=ident),
                         reads=[("ktT", tt // 4), "cb"], writes=["pT0"])
                P.op("act", lambda e, t8=t8: e.activation(out=kt[0:64, t8 * 1024:(t8 + 1) * 1024], in_=pT0[0:64, :], func=AF.Copy), reads=["pT0"], writes=[("kt", t8)])
                P.op("act", lambda e, t8=t8: e.activation(out=ktB[64:128, t8 * 1024:(t8 + 1) * 1024], in_=pT0[64:128, :], func=AF.Copy), reads=["pT0"], writes=[("kt", t8)])
            if limit == "h0b":
                return knext
            Uv = Ub.rearrange("p (v n) -> p n v", n=32)
            for ng in range(8):
                i = ng % 2
                for j in range(4):
                    n = ng * 4 + j
                    tt, half = n // 2, n % 2
                    ksrc = kt if half == 0 else ktB
                    P.op("pe", lambda e, j=j, tt=tt, ksrc=ksrc, i=i: e.matmul(pB[i][:, j * 128:(j + 1) * 128], lhsT=ksrc[:, tt * 128:(tt + 1) * 128],
                                                                            rhs=vv[:, tt * 128:(tt + 1) * 128], start=True, stop=True),
                         reads=[("kt", tt // 8), ("vv", tt // 4)], writes=[("pB", i)])
                top("dve", lambda e, ng=ng, i=i: e.tensor_tensor(out=Uv[:, ng * 4:(ng + 1) * 4, :], in0=pB[i][:].rearrange("p (n v) -> p n v", n=4),
                                                                 in1=Bl[:, ng * 4:(ng + 1) * 4].unsqueeze(2).to_broadcast([128, 4, 128]), op=ALU.mult),
                    reads=[("pB", i), "Bl"], writes=["Ub"])
            if limit == "h0c1":
                return knext
            for vq in range(4):
                Sq, ksq = (Shalf, "Shalf") if vq % 2 == 0 else (Shalf2, "Shalf2")
                top("dve", lambda e, vq=vq, Sq=Sq: e.tensor_tensor_scan(out=Sq, data0=arep, data1=Ub[:, vq * 1024:(vq + 1) * 1024], initial=0.0,
                                                                        op0=ALU.mult, op1=ALU.add),
                    reads=["arep", "Ub"], writes=[ksq])
                dstv = Sbf.rearrange("p (n v) -> p v n", n=32)[:, vq * 32:(vq + 1) * 32, :]
                if vq % 2 == 0:
                    top("act", lambda e, dstv=dstv, Sq=Sq: e.activation(out=dstv, in_=Sq.rearrange("p (v n) -> p v n", n=32), func=AF.Copy),
                        reads=[ksq], writes=[("Sbf", vq)])
                else:
                    top("pool", lambda e, dstv=dstv, Sq=Sq: e.tensor_copy(out=dstv, in_=Sq.rearrange("p (v n) -> p v n", n=32)),
                        reads=[ksq], writes=[("Sbf", vq)])
            def att_stage(pg):
                i = pg % 2
                for j in range(4):
                    pr = pg * 4 + j
                    P.op("pe", lambda e, j=j, pr=pr, i=i: e.matmul(pB[i][:, j * 128:(j + 1) * 128], lhsT=ktT[:, pr * 128:(pr + 1) * 128],
                                                                 rhs=qtT[:, pr * 128:(pr + 1) * 128], start=True, stop=True),
                         reads=[("ktT", pg), ("qtT", pg)], writes=[("pB", i)])
                top("dve", lambda e, i=i: e.tensor_tensor(out=attS[i].rearrange("p (n t) -> p n t", n=4), in0=pB[i][:].rearrange("p (n t) -> p n t", n=4),
                                                          in1=pmask.unsqueeze(1).to_broadcast([128, 4, 128]), op=ALU.mult),
                    reads=[("pB", i), "cb"], writes=[("attS", i)])

            att_stage(0)
            pT0f0 = pT0[:].bitcast(F32)
            for pg in range(4):
                i = pg % 2
                if pg % 2 == 0:
                    aO, aS, kO, kS = pO[:], pS[:], "pO", "pS"
                else:
                    aO, aS, kO, kS = pT0f0, pM[:], "pT0", "pM"
                if pg < 3:
                    att_stage(pg + 1)
                sbk = [("Sbf", q) for q in range(4)]
                for j in range(4):
                    pr = pg * 4 + j
                    P.op("pe", lambda e, j=j, pr=pr, i=i, aO=aO: e.matmul(aO[:, j * 128:(j + 1) * 128], lhsT=vv[:, pr * 128:(pr + 1) * 128],
                                                                        rhs=attS[i][:, j * 128:(j + 1) * 128], start=True, stop=False, skip_group_check=True),
                         reads=[("vv", pg), ("attS", i), H1A], writes=[kO])
                    if pr > 0:
                        P.op("pe", lambda e, j=j, pr=pr, aO=aO: e.matmul(aO[:, j * 128:j * 128 + 64], lhsT=Sbf[:, (2 * pr - 1) * 128:(2 * pr) * 128],
                                                                       rhs=qtT[:, pr * 128:pr * 128 + 64], start=False, stop=False, skip_group_check=True),
                             reads=sbk + [("qtT", pg), H1A], writes=[kO])
                    P.op("pe", lambda e, j=j, pr=pr, aO=aO: e.matmul(aO[:, j * 128 + 64:(j + 1) * 128], lhsT=Sbf[:, (2 * pr) * 128:(2 * pr + 1) * 128],
                                                                   rhs=qtT[:, pr * 128 + 64:(pr + 1) * 128], start=False, stop=True, skip_group_check=True),
                         reads=sbk + [("qtT", pg), H1A], writes=[kO])
                top("act", lambda e, aO=aO: e.activation(out=sqb, in_=aO, func=AF.Square), reads=[kO], writes=["sqb"])
                P.op("pe", lambda e, aS=aS: e.matmul(aS, lhsT=onesb, rhs=sqb, start=True, stop=True), reads=["sqb", "cb", H1A], writes=[kS])
                top("dve", lambda e, aS=aS: e.tensor_scalar(out=rst, in0=aS, scalar1=1.0 / 128, scalar2=EPS, op0=ALU.mult, op1=ALU.add), reads=[kS], writes=["rst"])
                top("act", lambda e: e.activation(out=rst, in_=rst, func=AF.Sqrt), reads=["rst"], writes=["rst"])
                top("dve", lambda e: e.reciprocal(out=rst, in_=rst), reads=["rst"], writes=["rst"])
                tbuf, tkey = (tt_, "tt_") if pg % 2 == 0 else (osb, "osb")
                top("dve", lambda e, aO=aO, tbuf=tbuf: e.scalar_tensor_tensor(out=tbuf, in0=aO, scalar=gn, in1=rst, op0=ALU.mult, op1=ALU.mult),
                    reads=[kO, "rst", "gn"], writes=[tkey])
                top("pool", lambda e, pg=pg, tbuf=tbuf: e.tensor_tensor(out=ogT[:, h, pg * 512:(pg + 1) * 512], in0=tbuf, in1=sgT[:, pg * 512:(pg + 1) * 512], op=ALU.mult),
                    reads=[tkey, ("sgT", pg)], writes=[("ogT", h, pg)])
            return knext

        pt_i = [0]

        def l1_pieces(h):
            return [b_w_in[:, h * 128:(h + 1) * 128], b_w_in[:, 1024 + h * 128:1024 + (h + 1) * 128],
                    w_kv[:, h * 128:(h + 1) * 128], w_kv[:, 1024 + h * 128:1024 + (h + 1) * 128]]

        def l1_head(h, k):
            QT, KT, szT, V = hb[0], hb[1], hb[2], hb[4]
            bias_cols(k, [0, 1])
            scale_w(k, 0, 256, aT, "aT")
            scale_w(k, 256, 512, gkT, "gkT")
            knext = load_head_weights(l1_pieces(h + 1)) if h < 7 else None
            if limit == "l1a0":
                return knext
            for tb in range(4):
                bs = slice(tb * 512, (tb + 1) * 512)
                for which in range(2):
                    if limit == "l1a1" and (tb, which) == (0, 1):
                        return knext
                    if limit == "l1a2" and (tb, which) == (1, 0):
                        return knext
                    dst = QT if which == 0 else KT
                    dkey = "QT" if which == 0 else "KT"
                    i = proj_fm(k, 0 if which == 0 else 2, tb)
                    j = (tb * 2 + which) % 2
                    if which == 0:
                        P.op("dve", lambda e, i=i: e.tensor_scalar(out=PT[0][:], in0=pA[i][:], scalar1=bcol[:, 0:1], scalar2=None, op0=ALU.add),
                            reads=[("pA", i), "bcol"], writes=[("PT", 0)])
                        P.op("dve", lambda e, i=i, bs=bs: e.scalar_tensor_tensor(out=rsb[:], in0=pA[i][:], scalar=bcol[:, 0:1], in1=cosT[:, bs], op0=ALU.add, op1=ALU.mult),
                            reads=[("pA", i), "bcol", "cosT"], writes=["rsb"])
                    else:
                        P.op("act", lambda e, i=i: e.activation(out=PT[0][:], in_=pA[i][:], func=AF.Copy), reads=[("pA", i)], writes=[("PT", 0)])
                        P.op("dve", lambda e, i=i, bs=bs: e.tensor_tensor(out=rsb[:], in0=pA[i][:], in1=cosT[:, bs], op=ALU.mult),
                            reads=[("pA", i), "cosT"], writes=["rsb"])
                    P.op("pe", lambda e, j=j: e.matmul(pB[j][:], lhsT=swp, rhs=PT[0][:], start=True, stop=True), reads=[("PT", 0), "cb"], writes=[("pB", j)])
                    P.op("dve", lambda e, j=j, bs=bs: e.tensor_tensor(out=otb[:], in0=pB[j][:], in1=sinT[:, bs], op=ALU.mult), reads=[("pB", j), "sinT"], writes=["otb"])
                    P.op("dve", lambda e, dst=dst, bs=bs: e.tensor_tensor(out=dst[:, bs], in0=rsb[:], in1=otb[:], op=ALU.add), reads=["rsb", "otb"], writes=[(dkey, tb)])
                i = proj_fm(k, 1, tb)
                P.op("act", lambda e, i=i, bs=bs: e.activation(out=szT[:, bs], in_=pA[i][:], func=AF.Silu, bias=bcol[:, 1:2]),
                     reads=[("pA", i), "bcol"], writes=[("szT", tb)])
            if limit == "l1a":
                return knext
            proj_tm(k, 3, V, "V")
            P.op("dve", lambda e: e.tensor_reduce(out=km, in_=KT[:].rearrange("p (n k) -> p n k", k=256), axis=AX.X, op=ALU.add),
                 reads=[("KT", q) for q in range(4)], writes=["km"])
            P.op("dve", lambda e: e.tensor_scalar(out=kmb, in0=km, scalar1=1.0 / 256, scalar2=None, op0=ALU.mult), reads=["km"], writes=["kmb"])
            for i8 in range(8):
                P.op("pe", lambda e, i8=i8: e.matmul(pM[:, i8 * 8:(i8 + 1) * 8], lhsT=QT[:, (8 + i8) * 128:(9 + i8) * 128], rhs=kmb, start=True, stop=True),
                     reads=[("QT", (8 + i8) // 4), "kmb"], writes=["pM"])
            P.op("dve", lambda e: e.tensor_tensor(out=gbuf[:], in0=pM[:, 0:64], in1=negm, op=ALU.add), reads=["pM", "cfs"], writes=["gbuf"])
            g3 = gbuf[:].rearrange("p (i n) -> p i n", n=8)
            P.op("dve", lambda e: e.tensor_tensor(out=cmpb[:], in0=g3.unsqueeze(2).to_broadcast([128, 8, 8, 8]),
                                                  in1=g3.unsqueeze(3).to_broadcast([128, 8, 8, 8]), op=ALU.is_gt), reads=["gbuf"], writes=["cmpb"])
            P.op("dve", lambda e: e.tensor_reduce(out=rankb[:], in_=cmpb[:].rearrange("p i n m -> p (i n) m"), axis=AX.X, op=ALU.add),
                 reads=["cmpb"], writes=["rankb"])
            bq_pad = cmpb[:].rearrange("p a b c -> p (a b c)").bitcast(BF16).rearrange("p (i c) -> p i c", c=128)
            P.op("dve", lambda e: e.memset(bq_pad, 0.0), reads=["rankb"], writes=["cmpb"])
            P.op("dve", lambda e: e.tensor_scalar(out=bq_pad[:, :, 0:8], in0=rankb[:].rearrange("p (i n) -> p i n", n=8), scalar1=2.5, scalar2=-BIG,
                                                  op0=ALU.is_ge, op1=ALU.mult), reads=["rankb", "cmpb"], writes=["cmpb"])
            for i8 in range(8):
                P.op("pe", lambda e, i8=i8: e.transpose(out=pT0[:, i8 * 128:(i8 + 1) * 128], in_=bq_pad[:, i8, :], identity=ident),
                     reads=["cmpb", "cb"], writes=["pT0"])
            P.op("act", lambda e: e.activation(out=biasT[:], in_=pT0[:], func=AF.Copy), reads=["pT0"], writes=["biasT"])
            if limit == "l1b":
                return knext
            sc = float(128 ** -0.5)
            pT0f = pT0[:].bitcast(F32)
            tiles = [(g, ktile) for g in range(4) for ktile in range(4 * g + 4)]

            def acc_of(g):
                if g % 2 == 0:
                    return pO[:], pS[:], "pO", "pS"
                return pT0f, pM[:], "pT0", "pM"

            def qk_stage(idx):
                g, ktile = tiles[idx]
                c0 = max(ktile - 4 * g, 0)
                cl = slice(c0 * 128, 512)
                j, r = idx % 2, idx % 3
                mm = [(cl, KT[:, ktile * 128:(ktile + 1) * 128], QT[:, g * 512 + c0 * 128:(g + 1) * 512], [("KT", ktile // 4), ("QT", g)])]
                if g >= 2:
                    n = ktile // 2
                    lo = max(2 * n + 2 - 4 * g, c0)
                    if lo < 4:
                        mm.append((slice(lo * 128, 512), cb[:, C_ES + n * 128:C_ES + (n + 1) * 128],
                                   biasT[:, (g - 2) * 512 + lo * 128:(g - 2) * 512 + 512], ["biasT", "cb"]))
                if ktile >= 4 * g:
                    mm.append((slice(c0 * 128, c0 * 128 + 128), ident, cmask, ["cb"]))
                for mi, (csl, l_, r_, rk) in enumerate(mm):
                    P.op("pe", lambda e, csl=csl, l_=l_, r_=r_, mi=mi, j=j, last=(mi == len(mm) - 1): e.matmul(
                        pB[j][:, csl], lhsT=l_, rhs=r_, start=(mi == 0), stop=last, skip_group_check=True),
                        reads=rk, writes=[("pB", j)])
                P.op("act", lambda e, j=j, r=r, cl=cl: e.activation(out=PT[r][:, cl], in_=pB[j][:, cl], func=AF.Exp, scale=sc),
                     reads=[("pB", j)], writes=[("PT", r)])

            def pv_stage(idx):
                g, ktile = tiles[idx]
                nkt = 4 * g + 4
                c0 = max(ktile - 4 * g, 0)
                cl = slice(c0 * 128, 512)
                r = idx % 3
                aO, aS, kO, kS = acc_of(g)
                P.op("pe", lambda e, r=r, cl=cl, ktile=ktile, nkt=nkt, aO=aO: e.matmul(aO[:, cl], lhsT=V[:, ktile * 128:(ktile + 1) * 128], rhs=PT[r][:, cl],
                                                                                      start=(ktile == 0), stop=(ktile == nkt - 1), skip_group_check=True),
                     reads=[("V", ktile // 4), ("PT", r)], writes=[kO])
                P.op("pe", lambda e, r=r, cl=cl, ktile=ktile, nkt=nkt, aS=aS: e.matmul(aS[:, cl], lhsT=onesb, rhs=PT[r][:, cl],
                                                                                      start=(ktile == 0), stop=(ktile == nkt - 1), skip_group_check=True),
                     reads=["cb", ("PT", r)], writes=[kS])
                if ktile == nkt - 1:
                    P.op("dve", lambda e, aS=aS: e.reciprocal(out=rsb[:], in_=aS), reads=[kS], writes=["rsb"])
                    P.op("dve", lambda e, aO=aO: e.tensor_tensor(out=otb[:], in0=aO, in1=rsb[:], op=ALU.mult), reads=[kO, "rsb"], writes=["otb"])
                    P.op("dve", lambda e, g=g: e.tensor_tensor(out=ogT[:, h, g * 512:(g + 1) * 512], in0=otb[:], in1=szT[:, g * 512:(g + 1) * 512], op=ALU.mult),
                         reads=["otb", ("szT", g)], writes=[("ogT", h, g)])

            qk_stage(0)
            for idx in range(len(tiles)):
                if idx + 1 < len(tiles):
                    qk_stage(idx + 1)
                pv_stage(idx)
            return knext

        for b in range(nseq):
            if limit == "setup":
                break
            rope_tables(b)
            layer_vectors(0, b)
            if limit == "rope":
                break
            kw = load_head_weights([a_w_in[:, g * 1024:g * 1024 + 128] for g in range(4)])
            pre_phase(0, b)
            if limit == "pre":
                break
            for h in range(8):
                kw = l0_head(h, kw)
                if limit in ("head0", "h0a", "h0b", "h0c", "h0c1", "h0c2", "h0c3"):
                    break
            if limit in ("head0", "heads", "h0a", "h0b", "h0c", "h0c1", "h0c2", "h0c3"):
                break
            post_phase(0, b, a_w_out, last=stop_after_l0)
            if stop_after_l0:
                continue
            layer_vectors(1, b)
            kw = load_head_weights(l1_pieces(0))
            pre_phase(1, b)
            if limit == "l1pre":
                break
            for h in range(8):
                kw = l1_head(h, kw)
                if limit in ("l1a", "l1b", "l1c", "l1a0", "l1a1", "l1a2"):
                    break
            if limit in ("l1a", "l1b", "l1c", "l1a0", "l1a1", "l1a2"):
                break
            post_phase(1, b, b_w_out, last=True)
        if limit is not None:
            P.dma("sp", "dbg", lambda e: e.dma_start(out=out[0, 0:128, :], in_=xin[0][:]), reads=[("xin", 0)], writes=[("out", 0, 0)])
            P.op("sp", lambda e: e.nop(), reads=[("out", 0, 0)])
            for e_ in ("act", "dve", "pool", "pe"):
                pass
        else:
            P.op("sp", lambda e: e.nop(), reads=[("out", b, tt) for b in range(nseq) for tt in range(NT)])

        cnt = P.analyze()
        names = set(cnt.keys()) | set(HW)
        sems = {s: es.enter_context(nc.semaphore(s.replace(":", "_"))) for s in sorted(names)}
        with nc.Block() as block:
            P.emit(block, sems)
    return nc, len(P.ops), cnt


_CACHE = {}


def kernel(x, c, positions, mod_w, mod_b, pre_norm_g, post_norm_g, a_w_in, a_w_out, a_out_norm_g,
           a_lb_logits, kv_norm_g, w_kv, b_w_in, b_w_out):
    if "nc" not in _CACHE:
        _CACHE["nc"] = build()[0]
    nc = _CACHE["nc"]
    f = lambda a: np.ascontiguousarray(np.asarray(a), dtype=np.float32)
    shared = {
        "mod_w": f(mod_w), "mod_b": f(mod_b), "pre_g": f(pre_norm_g), "post_g": f(post_norm_g),
        "a_w_in": f(a_w_in)[0], "a_w_out": f(a_w_out)[0], "a_gn": f(a_out_norm_g).reshape(128, 1),
        "a_lb": f(a_lb_logits), "kv_g": f(kv_norm_g).reshape(1, D), "w_kv": f(w_kv),
        "b_w_in": f(b_w_in)[0], "b_w_out": f(b_w_out)[0], "cf": make_consts(),
    }
    x = f(x)
    c = f(c)
    positions = np.ascontiguousarray(np.asarray(positions), dtype=np.int32)
    in_maps = []
    for i in range(8):
        m = dict(shared)
        m["x"] = x[i * NB:(i + 1) * NB]
        m["c"] = c[i * NB:(i + 1) * NB]
        m["pos"] = positions[i * NB:(i + 1) * NB]
        in_maps.append(m)
    res = run_bass_kernel_spmd(nc, in_maps, core_ids=list(range(8)))
    return np.concatenate([r["out"] for r in res.results], axis=0)
```

```python
import numpy as np
from contextlib import ExitStack
import concourse.bass as bass
import concourse.mybir as mybir
from concourse.bass_utils import run_bass_kernel_spmd

F32 = mybir.dt.float32
BF16 = mybir.dt.bfloat16
I32 = mybir.dt.int32
ALU = mybir.AluOpType
AF = mybir.ActivationFunctionType
AX = mybir.AxisListType

NB = 4
T = 2048
D = 1024
NT = 16
EPS = 1e-6
BIG = 30000.0
HW = ("pe", "act", "dve", "pool", "sp")
PSUM_KEYS = ("pA", "pB", "pO", "pS", "pT0", "pM")

C_ID, C_CM, C_PM, C_SW, C_ON, C_ES, C_NEG, C_INV, C_SGN, NCF = 0, 128, 256, 384, 512, 640, 1664, 1728, 1729, 1730
NCB = 1664


class Op:
    __slots__ = ("hw", "fn", "reads", "writes", "sem", "inc", "signal", "value", "waits", "idx", "is_dma")


class Prog:
    def __init__(self):
        self.ops = []

    def op(self, hw, fn, reads=(), writes=()):
        o = Op()
        o.hw, o.fn, o.reads, o.writes = hw, fn, tuple(reads), tuple(writes)
        o.sem, o.inc, o.signal, o.value, o.waits, o.is_dma = hw, 1, False, 0, [], False
        o.idx = len(self.ops)
        self.ops.append(o)
        return o

    def dma(self, hw, slot, fn, reads=(), writes=()):
        o = self.op(hw, fn, reads, writes)
        o.sem, o.inc, o.signal, o.is_dma = "dma:" + slot, 16, True, True
        return o

    def analyze(self):
        last_w, readers = {}, {}
        for o in self.ops:
            deps = {}
            for k in o.reads:
                p = last_w.get(k)
                if p is not None:
                    deps[p.idx] = (p, "raw")
                kn = k[0] if isinstance(k, tuple) else k
                if kn in PSUM_KEYS:
                    for r in readers.get(k, {}).values():
                        if r.hw != o.hw and r.idx not in deps:
                            deps[r.idx] = (r, "rar")
            for k in o.writes:
                p = last_w.get(k)
                if p is not None and p.idx not in deps:
                    deps[p.idx] = (p, "waw")
                for r in readers.get(k, {}).values():
                    if r.idx not in deps and r is not o:
                        deps[r.idx] = (r, "war")
            for p, kind in deps.values():
                if (not p.is_dma) and (not o.is_dma) and p.hw == o.hw:
                    if o.hw == "pe" or kind != "raw":
                        continue
                o.waits.append(p)
                p.signal = True
            for k in o.reads:
                readers.setdefault(k, {})[("d", o.idx) if o.is_dma else o.hw] = o
            for k in o.writes:
                last_w[k] = o
                readers[k] = {}
        cnt = {}
        for o in self.ops:
            if o.signal:
                cnt[o.sem] = cnt.get(o.sem, 0) + o.inc
                o.value = cnt[o.sem]
        return cnt

    def emit(self, block, sems):
        streams = {h: [] for h in HW}
        for o in self.ops:
            streams[o.hw].append(o)

        def make(hwname):
            def body(eng):
                known = {}
                for o in streams[hwname]:
                    need = {}
                    for p in o.waits:
                        if p.value > need.get(p.sem, 0):
                            need[p.sem] = p.value
                    for s, v in need.items():
                        if known.get(s, 0) < v:
                            eng.wait_ge(sems[s], v)
                            known[s] = v
                    ins = o.fn(eng)
                    if o.signal:
                        ins.then_inc(sems[o.sem], o.inc)
            return body

        block.tensor(make("pe"))
        block.scalar(make("act"))
        block.vector(make("dve"))
        block.gpsimd(make("pool"))
        block.sync(make("sp"))


def make_consts():
    cf = np.zeros((128, NCF), np.float32)
    p = np.arange(128)
    cf[:, C_ID:C_ID + 128] = np.eye(128, dtype=np.float32)
    cf[:, C_CM:C_CM + 128] = np.where(p[:, None] > p[None, :], -BIG, 0.0)
    cf[:, C_PM:C_PM + 128] = ((p[:, None] // 64 == p[None, :] // 64) & (p[:, None] <= p[None, :])).astype(np.float32)
    cf[:, C_SW:C_SW + 128] = (p[:, None] == (p[None, :] + 64) % 128).astype(np.float32)
    cf[:, C_ON:C_ON + 128] = 1.0
    for n in range(8):
        cf[n, C_ES + n * 128:C_ES + (n + 1) * 128] = 1.0
    neg = np.zeros((8, 8), np.float32)
    for i in range(8):
        j = (8 + i) // 2
        neg[i, j:] = -1e30
    cf[:, C_NEG:C_NEG + 64] = neg.reshape(1, 64)
    inv = np.float32(10000.0) ** (-np.arange(0, 128, 2, dtype=np.float32) / np.float32(128))
    cf[:, C_INV] = np.concatenate([inv, inv]).astype(np.float32)
    cf[:, C_SGN] = np.where(p < 64, -1.0, 1.0)
    return cf


def build(stop_after_l0=False, nseq=NB, limit=None):
    nc = bass.Bass("TRN2", target_bir_lowering=False)

    def din(name, shape, dt=F32):
        return nc.dram_tensor(name, list(shape), dt, kind="ExternalInput").ap()

    x = din("x", [NB, T, D])
    cin = din("c", [NB, D])
    pos = din("pos", [NB, T], I32)
    mod_w = din("mod_w", [2, D, 3 * D])
    mod_b = din("mod_b", [2, 3 * D])
    pre_g = din("pre_g", [2, D])
    post_g = din("post_g", [2, D])
    a_w_in = din("a_w_in", [D, 4 * D])
    a_w_out = din("a_w_out", [D, D])
    a_gn = din("a_gn", [128, 1])
    a_lb = din("a_lb", [2, D])
    kv_g = din("kv_g", [1, D])
    w_kv = din("w_kv", [D, 2 * D])
    b_w_in = din("b_w_in", [D, 2 * D])
    b_w_out = din("b_w_out", [D, D])
    cf = din("cf", [128, NCF])
    out = nc.dram_tensor("out", [NB, T, D], F32, kind="ExternalOutput").ap()
    scr = nc.dram_tensor("scr", [2, 3, NB, D], F32, kind="Internal").ap()

    P = Prog()
    es = ExitStack()
    with es:
        def sb(name, shape, dt):
            return es.enter_context(nc.sbuf_tensor(name, list(shape), dt))

        def ps(name, shape, dt):
            return es.enter_context(nc.psum_tensor(name, list(shape), dt))

        cb = sb("cb", [128, NCB], BF16)
        cfs = sb("cfs", [128, NCF - NCB], F32)
        ident = cb[:, C_ID:C_ID + 128]
        cmask = cb[:, C_CM:C_CM + 128]
        pmask = cb[:, C_PM:C_PM + 128]
        swp = cb[:, C_SW:C_SW + 128]
        onesb = cb[:, C_ON:C_ON + 128]
        negm = cfs[:, 0:64]
        invf = cfs[:, 64:65]
        sgn = cfs[:, 65:66]

        xnT = sb("xnT", [128, 8, T], BF16)
        ogT = sb("ogT", [128, 8, T], BF16)
        h1 = sb("h1", [128, NT * D], F32)
        wbf = [sb("wbf%d" % i, [128, 8, 512], BF16) for i in range(2)]
        hb = [sb("hb%d" % i, [128, T], BF16) for i in range(6)]
        cosT = sb("cosT", [128, T], BF16)
        sinT = sb("sinT", [128, T], BF16)
        xin = [sb("xin%d" % i, [128, D], F32) for i in range(2)]
        xs = [sb("xs0", [128, D], BF16)] * 2
        G_bc = sb("G_bc", [128, D], F32)
        PT = [sb("PT%d" % i, [128, 512], BF16) for i in range(3)]
        rsb = sb("rsb", [128, 512], F32)
        otb = sb("otb", [128, 512], F32)
        biasT = sb("biasT", [128, 1024], BF16)
        gbuf = sb("gbuf", [128, 64], F32)
        cmpb = sb("cmpb", [128, 8, 8, 8], F32)
        rankb = sb("rankb", [128, 64], F32)
        biasq = sb("biasq", [128, 64], BF16)
        small = sb("small", [128, 256], F32)
        smallb = sb("smallb", [128, 160], BF16)
        ss = small[:, 0:16]
        rstd = small[:, 16:32]
        tmpc = small[:, 32:48]
        aT = small[:, 48:56]
        shT = small[:, 56:64]
        gkT = small[:, 64:72]
        lbT = small[:, 72:88]
        oml = small[:, 88:96]
        noml = small[:, 184:192]
        gn = small[:, 96:97]
        bcol = small[:, 100:104]
        nbf = small[:, 104:105]
        ssy = small[:, 108:112]
        Bl = small[:, 112:144]
        Bl0 = small[:, 144:176]
        km = small[:, 176:184]
        cT = small[:, 192:224]
        shTb = smallb[:, 0:8]
        kmb = smallb[:, 8:16]
        birow = smallb[0:1, 16:144]
        scT = sb("scT", [128, 32], BF16)
        bmat = sb("bmat", [128, 128], BF16)

        def h1f(off, n):
            return h1[:, off:off + n]

        def h1b(off, n):
            return h1[:, off:off + n // 2].bitcast(BF16)

        o_ = 0
        T_q = h1f(o_, 512); o_ += 512
        T_s = h1f(o_, 512); o_ += 512
        T_d0 = h1f(o_, 512); o_ += 512
        T_d1 = h1f(o_, 512); o_ += 512
        T_B = h1f(o_, 512); o_ += 512
        attS = [h1b(o_, 512), h1b(o_ + 256, 512)]; o_ += 512
        osb = h1f(o_, 512); o_ += 512
        sqb = h1b(o_, 512); o_ += 256
        rst = h1f(o_, 512); o_ += 512
        tt_ = h1f(o_, 512); o_ += 512
        Ub = h1f(o_, 4096); o_ += 4096
        Sbf = h1b(o_, 4096); o_ += 2048
        arep = h1f(o_, 1024); RA = h1f(o_, 1024); RAi = h1[:, o_:o_ + 1024].bitcast(I32); o_ += 1024
        Shalf = h1f(o_, 1024); RB = h1f(o_, 1024); RBi = h1[:, o_:o_ + 1024].bitcast(I32); o_ += 1024
        Shalf2 = h1f(o_, 1024); o_ += 1024
        T_s2 = h1f(o_, 512); o_ += 512
        T_q2 = h1f(o_, 512); o_ += 512
        T_B2 = h1f(o_, 512); o_ += 512
        assert o_ <= NT * D
        msb = h1f(0, 3072)
        modbb = h1f(3072, 3072)
        pgb = h1f(6144, 2048)
        rows = h1f(8192, 3072)

        pA = [ps("pA%d" % i, [128, 512], F32) for i in range(2)]
        pB = [ps("pB%d" % i, [128, 512], F32) for i in range(2)]
        pO = ps("pO", [128, 512], F32)
        pS = ps("pS", [128, 512], F32)
        pT0 = ps("pT0", [128, 1024], BF16)
        pM = ps("pM", [128, 512], F32)

        H1A = "h1all"

        def WK(k, g0=0, g1=4):
            return [("wbf", k, g) for g in range(g0, g1)]

        def top(hw, fn, reads=(), writes=()):
            return P.op(hw, fn, tuple(reads) + (H1A,), writes)

        def tdma(hw, slot, fn, reads=(), writes=()):
            return P.dma(hw, slot, fn, tuple(reads) + (H1A,), writes)

        def barrier():
            for e in ("act", "dve", "pool"):
                P.op(e, (lambda eng, e=e: (eng.memset(small[:, 250:251], 0.0) if e != "act" else
                                            eng.activation(out=small[:, 251:252], in_=small[:, 252:253], func=AF.Copy))),
                     writes=[("bar", e)])
            P.op("pe", lambda eng: eng.matmul(pM[0:1, 0:1], lhsT=onesb[:, 0:1], rhs=onesb[:, 0:1], start=True, stop=True),
                 reads=["cb"], writes=["pM", ("bar", "pe")])
            for e in ("act", "dve", "pool", "pe", "sp"):
                if e == "pe":
                    P.op("pe", lambda eng: eng.matmul(pM[0:1, 0:1], lhsT=onesb[:, 0:1], rhs=onesb[:, 0:1], start=True, stop=True),
                         reads=["cb"] + [("bar", q) for q in ("act", "dve", "pool")], writes=["pM"])
                elif e == "sp":
                    P.op("sp", lambda eng: eng.nop(), reads=[("bar", q) for q in ("act", "dve", "pool", "pe")])
                else:
                    P.op(e, (lambda eng, e=e: (eng.memset(small[:, 253:254], 0.0) if e == "dve" else
                                                eng.memset(small[:, 254:255], 0.0) if e == "pool" else
                                                eng.activation(out=small[:, 255:256], in_=small[:, 252:253], func=AF.Copy))),
                         reads=[("bar", q) for q in ("act", "dve", "pool", "pe") if q != e])

        P.dma("pool", "cb", lambda e: e.dma_start(out=cb[:], in_=cf[:, 0:NCB]), writes=["cb"])
        P.dma("sp", "cfs", lambda e: e.dma_start(out=cfs[:], in_=cf[:, NCB:NCF]), writes=["cfs"])
        P.op("dve", lambda e: e.memset(small[:], 0.0), writes=["small"])
        P.op("dve", lambda e: e.memset(bmat[:], 0.0), writes=["birow"])
        P.op("dve", lambda e: e.memset(biasT[:], 0.0), writes=["biasT"])
        P.op("pool", lambda e: e.memset(hb[3][:], 0.0), writes=[("kt", 0), ("kt", 1)])
        P.op("pool", lambda e: e.memset(hb[5][:], 0.0), writes=[("kt", 0), ("kt", 1)])
        with nc.allow_non_contiguous_dma(reason="tiny one-time transposed loads"):
            pass
        for bb in range(NB):
            P.dma("sp", "s0", lambda e, bb=bb: e.dma_start(out=cT.rearrange("p (c b) -> p c b", b=NB)[:, :, bb], in_=cin[bb].rearrange("(c p) -> p c", p=128),
                                                         allow_slow_non_contiguous=True), reads=["small"], writes=[("cT", bb)])
        P.dma("sp", "s1", lambda e: e.dma_start(out=gkT, in_=kv_g[0].rearrange("(c p) -> p c", p=128), allow_slow_non_contiguous=True),
              reads=["small"], writes=["gkT"])
        for l in range(2):
            P.dma("sp", "s2", lambda e, l=l: e.dma_start(out=lbT[:, l * 8:(l + 1) * 8], in_=a_lb[l].rearrange("(c p) -> p c", p=128),
                                                       allow_slow_non_contiguous=True), reads=["small"], writes=[("lbT", l)])
        P.dma("sp", "s3", lambda e: e.dma_start(out=gn, in_=a_gn), reads=["small"], writes=["gn"])
        P.op("act", lambda e: e.activation(out=scT[:], in_=cT, func=AF.Silu), reads=[("cT", q) for q in range(NB)], writes=["scT"])
        P.op("dve", lambda e: e.tensor_tensor(out=oml, in0=lbT[:, 8:16], in1=lbT[:, 0:8], op=ALU.subtract), reads=[("lbT", 0), ("lbT", 1)], writes=["oml"])
        P.op("act", lambda e: e.activation(out=oml, in_=oml, func=AF.Sigmoid), reads=["oml"], writes=["oml"])
        P.op("dve", lambda e: e.tensor_scalar(out=noml, in0=oml, scalar1=-1.0, scalar2=None, op0=ALU.mult), reads=["oml", "small"], writes=["noml"])
        wi = 0
        for l in range(2):
            tdma("sp", "s4", lambda e, l=l: e.dma_start(out=modbb[0:NB, :], in_=mod_b[l].partition_broadcast(NB)), writes=["modbb"])
            tdma("sp", "s5", lambda e, l=l: e.dma_start(out=pgb[0:NB, 0:1024], in_=pre_g[l].partition_broadcast(NB)), writes=["pgb0"])
            tdma("sp", "s6", lambda e, l=l: e.dma_start(out=pgb[0:NB, 1024:2048], in_=post_g[l].partition_broadcast(NB)), writes=["pgb1"])
            for cbk in range(6):
                k = wi % 2
                wi += 1
                P.dma("pool", "wbf%d" % k, lambda e, l=l, cbk=cbk, k=k: e.dma_start(
                    out=wbf[k][:], in_=mod_w[l][:, cbk * 512:(cbk + 1) * 512].rearrange("(c p) n -> p c n", p=128)),
                    writes=WK(k))
                for c in range(8):
                    P.op("pe", lambda e, c=c, k=k: e.matmul(pM[0:NB, :], lhsT=scT[:, c * NB:(c + 1) * NB], rhs=wbf[k][:, c, :],
                                                            start=(c == 0), stop=(c == 7)),
                         reads=["scT"] + WK(k), writes=["pM"])
                top("dve", lambda e, cbk=cbk: e.tensor_tensor(
                    out=msb[0:NB, cbk * 512:(cbk + 1) * 512], in0=pM[0:NB, :],
                    in1=modbb[0:NB, cbk * 512:(cbk + 1) * 512], op=ALU.add),
                    reads=["pM", "modbb"], writes=[("msb", cbk)])
            mk = [("msb", i) for i in range(6)]
            top("dve", lambda e: e.scalar_tensor_tensor(out=rows[0:NB, 0:1024], in0=msb[0:NB, 1024:2048],
                                                        scalar=1.0, in1=pgb[0:NB, 0:1024], op0=ALU.add, op1=ALU.mult),
                reads=mk + ["pgb0"], writes=["rows"])
            top("dve", lambda e: e.tensor_copy(out=rows[0:NB, 1024:2048], in_=msb[0:NB, 0:1024]), reads=mk, writes=["rows"])
            top("dve", lambda e: e.tensor_tensor(out=rows[0:NB, 2048:3072], in0=msb[0:NB, 2048:3072], in1=pgb[0:NB, 1024:2048], op=ALU.mult),
                reads=mk + ["pgb1"], writes=["rows"])
            for kind in range(3):
                tdma("sp", "s7", lambda e, l=l, kind=kind: e.dma_start(out=scr[l, kind], in_=rows[0:NB, kind * 1024:(kind + 1) * 1024]),
                     reads=["rows"], writes=[("scr", l, kind)])
        barrier()

        xin_i = [0]

        def layer_vectors(l, b):
            P.dma("sp", "v0", lambda e: e.dma_start(out=aT, in_=scr[l, 0, b].rearrange("(c p) -> p c", p=128), allow_slow_non_contiguous=True),
                  reads=[("scr", l, 0)], writes=["aT"])
            P.dma("sp", "v1", lambda e: e.dma_start(out=shT, in_=scr[l, 1, b].rearrange("(c p) -> p c", p=128), allow_slow_non_contiguous=True),
                  reads=[("scr", l, 1)], writes=["shT"])
            P.dma("sp", "v2", lambda e: e.dma_start(out=G_bc[:], in_=scr[l, 2, b].partition_broadcast(128)), reads=[("scr", l, 2)], writes=["G_bc"])
            P.op("dve", lambda e: e.tensor_copy(out=shTb, in_=shT), reads=["shT"], writes=["shTb"])

        def rstd_from(col_in, col_out, keyin, keyout):
            P.op("dve", lambda e: e.tensor_scalar(out=col_out, in0=col_in, scalar1=1.0 / D, scalar2=EPS, op0=ALU.mult, op1=ALU.add),
                 reads=[keyin], writes=[keyout])
            P.op("act", lambda e: e.activation(out=col_out, in_=col_out, func=AF.Sqrt), reads=[keyout], writes=[keyout])
            P.op("dve", lambda e: e.reciprocal(out=col_out, in_=col_out), reads=[keyout], writes=[keyout])

        def pre_phase(l, b):
            pMb = pM[:].bitcast(BF16)
            jk = [("ogT", 7, 0), ("ogT", 7, 1)]
            for tt in range(NT):
                k = tt % 2
                ptb, pkey = (pT0[:], "pT0") if tt % 2 == 0 else (pMb, "pM")
                if l == 0:
                    xi = xin_i[0] % 2
                    xin_i[0] += 1
                    P.dma("sp", "xin%d" % xi, lambda e, tt=tt, xi=xi: e.dma_start(out=xin[xi][:], in_=x[b, tt * 128:(tt + 1) * 128, :]),
                          writes=[("xin", xi)])
                    src, skey = xin[xi][:], ("xin", xi)
                else:
                    src, skey = h1[:, tt * D:(tt + 1) * D], ("h1", tt)
                P.op("dve", lambda e, tt=tt: e.memset(ss[:, tt:tt + 1], 0.0), writes=[("ss", tt)])
                P.op("act", lambda e, src=src, tt=tt: e.activation(out=ogT[:, 7, 0:D], in_=src, func=AF.Square, accum_out=ss[:, tt:tt + 1]),
                     reads=[skey, ("ss", tt)], writes=jk + [("ss", tt)])
                rstd_from(ss[:, tt:tt + 1], rstd[:, tt:tt + 1], ("ss", tt), ("rstd", tt))
                P.op("dve", lambda e, src=src, tt=tt, k=k: e.tensor_scalar(out=xs[k][:], in0=src, scalar1=rstd[:, tt:tt + 1], scalar2=None, op0=ALU.mult),
                     reads=[skey, ("rstd", tt)], writes=[("xs", 0)])
                for c in range(8):
                    P.op("pe", lambda e, c=c, k=k, ptb=ptb: e.transpose(out=ptb[:, c * 128:(c + 1) * 128], in_=xs[k][:, c * 128:(c + 1) * 128], identity=ident),
                         reads=[("xs", 0), "cb"], writes=[pkey])
                P.op("act", lambda e, tt=tt, ptb=ptb: e.activation(out=xnT[:, :, tt * 128:(tt + 1) * 128], in_=ptb.rearrange("p (c t) -> p c t", c=8), func=AF.Copy),
                     reads=[pkey], writes=[("xnT", tt)])

        wslot = [0]

        def load_head_weights(pieces):
            k = wslot[0] % 2
            wslot[0] += 1
            for g, ap in enumerate(pieces):
                P.dma("pool", "wbf%d" % k, lambda e, g=g, ap=ap, k=k: e.dma_start(
                    out=wbf[k][:, :, g * 128:(g + 1) * 128], in_=ap.rearrange("(c p) n -> p c n", p=128)),
                    writes=[("wbf", k, g)])
            return k

        def bias_cols(k, groups):
            for j, g in enumerate(groups):
                for c in range(8):
                    P.op("pe", lambda e, j=j, g=g, c=c: e.matmul(pM[:, j:j + 1], lhsT=wbf[k][:, c, g * 128:(g + 1) * 128], rhs=shTb[:, c:c + 1],
                                                               start=(c == 0), stop=(c == 7)),
                         reads=[("wbf", k, g), "shTb"], writes=["pM"])
            P.op("dve", lambda e: e.tensor_copy(out=bcol[:, 0:len(groups)], in_=pM[:, 0:len(groups)]), reads=["pM"], writes=["bcol"])

        def scale_w(k, c0, c1, vec, vkey):
            P.op("pool", lambda e: e.tensor_tensor(out=wbf[k][:, :, c0:c1], in0=wbf[k][:, :, c0:c1],
                                                   in1=vec.unsqueeze(2).to_broadcast([128, 8, c1 - c0]), op=ALU.mult),
                 reads=WK(k, c0 // 128, c1 // 128) + [vkey], writes=WK(k, c0 // 128, c1 // 128))

        pa_i = [0]

        def proj_fm(k, g, tb):
            i = pa_i[0] % 2
            pa_i[0] += 1
            for c in range(8):
                P.op("pe", lambda e, c=c, i=i: e.matmul(pA[i][:], lhsT=wbf[k][:, c, g * 128:(g + 1) * 128], rhs=xnT[:, c, tb * 512:(tb + 1) * 512],
                                                      start=(c == 0), stop=(c == 7)),
                     reads=[("wbf", k, g)] + [("xnT", 4 * tb + q) for q in range(4)], writes=[("pA", i)])
            return i

        def proj_tm(k, g, dst, dkey, bias_row=False):
            for t4 in range(4):
                i = t4 % 2
                for j in range(4):
                    tt = t4 * 4 + j
                    for c in range(8):
                        P.op("pe", lambda e, c=c, tt=tt, j=j, i=i: e.matmul(pB[i][:, j * 128:(j + 1) * 128], lhsT=xnT[:, c, tt * 128:(tt + 1) * 128],
                                                                          rhs=wbf[k][:, c, g * 128:(g + 1) * 128], start=(c == 0),
                                                                          stop=(c == 7 and not bias_row)),
                             reads=[("wbf", k, g), ("xnT", tt)], writes=[("pB", i)])
                    if bias_row:
                        P.op("pe", lambda e, j=j, i=i: e.matmul(pB[i][:, j * 128:(j + 1) * 128], lhsT=onesb, rhs=bmat[:], start=False, stop=True),
                             reads=["cb", "birow"], writes=[("pB", i)])
                P.op("act", lambda e, t4=t4, i=i: e.activation(out=dst[:, t4 * 512:(t4 + 1) * 512], in_=pB[i][:], func=AF.Copy),
                     reads=[("pB", i)], writes=[(dkey, t4)])

        def post_phase(l, b, wout, last):
            for cbk in range(2):
                P.dma("pool", "wbf%d" % cbk, lambda e, cbk=cbk: e.dma_start(
                    out=wbf[cbk][:], in_=wout[:, cbk * 512:(cbk + 1) * 512].rearrange("(c p) n -> p c n", p=128)), writes=WK(cbk))
            wslot[0] = 0
            for tt in range(NT):
                pp, pk = (pA, "pA") if tt % 2 == 0 else (pB, "pB")
                for cbk in range(2):
                    for c in range(8):
                        P.op("pe", lambda e, c=c, cbk=cbk, tt=tt, pp=pp: e.matmul(pp[cbk][:], lhsT=ogT[:, c, tt * 128:(tt + 1) * 128], rhs=wbf[cbk][:, c, :],
                                                                         start=(c == 0), stop=(c == 7)),
                             reads=WK(cbk) + [("ogT", hh, tt // 4) for hh in range(8)], writes=[(pk, cbk)])
                    P.op("dve", lambda e, cbk=cbk: e.memset(ssy[:, cbk:cbk + 1], 0.0), writes=[("ssy", cbk)])
                    P.op("act", lambda e, cbk=cbk, pp=pp: e.activation(out=PT[cbk][:], in_=pp[cbk][:], func=AF.Square, accum_out=ssy[:, cbk:cbk + 1]),
                         reads=[(pk, cbk), ("ssy", cbk)], writes=[("PT", cbk), ("ssy", cbk)])
                P.op("dve", lambda e: e.tensor_tensor(out=ssy[:, 2:3], in0=ssy[:, 0:1], in1=ssy[:, 1:2], op=ALU.add),
                     reads=[("ssy", 0), ("ssy", 1)], writes=[("ssy", 2)])
                rstd_from(ssy[:, 2:3], ssy[:, 3:4], ("ssy", 2), ("ssy", 3))
                xi = xin_i[0] % 2
                xin_i[0] += 1
                if l == 0:
                    P.dma("sp", "xin%d" % xi, lambda e, tt=tt, xi=xi: e.dma_start(out=xin[xi][:], in_=x[b, tt * 128:(tt + 1) * 128, :]),
                          writes=[("xin", xi)])
                    dest, dkey, res, rkey = h1[:, tt * D:(tt + 1) * D], ("h1", tt), xin[xi][:], ("xin", xi)
                    extra_w = [H1A]
                else:
                    dest, dkey, res, rkey = xin[xi][:], ("xin", xi), h1[:, tt * D:(tt + 1) * D], ("h1", tt)
                    extra_w = [H1A]
                for cbk in range(2):
                    P.op("dve", lambda e, cbk=cbk, dest=dest, pp=pp: e.scalar_tensor_tensor(
                        out=dest[:, cbk * 512:(cbk + 1) * 512], in0=pp[cbk][:], scalar=ssy[:, 3:4], in1=G_bc[:, cbk * 512:(cbk + 1) * 512],
                        op0=ALU.mult, op1=ALU.mult), reads=[(pk, cbk), ("ssy", 3), "G_bc"], writes=[dkey] + (extra_w if l == 0 else []))
                P.op("dve", lambda e, dest=dest, res=res: e.tensor_tensor(out=dest, in0=dest, in1=res, op=ALU.add),
                     reads=[dkey, rkey], writes=[dkey] + extra_w)
                if last:
                    P.dma("sp", "xin%d" % xi, lambda e, tt=tt, dest=dest: e.dma_start(out=out[b, tt * 128:(tt + 1) * 128, :], in_=dest),
                          reads=[dkey], writes=[("out", b, tt)])

        def rope_tables(b):
            for hf in range(2):
                cs = slice(hf * 1024, (hf + 1) * 1024)
                tdma("sp", "ra", lambda e, cs=cs: e.dma_start(out=RAi, in_=pos[b, cs].partition_broadcast(128)), writes=["arep"])
                top("dve", lambda e: e.tensor_copy(out=RA, in_=RAi), reads=["arep"], writes=["arep"])
                top("dve", lambda e: e.tensor_scalar(out=RA, in0=RA, scalar1=invf, scalar2=None, op0=ALU.mult), reads=["arep", "cfs"], writes=["arep"])
                for which in range(2):
                    off = 0.0 if which == 0 else float(np.pi / 2)
                    top("dve", lambda e, off=off: e.tensor_scalar(out=RB, in0=RA, scalar1=off, scalar2=float(1.0 / (2 * np.pi)), op0=ALU.add, op1=ALU.mult),
                        reads=["arep"], writes=["Shalf"])
                    top("dve", lambda e: e.tensor_copy(out=RBi, in_=RB), reads=["Shalf"], writes=["Shalf"])
                    top("dve", lambda e: e.tensor_copy(out=RB, in_=RBi), reads=["Shalf"], writes=["Shalf"])
                    top("dve", lambda e: e.tensor_scalar(out=RB, in0=RB, scalar1=float(-2 * np.pi), scalar2=None, op0=ALU.mult), reads=["Shalf"], writes=["Shalf"])
                    top("dve", lambda e, off=off: e.scalar_tensor_tensor(out=RB, in0=RA, scalar=off, in1=RB, op0=ALU.add, op1=ALU.add),
                        reads=["arep", "Shalf"], writes=["Shalf"])
                    top("dve", lambda e: e.tensor_scalar(out=RB, in0=RB, scalar1=3.14159, scalar2=-3.14159, op0=ALU.min, op1=ALU.max),
                        reads=["Shalf"], writes=["Shalf"])
                    if which == 0:
                        top("act", lambda e: e.activation(out=RB, in_=RB, func=AF.Sin), reads=["Shalf"], writes=["Shalf"])
                        top("dve", lambda e, cs=cs: e.tensor_scalar(out=sinT[:, cs], in0=RB, scalar1=sgn, scalar2=None, op0=ALU.mult),
                            reads=["Shalf", "cfs"], writes=["sinT"])
                    else:
                        top("act", lambda e, cs=cs: e.activation(out=cosT[:, cs], in_=RB, func=AF.Sin), reads=["Shalf"], writes=["cosT"])

        def l0_prep(k):
            bias_cols(k, [0, 1, 3])
            P.op("dve", lambda e: e.tensor_scalar(out=nbf, in0=bcol[:, 1:2], scalar1=-1.0, scalar2=None, op0=ALU.mult), reads=["bcol"], writes=["nbf"])
            for c in range(8):
                P.op("pe", lambda e, c=c: e.matmul(pM[0:1, 128:256], lhsT=shTb[:, c:c + 1], rhs=wbf[k][:, c, 256:384], start=(c == 0), stop=(c == 7)),
                     reads=[("wbf", k, 2), "shTb"], writes=["pM"])
            P.op("dve", lambda e: e.tensor_copy(out=bmat[0:1, :], in_=pM[0:1, 128:256]), reads=["pM"], writes=["birow"])
            scale_w(k, 0, 512, aT, "aT")

        def l0_head(h, k):
            qtT, ktT, sgT, kt, vv, ktB = hb[0], hb[1], hb[2], hb[3], hb[4], hb[5]
            knext = None
            if h < 7:
                knext = load_head_weights([a_w_in[:, g * 1024 + (h + 1) * 128:g * 1024 + (h + 2) * 128] for g in range(4)])
            for tb in range(4):
                bs = slice(tb * 512, (tb + 1) * 512)
                Tq, TB, Ts = (T_q, T_B, T_s) if tb % 2 == 0 else (T_q2, T_B2, T_s2)
                kq, kB, ks = ("T_q", "T_B", "T_s") if tb % 2 == 0 else ("T_q2", "T_B2", "T_s2")
                i = proj_fm(k, 0, tb)
                top("act", lambda e, i=i, Tq=Tq: e.activation(out=Tq, in_=pA[i][:], func=AF.Silu, bias=bcol[:, 0:1]), reads=[("pA", i), "bcol"], writes=[kq])
                i = proj_fm(k, 3, tb)
                P.op("act", lambda e, i=i, bs=bs: e.activation(out=sgT[:, bs], in_=pA[i][:], func=AF.Silu, bias=bcol[:, 2:3]),
                     reads=[("pA", i), "bcol"], writes=[("sgT", tb)])
                i = proj_fm(k, 1, tb)
                top("act", lambda e, i=i, Ts=Ts: e.activation(out=Ts, in_=pA[i][:], func=AF.Sigmoid, bias=nbf, scale=-1.0), reads=[("pA", i), "nbf"], writes=[ks])
                top("dve", lambda e, Ts=Ts: e.tensor_scalar(out=T_d0, in0=Ts, scalar1=noml[:, h:h + 1], scalar2=1.0, op0=ALU.mult, op1=ALU.add),
                    reads=[ks, "noml"], writes=["T_d0"])
                if tb == 0 and h == 0:
                    top("dve", lambda e: e.memset(T_d1, 0.0), writes=["T_d1"])
                d0v = T_d0.rearrange("p (n c) -> p n c", c=64)[:, :, 0:1]
                d1v = T_d1.rearrange("p (n c) -> p n c", c=64)[:, :, 0:1]
                top("dve", lambda e, d0v=d0v, d1v=d1v: e.tensor_copy(out=d1v, in_=d0v), reads=["T_d0", "T_d1"], writes=["T_d1"])
                top("dve", lambda e, d0v=d0v: e.memset(d0v, 0.0), reads=["T_d0", "T_d1"], writes=["T_d0"])
                top("dve", lambda e, TB=TB: e.tensor_tensor_scan(out=TB, data0=T_d0, data1=T_d1, initial=0.0, op0=ALU.mult, op1=ALU.add),
                    reads=["T_d0", "T_d1"], writes=[kB])
                top("dve", lambda e, tb=tb, TB=TB: e.tensor_copy(out=Bl[:, tb * 8:(tb + 1) * 8].unsqueeze(2),
                                                                 in_=TB.rearrange("p (n c) -> p n c", c=64)[:, :, 63:64]), reads=[kB], writes=["Bl"])
                top("pool", lambda e, bs=bs, Tq=Tq, TB=TB: e.tensor_tensor(out=qtT[:, bs], in0=Tq, in1=TB, op=ALU.mult), reads=[kq, kB], writes=[("qtT", tb)])
                top("dve", lambda e, TB=TB: e.reciprocal(out=T_d0, in_=TB), reads=[kB], writes=["T_d0"])
                top("dve", lambda e, bs=bs, Ts=Ts: e.scalar_tensor_tensor(out=ktT[:, bs], in0=Ts, scalar=oml[:, h:h + 1], in1=T_d0, op0=ALU.mult, op1=ALU.mult),
                    reads=[ks, "T_d0", "oml"], writes=[("ktT", tb)])
            top("dve", lambda e: e.tensor_copy(out=Bl0, in_=Bl), reads=["Bl"], writes=["Bl0"])
            top("dve", lambda e: e.memset(Bl0[:, 0:1], 0.0), reads=["Bl0"], writes=["Bl0"])
            top("pool", lambda e: e.tensor_copy(out=arep.rearrange("p (v n) -> p v n", n=32), in_=Bl0.unsqueeze(1).to_broadcast([128, 32, 32])),
                reads=["Bl0"], writes=["arep"])
            if limit == "h0a":
                return knext
            proj_tm(k, 2, vv, "vv", bias_row=True)
            for t8 in range(2):
                for j in range(8):
                    tt = t8 * 8 + j
                    P.op("pe", lambda e, tt=tt, j=j: e.transpose(out=pT0[:, j * 128:(j + 1) * 128], in_=ktT[:, tt * 128:(tt + 1) * 128], identity=ident),
                         reads=[("ktT", tt // 4), "cb"], writes=["pT0"])
                P.op("act", lambda e, t8=t8: e.activation(out=kt[0:64, t8 * 1024:(t8 + 1) * 1024], in_=pT0[0:64, :], func=AF.Copy), reads=["pT0"], writes=[("kt", t8)])
                P.op("act", lambda e, t8=t8: e.activation(out=ktB[64:128, t8 * 1024:(t8 + 1) * 1024], in_=pT0[64:128, :], func=AF.Copy), reads=["pT0"], writes=[("kt", t8)])
            if knext is not None:
                l0_prep(knext)
            if limit == "h0b":
                return knext
            Uv = Ub.rearrange("p (v n) -> p n v", n=32)
            for ng in range(8):
                i = ng % 2
                for j in range(4):
                    n = ng * 4 + j
                    tt, half = n // 2, n % 2
                    ksrc = kt if half == 0 else ktB
                    P.op("pe", lambda e, j=j, tt=tt, ksrc=ksrc, i=i: e.matmul(pB[i][:, j * 128:(j + 1) * 128], lhsT=ksrc[:, tt * 128:(tt + 1) * 128],
                                                                            rhs=vv[:, tt * 128:(tt + 1) * 128], start=True, stop=True),
                         reads=[("kt", tt // 8), ("vv", tt // 4)], writes=[("pB", i)])
                top("dve", lambda e, ng=ng, i=i: e.tensor_tensor(out=Uv[:, ng * 4:(ng + 1) * 4, :], in0=pB[i][:].rearrange("p (n v) -> p n v", n=4),
                                                                 in1=Bl[:, ng * 4:(ng + 1) * 4].unsqueeze(2).to_broadcast([128, 4, 128]), op=ALU.mult),
                    reads=[("pB", i), "Bl"], writes=["Ub"])
            if limit == "h0c1":
                return knext
            for vq in range(4):
                Sq, ksq = (Shalf, "Shalf") if vq % 2 == 0 else (Shalf2, "Shalf2")
                top("dve", lambda e, vq=vq, Sq=Sq: e.tensor_tensor_scan(out=Sq, data0=arep, data1=Ub[:, vq * 1024:(vq + 1) * 1024], initial=0.0,
                                                                        op0=ALU.mult, op1=ALU.add),
                    reads=["arep", "Ub"], writes=[ksq])
                dstv = Sbf.rearrange("p (n v) -> p v n", n=32)[:, vq * 32:(vq + 1) * 32, :]
                if vq % 2 == 0:
                    top("act", lambda e, dstv=dstv, Sq=Sq: e.activation(out=dstv, in_=Sq.rearrange("p (v n) -> p v n", n=32), func=AF.Copy),
                        reads=[ksq], writes=[("Sbf", vq)])
                else:
                    top("pool", lambda e, dstv=dstv, Sq=Sq: e.tensor_copy(out=dstv, in_=Sq.rearrange("p (v n) -> p v n", n=32)),
                        reads=[ksq], writes=[("Sbf", vq)])
            def att_stage(pg):
                i = pg % 2
                for j in range(4):
                    pr = pg * 4 + j
                    P.op("pe", lambda e, j=j, pr=pr, i=i: e.matmul(pB[i][:, j * 128:(j + 1) * 128], lhsT=ktT[:, pr * 128:(pr + 1) * 128],
                                                                 rhs=qtT[:, pr * 128:(pr + 1) * 128], start=True, stop=True),
                         reads=[("ktT", pg), ("qtT", pg)], writes=[("pB", i)])
                top("dve", lambda e, i=i: e.tensor_tensor(out=attS[i].rearrange("p (n t) -> p n t", n=4), in0=pB[i][:].rearrange("p (n t) -> p n t", n=4),
                                                          in1=pmask.unsqueeze(1).to_broadcast([128, 4, 128]), op=ALU.mult),
                    reads=[("pB", i), "cb"], writes=[("attS", i)])

            att_stage(0)
            pT0f0 = pT0[:].bitcast(F32)
            for pg in range(4):
                i = pg % 2
                if pg % 2 == 0:
                    aO, aS, kO, kS = pO[:], pS[:], "pO", "pS"
                else:
                    aO, aS, kO, kS = pT0f0, pM[:], "pT0", "pM"
                if pg < 3:
                    att_stage(pg + 1)
                sbk = [("Sbf", q) for q in range(4)]
                for j in range(4):
                    pr = pg * 4 + j
                    P.op("pe", lambda e, j=j, pr=pr, i=i, aO=aO: e.matmul(aO[:, j * 128:(j + 1) * 128], lhsT=vv[:, pr * 128:(pr + 1) * 128],
                                                                        rhs=attS[i][:, j * 128:(j + 1) * 128], start=True, stop=False, skip_group_check=True),
                         reads=[("vv", pg), ("attS", i), H1A], writes=[kO])
                    if pr > 0:
                        P.op("pe", lambda e, j=j, pr=pr, aO=aO: e.matmul(aO[:, j * 128:j * 128 + 64], lhsT=Sbf[:, (2 * pr - 1) * 128:(2 * pr) * 128],
                                                                       rhs=qtT[:, pr * 128:pr * 128 + 64], start=False, stop=False, skip_group_check=True),
                             reads=sbk + [("qtT", pg), H1A], writes=[kO])
                    P.op("pe", lambda e, j=j, pr=pr, aO=aO: e.matmul(aO[:, j * 128 + 64:(j + 1) * 128], lhsT=Sbf[:, (2 * pr) * 128:(2 * pr + 1) * 128],
                                                                   rhs=qtT[:, pr * 128 + 64:(pr + 1) * 128], start=False, stop=True, skip_group_check=True),
                         reads=sbk + [("qtT", pg), H1A], writes=[kO])
                top("act", lambda e, aO=aO: e.activation(out=sqb, in_=aO, func=AF.Square), reads=[kO], writes=["sqb"])
                P.op("pe", lambda e, aS=aS: e.matmul(aS, lhsT=onesb, rhs=sqb, start=True, stop=True), reads=["sqb", "cb", H1A], writes=[kS])
                top("dve", lambda e, aS=aS: e.tensor_scalar(out=rst, in0=aS, scalar1=1.0 / 128, scalar2=EPS, op0=ALU.mult, op1=ALU.add), reads=[kS], writes=["rst"])
                top("act", lambda e: e.activation(out=rst, in_=rst, func=AF.Sqrt), reads=["rst"], writes=["rst"])
                top("dve", lambda e: e.reciprocal(out=rst, in_=rst), reads=["rst"], writes=["rst"])
                tbuf, tkey = (tt_, "tt_") if pg % 2 == 0 else (osb, "osb")
                top("dve", lambda e, aO=aO, tbuf=tbuf: e.scalar_tensor_tensor(out=tbuf, in0=aO, scalar=gn, in1=rst, op0=ALU.mult, op1=ALU.mult),
                    reads=[kO, "rst", "gn"], writes=[tkey])
                top("pool", lambda e, pg=pg, tbuf=tbuf: e.tensor_tensor(out=ogT[:, h, pg * 512:(pg + 1) * 512], in0=tbuf, in1=sgT[:, pg * 512:(pg + 1) * 512], op=ALU.mult),
                    reads=[tkey, ("sgT", pg)], writes=[("ogT", h, pg)])
            return knext

        pt_i = [0]

        def l1_pieces(h):
            return [b_w_in[:, h * 128:(h + 1) * 128], b_w_in[:, 1024 + h * 128:1024 + (h + 1) * 128],
                    w_kv[:, h * 128:(h + 1) * 128], w_kv[:, 1024 + h * 128:1024 + (h + 1) * 128]]

        def l1_prep(k):
            bias_cols(k, [0, 1])
            scale_w(k, 0, 256, aT, "aT")
            scale_w(k, 256, 512, gkT, "gkT")

        def l1_head(h, k):
            QT, KT, szT, V = hb[0], hb[1], hb[2], hb[4]
            knext = load_head_weights(l1_pieces(h + 1)) if h < 7 else None
            if limit == "l1a0":
                return knext
            for tb in range(4):
                bs = slice(tb * 512, (tb + 1) * 512)
                for which in range(2):
                    if limit == "l1a1" and (tb, which) == (0, 1):
                        return knext
                    if limit == "l1a2" and (tb, which) == (1, 0):
                        return knext
                    dst = QT if which == 0 else KT
                    dkey = "QT" if which == 0 else "KT"
                    i = proj_fm(k, 0 if which == 0 else 2, tb)
                    j = (tb * 2 + which) % 2
                    if which == 0:
                        P.op("dve", lambda e, i=i: e.tensor_scalar(out=PT[0][:], in0=pA[i][:], scalar1=bcol[:, 0:1], scalar2=None, op0=ALU.add),
                            reads=[("pA", i), "bcol"], writes=[("PT", 0)])
                        P.op("dve", lambda e, i=i, bs=bs: e.scalar_tensor_tensor(out=rsb[:], in0=pA[i][:], scalar=bcol[:, 0:1], in1=cosT[:, bs], op0=ALU.add, op1=ALU.mult),
                            reads=[("pA", i), "bcol", "cosT"], writes=["rsb"])
                    else:
                        P.op("act", lambda e, i=i: e.activation(out=PT[0][:], in_=pA[i][:], func=AF.Copy), reads=[("pA", i)], writes=[("PT", 0)])
                        P.op("dve", lambda e, i=i, bs=bs: e.tensor_tensor(out=rsb[:], in0=pA[i][:], in1=cosT[:, bs], op=ALU.mult),
                            reads=[("pA", i), "cosT"], writes=["rsb"])
                    P.op("pe", lambda e, j=j: e.matmul(pB[j][:], lhsT=swp, rhs=PT[0][:], start=True, stop=True), reads=[("PT", 0), "cb"], writes=[("pB", j)])
                    P.op("dve", lambda e, j=j, bs=bs: e.tensor_tensor(out=otb[:], in0=pB[j][:], in1=sinT[:, bs], op=ALU.mult), reads=[("pB", j), "sinT"], writes=["otb"])
                    P.op("dve", lambda e, dst=dst, bs=bs: e.tensor_tensor(out=dst[:, bs], in0=rsb[:], in1=otb[:], op=ALU.add), reads=["rsb", "otb"], writes=[(dkey, tb)])
                i = proj_fm(k, 1, tb)
                P.op("act", lambda e, i=i, bs=bs: e.activation(out=szT[:, bs], in_=pA[i][:], func=AF.Silu, bias=bcol[:, 1:2]),
                     reads=[("pA", i), "bcol"], writes=[("szT", tb)])
            if limit == "l1a":
                return knext
            proj_tm(k, 3, V, "V")
            if knext is not None:
                l1_prep(knext)
            P.op("dve", lambda e: e.tensor_reduce(out=km, in_=KT[:].rearrange("p (n k) -> p n k", k=256), axis=AX.X, op=ALU.add),
                 reads=[("KT", q) for q in range(4)], writes=["km"])
            P.op("dve", lambda e: e.tensor_scalar(out=kmb, in0=km, scalar1=1.0 / 256, scalar2=None, op0=ALU.mult), reads=["km"], writes=["kmb"])
            for i8 in range(8):
                P.op("pe", lambda e, i8=i8: e.matmul(pM[:, i8 * 8:(i8 + 1) * 8], lhsT=QT[:, (8 + i8) * 128:(9 + i8) * 128], rhs=kmb, start=True, stop=True),
                     reads=[("QT", (8 + i8) // 4), "kmb"], writes=["pM"])
            P.op("dve", lambda e: e.tensor_tensor(out=gbuf[:], in0=pM[:, 0:64], in1=negm, op=ALU.add), reads=["pM", "cfs"], writes=["gbuf"])
            g3 = gbuf[:].rearrange("p (i n) -> p i n", n=8)
            P.op("dve", lambda e: e.tensor_tensor(out=cmpb[:], in0=g3.unsqueeze(2).to_broadcast([128, 8, 8, 8]),
                                                  in1=g3.unsqueeze(3).to_broadcast([128, 8, 8, 8]), op=ALU.is_gt), reads=["gbuf"], writes=["cmpb"])
            P.op("dve", lambda e: e.tensor_reduce(out=rankb[:], in_=cmpb[:].rearrange("p i n m -> p (i n) m"), axis=AX.X, op=ALU.add),
                 reads=["cmpb"], writes=["rankb"])
            bq_pad = cmpb[:].rearrange("p a b c -> p (a b c)").bitcast(BF16).rearrange("p (i c) -> p i c", c=128)
            P.op("dve", lambda e: e.memset(bq_pad, 0.0), reads=["rankb"], writes=["cmpb"])
            P.op("dve", lambda e: e.tensor_scalar(out=bq_pad[:, :, 0:8], in0=rankb[:].rearrange("p (i n) -> p i n", n=8), scalar1=2.5, scalar2=-BIG,
                                                  op0=ALU.is_ge, op1=ALU.mult), reads=["rankb", "cmpb"], writes=["cmpb"])
            for i8 in range(8):
                P.op("pe", lambda e, i8=i8: e.transpose(out=pT0[:, i8 * 128:(i8 + 1) * 128], in_=bq_pad[:, i8, :], identity=ident),
                     reads=["cmpb", "cb"], writes=["pT0"])
            P.op("act", lambda e: e.activation(out=biasT[:], in_=pT0[:], func=AF.Copy), reads=["pT0"], writes=["biasT"])
            if limit == "l1b":
                return knext
            sc = float(128 ** -0.5)
            pT0f = pT0[:].bitcast(F32)
            tiles = [(g, ktile) for g in range(4) for ktile in range(4 * g + 4)]

            def acc_of(g):
                if g % 2 == 0:
                    return pO[:], pS[:], "pO", "pS"
                return pT0f, pM[:], "pT0", "pM"

            def qk_stage(idx):
                g, ktile = tiles[idx]
                c0 = max(ktile - 4 * g, 0)
                cl = slice(c0 * 128, 512)
                j, r = idx % 2, idx % 3
                mm = [(cl, KT[:, ktile * 128:(ktile + 1) * 128], QT[:, g * 512 + c0 * 128:(g + 1) * 512], [("KT", ktile // 4), ("QT", g)])]
                if g >= 2:
                    n = ktile // 2
                    lo = max(2 * n + 2 - 4 * g, c0)
                    if lo < 4:
                        mm.append((slice(lo * 128, 512), cb[:, C_ES + n * 128:C_ES + (n + 1) * 128],
                                   biasT[:, (g - 2) * 512 + lo * 128:(g - 2) * 512 + 512], ["biasT", "cb"]))
                if ktile >= 4 * g:
                    mm.append((slice(c0 * 128, c0 * 128 + 128), ident, cmask, ["cb"]))
                for mi, (csl, l_, r_, rk) in enumerate(mm):
                    P.op("pe", lambda e, csl=csl, l_=l_, r_=r_, mi=mi, j=j, last=(mi == len(mm) - 1): e.matmul(
                        pB[j][:, csl], lhsT=l_, rhs=r_, start=(mi == 0), stop=last, skip_group_check=True),
                        reads=rk, writes=[("pB", j)])
                P.op("act", lambda e, j=j, r=r, cl=cl: e.activation(out=PT[r][:, cl], in_=pB[j][:, cl], func=AF.Exp, scale=sc),
                     reads=[("pB", j)], writes=[("PT", r)])

            def pv_stage(idx):
                g, ktile = tiles[idx]
                nkt = 4 * g + 4
                c0 = max(ktile - 4 * g, 0)
                cl = slice(c0 * 128, 512)
                r = idx % 3
                aO, aS, kO, kS = acc_of(g)
                P.op("pe", lambda e, r=r, cl=cl, ktile=ktile, nkt=nkt, aO=aO: e.matmul(aO[:, cl], lhsT=V[:, ktile * 128:(ktile + 1) * 128], rhs=PT[r][:, cl],
                                                                                      start=(ktile == 0), stop=(ktile == nkt - 1), skip_group_check=True),
                     reads=[("V", ktile // 4), ("PT", r)], writes=[kO])
                P.op("pe", lambda e, r=r, cl=cl, ktile=ktile, nkt=nkt, aS=aS: e.matmul(aS[:, cl], lhsT=onesb, rhs=PT[r][:, cl],
                                                                                      start=(ktile == 0), stop=(ktile == nkt - 1), skip_group_check=True),
                     reads=["cb", ("PT", r)], writes=[kS])
                if ktile == nkt - 1:
                    P.op("dve", lambda e, aS=aS: e.reciprocal(out=rsb[:], in_=aS), reads=[kS], writes=["rsb"])
                    P.op("dve", lambda e, aO=aO: e.tensor_tensor(out=otb[:], in0=aO, in1=rsb[:], op=ALU.mult), reads=[kO, "rsb"], writes=["otb"])
                    P.op("dve", lambda e, g=g: e.tensor_tensor(out=ogT[:, h, g * 512:(g + 1) * 512], in0=otb[:], in1=szT[:, g * 512:(g + 1) * 512], op=ALU.mult),
                         reads=["otb", ("szT", g)], writes=[("ogT", h, g)])

            qk_stage(0)
            for idx in range(len(tiles)):
                if idx + 1 < len(tiles):
                    qk_stage(idx + 1)
                pv_stage(idx)
            return knext

        for b in range(nseq):
            if limit == "setup":
                break
            rope_tables(b)
            layer_vectors(0, b)
            if limit == "rope":
                break
            kw = load_head_weights([a_w_in[:, g * 1024:g * 1024 + 128] for g in range(4)])
            l0_prep(kw)
            pre_phase(0, b)
            if limit == "pre":
                break
            for h in range(8):
                kw = l0_head(h, kw)
                if limit in ("head0", "h0a", "h0b", "h0c", "h0c1", "h0c2", "h0c3"):
                    break
            if limit in ("head0", "heads", "h0a", "h0b", "h0c", "h0c1", "h0c2", "h0c3"):
                break
            post_phase(0, b, a_w_out, last=stop_after_l0)
            if stop_after_l0:
                continue
            layer_vectors(1, b)
            kw = load_head_weights(l1_pieces(0))
            l1_prep(kw)
            pre_phase(1, b)
            if limit == "l1pre":
                break
            for h in range(8):
                kw = l1_head(h, kw)
                if limit in ("l1a", "l1b", "l1c", "l1a0", "l1a1", "l1a2"):
                    break
            if limit in ("l1a", "l1b", "l1c", "l1a0", "l1a1", "l1a2"):
                break
            post_phase(1, b, b_w_out, last=True)
        if limit is not None:
            P.dma("sp", "dbg", lambda e: e.dma_start(out=out[0, 0:128, :], in_=xin[0][:]), reads=[("xin", 0)], writes=[("out", 0, 0)])
            P.op("sp", lambda e: e.nop(), reads=[("out", 0, 0)])
            for e_ in ("act", "dve", "pool", "pe"):
                pass
        else:
            P.op("sp", lambda e: e.nop(), reads=[("out", b, tt) for b in range(nseq) for tt in range(NT)])

        cnt = P.analyze()
        names = set(cnt.keys()) | set(HW)
        sems = {s: es.enter_context(nc.semaphore(s.replace(":", "_"))) for s in sorted(names)}
        with nc.Block() as block:
            P.emit(block, sems)
    return nc, len(P.ops), cnt


_CACHE = {}


def kernel(x, c, positions, mod_w, mod_b, pre_norm_g, post_norm_g, a_w_in, a_w_out, a_out_norm_g,
           a_lb_logits, kv_norm_g, w_kv, b_w_in, b_w_out):
    if "nc" not in _CACHE:
        _CACHE["nc"] = build()[0]
    nc = _CACHE["nc"]
    f = lambda a: np.ascontiguousarray(np.asarray(a), dtype=np.float32)
    shared = {
        "mod_w": f(mod_w), "mod_b": f(mod_b), "pre_g": f(pre_norm_g), "post_g": f(post_norm_g),
        "a_w_in": f(a_w_in)[0], "a_w_out": f(a_w_out)[0], "a_gn": f(a_out_norm_g).reshape(128, 1),
        "a_lb": f(a_lb_logits), "kv_g": f(kv_norm_g).reshape(1, D), "w_kv": f(w_kv),
        "b_w_in": f(b_w_in)[0], "b_w_out": f(b_w_out)[0], "cf": make_consts(),
    }
    x = f(x)
    c = f(c)
    positions = np.ascontiguousarray(np.asarray(positions), dtype=np.int32)
    in_maps = []
    for i in range(8):
        m = dict(shared)
        m["x"] = x[i * NB:(i + 1) * NB]
        m["c"] = c[i * NB:(i + 1) * NB]
        m["pos"] = positions[i * NB:(i + 1) * NB]
        in_maps.append(m)
    res = run_bass_kernel_spmd(nc, in_maps, core_ids=list(range(8)))
    return np.concatenate([r["out"] for r in res.results], axis=0)
```

```python
import numpy as np
from contextlib import ExitStack
import concourse.bass as bass
import concourse.mybir as mybir
from concourse.bass_utils import run_bass_kernel_spmd

F32 = mybir.dt.float32
BF16 = mybir.dt.bfloat16
I32 = mybir.dt.int32
ALU = mybir.AluOpType
AF = mybir.ActivationFunctionType
AX = mybir.AxisListType

NB = 4
T = 2048
D = 1024
NT = 16
EPS = 1e-6
BIG = 30000.0
HW = ("pe", "act", "dve", "pool", "sp")
PSUM_KEYS = ("pA", "pB", "pO", "pS", "pT0", "pM")

C_ID, C_CM, C_PM, C_SW, C_ON, C_ES, C_NEG, C_INV, C_SGN, NCF = 0, 128, 256, 384, 512, 640, 1664, 1728, 1729, 1730
NCB = 1664


class Op:
    __slots__ = ("hw", "fn", "reads", "writes", "sem", "inc", "signal", "value", "waits", "idx", "is_dma")


class Prog:
    def __init__(self):
        self.ops = []

    def op(self, hw, fn, reads=(), writes=()):
        o = Op()
        o.hw, o.fn, o.reads, o.writes = hw, fn, tuple(reads), tuple(writes)
        o.sem, o.inc, o.signal, o.value, o.waits, o.is_dma = hw, 1, False, 0, [], False
        o.idx = len(self.ops)
        self.ops.append(o)
        return o

    def dma(self, hw, slot, fn, reads=(), writes=()):
        o = self.op(hw, fn, reads, writes)
        o.sem, o.inc, o.signal, o.is_dma = "dma:" + slot, 16, True, True
        return o

    def analyze(self):
        last_w, readers = {}, {}
        for o in self.ops:
            deps = {}
            for k in o.reads:
                p = last_w.get(k)
                if p is not None:
                    deps[p.idx] = (p, "raw")
                kn = k[0] if isinstance(k, tuple) else k
                if kn in PSUM_KEYS:
                    for r in readers.get(k, {}).values():
                        if r.hw != o.hw and r.idx not in deps:
                            deps[r.idx] = (r, "rar")
            for k in o.writes:
                p = last_w.get(k)
                if p is not None and p.idx not in deps:
                    deps[p.idx] = (p, "waw")
                for r in readers.get(k, {}).values():
                    if r.idx not in deps and r is not o:
                        deps[r.idx] = (r, "war")
            for p, kind in deps.values():
                if (not p.is_dma) and (not o.is_dma) and p.hw == o.hw:
                    if o.hw == "pe" or kind != "raw":
                        continue
                o.waits.append(p)
                p.signal = True
            for k in o.reads:
                readers.setdefault(k, {})[("d", o.idx) if o.is_dma else o.hw] = o
            for k in o.writes:
                last_w[k] = o
                readers[k] = {}
        cnt = {}
        for o in self.ops:
            if o.signal:
                cnt[o.sem] = cnt.get(o.sem, 0) + o.inc
                o.value = cnt[o.sem]
        return cnt

    def emit(self, block, sems):
        streams = {h: [] for h in HW}
        for o in self.ops:
            streams[o.hw].append(o)

        def make(hwname):
            def body(eng):
                known = {}
                for o in streams[hwname]:
                    need = {}
                    for p in o.waits:
                        if p.value > need.get(p.sem, 0):
                            need[p.sem] = p.value
                    for s, v in need.items():
                        if known.get(s, 0) < v:
                            eng.wait_ge(sems[s], v)
                            known[s] = v
                    ins = o.fn(eng)
                    if o.signal:
                        ins.then_inc(sems[o.sem], o.inc)
            return body

        block.tensor(make("pe"))
        block.scalar(make("act"))
        block.vector(make("dve"))
        block.gpsimd(make("pool"))
        block.sync(make("sp"))


def make_consts():
    cf = np.zeros((128, NCF), np.float32)
    p = np.arange(128)
    cf[:, C_ID:C_ID + 128] = np.eye(128, dtype=np.float32)
    cf[:, C_CM:C_CM + 128] = np.where(p[:, None] > p[None, :], -BIG, 0.0)
    cf[:, C_PM:C_PM + 128] = ((p[:, None] // 64 == p[None, :] // 64) & (p[:, None] <= p[None, :])).astype(np.float32)
    cf[:, C_SW:C_SW + 128] = (p[:, None] == (p[None, :] + 64) % 128).astype(np.float32)
    cf[:, C_ON:C_ON + 128] = 1.0
    for n in range(8):
        cf[n, C_ES + n * 128:C_ES + (n + 1) * 128] = 1.0
    neg = np.zeros((8, 8), np.float32)
    for i in range(8):
        j = (8 + i) // 2
        neg[i, j:] = -1e30
    cf[:, C_NEG:C_NEG + 64] = neg.reshape(1, 64)
    inv = np.float32(10000.0) ** (-np.arange(0, 128, 2, dtype=np.float32) / np.float32(128))
    cf[:, C_INV] = np.concatenate([inv, inv]).astype(np.float32)
    cf[:, C_SGN] = np.where(p < 64, -1.0, 1.0)
    return cf


def build(stop_after_l0=False, nseq=NB, limit=None):
    nc = bass.Bass("TRN2", target_bir_lowering=False)

    def din(name, shape, dt=F32):
        return nc.dram_tensor(name, list(shape), dt, kind="ExternalInput").ap()

    x = din("x", [NB, T, D])
    cin = din("c", [NB, D])
    pos = din("pos", [NB, T], I32)
    mod_w = din("mod_w", [2, D, 3 * D])
    mod_b = din("mod_b", [2, 3 * D])
    pre_g = din("pre_g", [2, D])
    post_g = din("post_g", [2, D])
    a_w_in = din("a_w_in", [D, 4 * D])
    a_w_out = din("a_w_out", [D, D])
    a_gn = din("a_gn", [128, 1])
    a_lb = din("a_lb", [2, D])
    kv_g = din("kv_g", [1, D])
    w_kv = din("w_kv", [D, 2 * D])
    b_w_in = din("b_w_in", [D, 2 * D])
    b_w_out = din("b_w_out", [D, D])
    cf = din("cf", [128, NCF])
    out = nc.dram_tensor("out", [NB, T, D], F32, kind="ExternalOutput").ap()
    scr = nc.dram_tensor("scr", [2, 3, NB, D], F32, kind="Internal").ap()

    P = Prog()
    es = ExitStack()
    with es:
        def sb(name, shape, dt):
            return es.enter_context(nc.sbuf_tensor(name, list(shape), dt))

        def ps(name, shape, dt):
            return es.enter_context(nc.psum_tensor(name, list(shape), dt))

        cb = sb("cb", [128, NCB], BF16)
        cfs = sb("cfs", [128, NCF - NCB], F32)
        ident = cb[:, C_ID:C_ID + 128]
        cmask = cb[:, C_CM:C_CM + 128]
        pmask = cb[:, C_PM:C_PM + 128]
        swp = cb[:, C_SW:C_SW + 128]
        onesb = cb[:, C_ON:C_ON + 128]
        negm = cfs[:, 0:64]
        invf = cfs[:, 64:65]
        sgn = cfs[:, 65:66]

        xnT = sb("xnT", [128, 8, T], BF16)
        ogT = sb("ogT", [128, 8, T], BF16)
        h1 = sb("h1", [128, NT * D], F32)
        wbf = [sb("wbf%d" % i, [128, 8, 512], BF16) for i in range(2)]
        hb = [sb("hb%d" % i, [128, T], BF16) for i in range(6)]
        cosT = sb("cosT", [128, T], BF16)
        sinT = sb("sinT", [128, T], BF16)
        xin = [sb("xin%d" % i, [128, D], F32) for i in range(2)]
        xs = [sb("xs0", [128, D], BF16)] * 2
        G_bc = sb("G_bc", [128, D], F32)
        PT = [sb("PT%d" % i, [128, 512], BF16) for i in range(3)]
        rsb = sb("rsb", [128, 512], F32)
        otb = sb("otb", [128, 512], F32)
        biasT = sb("biasT", [128, 1024], BF16)
        gbuf = sb("gbuf", [128, 64], F32)
        cmpb = sb("cmpb", [128, 8, 8, 8], F32)
        rankb = sb("rankb", [128, 64], F32)
        biasq = sb("biasq", [128, 64], BF16)
        small = sb("small", [128, 256], F32)
        smallb = sb("smallb", [128, 160], BF16)
        ss = small[:, 0:16]
        rstd = small[:, 16:32]
        tmpc = small[:, 32:48]
        aT = small[:, 48:56]
        shT = small[:, 56:64]
        gkT = small[:, 64:72]
        lbT = small[:, 72:88]
        oml = small[:, 88:96]
        noml = small[:, 184:192]
        gn = small[:, 96:97]
        bcol = small[:, 100:104]
        nbf = small[:, 104:105]
        ssy = small[:, 108:112]
        Bl = small[:, 112:144]
        Bl0 = small[:, 144:176]
        km = small[:, 176:184]
        cT = small[:, 192:224]
        shTb = smallb[:, 0:8]
        kmb = smallb[:, 8:16]
        birow = smallb[0:1, 16:144]
        scT = sb("scT", [128, 32], BF16)
        bmat = sb("bmat", [128, 128], BF16)

        def h1f(off, n):
            return h1[:, off:off + n]

        def h1b(off, n):
            return h1[:, off:off + n // 2].bitcast(BF16)

        o_ = 0
        T_q = h1f(o_, 512); o_ += 512
        T_s = h1f(o_, 512); o_ += 512
        T_d0 = h1f(o_, 512); o_ += 512
        T_d1 = h1f(o_, 512); o_ += 512
        T_B = h1f(o_, 512); o_ += 512
        attS = [h1b(o_, 512), h1b(o_ + 256, 512)]; o_ += 512
        osb = h1f(o_, 512); o_ += 512
        sqb = h1b(o_, 512); o_ += 256
        rst = h1f(o_, 512); o_ += 512
        tt_ = h1f(o_, 512); o_ += 512
        Ub = h1f(o_, 4096); o_ += 4096
        Sbf = h1b(o_, 4096); o_ += 2048
        arep = h1f(o_, 1024); RA = h1f(o_, 1024); RAi = h1[:, o_:o_ + 1024].bitcast(I32); o_ += 1024
        Shalf = h1f(o_, 1024); RB = h1f(o_, 1024); RBi = h1[:, o_:o_ + 1024].bitcast(I32); o_ += 1024
        Shalf2 = h1f(o_, 1024); o_ += 1024
        T_s2 = h1f(o_, 512); o_ += 512
        T_q2 = h1f(o_, 512); o_ += 512
        T_B2 = h1f(o_, 512); o_ += 512
        assert o_ <= NT * D
        msb = h1f(0, 3072)
        modbb = h1f(3072, 3072)
        pgb = h1f(6144, 2048)
        rows = h1f(8192, 3072)

        pA = [ps("pA%d" % i, [128, 512], F32) for i in range(2)]
        pB = [ps("pB%d" % i, [128, 512], F32) for i in range(2)]
        pO = ps("pO", [128, 512], F32)
        pS = ps("pS", [128, 512], F32)
        pT0 = ps("pT0", [128, 1024], BF16)
        pM = ps("pM", [128, 512], F32)

        H1A = "h1all"

        def WK(k, g0=0, g1=4):
            return [("wbf", k, g) for g in range(g0, g1)]

        def top(hw, fn, reads=(), writes=()):
            return P.op(hw, fn, tuple(reads) + (H1A,), writes)

        def tdma(hw, slot, fn, reads=(), writes=()):
            return P.dma(hw, slot, fn, tuple(reads) + (H1A,), writes)

        def barrier():
            for e in ("act", "dve", "pool"):
                P.op(e, (lambda eng, e=e: (eng.memset(small[:, 250:251], 0.0) if e != "act" else
                                            eng.activation(out=small[:, 251:252], in_=small[:, 252:253], func=AF.Copy))),
                     writes=[("bar", e)])
            P.op("pe", lambda eng: eng.matmul(pM[0:1, 0:1], lhsT=onesb[:, 0:1], rhs=onesb[:, 0:1], start=True, stop=True),
                 reads=["cb"], writes=["pM", ("bar", "pe")])
            for e in ("act", "dve", "pool", "pe", "sp"):
                if e == "pe":
                    P.op("pe", lambda eng: eng.matmul(pM[0:1, 0:1], lhsT=onesb[:, 0:1], rhs=onesb[:, 0:1], start=True, stop=True),
                         reads=["cb"] + [("bar", q) for q in ("act", "dve", "pool")], writes=["pM"])
                elif e == "sp":
                    P.op("sp", lambda eng: eng.nop(), reads=[("bar", q) for q in ("act", "dve", "pool", "pe")])
                else:
                    P.op(e, (lambda eng, e=e: (eng.memset(small[:, 253:254], 0.0) if e == "dve" else
                                                eng.memset(small[:, 254:255], 0.0) if e == "pool" else
                                                eng.activation(out=small[:, 255:256], in_=small[:, 252:253], func=AF.Copy))),
                         reads=[("bar", q) for q in ("act", "dve", "pool", "pe") if q != e])

        P.dma("pool", "cb", lambda e: e.dma_start(out=cb[:], in_=cf[:, 0:NCB]), writes=["cb"])
        P.dma("sp", "cfs", lambda e: e.dma_start(out=cfs[:], in_=cf[:, NCB:NCF]), writes=["cfs"])
        P.op("dve", lambda e: e.memset(small[:], 0.0), writes=["small"])
        P.op("dve", lambda e: e.memset(bmat[:], 0.0), writes=["birow"])
        P.op("dve", lambda e: e.memset(biasT[:], 0.0), writes=["biasT"])
        P.op("pool", lambda e: e.memset(hb[3][:], 0.0), writes=[("kt", 0), ("kt", 1)])
        P.op("pool", lambda e: e.memset(hb[5][:], 0.0), writes=[("kt", 0), ("kt", 1)])
        with nc.allow_non_contiguous_dma(reason="tiny one-time transposed loads"):
            pass
        for bb in range(NB):
            P.dma("sp", "s0", lambda e, bb=bb: e.dma_start(out=cT.rearrange("p (c b) -> p c b", b=NB)[:, :, bb], in_=cin[bb].rearrange("(c p) -> p c", p=128),
                                                         allow_slow_non_contiguous=True), reads=["small"], writes=[("cT", bb)])
        P.dma("sp", "s1", lambda e: e.dma_start(out=gkT, in_=kv_g[0].rearrange("(c p) -> p c", p=128), allow_slow_non_contiguous=True),
              reads=["small"], writes=["gkT"])
        for l in range(2):
            P.dma("sp", "s2", lambda e, l=l: e.dma_start(out=lbT[:, l * 8:(l + 1) * 8], in_=a_lb[l].rearrange("(c p) -> p c", p=128),
                                                       allow_slow_non_contiguous=True), reads=["small"], writes=[("lbT", l)])
        P.dma("sp", "s3", lambda e: e.dma_start(out=gn, in_=a_gn), reads=["small"], writes=["gn"])
        P.op("act", lambda e: e.activation(out=scT[:], in_=cT, func=AF.Silu), reads=[("cT", q) for q in range(NB)], writes=["scT"])
        P.op("dve", lambda e: e.tensor_tensor(out=oml, in0=lbT[:, 8:16], in1=lbT[:, 0:8], op=ALU.subtract), reads=[("lbT", 0), ("lbT", 1)], writes=["oml"])
        P.op("act", lambda e: e.activation(out=oml, in_=oml, func=AF.Sigmoid), reads=["oml"], writes=["oml"])
        P.op("dve", lambda e: e.tensor_scalar(out=noml, in0=oml, scalar1=-1.0, scalar2=None, op0=ALU.mult), reads=["oml", "small"], writes=["noml"])
        wi = 0
        for l in range(2):
            tdma("sp", "s4", lambda e, l=l: e.dma_start(out=modbb[0:NB, :], in_=mod_b[l].partition_broadcast(NB)), writes=["modbb"])
            tdma("sp", "s5", lambda e, l=l: e.dma_start(out=pgb[0:NB, 0:1024], in_=pre_g[l].partition_broadcast(NB)), writes=["pgb0"])
            tdma("sp", "s6", lambda e, l=l: e.dma_start(out=pgb[0:NB, 1024:2048], in_=post_g[l].partition_broadcast(NB)), writes=["pgb1"])
            for cbk in range(6):
                k = wi % 2
                wi += 1
                P.dma("pool", "wbf%d" % k, lambda e, l=l, cbk=cbk, k=k: e.dma_start(
                    out=wbf[k][:], in_=mod_w[l][:, cbk * 512:(cbk + 1) * 512].rearrange("(c p) n -> p c n", p=128)),
                    writes=WK(k))
                for c in range(8):
                    P.op("pe", lambda e, c=c, k=k: e.matmul(pM[0:NB, :], lhsT=scT[:, c * NB:(c + 1) * NB], rhs=wbf[k][:, c, :],
                                                            start=(c == 0), stop=(c == 7)),
                         reads=["scT"] + WK(k), writes=["pM"])
                top("dve", lambda e, cbk=cbk: e.tensor_tensor(
                    out=msb[0:NB, cbk * 512:(cbk + 1) * 512], in0=pM[0:NB, :],
                    in1=modbb[0:NB, cbk * 512:(cbk + 1) * 512], op=ALU.add),
                    reads=["pM", "modbb"], writes=[("msb", cbk)])
            mk = [("msb", i) for i in range(6)]
            top("dve", lambda e: e.scalar_tensor_tensor(out=rows[0:NB, 0:1024], in0=msb[0:NB, 1024:2048],
                                                        scalar=1.0, in1=pgb[0:NB, 0:1024], op0=ALU.add, op1=ALU.mult),
                reads=mk + ["pgb0"], writes=["rows"])
            top("dve", lambda e: e.tensor_copy(out=rows[0:NB, 1024:2048], in_=msb[0:NB, 0:1024]), reads=mk, writes=["rows"])
            top("dve", lambda e: e.tensor_tensor(out=rows[0:NB, 2048:3072], in0=msb[0:NB, 2048:3072], in1=pgb[0:NB, 1024:2048], op=ALU.mult),
                reads=mk + ["pgb1"], writes=["rows"])
            for kind in range(3):
                tdma("sp", "s7", lambda e, l=l, kind=kind: e.dma_start(out=scr[l, kind], in_=rows[0:NB, kind * 1024:(kind + 1) * 1024]),
                     reads=["rows"], writes=[("scr", l, kind)])
        barrier()

        xin_i = [0]

        def layer_vectors(l, b):
            P.dma("sp", "v0", lambda e: e.dma_start(out=aT, in_=scr[l, 0, b].rearrange("(c p) -> p c", p=128), allow_slow_non_contiguous=True),
                  reads=[("scr", l, 0)], writes=["aT"])
            P.dma("sp", "v1", lambda e: e.dma_start(out=shT, in_=scr[l, 1, b].rearrange("(c p) -> p c", p=128), allow_slow_non_contiguous=True),
                  reads=[("scr", l, 1)], writes=["shT"])
            P.dma("sp", "v2", lambda e: e.dma_start(out=G_bc[:], in_=scr[l, 2, b].partition_broadcast(128)), reads=[("scr", l, 2)], writes=["G_bc"])
            P.op("dve", lambda e: e.tensor_copy(out=shTb, in_=shT), reads=["shT"], writes=["shTb"])

        def rstd_from(col_in, col_out, keyin, keyout):
            P.op("dve", lambda e: e.tensor_scalar(out=col_out, in0=col_in, scalar1=1.0 / D, scalar2=EPS, op0=ALU.mult, op1=ALU.add),
                 reads=[keyin], writes=[keyout])
            P.op("act", lambda e: e.activation(out=col_out, in_=col_out, func=AF.Sqrt), reads=[keyout], writes=[keyout])
            P.op("dve", lambda e: e.reciprocal(out=col_out, in_=col_out), reads=[keyout], writes=[keyout])

        def pre_phase(l, b):
            pMb = pM[:].bitcast(BF16)
            jk = [("ogT", 7, 0), ("ogT", 7, 1)]
            xsb = [(xs[0][:], [("xs", 0)]), (ogT[:, 6, 0:D], [("ogT", 6, 0), ("ogT", 6, 1)])]
            srcs = {}

            def stage_a(tt):
                if l == 0:
                    xi = xin_i[0] % 2
                    xin_i[0] += 1
                    P.dma("sp", "xin%d" % xi, lambda e, tt=tt, xi=xi: e.dma_start(out=xin[xi][:], in_=x[b, tt * 128:(tt + 1) * 128, :]),
                          writes=[("xin", xi)])
                    src, skey = xin[xi][:], ("xin", xi)
                else:
                    src, skey = h1[:, tt * D:(tt + 1) * D], ("h1", tt)
                xb, xk = xsb[tt % 2]
                P.op("dve", lambda e, tt=tt: e.memset(ss[:, tt:tt + 1], 0.0), writes=[("ss", tt)])
                P.op("act", lambda e, src=src, tt=tt: e.activation(out=ogT[:, 7, 0:D], in_=src, func=AF.Square, accum_out=ss[:, tt:tt + 1]),
                     reads=[skey, ("ss", tt)], writes=jk + [("ss", tt)])
                rstd_from(ss[:, tt:tt + 1], rstd[:, tt:tt + 1], ("ss", tt), ("rstd", tt))
                P.op("dve", lambda e, src=src, tt=tt, xb=xb: e.tensor_scalar(out=xb, in0=src, scalar1=rstd[:, tt:tt + 1], scalar2=None, op0=ALU.mult),
                     reads=[skey, ("rstd", tt)], writes=xk)

            def stage_b(tt):
                xb, xk = xsb[tt % 2]
                ptb, pkey = (pT0[:], "pT0") if tt % 2 == 0 else (pMb, "pM")
                for c in range(8):
                    P.op("pe", lambda e, c=c, xb=xb, ptb=ptb: e.transpose(out=ptb[:, c * 128:(c + 1) * 128], in_=xb[:, c * 128:(c + 1) * 128], identity=ident),
                         reads=xk + ["cb"], writes=[pkey])
                if tt % 2 == 0:
                    P.op("act", lambda e, tt=tt, ptb=ptb: e.activation(out=xnT[:, :, tt * 128:(tt + 1) * 128], in_=ptb.rearrange("p (c t) -> p c t", c=8), func=AF.Copy),
                         reads=[pkey], writes=[("xnT", tt)])
                else:
                    P.op("dve", lambda e, tt=tt, ptb=ptb: e.tensor_copy(out=xnT[:, :, tt * 128:(tt + 1) * 128], in_=ptb.rearrange("p (c t) -> p c t", c=8)),
                         reads=[pkey], writes=[("xnT", tt)])

            stage_a(0)
            for tt in range(NT):
                if tt + 1 < NT:
                    stage_a(tt + 1)
                stage_b(tt)

        wslot = [0]
        kfree = [0]

        def load_head_weights(pieces):
            k = wslot[0] % 2
            wslot[0] += 1
            for g, ap in enumerate(pieces):
                P.dma("pool", "wbf%d" % k, lambda e, g=g, ap=ap, k=k: e.dma_start(
                    out=wbf[k][:, :, g * 128:(g + 1) * 128], in_=ap.rearrange("(c p) n -> p c n", p=128)),
                    writes=[("wbf", k, g)])
            return k

        def bias_cols(k, groups):
            for j, g in enumerate(groups):
                for c in range(8):
                    P.op("pe", lambda e, j=j, g=g, c=c: e.matmul(pM[:, j:j + 1], lhsT=wbf[k][:, c, g * 128:(g + 1) * 128], rhs=shTb[:, c:c + 1],
                                                               start=(c == 0), stop=(c == 7)),
                         reads=[("wbf", k, g), "shTb"], writes=["pM"])
            P.op("dve", lambda e: e.tensor_copy(out=bcol[:, 0:len(groups)], in_=pM[:, 0:len(groups)]), reads=["pM"], writes=["bcol"])

        def scale_w(k, c0, c1, vec, vkey):
            P.op("pool", lambda e: e.tensor_tensor(out=wbf[k][:, :, c0:c1], in0=wbf[k][:, :, c0:c1],
                                                   in1=vec.unsqueeze(2).to_broadcast([128, 8, c1 - c0]), op=ALU.mult),
                 reads=WK(k, c0 // 128, c1 // 128) + [vkey], writes=WK(k, c0 // 128, c1 // 128))

        pa_i = [0]

        def proj_fm(k, g, tb):
            i = pa_i[0] % 2
            pa_i[0] += 1
            for c in range(8):
                P.op("pe", lambda e, c=c, i=i: e.matmul(pA[i][:], lhsT=wbf[k][:, c, g * 128:(g + 1) * 128], rhs=xnT[:, c, tb * 512:(tb + 1) * 512],
                                                      start=(c == 0), stop=(c == 7)),
                     reads=[("wbf", k, g)] + [("xnT", 4 * tb + q) for q in range(4)], writes=[("pA", i)])
            return i

        def proj_tm(k, g, dst, dkey, bias_row=False):
            for t4 in range(4):
                i = t4 % 2
                for j in range(4):
                    tt = t4 * 4 + j
                    for c in range(8):
                        P.op("pe", lambda e, c=c, tt=tt, j=j, i=i: e.matmul(pB[i][:, j * 128:(j + 1) * 128], lhsT=xnT[:, c, tt * 128:(tt + 1) * 128],
                                                                          rhs=wbf[k][:, c, g * 128:(g + 1) * 128], start=(c == 0),
                                                                          stop=(c == 7 and not bias_row)),
                             reads=[("wbf", k, g), ("xnT", tt)], writes=[("pB", i)])
                    if bias_row:
                        P.op("pe", lambda e, j=j, i=i: e.matmul(pB[i][:, j * 128:(j + 1) * 128], lhsT=onesb, rhs=bmat[:], start=False, stop=True),
                             reads=["cb", "birow"], writes=[("pB", i)])
                P.op("act", lambda e, t4=t4, i=i: e.activation(out=dst[:, t4 * 512:(t4 + 1) * 512], in_=pB[i][:], func=AF.Copy),
                     reads=[("pB", i)], writes=[(dkey, t4)])

        def prefetch_wout(wout):
            kf = wslot[0] % 2
            wslot[0] += 1
            P.dma("pool", "wbf%d" % kf, lambda e: e.dma_start(out=wbf[kf][:], in_=wout[:, 0:512].rearrange("(c p) n -> p c n", p=128)), writes=WK(kf))
            return kf

        def post_phase(l, b, wout, last, kf):
            sl = [kf, 1 - kf]
            P.dma("pool", "wbf%d" % sl[1], lambda e: e.dma_start(
                out=wbf[sl[1]][:], in_=wout[:, 512:1024].rearrange("(c p) n -> p c n", p=128)), writes=WK(sl[1]))
            wslot[0] = 0
            for tt in range(NT):
                pp, pk = (pA, "pA") if tt % 2 == 0 else (pB, "pB")
                for cbk in range(2):
                    for c in range(8):
                        P.op("pe", lambda e, c=c, cbk=cbk, tt=tt, pp=pp: e.matmul(pp[cbk][:], lhsT=ogT[:, c, tt * 128:(tt + 1) * 128], rhs=wbf[sl[cbk]][:, c, :],
                                                                         start=(c == 0), stop=(c == 7)),
                             reads=WK(sl[cbk]) + [("ogT", hh, tt // 4) for hh in range(8)], writes=[(pk, cbk)])
                    P.op("dve", lambda e, cbk=cbk: e.memset(ssy[:, cbk:cbk + 1], 0.0), writes=[("ssy", cbk)])
                    P.op("act", lambda e, cbk=cbk, pp=pp: e.activation(out=PT[cbk][:], in_=pp[cbk][:], func=AF.Square, accum_out=ssy[:, cbk:cbk + 1]),
                         reads=[(pk, cbk), ("ssy", cbk)], writes=[("PT", cbk), ("ssy", cbk)])
                P.op("dve", lambda e: e.tensor_tensor(out=ssy[:, 2:3], in0=ssy[:, 0:1], in1=ssy[:, 1:2], op=ALU.add),
                     reads=[("ssy", 0), ("ssy", 1)], writes=[("ssy", 2)])
                rstd_from(ssy[:, 2:3], ssy[:, 3:4], ("ssy", 2), ("ssy", 3))
                xi = xin_i[0] % 2
                xin_i[0] += 1
                if l == 0:
                    P.dma("sp", "xin%d" % xi, lambda e, tt=tt, xi=xi: e.dma_start(out=xin[xi][:], in_=x[b, tt * 128:(tt + 1) * 128, :]),
                          writes=[("xin", xi)])
                    dest, dkey, res, rkey = h1[:, tt * D:(tt + 1) * D], ("h1", tt), xin[xi][:], ("xin", xi)
                    extra_w = [H1A]
                else:
                    dest, dkey, res, rkey = xin[xi][:], ("xin", xi), h1[:, tt * D:(tt + 1) * D], ("h1", tt)
                    extra_w = [H1A]
                for cbk in range(2):
                    P.op("dve", lambda e, cbk=cbk, dest=dest, pp=pp: e.scalar_tensor_tensor(
                        out=dest[:, cbk * 512:(cbk + 1) * 512], in0=pp[cbk][:], scalar=ssy[:, 3:4], in1=G_bc[:, cbk * 512:(cbk + 1) * 512],
                        op0=ALU.mult, op1=ALU.mult), reads=[(pk, cbk), ("ssy", 3), "G_bc"], writes=[dkey] + (extra_w if l == 0 else []))
                P.op("dve", lambda e, dest=dest, res=res: e.tensor_tensor(out=dest, in0=dest, in1=res, op=ALU.add),
                     reads=[dkey, rkey], writes=[dkey] + extra_w)
                if last:
                    P.dma("sp", "xin%d" % xi, lambda e, tt=tt, dest=dest: e.dma_start(out=out[b, tt * 128:(tt + 1) * 128, :], in_=dest),
                          reads=[dkey], writes=[("out", b, tt)])

        def rope_tables(b):
            for hf in range(2):
                cs = slice(hf * 1024, (hf + 1) * 1024)
                tdma("sp", "ra", lambda e, cs=cs: e.dma_start(out=RAi, in_=pos[b, cs].partition_broadcast(128)), writes=["arep"])
                top("dve", lambda e: e.tensor_copy(out=RA, in_=RAi), reads=["arep"], writes=["arep"])
                top("dve", lambda e: e.tensor_scalar(out=RA, in0=RA, scalar1=invf, scalar2=None, op0=ALU.mult), reads=["arep", "cfs"], writes=["arep"])
                for which in range(2):
                    off = 0.0 if which == 0 else float(np.pi / 2)
                    top("dve", lambda e, off=off: e.tensor_scalar(out=RB, in0=RA, scalar1=off, scalar2=float(1.0 / (2 * np.pi)), op0=ALU.add, op1=ALU.mult),
                        reads=["arep"], writes=["Shalf"])
                    top("dve", lambda e: e.tensor_copy(out=RBi, in_=RB), reads=["Shalf"], writes=["Shalf"])
                    top("dve", lambda e: e.tensor_copy(out=RB, in_=RBi), reads=["Shalf"], writes=["Shalf"])
                    top("dve", lambda e: e.tensor_scalar(out=RB, in0=RB, scalar1=float(-2 * np.pi), scalar2=None, op0=ALU.mult), reads=["Shalf"], writes=["Shalf"])
                    top("dve", lambda e, off=off: e.scalar_tensor_tensor(out=RB, in0=RA, scalar=off, in1=RB, op0=ALU.add, op1=ALU.add),
                        reads=["arep", "Shalf"], writes=["Shalf"])
                    top("dve", lambda e: e.tensor_scalar(out=RB, in0=RB, scalar1=3.14159, scalar2=-3.14159, op0=ALU.min, op1=ALU.max),
                        reads=["Shalf"], writes=["Shalf"])
                    if which == 0:
                        top("act", lambda e: e.activation(out=RB, in_=RB, func=AF.Sin), reads=["Shalf"], writes=["Shalf"])
                        top("dve", lambda e, cs=cs: e.tensor_scalar(out=sinT[:, cs], in0=RB, scalar1=sgn, scalar2=None, op0=ALU.mult),
                            reads=["Shalf", "cfs"], writes=["sinT"])
                    else:
                        top("act", lambda e, cs=cs: e.activation(out=cosT[:, cs], in_=RB, func=AF.Sin), reads=["Shalf"], writes=["cosT"])

        def l0_prep(k):
            bias_cols(k, [0, 1, 3])
            P.op("dve", lambda e: e.tensor_scalar(out=nbf, in0=bcol[:, 1:2], scalar1=-1.0, scalar2=None, op0=ALU.mult), reads=["bcol"], writes=["nbf"])
            for c in range(8):
                P.op("pe", lambda e, c=c: e.matmul(pM[0:1, 128:256], lhsT=shTb[:, c:c + 1], rhs=wbf[k][:, c, 256:384], start=(c == 0), stop=(c == 7)),
                     reads=[("wbf", k, 2), "shTb"], writes=["pM"])
            P.op("dve", lambda e: e.tensor_copy(out=bmat[0:1, :], in_=pM[0:1, 128:256]), reads=["pM"], writes=["birow"])
            scale_w(k, 0, 512, aT, "aT")

        def l0_head(h, k):
            qtT, ktT, sgT, kt, vv, ktB = hb[0], hb[1], hb[2], hb[3], hb[4], hb[5]
            knext = None
            if h == 7:
                kfree[0] = prefetch_wout(a_w_out)
            if h < 7:
                knext = load_head_weights([a_w_in[:, g * 1024 + (h + 1) * 128:g * 1024 + (h + 2) * 128] for g in range(4)])
            for tb in range(4):
                bs = slice(tb * 512, (tb + 1) * 512)
                Tq, TB, Ts = (T_q, T_B, T_s) if tb % 2 == 0 else (T_q2, T_B2, T_s2)
                kq, kB, ks = ("T_q", "T_B", "T_s") if tb % 2 == 0 else ("T_q2", "T_B2", "T_s2")
                i = proj_fm(k, 0, tb)
                top("act", lambda e, i=i, Tq=Tq: e.activation(out=Tq, in_=pA[i][:], func=AF.Silu, bias=bcol[:, 0:1]), reads=[("pA", i), "bcol"], writes=[kq])
                i = proj_fm(k, 3, tb)
                P.op("act", lambda e, i=i, bs=bs: e.activation(out=sgT[:, bs], in_=pA[i][:], func=AF.Silu, bias=bcol[:, 2:3]),
                     reads=[("pA", i), "bcol"], writes=[("sgT", tb)])
                i = proj_fm(k, 1, tb)
                top("act", lambda e, i=i, Ts=Ts: e.activation(out=Ts, in_=pA[i][:], func=AF.Sigmoid, bias=nbf, scale=-1.0), reads=[("pA", i), "nbf"], writes=[ks])
                top("dve", lambda e, Ts=Ts: e.tensor_scalar(out=T_d0, in0=Ts, scalar1=noml[:, h:h + 1], scalar2=1.0, op0=ALU.mult, op1=ALU.add),
                    reads=[ks, "noml"], writes=["T_d0"])
                if tb == 0 and h == 0:
                    top("dve", lambda e: e.memset(T_d1, 0.0), writes=["T_d1"])
                d0v = T_d0.rearrange("p (n c) -> p n c", c=64)[:, :, 0:1]
                d1v = T_d1.rearrange("p (n c) -> p n c", c=64)[:, :, 0:1]
                top("dve", lambda e, d0v=d0v, d1v=d1v: e.tensor_copy(out=d1v, in_=d0v), reads=["T_d0", "T_d1"], writes=["T_d1"])
                top("dve", lambda e, d0v=d0v: e.memset(d0v, 0.0), reads=["T_d0", "T_d1"], writes=["T_d0"])
                top("dve", lambda e, TB=TB: e.tensor_tensor_scan(out=TB, data0=T_d0, data1=T_d1, initial=0.0, op0=ALU.mult, op1=ALU.add),
                    reads=["T_d0", "T_d1"], writes=[kB])
                top("dve", lambda e, tb=tb, TB=TB: e.tensor_copy(out=Bl[:, tb * 8:(tb + 1) * 8].unsqueeze(2),
                                                                 in_=TB.rearrange("p (n c) -> p n c", c=64)[:, :, 63:64]), reads=[kB], writes=["Bl"])
                top("pool", lambda e, bs=bs, Tq=Tq, TB=TB: e.tensor_tensor(out=qtT[:, bs], in0=Tq, in1=TB, op=ALU.mult), reads=[kq, kB], writes=[("qtT", tb)])
                top("dve", lambda e, TB=TB: e.reciprocal(out=T_d0, in_=TB), reads=[kB], writes=["T_d0"])
                top("dve", lambda e, bs=bs, Ts=Ts: e.scalar_tensor_tensor(out=ktT[:, bs], in0=Ts, scalar=oml[:, h:h + 1], in1=T_d0, op0=ALU.mult, op1=ALU.mult),
                    reads=[ks, "T_d0", "oml"], writes=[("ktT", tb)])
            top("dve", lambda e: e.tensor_copy(out=Bl0, in_=Bl), reads=["Bl"], writes=["Bl0"])
            top("dve", lambda e: e.memset(Bl0[:, 0:1], 0.0), reads=["Bl0"], writes=["Bl0"])
            top("pool", lambda e: e.tensor_copy(out=arep.rearrange("p (v n) -> p v n", n=32), in_=Bl0.unsqueeze(1).to_broadcast([128, 32, 32])),
                reads=["Bl0"], writes=["arep"])
            if limit == "h0a":
                return knext
            proj_tm(k, 2, vv, "vv", bias_row=True)
            for t8 in range(2):
                for j in range(8):
                    tt = t8 * 8 + j
                    P.op("pe", lambda e, tt=tt, j=j: e.transpose(out=pT0[:, j * 128:(j + 1) * 128], in_=ktT[:, tt * 128:(tt + 1) * 128], identity=ident),
                         reads=[("ktT", tt // 4), "cb"], writes=["pT0"])
                P.op("act", lambda e, t8=t8: e.activation(out=kt[0:64, t8 * 1024:(t8 + 1) * 1024], in_=pT0[0:64, :], func=AF.Copy), reads=["pT0"], writes=[("kt", t8)])
                P.op("act", lambda e, t8=t8: e.activation(out=ktB[64:128, t8 * 1024:(t8 + 1) * 1024], in_=pT0[64:128, :], func=AF.Copy), reads=["pT0"], writes=[("kt", t8)])
            if knext is not None:
                l0_prep(knext)
            if limit == "h0b":
                return knext
            Uv = Ub.rearrange("p (v n) -> p n v", n=32)
            for ng in range(8):
                i = ng % 2
                for j in range(4):
                    n = ng * 4 + j
                    tt, half = n // 2, n % 2
                    ksrc = kt if half == 0 else ktB
                    P.op("pe", lambda e, j=j, tt=tt, ksrc=ksrc, i=i: e.matmul(pB[i][:, j * 128:(j + 1) * 128], lhsT=ksrc[:, tt * 128:(tt + 1) * 128],
                                                                            rhs=vv[:, tt * 128:(tt + 1) * 128], start=True, stop=True),
                         reads=[("kt", tt // 8), ("vv", tt // 4)], writes=[("pB", i)])
                top("dve", lambda e, ng=ng, i=i: e.tensor_tensor(out=Uv[:, ng * 4:(ng + 1) * 4, :], in0=pB[i][:].rearrange("p (n v) -> p n v", n=4),
                                                                 in1=Bl[:, ng * 4:(ng + 1) * 4].unsqueeze(2).to_broadcast([128, 4, 128]), op=ALU.mult),
                    reads=[("pB", i), "Bl"], writes=["Ub"])
            if limit == "h0c1":
                return knext
            for vq in range(4):
                Sq, ksq = (Shalf, "Shalf") if vq % 2 == 0 else (Shalf2, "Shalf2")
                top("dve", lambda e, vq=vq, Sq=Sq: e.tensor_tensor_scan(out=Sq, data0=arep, data1=Ub[:, vq * 1024:(vq + 1) * 1024], initial=0.0,
                                                                        op0=ALU.mult, op1=ALU.add),
                    reads=["arep", "Ub"], writes=[ksq])
                dstv = Sbf.rearrange("p (n v) -> p v n", n=32)[:, vq * 32:(vq + 1) * 32, :]
                if vq % 2 == 0:
                    top("act", lambda e, dstv=dstv, Sq=Sq: e.activation(out=dstv, in_=Sq.rearrange("p (v n) -> p v n", n=32), func=AF.Copy),
                        reads=[ksq], writes=[("Sbf", vq)])
                else:
                    top("pool", lambda e, dstv=dstv, Sq=Sq: e.tensor_copy(out=dstv, in_=Sq.rearrange("p (v n) -> p v n", n=32)),
                        reads=[ksq], writes=[("Sbf", vq)])
            def att_stage(pg):
                i = pg % 2
                for j in range(4):
                    pr = pg * 4 + j
                    P.op("pe", lambda e, j=j, pr=pr, i=i: e.matmul(pB[i][:, j * 128:(j + 1) * 128], lhsT=ktT[:, pr * 128:(pr + 1) * 128],
                                                                 rhs=qtT[:, pr * 128:(pr + 1) * 128], start=True, stop=True),
                         reads=[("ktT", pg), ("qtT", pg)], writes=[("pB", i)])
                top("dve", lambda e, i=i: e.tensor_tensor(out=attS[i].rearrange("p (n t) -> p n t", n=4), in0=pB[i][:].rearrange("p (n t) -> p n t", n=4),
                                                          in1=pmask.unsqueeze(1).to_broadcast([128, 4, 128]), op=ALU.mult),
                    reads=[("pB", i), "cb"], writes=[("attS", i)])

            att_stage(0)
            pT0f0 = pT0[:].bitcast(F32)
            for pg in range(4):
                i = pg % 2
                if pg % 2 == 0:
                    aO, aS, kO, kS = pO[:], pS[:], "pO", "pS"
                else:
                    aO, aS, kO, kS = pT0f0, pM[:], "pT0", "pM"
                if pg < 3:
                    att_stage(pg + 1)
                sbk = [("Sbf", q) for q in range(4)]
                for j in range(4):
                    pr = pg * 4 + j
                    P.op("pe", lambda e, j=j, pr=pr, i=i, aO=aO: e.matmul(aO[:, j * 128:(j + 1) * 128], lhsT=vv[:, pr * 128:(pr + 1) * 128],
                                                                        rhs=attS[i][:, j * 128:(j + 1) * 128], start=True, stop=False, skip_group_check=True),
                         reads=[("vv", pg), ("attS", i), H1A], writes=[kO])
                    if pr > 0:
                        P.op("pe", lambda e, j=j, pr=pr, aO=aO: e.matmul(aO[:, j * 128:j * 128 + 64], lhsT=Sbf[:, (2 * pr - 1) * 128:(2 * pr) * 128],
                                                                       rhs=qtT[:, pr * 128:pr * 128 + 64], start=False, stop=False, skip_group_check=True),
                             reads=sbk + [("qtT", pg), H1A], writes=[kO])
                    P.op("pe", lambda e, j=j, pr=pr, aO=aO: e.matmul(aO[:, j * 128 + 64:(j + 1) * 128], lhsT=Sbf[:, (2 * pr) * 128:(2 * pr + 1) * 128],
                                                                   rhs=qtT[:, pr * 128 + 64:(pr + 1) * 128], start=False, stop=True, skip_group_check=True),
                         reads=sbk + [("qtT", pg), H1A], writes=[kO])
                top("act", lambda e, aO=aO: e.activation(out=sqb, in_=aO, func=AF.Square), reads=[kO], writes=["sqb"])
                P.op("pe", lambda e, aS=aS: e.matmul(aS, lhsT=onesb, rhs=sqb, start=True, stop=True), reads=["sqb", "cb", H1A], writes=[kS])
                top("dve", lambda e, aS=aS: e.tensor_scalar(out=rst, in0=aS, scalar1=1.0 / 128, scalar2=EPS, op0=ALU.mult, op1=ALU.add), reads=[kS], writes=["rst"])
                top("act", lambda e: e.activation(out=rst, in_=rst, func=AF.Sqrt), reads=["rst"], writes=["rst"])
                top("dve", lambda e: e.reciprocal(out=rst, in_=rst), reads=["rst"], writes=["rst"])
                tbuf, tkey = (tt_, "tt_") if pg % 2 == 0 else (osb, "osb")
                top("dve", lambda e, aO=aO, tbuf=tbuf: e.scalar_tensor_tensor(out=tbuf, in0=aO, scalar=gn, in1=rst, op0=ALU.mult, op1=ALU.mult),
                    reads=[kO, "rst", "gn"], writes=[tkey])
                top("pool", lambda e, pg=pg, tbuf=tbuf: e.tensor_tensor(out=ogT[:, h, pg * 512:(pg + 1) * 512], in0=tbuf, in1=sgT[:, pg * 512:(pg + 1) * 512], op=ALU.mult),
                    reads=[tkey, ("sgT", pg)], writes=[("ogT", h, pg)])
            return knext

        pt_i = [0]

        def l1_pieces(h):
            return [b_w_in[:, h * 128:(h + 1) * 128], b_w_in[:, 1024 + h * 128:1024 + (h + 1) * 128],
                    w_kv[:, h * 128:(h + 1) * 128], w_kv[:, 1024 + h * 128:1024 + (h + 1) * 128]]

        def l1_prep(k):
            bias_cols(k, [0, 1])
            scale_w(k, 0, 256, aT, "aT")
            scale_w(k, 256, 512, gkT, "gkT")

        def l1_head(h, k):
            QT, KT, szT, V = hb[0], hb[1], hb[2], hb[4]
            knext = load_head_weights(l1_pieces(h + 1)) if h < 7 else None
            if h == 7:
                kfree[0] = prefetch_wout(b_w_out)
            if limit == "l1a0":
                return knext
            for tb in range(4):
                bs = slice(tb * 512, (tb + 1) * 512)
                for which in range(2):
                    if limit == "l1a1" and (tb, which) == (0, 1):
                        return knext
                    if limit == "l1a2" and (tb, which) == (1, 0):
                        return knext
                    dst = QT if which == 0 else KT
                    dkey = "QT" if which == 0 else "KT"
                    i = proj_fm(k, 0 if which == 0 else 2, tb)
                    j = (tb * 2 + which) % 2
                    if which == 0:
                        P.op("dve", lambda e, i=i: e.tensor_scalar(out=PT[0][:], in0=pA[i][:], scalar1=bcol[:, 0:1], scalar2=None, op0=ALU.add),
                            reads=[("pA", i), "bcol"], writes=[("PT", 0)])
                        P.op("dve", lambda e, i=i, bs=bs: e.scalar_tensor_tensor(out=rsb[:], in0=pA[i][:], scalar=bcol[:, 0:1], in1=cosT[:, bs], op0=ALU.add, op1=ALU.mult),
                            reads=[("pA", i), "bcol", "cosT"], writes=["rsb"])
                    else:
                        P.op("act", lambda e, i=i: e.activation(out=PT[0][:], in_=pA[i][:], func=AF.Copy), reads=[("pA", i)], writes=[("PT", 0)])
                        P.op("dve", lambda e, i=i, bs=bs: e.tensor_tensor(out=rsb[:], in0=pA[i][:], in1=cosT[:, bs], op=ALU.mult),
                            reads=[("pA", i), "cosT"], writes=["rsb"])
                    P.op("pe", lambda e, j=j: e.matmul(pB[j][:], lhsT=swp, rhs=PT[0][:], start=True, stop=True), reads=[("PT", 0), "cb"], writes=[("pB", j)])
                    P.op("dve", lambda e, j=j, bs=bs: e.tensor_tensor(out=otb[:], in0=pB[j][:], in1=sinT[:, bs], op=ALU.mult), reads=[("pB", j), "sinT"], writes=["otb"])
                    P.op("dve", lambda e, dst=dst, bs=bs: e.tensor_tensor(out=dst[:, bs], in0=rsb[:], in1=otb[:], op=ALU.add), reads=["rsb", "otb"], writes=[(dkey, tb)])
                i = proj_fm(k, 1, tb)
                P.op("act", lambda e, i=i, bs=bs: e.activation(out=szT[:, bs], in_=pA[i][:], func=AF.Silu, bias=bcol[:, 1:2]),
                     reads=[("pA", i), "bcol"], writes=[("szT", tb)])
            if limit == "l1a":
                return knext
            proj_tm(k, 3, V, "V")
            if knext is not None:
                l1_prep(knext)
            P.op("dve", lambda e: e.tensor_reduce(out=km, in_=KT[:].rearrange("p (n k) -> p n k", k=256), axis=AX.X, op=ALU.add),
                 reads=[("KT", q) for q in range(4)], writes=["km"])
            P.op("dve", lambda e: e.tensor_scalar(out=kmb, in0=km, scalar1=1.0 / 256, scalar2=None, op0=ALU.mult), reads=["km"], writes=["kmb"])
            for i8 in range(8):
                P.op("pe", lambda e, i8=i8: e.matmul(pM[:, i8 * 8:(i8 + 1) * 8], lhsT=QT[:, (8 + i8) * 128:(9 + i8) * 128], rhs=kmb, start=True, stop=True),
                     reads=[("QT", (8 + i8) // 4), "kmb"], writes=["pM"])
            P.op("dve", lambda e: e.tensor_tensor(out=gbuf[:], in0=pM[:, 0:64], in1=negm, op=ALU.add), reads=["pM", "cfs"], writes=["gbuf"])
            g3 = gbuf[:].rearrange("p (i n) -> p i n", n=8)
            P.op("dve", lambda e: e.tensor_tensor(out=cmpb[:], in0=g3.unsqueeze(2).to_broadcast([128, 8, 8, 8]),
                                                  in1=g3.unsqueeze(3).to_broadcast([128, 8, 8, 8]), op=ALU.is_gt), reads=["gbuf"], writes=["cmpb"])
            P.op("dve", lambda e: e.tensor_reduce(out=rankb[:], in_=cmpb[:].rearrange("p i n m -> p (i n) m"), axis=AX.X, op=ALU.add),
                 reads=["cmpb"], writes=["rankb"])
            bq_pad = cmpb[:].rearrange("p a b c -> p (a b c)").bitcast(BF16).rearrange("p (i c) -> p i c", c=128)
            P.op("dve", lambda e: e.memset(bq_pad, 0.0), reads=["rankb"], writes=["cmpb"])
            P.op("dve", lambda e: e.tensor_scalar(out=bq_pad[:, :, 0:8], in0=rankb[:].rearrange("p (i n) -> p i n", n=8), scalar1=2.5, scalar2=-BIG,
                                                  op0=ALU.is_ge, op1=ALU.mult), reads=["rankb", "cmpb"], writes=["cmpb"])
            for i8 in range(8):
                P.op("pe", lambda e, i8=i8: e.transpose(out=pT0[:, i8 * 128:(i8 + 1) * 128], in_=bq_pad[:, i8, :], identity=ident),
                     reads=["cmpb", "cb"], writes=["pT0"])
            P.op("act", lambda e: e.activation(out=biasT[:], in_=pT0[:], func=AF.Copy), reads=["pT0"], writes=["biasT"])
            if limit == "l1b":
                return knext
            sc = float(128 ** -0.5)
            pT0f = pT0[:].bitcast(F32)
            tiles = [(g, ktile) for g in range(4) for ktile in range(4 * g + 4)]

            def acc_of(g):
                if g % 2 == 0:
                    return pO[:], pS[:], "pO", "pS"
                return pT0f, pM[:], "pT0", "pM"

            def qk_stage(idx):
                g, ktile = tiles[idx]
                c0 = max(ktile - 4 * g, 0)
                cl = slice(c0 * 128, 512)
                j, r = idx % 2, idx % 3
                mm = [(cl, KT[:, ktile * 128:(ktile + 1) * 128], QT[:, g * 512 + c0 * 128:(g + 1) * 512], [("KT", ktile // 4), ("QT", g)])]
                if g >= 2:
                    n = ktile // 2
                    lo = max(2 * n + 2 - 4 * g, c0)
                    if lo < 4:
                        mm.append((slice(lo * 128, 512), cb[:, C_ES + n * 128:C_ES + (n + 1) * 128],
                                   biasT[:, (g - 2) * 512 + lo * 128:(g - 2) * 512 + 512], ["biasT", "cb"]))
                if ktile >= 4 * g:
                    mm.append((slice(c0 * 128, c0 * 128 + 128), ident, cmask, ["cb"]))
                for mi, (csl, l_, r_, rk) in enumerate(mm):
                    P.op("pe", lambda e, csl=csl, l_=l_, r_=r_, mi=mi, j=j, last=(mi == len(mm) - 1): e.matmul(
                        pB[j][:, csl], lhsT=l_, rhs=r_, start=(mi == 0), stop=last, skip_group_check=True),
                        reads=rk, writes=[("pB", j)])
                P.op("act", lambda e, j=j, r=r, cl=cl: e.activation(out=PT[r][:, cl], in_=pB[j][:, cl], func=AF.Exp, scale=sc),
                     reads=[("pB", j)], writes=[("PT", r)])

            def pv_stage(idx):
                g, ktile = tiles[idx]
                nkt = 4 * g + 4
                c0 = max(ktile - 4 * g, 0)
                cl = slice(c0 * 128, 512)
                r = idx % 3
                aO, aS, kO, kS = acc_of(g)
                P.op("pe", lambda e, r=r, cl=cl, ktile=ktile, nkt=nkt, aO=aO: e.matmul(aO[:, cl], lhsT=V[:, ktile * 128:(ktile + 1) * 128], rhs=PT[r][:, cl],
                                                                                      start=(ktile == 0), stop=(ktile == nkt - 1), skip_group_check=True),
                     reads=[("V", ktile // 4), ("PT", r)], writes=[kO])
                P.op("pe", lambda e, r=r, cl=cl, ktile=ktile, nkt=nkt, aS=aS: e.matmul(aS[:, cl], lhsT=onesb, rhs=PT[r][:, cl],
                                                                                      start=(ktile == 0), stop=(ktile == nkt - 1), skip_group_check=True),
                     reads=["cb", ("PT", r)], writes=[kS])
                if ktile == nkt - 1:
                    P.op("dve", lambda e, aS=aS: e.reciprocal(out=rsb[:], in_=aS), reads=[kS], writes=["rsb"])
                    P.op("dve", lambda e, aO=aO: e.tensor_tensor(out=otb[:], in0=aO, in1=rsb[:], op=ALU.mult), reads=[kO, "rsb"], writes=["otb"])
                    P.op("dve", lambda e, g=g: e.tensor_tensor(out=ogT[:, h, g * 512:(g + 1) * 512], in0=otb[:], in1=szT[:, g * 512:(g + 1) * 512], op=ALU.mult),
                         reads=["otb", ("szT", g)], writes=[("ogT", h, g)])

            qk_stage(0)
            for idx in range(len(tiles)):
                if idx + 1 < len(tiles):
                    qk_stage(idx + 1)
                pv_stage(idx)
            return knext

        for b in range(nseq):
            if limit == "setup":
                break
            rope_tables(b)
            layer_vectors(0, b)
            if limit == "rope":
                break
            kw = load_head_weights([a_w_in[:, g * 1024:g * 1024 + 128] for g in range(4)])
            l0_prep(kw)
            pre_phase(0, b)
            if limit == "pre":
                break
            for h in range(8):
                kw = l0_head(h, kw)
                if limit in ("head0", "h0a", "h0b", "h0c", "h0c1", "h0c2", "h0c3"):
                    break
            if limit in ("head0", "heads", "h0a", "h0b", "h0c", "h0c1", "h0c2", "h0c3"):
                break
            post_phase(0, b, a_w_out, stop_after_l0, kfree[0])
            if stop_after_l0:
                continue
            layer_vectors(1, b)
            kw = load_head_weights(l1_pieces(0))
            l1_prep(kw)
            pre_phase(1, b)
            if limit == "l1pre":
                break
            for h in range(8):
                kw = l1_head(h, kw)
                if limit in ("l1a", "l1b", "l1c", "l1a0", "l1a1", "l1a2"):
                    break
            if limit in ("l1a", "l1b", "l1c", "l1a0", "l1a1", "l1a2"):
                break
            post_phase(1, b, b_w_out, True, kfree[0])
        if limit is not None:
            P.dma("sp", "dbg", lambda e: e.dma_start(out=out[0, 0:128, :], in_=xin[0][:]), reads=[("xin", 0)], writes=[("out", 0, 0)])
            P.op("sp", lambda e: e.nop(), reads=[("out", 0, 0)])
            for e_ in ("act", "dve", "pool", "pe"):
                pass
        else:
            P.op("sp", lambda e: e.nop(), reads=[("out", b, tt) for b in range(nseq) for tt in range(NT)])

        cnt = P.analyze()
        names = set(cnt.keys()) | set(HW)
        sems = {s: es.enter_context(nc.semaphore(s.replace(":", "_"))) for s in sorted(names)}
        with nc.Block() as block:
            P.emit(block, sems)
    return nc, len(P.ops), cnt


_CACHE = {}


def kernel(x, c, positions, mod_w, mod_b, pre_norm_g, post_norm_g, a_w_in, a_w_out, a_out_norm_g,
           a_lb_logits, kv_norm_g, w_kv, b_w_in, b_w_out):
    if "nc" not in _CACHE:
        _CACHE["nc"] = build()[0]
    nc = _CACHE["nc"]
    f = lambda a: np.ascontiguousarray(np.asarray(a), dtype=np.float32)
    shared = {
        "mod_w": f(mod_w), "mod_b": f(mod_b), "pre_g": f(pre_norm_g), "post_g": f(post_norm_g),
        "a_w_in": f(a_w_in)[0], "a_w_out": f(a_w_out)[0], "a_gn": f(a_out_norm_g).reshape(128, 1),
        "a_lb": f(a_lb_logits), "kv_g": f(kv_norm_g).reshape(1, D), "w_kv": f(w_kv),
        "b_w_in": f(b_w_in)[0], "b_w_out": f(b_w_out)[0], "cf": make_consts(),
    }
    x = f(x)
    c = f(c)
    positions = np.ascontiguousarray(np.asarray(positions), dtype=np.int32)
    in_maps = []
    for i in range(8):
        m = dict(shared)
        m["x"] = x[i * NB:(i + 1) * NB]
        m["c"] = c[i * NB:(i + 1) * NB]
        m["pos"] = positions[i * NB:(i + 1) * NB]
        in_maps.append(m)
    res = run_bass_kernel_spmd(nc, in_maps, core_ids=list(range(8)))
    return np.concatenate([r["out"] for r in res.results], axis=0)
```

```python
import numpy as np
from contextlib import ExitStack
import concourse.bass as bass
import concourse.mybir as mybir
from concourse.bass_utils import run_bass_kernel_spmd

F32 = mybir.dt.float32
BF16 = mybir.dt.bfloat16
I32 = mybir.dt.int32
ALU = mybir.AluOpType
AF = mybir.ActivationFunctionType
AX = mybir.AxisListType

NB = 4
T = 2048
D = 1024
NT = 16
EPS = 1e-6
BIG = 30000.0
HW = ("pe", "act", "dve", "pool", "sp")
PSUM_KEYS = ("pA", "pB", "pO", "pS", "pT0", "pM")

C_ID, C_CM, C_PM, C_SW, C_ON, C_ES, C_NEG, C_INV, C_SGN, NCF = 0, 128, 256, 384, 512, 640, 1664, 1728, 1729, 1730
NCB = 1664


class Op:
    __slots__ = ("hw", "fn", "reads", "writes", "sem", "inc", "signal", "value", "waits", "idx", "is_dma")


class Prog:
    def __init__(self):
        self.ops = []

    def op(self, hw, fn, reads=(), writes=()):
        o = Op()
        o.hw, o.fn, o.reads, o.writes = hw, fn, tuple(reads), tuple(writes)
        o.sem, o.inc, o.signal, o.value, o.waits, o.is_dma = hw, 1, False, 0, [], False
        o.idx = len(self.ops)
        self.ops.append(o)
        return o

    def dma(self, hw, slot, fn, reads=(), writes=()):
        o = self.op(hw, fn, reads, writes)
        o.sem, o.inc, o.signal, o.is_dma = "dma:" + slot, 16, True, True
        return o

    def analyze(self):
        last_w, readers = {}, {}
        for o in self.ops:
            deps = {}
            for k in o.reads:
                p = last_w.get(k)
                if p is not None:
                    deps[p.idx] = (p, "raw")
                kn = k[0] if isinstance(k, tuple) else k
                if kn in PSUM_KEYS:
                    for r in readers.get(k, {}).values():
                        if r.hw != o.hw and r.idx not in deps:
                            deps[r.idx] = (r, "rar")
            for k in o.writes:
                p = last_w.get(k)
                if p is not None and p.idx not in deps:
                    deps[p.idx] = (p, "waw")
                for r in readers.get(k, {}).values():
                    if r.idx not in deps and r is not o:
                        deps[r.idx] = (r, "war")
            for p, kind in deps.values():
                if (not p.is_dma) and (not o.is_dma) and p.hw == o.hw:
                    if o.hw == "pe" or kind != "raw":
                        continue
                o.waits.append(p)
                p.signal = True
            for k in o.reads:
                readers.setdefault(k, {})[("d", o.idx) if o.is_dma else o.hw] = o
            for k in o.writes:
                last_w[k] = o
                readers[k] = {}
        cnt = {}
        for o in self.ops:
            if o.signal:
                cnt[o.sem] = cnt.get(o.sem, 0) + o.inc
                o.value = cnt[o.sem]
        return cnt

    def emit(self, block, sems):
        streams = {h: [] for h in HW}
        for o in self.ops:
            streams[o.hw].append(o)

        def make(hwname):
            def body(eng):
                known = {}
                for o in streams[hwname]:
                    need = {}
                    for p in o.waits:
                        if p.value > need.get(p.sem, 0):
                            need[p.sem] = p.value
                    for s, v in need.items():
                        if known.get(s, 0) < v:
                            eng.wait_ge(sems[s], v)
                            known[s] = v
                    ins = o.fn(eng)
                    if o.signal:
                        ins.then_inc(sems[o.sem], o.inc)
            return body

        block.tensor(make("pe"))
        block.scalar(make("act"))
        block.vector(make("dve"))
        block.gpsimd(make("pool"))
        block.sync(make("sp"))


def make_consts():
    cf = np.zeros((128, NCF), np.float32)
    p = np.arange(128)
    cf[:, C_ID:C_ID + 128] = np.eye(128, dtype=np.float32)
    cf[:, C_CM:C_CM + 128] = np.where(p[:, None] > p[None, :], -BIG, 0.0)
    cf[:, C_PM:C_PM + 128] = ((p[:, None] // 64 == p[None, :] // 64) & (p[:, None] <= p[None, :])).astype(np.float32)
    cf[:, C_SW:C_SW + 128] = (p[:, None] == (p[None, :] + 64) % 128).astype(np.float32)
    cf[:, C_ON:C_ON + 128] = 1.0
    for n in range(8):
        cf[n, C_ES + n * 128:C_ES + (n + 1) * 128] = 1.0
    neg = np.zeros((8, 8), np.float32)
    for i in range(8):
        j = (8 + i) // 2
        neg[i, j:] = -1e30
    cf[:, C_NEG:C_NEG + 64] = neg.reshape(1, 64)
    inv = np.float32(10000.0) ** (-np.arange(0, 128, 2, dtype=np.float32) / np.float32(128))
    cf[:, C_INV] = np.concatenate([inv, inv]).astype(np.float32)
    cf[:, C_SGN] = np.where(p < 64, -1.0, 1.0)
    return cf


def build(stop_after_l0=False, nseq=NB, limit=None):
    nc = bass.Bass("TRN2", target_bir_lowering=False)

    def din(name, shape, dt=F32):
        return nc.dram_tensor(name, list(shape), dt, kind="ExternalInput").ap()

    x = din("x", [NB, T, D])
    cin = din("c", [NB, D])
    pos = din("pos", [NB, T], I32)
    mod_w = din("mod_w", [2, D, 3 * D])
    mod_b = din("mod_b", [2, 3 * D])
    pre_g = din("pre_g", [2, D])
    post_g = din("post_g", [2, D])
    a_w_in = din("a_w_in", [D, 4 * D])
    a_w_out = din("a_w_out", [D, D])
    a_gn = din("a_gn", [128, 1])
    a_lb = din("a_lb", [2, D])
    kv_g = din("kv_g", [1, D])
    w_kv = din("w_kv", [D, 2 * D])
    b_w_in = din("b_w_in", [D, 2 * D])
    b_w_out = din("b_w_out", [D, D])
    cf = din("cf", [128, NCF])
    out = nc.dram_tensor("out", [NB, T, D], F32, kind="ExternalOutput").ap()
    scr = nc.dram_tensor("scr", [2, 3, NB, D], F32, kind="Internal").ap()

    P = Prog()
    es = ExitStack()
    with es:
        def sb(name, shape, dt):
            return es.enter_context(nc.sbuf_tensor(name, list(shape), dt))

        def ps(name, shape, dt):
            return es.enter_context(nc.psum_tensor(name, list(shape), dt))

        cb = sb("cb", [128, NCB], BF16)
        cfs = sb("cfs", [128, NCF - NCB], F32)
        ident = cb[:, C_ID:C_ID + 128]
        cmask = cb[:, C_CM:C_CM + 128]
        pmask = cb[:, C_PM:C_PM + 128]
        swp = cb[:, C_SW:C_SW + 128]
        onesb = cb[:, C_ON:C_ON + 128]
        negm = cfs[:, 0:64]
        invf = cfs[:, 64:65]
        sgn = cfs[:, 65:66]

        xnT = sb("xnT", [128, 8, T], BF16)
        ogT = sb("ogT", [128, 8, T], BF16)
        h1 = sb("h1", [128, NT * D], F32)
        wbf = [sb("wbf%d" % i, [128, 8, 512], BF16) for i in range(2)]
        hb = [sb("hb%d" % i, [128, T], BF16) for i in range(6)]
        cosT = sb("cosT", [128, T], BF16)
        sinT = sb("sinT", [128, T], BF16)
        xin = [sb("xin%d" % i, [128, D], F32) for i in range(2)]
        xs = [sb("xs0", [128, D], BF16)] * 2
        G_bc = sb("G_bc", [128, D], F32)
        PT = [sb("PT%d" % i, [128, 512], BF16) for i in range(3)]
        rsb = sb("rsb", [128, 512], F32)
        otb = sb("otb", [128, 512], F32)
        biasT = sb("biasT", [128, 1024], BF16)
        gbuf = sb("gbuf", [128, 64], F32)
        cmpb = sb("cmpb", [128, 8, 8, 8], F32)
        rankb = sb("rankb", [128, 64], F32)
        biasq = sb("biasq", [128, 64], BF16)
        small = sb("small", [128, 256], F32)
        smallb = sb("smallb", [128, 160], BF16)
        ss = small[:, 0:16]
        rstd = small[:, 16:32]
        tmpc = small[:, 32:48]
        aT = small[:, 48:56]
        shT = small[:, 56:64]
        gkT = small[:, 64:72]
        lbT = small[:, 72:88]
        oml = small[:, 88:96]
        noml = small[:, 184:192]
        gn = small[:, 96:97]
        bcol = small[:, 100:104]
        nbf = small[:, 104:105]
        ssy = small[:, 108:112]
        Bl = small[:, 112:144]
        Bl0 = small[:, 144:176]
        km = small[:, 176:184]
        cT = small[:, 192:224]
        shTb = smallb[:, 0:8]
        kmb = smallb[:, 8:16]
        birow = smallb[0:1, 16:144]
        scT = sb("scT", [128, 32], BF16)
        bmat = sb("bmat", [128, 128], BF16)

        def h1f(off, n):
            return h1[:, off:off + n]

        def h1b(off, n):
            return h1[:, off:off + n // 2].bitcast(BF16)

        o_ = 0
        T_q = h1f(o_, 512); o_ += 512
        T_s = h1f(o_, 512); o_ += 512
        T_d0 = h1f(o_, 512); o_ += 512
        T_d1 = h1f(o_, 512); o_ += 512
        T_B = h1f(o_, 512); o_ += 512
        attS = [h1b(o_, 512), h1b(o_ + 256, 512)]; o_ += 512
        osb = h1f(o_, 512); o_ += 512
        sqb = h1b(o_, 512); o_ += 256
        rst = h1f(o_, 512); o_ += 512
        tt_ = h1f(o_, 512); o_ += 512
        Ub = h1f(o_, 4096); o_ += 4096
        Sbf = h1b(o_, 4096); o_ += 2048
        arep = h1f(o_, 1024); RA = h1f(o_, 1024); RAi = h1[:, o_:o_ + 1024].bitcast(I32); o_ += 1024
        Shalf = h1f(o_, 1024); RB = h1f(o_, 1024); RBi = h1[:, o_:o_ + 1024].bitcast(I32); o_ += 1024
        Shalf2 = h1f(o_, 1024); o_ += 1024
        T_s2 = h1f(o_, 512); o_ += 512
        T_q2 = h1f(o_, 512); o_ += 512
        T_B2 = h1f(o_, 512); o_ += 512
        assert o_ <= NT * D
        msb = h1f(0, 3072)
        modbb = h1f(3072, 3072)
        pgb = h1f(6144, 2048)
        rows = h1f(8192, 3072)

        pA = [ps("pA%d" % i, [128, 512], F32) for i in range(2)]
        pB = [ps("pB%d" % i, [128, 512], F32) for i in range(2)]
        pO = ps("pO", [128, 512], F32)
        pS = ps("pS", [128, 512], F32)
        pT0 = ps("pT0", [128, 1024], BF16)
        pM = ps("pM", [128, 512], F32)

        H1A = "h1all"

        def WK(k, g0=0, g1=4):
            return [("wbf", k, g) for g in range(g0, g1)]

        def top(hw, fn, reads=(), writes=()):
            return P.op(hw, fn, tuple(reads) + (H1A,), writes)

        def tdma(hw, slot, fn, reads=(), writes=()):
            return P.dma(hw, slot, fn, tuple(reads) + (H1A,), writes)

        def barrier():
            for e in ("act", "dve", "pool"):
                P.op(e, (lambda eng, e=e: (eng.memset(small[:, 250:251], 0.0) if e != "act" else
                                            eng.activation(out=small[:, 251:252], in_=small[:, 252:253], func=AF.Copy))),
                     writes=[("bar", e)])
            P.op("pe", lambda eng: eng.matmul(pM[0:1, 0:1], lhsT=onesb[:, 0:1], rhs=onesb[:, 0:1], start=True, stop=True),
                 reads=["cb"], writes=["pM", ("bar", "pe")])
            for e in ("act", "dve", "pool", "pe", "sp"):
                if e == "pe":
                    P.op("pe", lambda eng: eng.matmul(pM[0:1, 0:1], lhsT=onesb[:, 0:1], rhs=onesb[:, 0:1], start=True, stop=True),
                         reads=["cb"] + [("bar", q) for q in ("act", "dve", "pool")], writes=["pM"])
                elif e == "sp":
                    P.op("sp", lambda eng: eng.nop(), reads=[("bar", q) for q in ("act", "dve", "pool", "pe")])
                else:
                    P.op(e, (lambda eng, e=e: (eng.memset(small[:, 253:254], 0.0) if e == "dve" else
                                                eng.memset(small[:, 254:255], 0.0) if e == "pool" else
                                                eng.activation(out=small[:, 255:256], in_=small[:, 252:253], func=AF.Copy))),
                         reads=[("bar", q) for q in ("act", "dve", "pool", "pe") if q != e])

        P.dma("pool", "cb", lambda e: e.dma_start(out=cb[:], in_=cf[:, 0:NCB]), writes=["cb"])
        P.dma("sp", "cfs", lambda e: e.dma_start(out=cfs[:], in_=cf[:, NCB:NCF]), writes=["cfs"])
        P.op("dve", lambda e: e.memset(small[:], 0.0), writes=["small"])
        P.op("dve", lambda e: e.memset(bmat[:], 0.0), writes=["birow"])
        P.op("dve", lambda e: e.memset(biasT[:], 0.0), writes=["biasT"])
        P.op("pool", lambda e: e.memset(hb[3][:], 0.0), writes=[("kt", 0), ("kt", 1)])
        P.op("pool", lambda e: e.memset(hb[5][:], 0.0), writes=[("kt", 0), ("kt", 1)])
        with nc.allow_non_contiguous_dma(reason="tiny one-time transposed loads"):
            pass
        for bb in range(NB):
            P.dma("sp", "s0", lambda e, bb=bb: e.dma_start(out=cT.rearrange("p (c b) -> p c b", b=NB)[:, :, bb], in_=cin[bb].rearrange("(c p) -> p c", p=128),
                                                         allow_slow_non_contiguous=True), reads=["small"], writes=[("cT", bb)])
        P.dma("sp", "s1", lambda e: e.dma_start(out=gkT, in_=kv_g[0].rearrange("(c p) -> p c", p=128), allow_slow_non_contiguous=True),
              reads=["small"], writes=["gkT"])
        for l in range(2):
            P.dma("sp", "s2", lambda e, l=l: e.dma_start(out=lbT[:, l * 8:(l + 1) * 8], in_=a_lb[l].rearrange("(c p) -> p c", p=128),
                                                       allow_slow_non_contiguous=True), reads=["small"], writes=[("lbT", l)])
        P.dma("sp", "s3", lambda e: e.dma_start(out=gn, in_=a_gn), reads=["small"], writes=["gn"])
        P.op("act", lambda e: e.activation(out=scT[:], in_=cT, func=AF.Silu), reads=[("cT", q) for q in range(NB)], writes=["scT"])
        P.op("dve", lambda e: e.tensor_tensor(out=oml, in0=lbT[:, 8:16], in1=lbT[:, 0:8], op=ALU.subtract), reads=[("lbT", 0), ("lbT", 1)], writes=["oml"])
        P.op("act", lambda e: e.activation(out=oml, in_=oml, func=AF.Sigmoid), reads=["oml"], writes=["oml"])
        P.op("dve", lambda e: e.tensor_scalar(out=noml, in0=oml, scalar1=-1.0, scalar2=None, op0=ALU.mult), reads=["oml", "small"], writes=["noml"])
        wi = 0
        for l in range(2):
            tdma("sp", "s4", lambda e, l=l: e.dma_start(out=modbb[0:NB, :], in_=mod_b[l].partition_broadcast(NB)), writes=["modbb"])
            tdma("sp", "s5", lambda e, l=l: e.dma_start(out=pgb[0:NB, 0:1024], in_=pre_g[l].partition_broadcast(NB)), writes=["pgb0"])
            tdma("sp", "s6", lambda e, l=l: e.dma_start(out=pgb[0:NB, 1024:2048], in_=post_g[l].partition_broadcast(NB)), writes=["pgb1"])
            for cbk in range(6):
                k = wi % 2
                wi += 1
                P.dma("pool", "wbf%d" % k, lambda e, l=l, cbk=cbk, k=k: e.dma_start(
                    out=wbf[k][:], in_=mod_w[l][:, cbk * 512:(cbk + 1) * 512].rearrange("(c p) n -> p c n", p=128)),
                    writes=WK(k))
                for c in range(8):
                    P.op("pe", lambda e, c=c, k=k: e.matmul(pM[0:NB, :], lhsT=scT[:, c * NB:(c + 1) * NB], rhs=wbf[k][:, c, :],
                                                            start=(c == 0), stop=(c == 7)),
                         reads=["scT"] + WK(k), writes=["pM"])
                top("dve", lambda e, cbk=cbk: e.tensor_tensor(
                    out=msb[0:NB, cbk * 512:(cbk + 1) * 512], in0=pM[0:NB, :],
                    in1=modbb[0:NB, cbk * 512:(cbk + 1) * 512], op=ALU.add),
                    reads=["pM", "modbb"], writes=[("msb", cbk)])
            mk = [("msb", i) for i in range(6)]
            top("dve", lambda e: e.scalar_tensor_tensor(out=rows[0:NB, 0:1024], in0=msb[0:NB, 1024:2048],
                                                        scalar=1.0, in1=pgb[0:NB, 0:1024], op0=ALU.add, op1=ALU.mult),
                reads=mk + ["pgb0"], writes=["rows"])
            top("dve", lambda e: e.tensor_copy(out=rows[0:NB, 1024:2048], in_=msb[0:NB, 0:1024]), reads=mk, writes=["rows"])
            top("dve", lambda e: e.tensor_tensor(out=rows[0:NB, 2048:3072], in0=msb[0:NB, 2048:3072], in1=pgb[0:NB, 1024:2048], op=ALU.mult),
                reads=mk + ["pgb1"], writes=["rows"])
            for kind in range(3):
                tdma("sp", "s7", lambda e, l=l, kind=kind: e.dma_start(out=scr[l, kind], in_=rows[0:NB, kind * 1024:(kind + 1) * 1024]),
                     reads=["rows"], writes=[("scr", l, kind)])
        barrier()

        xin_i = [0]

        def layer_vectors(l, b):
            P.dma("sp", "v0", lambda e: e.dma_start(out=aT, in_=scr[l, 0, b].rearrange("(c p) -> p c", p=128), allow_slow_non_contiguous=True),
                  reads=[("scr", l, 0)], writes=["aT"])
            P.dma("sp", "v1", lambda e: e.dma_start(out=shT, in_=scr[l, 1, b].rearrange("(c p) -> p c", p=128), allow_slow_non_contiguous=True),
                  reads=[("scr", l, 1)], writes=["shT"])
            P.dma("sp", "v2", lambda e: e.dma_start(out=G_bc[:], in_=scr[l, 2, b].partition_broadcast(128)), reads=[("scr", l, 2)], writes=["G_bc"])
            P.op("dve", lambda e: e.tensor_copy(out=shTb, in_=shT), reads=["shT"], writes=["shTb"])

        def rstd_from(col_in, col_out, keyin, keyout):
            P.op("dve", lambda e: e.tensor_scalar(out=col_out, in0=col_in, scalar1=1.0 / D, scalar2=EPS, op0=ALU.mult, op1=ALU.add),
                 reads=[keyin], writes=[keyout])
            P.op("act", lambda e: e.activation(out=col_out, in_=col_out, func=AF.Sqrt), reads=[keyout], writes=[keyout])
            P.op("dve", lambda e: e.reciprocal(out=col_out, in_=col_out), reads=[keyout], writes=[keyout])

        def pre_phase(l, b):
            pMb = pM[:].bitcast(BF16)
            jk = [("ogT", 7, 0), ("ogT", 7, 1)]
            xsb = [(xs[0][:], [("xs", 0)]), (ogT[:, 6, 0:D], [("ogT", 6, 0), ("ogT", 6, 1)])]
            srcs = {}

            def stage_a(tt):
                if l == 0:
                    xi = xin_i[0] % 2
                    xin_i[0] += 1
                    P.dma("sp", "xin%d" % xi, lambda e, tt=tt, xi=xi: e.dma_start(out=xin[xi][:], in_=x[b, tt * 128:(tt + 1) * 128, :]),
                          writes=[("xin", xi)])
                    src, skey = xin[xi][:], ("xin", xi)
                else:
                    src, skey = h1[:, tt * D:(tt + 1) * D], ("h1", tt)
                xb, xk = xsb[tt % 2]
                P.op("dve", lambda e, tt=tt: e.memset(ss[:, tt:tt + 1], 0.0), writes=[("ss", tt)])
                P.op("act", lambda e, src=src, tt=tt: e.activation(out=ogT[:, 7, 0:D], in_=src, func=AF.Square, accum_out=ss[:, tt:tt + 1]),
                     reads=[skey, ("ss", tt)], writes=jk + [("ss", tt)])
                rstd_from(ss[:, tt:tt + 1], rstd[:, tt:tt + 1], ("ss", tt), ("rstd", tt))
                P.op("dve", lambda e, src=src, tt=tt, xb=xb: e.tensor_scalar(out=xb, in0=src, scalar1=rstd[:, tt:tt + 1], scalar2=None, op0=ALU.mult),
                     reads=[skey, ("rstd", tt)], writes=xk)

            def stage_b(tt):
                xb, xk = xsb[tt % 2]
                ptb, pkey = (pT0[:], "pT0") if tt % 2 == 0 else (pMb, "pM")
                for c in range(8):
                    P.op("pe", lambda e, c=c, xb=xb, ptb=ptb: e.transpose(out=ptb[:, c * 128:(c + 1) * 128], in_=xb[:, c * 128:(c + 1) * 128], identity=ident),
                         reads=xk + ["cb"], writes=[pkey])
                if tt % 2 == 0:
                    P.op("act", lambda e, tt=tt, ptb=ptb: e.activation(out=xnT[:, :, tt * 128:(tt + 1) * 128], in_=ptb.rearrange("p (c t) -> p c t", c=8), func=AF.Copy),
                         reads=[pkey], writes=[("xnT", tt)])
                else:
                    P.op("dve", lambda e, tt=tt, ptb=ptb: e.tensor_copy(out=xnT[:, :, tt * 128:(tt + 1) * 128], in_=ptb.rearrange("p (c t) -> p c t", c=8)),
                         reads=[pkey], writes=[("xnT", tt)])

            stage_a(0)
            for tt in range(NT):
                if tt + 1 < NT:
                    stage_a(tt + 1)
                stage_b(tt)

        wslot = [0]
        kfree = [0]

        def load_head_weights(pieces):
            k = wslot[0] % 2
            wslot[0] += 1
            for g, ap in enumerate(pieces):
                P.dma("pool", "wbf%d" % k, lambda e, g=g, ap=ap, k=k: e.dma_start(
                    out=wbf[k][:, :, g * 128:(g + 1) * 128], in_=ap.rearrange("(c p) n -> p c n", p=128)),
                    writes=[("wbf", k, g)])
            return k

        def bias_cols(k, groups):
            for j, g in enumerate(groups):
                for c in range(8):
                    P.op("pe", lambda e, j=j, g=g, c=c: e.matmul(pM[:, j:j + 1], lhsT=wbf[k][:, c, g * 128:(g + 1) * 128], rhs=shTb[:, c:c + 1],
                                                               start=(c == 0), stop=(c == 7)),
                         reads=[("wbf", k, g), "shTb"], writes=["pM"])
            P.op("dve", lambda e: e.tensor_copy(out=bcol[:, 0:len(groups)], in_=pM[:, 0:len(groups)]), reads=["pM"], writes=["bcol"])

        def scale_w(k, c0, c1, vec, vkey):
            P.op("pool", lambda e: e.tensor_tensor(out=wbf[k][:, :, c0:c1], in0=wbf[k][:, :, c0:c1],
                                                   in1=vec.unsqueeze(2).to_broadcast([128, 8, c1 - c0]), op=ALU.mult),
                 reads=WK(k, c0 // 128, c1 // 128) + [vkey], writes=WK(k, c0 // 128, c1 // 128))

        pa_i = [0]

        def proj_fm(k, g, tb):
            i = pa_i[0] % 2
            pa_i[0] += 1
            for c in range(8):
                P.op("pe", lambda e, c=c, i=i: e.matmul(pA[i][:], lhsT=wbf[k][:, c, g * 128:(g + 1) * 128], rhs=xnT[:, c, tb * 512:(tb + 1) * 512],
                                                      start=(c == 0), stop=(c == 7)),
                     reads=[("wbf", k, g)] + [("xnT", 4 * tb + q) for q in range(4)], writes=[("pA", i)])
            return i

        def proj_tm(k, g, dst, dkey, bias_row=False):
            for t4 in range(4):
                i = t4 % 2
                for j in range(4):
                    tt = t4 * 4 + j
                    for c in range(8):
                        P.op("pe", lambda e, c=c, tt=tt, j=j, i=i: e.matmul(pB[i][:, j * 128:(j + 1) * 128], lhsT=xnT[:, c, tt * 128:(tt + 1) * 128],
                                                                          rhs=wbf[k][:, c, g * 128:(g + 1) * 128], start=(c == 0),
                                                                          stop=(c == 7 and not bias_row)),
                             reads=[("wbf", k, g), ("xnT", tt)], writes=[("pB", i)])
                    if bias_row:
                        P.op("pe", lambda e, j=j, i=i: e.matmul(pB[i][:, j * 128:(j + 1) * 128], lhsT=onesb, rhs=bmat[:], start=False, stop=True),
                             reads=["cb", "birow"], writes=[("pB", i)])
                P.op("act", lambda e, t4=t4, i=i: e.activation(out=dst[:, t4 * 512:(t4 + 1) * 512], in_=pB[i][:], func=AF.Copy),
                     reads=[("pB", i)], writes=[(dkey, t4)])

        def prefetch_wout(wout):
            kf = wslot[0] % 2
            wslot[0] += 1
            P.dma("pool", "wbf%d" % kf, lambda e: e.dma_start(out=wbf[kf][:], in_=wout[:, 0:512].rearrange("(c p) n -> p c n", p=128)), writes=WK(kf))
            return kf

        def post_phase(l, b, wout, last, kf):
            sl = [kf, 1 - kf]
            P.dma("pool", "wbf%d" % sl[1], lambda e: e.dma_start(
                out=wbf[sl[1]][:], in_=wout[:, 512:1024].rearrange("(c p) n -> p c n", p=128)), writes=WK(sl[1]))
            wslot[0] = 0
            def sy(tt):
                base = 108 if tt % 2 == 0 else 228
                return small[:, base:base + 4], ("ssy", tt % 2)

            def stage_a(tt):
                pp, pk = (pA, "pA") if tt % 2 == 0 else (pB, "pB")
                sc_, sk = sy(tt)
                for cbk in range(2):
                    for c in range(8):
                        P.op("pe", lambda e, c=c, cbk=cbk, tt=tt, pp=pp: e.matmul(pp[cbk][:], lhsT=ogT[:, c, tt * 128:(tt + 1) * 128], rhs=wbf[sl[cbk]][:, c, :],
                                                                         start=(c == 0), stop=(c == 7)),
                             reads=WK(sl[cbk]) + [("ogT", hh, tt // 4) for hh in range(8)], writes=[(pk, cbk)])
                    P.op("dve", lambda e, cbk=cbk, sc_=sc_: e.memset(sc_[:, cbk:cbk + 1], 0.0), writes=[sk + (cbk,)])
                    P.op("act", lambda e, cbk=cbk, pp=pp, sc_=sc_: e.activation(out=PT[cbk][:], in_=pp[cbk][:], func=AF.Square, accum_out=sc_[:, cbk:cbk + 1]),
                         reads=[(pk, cbk), sk + (cbk,)], writes=[("PT", cbk), sk + (cbk,)])

            def stage_b(tt):
                pp, pk = (pA, "pA") if tt % 2 == 0 else (pB, "pB")
                sc_, sk = sy(tt)
                P.op("dve", lambda e, sc_=sc_: e.tensor_tensor(out=sc_[:, 2:3], in0=sc_[:, 0:1], in1=sc_[:, 1:2], op=ALU.add),
                     reads=[sk + (0,), sk + (1,)], writes=[sk + (2,)])
                rstd_from(sc_[:, 2:3], sc_[:, 3:4], sk + (2,), sk + (3,))
                xi = xin_i[0] % 2
                xin_i[0] += 1
                if l == 0:
                    P.dma("sp", "xin%d" % xi, lambda e, tt=tt, xi=xi: e.dma_start(out=xin[xi][:], in_=x[b, tt * 128:(tt + 1) * 128, :]),
                          writes=[("xin", xi)])
                    dest, dkey, res, rkey = h1[:, tt * D:(tt + 1) * D], ("h1", tt), xin[xi][:], ("xin", xi)
                else:
                    dest, dkey, res, rkey = xin[xi][:], ("xin", xi), h1[:, tt * D:(tt + 1) * D], ("h1", tt)
                extra_w = [H1A]
                for cbk in range(2):
                    P.op("dve", lambda e, cbk=cbk, dest=dest, pp=pp, sc_=sc_: e.scalar_tensor_tensor(
                        out=dest[:, cbk * 512:(cbk + 1) * 512], in0=pp[cbk][:], scalar=sc_[:, 3:4], in1=G_bc[:, cbk * 512:(cbk + 1) * 512],
                        op0=ALU.mult, op1=ALU.mult), reads=[(pk, cbk), sk + (3,), "G_bc"], writes=[dkey] + (extra_w if l == 0 else []))
                P.op("dve", lambda e, dest=dest, res=res: e.tensor_tensor(out=dest, in0=dest, in1=res, op=ALU.add),
                     reads=[dkey, rkey], writes=[dkey] + extra_w)
                if last:
                    P.dma("sp", "xin%d" % xi, lambda e, tt=tt, dest=dest: e.dma_start(out=out[b, tt * 128:(tt + 1) * 128, :], in_=dest),
                          reads=[dkey], writes=[("out", b, tt)])

            stage_a(0)
            for tt in range(NT):
                if tt + 1 < NT:
                    stage_a(tt + 1)
                stage_b(tt)

        def rope_tables(b):
            for hf in range(2):
                cs = slice(hf * 1024, (hf + 1) * 1024)
                tdma("sp", "ra", lambda e, cs=cs: e.dma_start(out=RAi, in_=pos[b, cs].partition_broadcast(128)), writes=["arep"])
                top("dve", lambda e: e.tensor_copy(out=RA, in_=RAi), reads=["arep"], writes=["arep"])
                top("dve", lambda e: e.tensor_scalar(out=RA, in0=RA, scalar1=invf, scalar2=None, op0=ALU.mult), reads=["arep", "cfs"], writes=["arep"])
                for which in range(2):
                    off = 0.0 if which == 0 else float(np.pi / 2)
                    top("dve", lambda e, off=off: e.tensor_scalar(out=RB, in0=RA, scalar1=off, scalar2=float(1.0 / (2 * np.pi)), op0=ALU.add, op1=ALU.mult),
                        reads=["arep"], writes=["Shalf"])
                    top("dve", lambda e: e.tensor_copy(out=RBi, in_=RB), reads=["Shalf"], writes=["Shalf"])
                    top("dve", lambda e: e.tensor_copy(out=RB, in_=RBi), reads=["Shalf"], writes=["Shalf"])
                    top("dve", lambda e: e.tensor_scalar(out=RB, in0=RB, scalar1=float(-2 * np.pi), scalar2=None, op0=ALU.mult), reads=["Shalf"], writes=["Shalf"])
                    top("dve", lambda e, off=off: e.scalar_tensor_tensor(out=RB, in0=RA, scalar=off, in1=RB, op0=ALU.add, op1=ALU.add),
                        reads=["arep", "Shalf"], writes=["Shalf"])
                    top("dve", lambda e: e.tensor_scalar(out=RB, in0=RB, scalar1=3.14159, scalar2=-3.14159, op0=ALU.min, op1=ALU.max),
                        reads=["Shalf"], writes=["Shalf"])
                    if which == 0:
                        top("act", lambda e: e.activation(out=RB, in_=RB, func=AF.Sin), reads=["Shalf"], writes=["Shalf"])
                        top("dve", lambda e, cs=cs: e.tensor_scalar(out=sinT[:, cs], in0=RB, scalar1=sgn, scalar2=None, op0=ALU.mult),
                            reads=["Shalf", "cfs"], writes=["sinT"])
                    else:
                        top("act", lambda e, cs=cs: e.activation(out=cosT[:, cs], in_=RB, func=AF.Sin), reads=["Shalf"], writes=["cosT"])

        def l0_prep(k):
            bias_cols(k, [0, 1, 3])
            P.op("dve", lambda e: e.tensor_scalar(out=nbf, in0=bcol[:, 1:2], scalar1=-1.0, scalar2=None, op0=ALU.mult), reads=["bcol"], writes=["nbf"])
            for c in range(8):
                P.op("pe", lambda e, c=c: e.matmul(pM[0:1, 128:256], lhsT=shTb[:, c:c + 1], rhs=wbf[k][:, c, 256:384], start=(c == 0), stop=(c == 7)),
                     reads=[("wbf", k, 2), "shTb"], writes=["pM"])
            P.op("dve", lambda e: e.tensor_copy(out=bmat[0:1, :], in_=pM[0:1, 128:256]), reads=["pM"], writes=["birow"])
            scale_w(k, 0, 512, aT, "aT")

        def l0_head(h, k):
            qtT, ktT, sgT, kt, vv, ktB = hb[0], hb[1], hb[2], hb[3], hb[4], hb[5]
            knext = None
            if h == 7:
                kfree[0] = prefetch_wout(a_w_out)
            if h < 7:
                knext = load_head_weights([a_w_in[:, g * 1024 + (h + 1) * 128:g * 1024 + (h + 2) * 128] for g in range(4)])
            for tb in range(4):
                bs = slice(tb * 512, (tb + 1) * 512)
                Tq, TB, Ts = (T_q, T_B, T_s) if tb % 2 == 0 else (T_q2, T_B2, T_s2)
                kq, kB, ks = ("T_q", "T_B", "T_s") if tb % 2 == 0 else ("T_q2", "T_B2", "T_s2")
                i = proj_fm(k, 0, tb)
                top("act", lambda e, i=i, Tq=Tq: e.activation(out=Tq, in_=pA[i][:], func=AF.Silu, bias=bcol[:, 0:1]), reads=[("pA", i), "bcol"], writes=[kq])
                i = proj_fm(k, 3, tb)
                P.op("act", lambda e, i=i, bs=bs: e.activation(out=sgT[:, bs], in_=pA[i][:], func=AF.Silu, bias=bcol[:, 2:3]),
                     reads=[("pA", i), "bcol"], writes=[("sgT", tb)])
                i = proj_fm(k, 1, tb)
                top("act", lambda e, i=i, Ts=Ts: e.activation(out=Ts, in_=pA[i][:], func=AF.Sigmoid, bias=nbf, scale=-1.0), reads=[("pA", i), "nbf"], writes=[ks])
                top("dve", lambda e, Ts=Ts: e.tensor_scalar(out=T_d0, in0=Ts, scalar1=noml[:, h:h + 1], scalar2=1.0, op0=ALU.mult, op1=ALU.add),
                    reads=[ks, "noml"], writes=["T_d0"])
                if tb == 0 and h == 0:
                    top("dve", lambda e: e.memset(T_d1, 0.0), writes=["T_d1"])
                d0v = T_d0.rearrange("p (n c) -> p n c", c=64)[:, :, 0:1]
                d1v = T_d1.rearrange("p (n c) -> p n c", c=64)[:, :, 0:1]
                top("dve", lambda e, d0v=d0v, d1v=d1v: e.tensor_copy(out=d1v, in_=d0v), reads=["T_d0", "T_d1"], writes=["T_d1"])
                top("dve", lambda e, d0v=d0v: e.memset(d0v, 0.0), reads=["T_d0", "T_d1"], writes=["T_d0"])
                top("dve", lambda e, TB=TB: e.tensor_tensor_scan(out=TB, data0=T_d0, data1=T_d1, initial=0.0, op0=ALU.mult, op1=ALU.add),
                    reads=["T_d0", "T_d1"], writes=[kB])
                top("dve", lambda e, tb=tb, TB=TB: e.tensor_copy(out=Bl[:, tb * 8:(tb + 1) * 8].unsqueeze(2),
                                                                 in_=TB.rearrange("p (n c) -> p n c", c=64)[:, :, 63:64]), reads=[kB], writes=["Bl"])
                top("pool", lambda e, bs=bs, Tq=Tq, TB=TB: e.tensor_tensor(out=qtT[:, bs], in0=Tq, in1=TB, op=ALU.mult), reads=[kq, kB], writes=[("qtT", tb)])
                top("dve", lambda e, TB=TB: e.reciprocal(out=T_d0, in_=TB), reads=[kB], writes=["T_d0"])
                top("dve", lambda e, bs=bs, Ts=Ts: e.scalar_tensor_tensor(out=ktT[:, bs], in0=Ts, scalar=oml[:, h:h + 1], in1=T_d0, op0=ALU.mult, op1=ALU.mult),
                    reads=[ks, "T_d0", "oml"], writes=[("ktT", tb)])
            top("dve", lambda e: e.tensor_copy(out=Bl0, in_=Bl), reads=["Bl"], writes=["Bl0"])
            top("dve", lambda e: e.memset(Bl0[:, 0:1], 0.0), reads=["Bl0"], writes=["Bl0"])
            top("pool", lambda e: e.tensor_copy(out=arep.rearrange("p (v n) -> p v n", n=32), in_=Bl0.unsqueeze(1).to_broadcast([128, 32, 32])),
                reads=["Bl0"], writes=["arep"])
            if limit == "h0a":
                return knext
            proj_tm(k, 2, vv, "vv", bias_row=True)
            for t8 in range(2):
                for j in range(8):
                    tt = t8 * 8 + j
                    P.op("pe", lambda e, tt=tt, j=j: e.transpose(out=pT0[:, j * 128:(j + 1) * 128], in_=ktT[:, tt * 128:(tt + 1) * 128], identity=ident),
                         reads=[("ktT", tt // 4), "cb"], writes=["pT0"])
                P.op("act", lambda e, t8=t8: e.activation(out=kt[0:64, t8 * 1024:(t8 + 1) * 1024], in_=pT0[0:64, :], func=AF.Copy), reads=["pT0"], writes=[("kt", t8)])
                P.op("act", lambda e, t8=t8: e.activation(out=ktB[64:128, t8 * 1024:(t8 + 1) * 1024], in_=pT0[64:128, :], func=AF.Copy), reads=["pT0"], writes=[("kt", t8)])
            if knext is not None:
                l0_prep(knext)
            if limit == "h0b":
                return knext
            Uv = Ub.rearrange("p (v n) -> p n v", n=32)
            for ng in range(8):
                i = ng % 2
                for j in range(4):
                    n = ng * 4 + j
                    tt, half = n // 2, n % 2
                    ksrc = kt if half == 0 else ktB
                    P.op("pe", lambda e, j=j, tt=tt, ksrc=ksrc, i=i: e.matmul(pB[i][:, j * 128:(j + 1) * 128], lhsT=ksrc[:, tt * 128:(tt + 1) * 128],
                                                                            rhs=vv[:, tt * 128:(tt + 1) * 128], start=True, stop=True),
                         reads=[("kt", tt // 8), ("vv", tt // 4)], writes=[("pB", i)])
                top("dve", lambda e, ng=ng, i=i: e.tensor_tensor(out=Uv[:, ng * 4:(ng + 1) * 4, :], in0=pB[i][:].rearrange("p (n v) -> p n v", n=4),
                                                                 in1=Bl[:, ng * 4:(ng + 1) * 4].unsqueeze(2).to_broadcast([128, 4, 128]), op=ALU.mult),
                    reads=[("pB", i), "Bl"], writes=["Ub"])
            if limit == "h0c1":
                return knext
            for vq in range(4):
                Sq, ksq = (Shalf, "Shalf") if vq % 2 == 0 else (Shalf2, "Shalf2")
                top("dve", lambda e, vq=vq, Sq=Sq: e.tensor_tensor_scan(out=Sq, data0=arep, data1=Ub[:, vq * 1024:(vq + 1) * 1024], initial=0.0,
                                                                        op0=ALU.mult, op1=ALU.add),
                    reads=["arep", "Ub"], writes=[ksq])
                dstv = Sbf.rearrange("p (n v) -> p v n", n=32)[:, vq * 32:(vq + 1) * 32, :]
                if vq % 2 == 0:
                    top("act", lambda e, dstv=dstv, Sq=Sq: e.activation(out=dstv, in_=Sq.rearrange("p (v n) -> p v n", n=32), func=AF.Copy),
                        reads=[ksq], writes=[("Sbf", vq)])
                else:
                    top("pool", lambda e, dstv=dstv, Sq=Sq: e.tensor_copy(out=dstv, in_=Sq.rearrange("p (v n) -> p v n", n=32)),
                        reads=[ksq], writes=[("Sbf", vq)])
            def att_stage(pg):
                i = pg % 2
                for j in range(4):
                    pr = pg * 4 + j
                    P.op("pe", lambda e, j=j, pr=pr, i=i: e.matmul(pB[i][:, j * 128:(j + 1) * 128], lhsT=ktT[:, pr * 128:(pr + 1) * 128],
                                                                 rhs=qtT[:, pr * 128:(pr + 1) * 128], start=True, stop=True),
                         reads=[("ktT", pg), ("qtT", pg)], writes=[("pB", i)])
                top("dve", lambda e, i=i: e.tensor_tensor(out=attS[i].rearrange("p (n t) -> p n t", n=4), in0=pB[i][:].rearrange("p (n t) -> p n t", n=4),
                                                          in1=pmask.unsqueeze(1).to_broadcast([128, 4, 128]), op=ALU.mult),
                    reads=[("pB", i), "cb"], writes=[("attS", i)])

            att_stage(0)
            pT0f0 = pT0[:].bitcast(F32)
            for pg in range(4):
                i = pg % 2
                if pg % 2 == 0:
                    aO, aS, kO, kS = pO[:], pS[:], "pO", "pS"
                else:
                    aO, aS, kO, kS = pT0f0, pM[:], "pT0", "pM"
                if pg < 3:
                    att_stage(pg + 1)
                sbk = [("Sbf", q) for q in range(4)]
                for j in range(4):
                    pr = pg * 4 + j
                    P.op("pe", lambda e, j=j, pr=pr, i=i, aO=aO: e.matmul(aO[:, j * 128:(j + 1) * 128], lhsT=vv[:, pr * 128:(pr + 1) * 128],
                                                                        rhs=attS[i][:, j * 128:(j + 1) * 128], start=True, stop=False, skip_group_check=True),
                         reads=[("vv", pg), ("attS", i), H1A], writes=[kO])
                    if pr > 0:
                        P.op("pe", lambda e, j=j, pr=pr, aO=aO: e.matmul(aO[:, j * 128:j * 128 + 64], lhsT=Sbf[:, (2 * pr - 1) * 128:(2 * pr) * 128],
                                                                       rhs=qtT[:, pr * 128:pr * 128 + 64], start=False, stop=False, skip_group_check=True),
                             reads=sbk + [("qtT", pg), H1A], writes=[kO])
                    P.op("pe", lambda e, j=j, pr=pr, aO=aO: e.matmul(aO[:, j * 128 + 64:(j + 1) * 128], lhsT=Sbf[:, (2 * pr) * 128:(2 * pr + 1) * 128],
                                                                   rhs=qtT[:, pr * 128 + 64:(pr + 1) * 128], start=False, stop=True, skip_group_check=True),
                         reads=sbk + [("qtT", pg), H1A], writes=[kO])
                top("act", lambda e, aO=aO: e.activation(out=sqb, in_=aO, func=AF.Square), reads=[kO], writes=["sqb"])
                P.op("pe", lambda e, aS=aS: e.matmul(aS, lhsT=onesb, rhs=sqb, start=True, stop=True), reads=["sqb", "cb", H1A], writes=[kS])
                top("dve", lambda e, aS=aS: e.tensor_scalar(out=rst, in0=aS, scalar1=1.0 / 128, scalar2=EPS, op0=ALU.mult, op1=ALU.add), reads=[kS], writes=["rst"])
                top("act", lambda e: e.activation(out=rst, in_=rst, func=AF.Sqrt), reads=["rst"], writes=["rst"])
                top("dve", lambda e: e.reciprocal(out=rst, in_=rst), reads=["rst"], writes=["rst"])
                tbuf, tkey = (tt_, "tt_") if pg % 2 == 0 else (osb, "osb")
                top("dve", lambda e, aO=aO, tbuf=tbuf: e.scalar_tensor_tensor(out=tbuf, in0=aO, scalar=gn, in1=rst, op0=ALU.mult, op1=ALU.mult),
                    reads=[kO, "rst", "gn"], writes=[tkey])
                top("pool", lambda e, pg=pg, tbuf=tbuf: e.tensor_tensor(out=ogT[:, h, pg * 512:(pg + 1) * 512], in0=tbuf, in1=sgT[:, pg * 512:(pg + 1) * 512], op=ALU.mult),
                    reads=[tkey, ("sgT", pg)], writes=[("ogT", h, pg)])
            return knext

        pt_i = [0]

        def l1_pieces(h):
            return [b_w_in[:, h * 128:(h + 1) * 128], b_w_in[:, 1024 + h * 128:1024 + (h + 1) * 128],
                    w_kv[:, h * 128:(h + 1) * 128], w_kv[:, 1024 + h * 128:1024 + (h + 1) * 128]]

        def l1_prep(k):
            bias_cols(k, [0, 1])
            scale_w(k, 0, 256, aT, "aT")
            scale_w(k, 256, 512, gkT, "gkT")

        def l1_head(h, k):
            QT, KT, szT, V = hb[0], hb[1], hb[2], hb[4]
            knext = load_head_weights(l1_pieces(h + 1)) if h < 7 else None
            if h == 7:
                kfree[0] = prefetch_wout(b_w_out)
            if limit == "l1a0":
                return knext
            for tb in range(4):
                bs = slice(tb * 512, (tb + 1) * 512)
                iq = proj_fm(k, 0, tb)
                P.op("dve", lambda e, i=iq: e.tensor_scalar(out=PT[0][:], in0=pA[i][:], scalar1=bcol[:, 0:1], scalar2=None, op0=ALU.add),
                     reads=[("pA", iq), "bcol"], writes=[("PT", 0)])
                P.op("dve", lambda e, i=iq, bs=bs: e.scalar_tensor_tensor(out=rsb[:], in0=pA[i][:], scalar=bcol[:, 0:1], in1=cosT[:, bs], op0=ALU.add, op1=ALU.mult),
                     reads=[("pA", iq), "bcol", "cosT"], writes=["rsb"])
                ik = proj_fm(k, 2, tb)
                P.op("act", lambda e, i=ik: e.activation(out=PT[1][:], in_=pA[i][:], func=AF.Copy), reads=[("pA", ik)], writes=[("PT", 1)])
                P.op("pe", lambda e: e.matmul(pB[0][:], lhsT=swp, rhs=PT[0][:], start=True, stop=True), reads=[("PT", 0), "cb"], writes=[("pB", 0)])
                P.op("dve", lambda e, bs=bs: e.tensor_tensor(out=otb[:], in0=pB[0][:], in1=sinT[:, bs], op=ALU.mult), reads=[("pB", 0), "sinT"], writes=["otb"])
                P.op("dve", lambda e, bs=bs: e.tensor_tensor(out=QT[:, bs], in0=rsb[:], in1=otb[:], op=ALU.add), reads=["rsb", "otb"], writes=[("QT", tb)])
                P.op("dve", lambda e, i=ik, bs=bs: e.tensor_tensor(out=rsb[:], in0=pA[i][:], in1=cosT[:, bs], op=ALU.mult),
                     reads=[("pA", ik), "cosT"], writes=["rsb"])
                iz = proj_fm(k, 1, tb)
                P.op("pe", lambda e: e.matmul(pB[1][:], lhsT=swp, rhs=PT[1][:], start=True, stop=True), reads=[("PT", 1), "cb"], writes=[("pB", 1)])
                P.op("dve", lambda e, bs=bs: e.tensor_tensor(out=otb[:], in0=pB[1][:], in1=sinT[:, bs], op=ALU.mult), reads=[("pB", 1), "sinT"], writes=["otb"])
                P.op("dve", lambda e, bs=bs: e.tensor_tensor(out=KT[:, bs], in0=rsb[:], in1=otb[:], op=ALU.add), reads=["rsb", "otb"], writes=[("KT", tb)])
                P.op("act", lambda e, i=iz, bs=bs: e.activation(out=szT[:, bs], in_=pA[i][:], func=AF.Silu, bias=bcol[:, 1:2]),
                     reads=[("pA", iz), "bcol"], writes=[("szT", tb)])
            if limit == "l1a":
                return knext
            proj_tm(k, 3, V, "V")
            if knext is not None:
                l1_prep(knext)
            P.op("dve", lambda e: e.tensor_reduce(out=km, in_=KT[:].rearrange("p (n k) -> p n k", k=256), axis=AX.X, op=ALU.add),
                 reads=[("KT", q) for q in range(4)], writes=["km"])
            P.op("dve", lambda e: e.tensor_scalar(out=kmb, in0=km, scalar1=1.0 / 256, scalar2=None, op0=ALU.mult), reads=["km"], writes=["kmb"])
            for i8 in range(8):
                P.op("pe", lambda e, i8=i8: e.matmul(pM[:, i8 * 8:(i8 + 1) * 8], lhsT=QT[:, (8 + i8) * 128:(9 + i8) * 128], rhs=kmb, start=True, stop=True),
                     reads=[("QT", (8 + i8) // 4), "kmb"], writes=["pM"])
            P.op("dve", lambda e: e.tensor_tensor(out=gbuf[:], in0=pM[:, 0:64], in1=negm, op=ALU.add), reads=["pM", "cfs"], writes=["gbuf"])
            g3 = gbuf[:].rearrange("p (i n) -> p i n", n=8)
            P.op("dve", lambda e: e.tensor_tensor(out=cmpb[:], in0=g3.unsqueeze(2).to_broadcast([128, 8, 8, 8]),
                                                  in1=g3.unsqueeze(3).to_broadcast([128, 8, 8, 8]), op=ALU.is_gt), reads=["gbuf"], writes=["cmpb"])
            P.op("dve", lambda e: e.tensor_reduce(out=rankb[:], in_=cmpb[:].rearrange("p i n m -> p (i n) m"), axis=AX.X, op=ALU.add),
                 reads=["cmpb"], writes=["rankb"])
            bq_pad = cmpb[:].rearrange("p a b c -> p (a b c)").bitcast(BF16).rearrange("p (i c) -> p i c", c=128)
            P.op("dve", lambda e: e.memset(bq_pad, 0.0), reads=["rankb"], writes=["cmpb"])
            P.op("dve", lambda e: e.tensor_scalar(out=bq_pad[:, :, 0:8], in0=rankb[:].rearrange("p (i n) -> p i n", n=8), scalar1=2.5, scalar2=-BIG,
                                                  op0=ALU.is_ge, op1=ALU.mult), reads=["rankb", "cmpb"], writes=["cmpb"])
            for i8 in range(8):
                P.op("pe", lambda e, i8=i8: e.transpose(out=pT0[:, i8 * 128:(i8 + 1) * 128], in_=bq_pad[:, i8, :], identity=ident),
                     reads=["cmpb", "cb"], writes=["pT0"])
            P.op("act", lambda e: e.activation(out=biasT[:], in_=pT0[:], func=AF.Copy), reads=["pT0"], writes=["biasT"])
            if limit == "l1b":
                return knext
            sc = float(128 ** -0.5)
            pT0f = pT0[:].bitcast(F32)
            tiles = [(g, ktile) for g in range(4) for ktile in range(4 * g + 4)]

            def acc_of(g):
                if g % 2 == 0:
                    return pO[:], pS[:], "pO", "pS"
                return pT0f, pM[:], "pT0", "pM"

            def qk_stage(idx):
                g, ktile = tiles[idx]
                c0 = max(ktile - 4 * g, 0)
                cl = slice(c0 * 128, 512)
                j, r = idx % 2, idx % 3
                mm = [(cl, KT[:, ktile * 128:(ktile + 1) * 128], QT[:, g * 512 + c0 * 128:(g + 1) * 512], [("KT", ktile // 4), ("QT", g)])]
                if g >= 2:
                    n = ktile // 2
                    lo = max(2 * n + 2 - 4 * g, c0)
                    if lo < 4:
                        mm.append((slice(lo * 128, 512), cb[:, C_ES + n * 128:C_ES + (n + 1) * 128],
                                   biasT[:, (g - 2) * 512 + lo * 128:(g - 2) * 512 + 512], ["biasT", "cb"]))
                if ktile >= 4 * g:
                    mm.append((slice(c0 * 128, c0 * 128 + 128), ident, cmask, ["cb"]))
                for mi, (csl, l_, r_, rk) in enumerate(mm):
                    P.op("pe", lambda e, csl=csl, l_=l_, r_=r_, mi=mi, j=j, last=(mi == len(mm) - 1): e.matmul(
                        pB[j][:, csl], lhsT=l_, rhs=r_, start=(mi == 0), stop=last, skip_group_check=True),
                        reads=rk, writes=[("pB", j)])
                P.op("act", lambda e, j=j, r=r, cl=cl: e.activation(out=PT[r][:, cl], in_=pB[j][:, cl], func=AF.Exp, scale=sc),
                     reads=[("pB", j)], writes=[("PT", r)])

            def pv_stage(idx):
                g, ktile = tiles[idx]
                nkt = 4 * g + 4
                c0 = max(ktile - 4 * g, 0)
                cl = slice(c0 * 128, 512)
                r = idx % 3
                aO, aS, kO, kS = acc_of(g)
                P.op("pe", lambda e, r=r, cl=cl, ktile=ktile, nkt=nkt, aO=aO: e.matmul(aO[:, cl], lhsT=V[:, ktile * 128:(ktile + 1) * 128], rhs=PT[r][:, cl],
                                                                                      start=(ktile == 0), stop=(ktile == nkt - 1), skip_group_check=True),
                     reads=[("V", ktile // 4), ("PT", r)], writes=[kO])
                P.op("pe", lambda e, r=r, cl=cl, ktile=ktile, nkt=nkt, aS=aS: e.matmul(aS[:, cl], lhsT=onesb, rhs=PT[r][:, cl],
                                                                                      start=(ktile == 0), stop=(ktile == nkt - 1), skip_group_check=True),
                     reads=["cb", ("PT", r)], writes=[kS])
                if ktile == nkt - 1:
                    P.op("dve", lambda e, aS=aS: e.reciprocal(out=rsb[:], in_=aS), reads=[kS], writes=["rsb"])
                    P.op("dve", lambda e, aO=aO: e.tensor_tensor(out=otb[:], in0=aO, in1=rsb[:], op=ALU.mult), reads=[kO, "rsb"], writes=["otb"])
                    P.op("dve", lambda e, g=g: e.tensor_tensor(out=ogT[:, h, g * 512:(g + 1) * 512], in0=otb[:], in1=szT[:, g * 512:(g + 1) * 512], op=ALU.mult),
                         reads=["otb", ("szT", g)], writes=[("ogT", h, g)])

            qk_stage(0)
            for idx in range(len(tiles)):
                if idx + 1 < len(tiles):
                    qk_stage(idx + 1)
                pv_stage(idx)
            return knext

        for b in range(nseq):
            if limit == "setup":
                break
            rope_tables(b)
            layer_vectors(0, b)
            if limit == "rope":
                break
            kw = load_head_weights([a_w_in[:, g * 1024:g * 1024 + 128] for g in range(4)])
            l0_prep(kw)
            pre_phase(0, b)
            if limit == "pre":
                break
            for h in range(8):
                kw = l0_head(h, kw)
                if limit in ("head0", "h0a", "h0b", "h0c", "h0c1", "h0c2", "h0c3"):
                    break
            if limit in ("head0", "heads", "h0a", "h0b", "h0c", "h0c1", "h0c2", "h0c3"):
                break
            post_phase(0, b, a_w_out, stop_after_l0, kfree[0])
            if stop_after_l0:
                continue
            layer_vectors(1, b)
            kw = load_head_weights(l1_pieces(0))
            l1_prep(kw)
            pre_phase(1, b)
            if limit == "l1pre":
                break
            for h in range(8):
                kw = l1_head(h, kw)
                if limit in ("l1a", "l1b", "l1c", "l1a0", "l1a1", "l1a2"):
                    break
            if limit in ("l1a", "l1b", "l1c", "l1a0", "l1a1", "l1a2"):
                break
            post_phase(1, b, b_w_out, True, kfree[0])
        if limit is not None:
            P.dma("sp", "dbg", lambda e: e.dma_start(out=out[0, 0:128, :], in_=xin[0][:]), reads=[("xin", 0)], writes=[("out", 0, 0)])
            P.op("sp", lambda e: e.nop(), reads=[("out", 0, 0)])
            for e_ in ("act", "dve", "pool", "pe"):
                pass
        else:
            P.op("sp", lambda e: e.nop(), reads=[("out", b, tt) for b in range(nseq) for tt in range(NT)])

        cnt = P.analyze()
        names = set(cnt.keys()) | set(HW)
        sems = {s: es.enter_context(nc.semaphore(s.replace(":", "_"))) for s in sorted(names)}
        with nc.Block() as block:
            P.emit(block, sems)
    return nc, len(P.ops), cnt


_CACHE = {}


def kernel(x, c, positions, mod_w, mod_b, pre_norm_g, post_norm_g, a_w_in, a_w_out, a_out_norm_g,
           a_lb_logits, kv_norm_g, w_kv, b_w_in, b_w_out):
    if "nc" not in _CACHE:
        _CACHE["nc"] = build()[0]
    nc = _CACHE["nc"]
    f = lambda a: np.ascontiguousarray(np.asarray(a), dtype=np.float32)
    shared = {
        "mod_w": f(mod_w), "mod_b": f(mod_b), "pre_g": f(pre_norm_g), "post_g": f(post_norm_g),
        "a_w_in": f(a_w_in)[0], "a_w_out": f(a_w_out)[0], "a_gn": f(a_out_norm_g).reshape(128, 1),
        "a_lb": f(a_lb_logits), "kv_g": f(kv_norm_g).reshape(1, D), "w_kv": f(w_kv),
        "b_w_in": f(b_w_in)[0], "b_w_out": f(b_w_out)[0], "cf": make_consts(),
    }
    x = f(x)
    c = f(c)
    positions = np.ascontiguousarray(np.asarray(positions), dtype=np.int32)
    in_maps = []
    for i in range(8):
        m = dict(shared)
        m["x"] = x[i * NB:(i + 1) * NB]
        m["c"] = c[i * NB:(i + 1) * NB]
        m["pos"] = positions[i * NB:(i + 1) * NB]
        in_maps.append(m)
    res = run_bass_kernel_spmd(nc, in_maps, core_ids=list(range(8)))
    return np.concatenate([r["out"] for r in res.results], axis=0)
```
